# Optimizing a Trainium2 kernel written in Bass

```python
import math
import jax, jax.numpy as jnp
from jax import lax
import numpy as np

D_MODEL = 1024
BATCH = 16
SEQ = 2048
DEPTH = 2

M_HEADS = 4
M_QK = 64
M_V = 128
M_WIDTH = M_HEADS * M_V
CONV_W = 4
A_HEADS = 8
A_HEAD_DIM = 64
A_WIDTH = A_HEADS * A_HEAD_DIM
Q_RANK = 256
KV_RANK = 128
IDX_HEADS = 8
IDX_DIM = 64
TOPK_MAX = 256
R_HEADS = 4
R_QK = 128
R_V = 128
R_WIDTH = R_HEADS * R_V
CHUNK = 128
Q_BLOCK = 128
N_BUCKETS = 32
MAX_DISTANCE = 128
ROPE_BASE = 10000.0
EPS = 1e-6
D_FF = -(((-8 * D_MODEL) // 3) // 256) * 256
IN_SPLITS = (M_HEADS * M_QK, M_HEADS * M_QK, M_WIDTH, M_HEADS, M_HEADS, M_WIDTH,
             Q_RANK, IDX_DIM, IDX_HEADS, KV_RANK,
             R_HEADS * R_QK, R_HEADS * R_QK, R_WIDTH, R_WIDTH,
             D_MODEL, D_MODEL, D_MODEL)
D_IN = sum(IN_SPLITS)

kernel_name = 'hybrid_mlstm_dsa_retention_block'


def rms_norm(x, g):
    xf = x.astype(jnp.float32)
    y = xf * lax.rsqrt(jnp.mean(xf * xf, axis=-1, keepdims=True) + EPS)
    return (y * g.astype(jnp.float32)).astype(x.dtype)


def layer_norm(x, g):
    xf = x.astype(jnp.float32)
    mu = jnp.mean(xf, axis=-1, keepdims=True)
    xc = xf - mu
    y = xc * lax.rsqrt(jnp.mean(xc * xc, axis=-1, keepdims=True) + EPS)
    return (y * g.astype(jnp.float32)).astype(x.dtype)


def head_layer_norm(x, g):
    return layer_norm(x, g.reshape(x.shape[-2:]))


def causal_conv(x, w):
    k_w, c = w.shape
    return lax.conv_general_dilated(x, w[:, None, :].astype(x.dtype), window_strides=(1,),
                                    padding=[(k_w - 1, 0)],
                                    dimension_numbers=('NWC', 'WIO', 'NWC'),
                                    feature_group_count=c)


def rotary(x, pos):
    half = x.shape[-1] // 2
    freqs = ROPE_BASE ** (-jnp.linspace(0.0, 1.0, half, dtype=jnp.float32))
    ang = pos[:, None] * freqs[None, :]
    cos = jnp.cos(ang)[:, None, :]
    sin = jnp.sin(ang)[:, None, :]
    xf = x.astype(jnp.float32)
    x1, x2 = xf[..., :half], xf[..., half:]
    return jnp.concatenate([x1 * cos - x2 * sin, x1 * sin + x2 * cos], axis=-1).astype(x.dtype)


def t5_bucket(dist):
    max_exact = N_BUCKETS // 2
    d_f = jnp.maximum(dist, 1).astype(jnp.float32)
    large = max_exact + (jnp.log(d_f / max_exact) / math.log(MAX_DISTANCE / max_exact)
                         * (N_BUCKETS - max_exact)).astype(jnp.int32)
    large = jnp.minimum(large, N_BUCKETS - 1)
    return jnp.where(dist < max_exact, dist, large)


def to_chunks(x):
    b, s, h = x.shape[:3]
    rest = x.shape[3:]
    x = x.reshape((b, s // CHUNK, CHUNK, h) + rest)
    return jnp.transpose(x, (1, 0, 3, 2) + tuple(range(4, x.ndim)))


def from_chunks(x):
    n, b, h, l, d = x.shape
    return jnp.transpose(x, (1, 0, 3, 2, 4)).reshape(b, n * l, h, d)


def mlstm(q, k, v, i_pre, f_pre):
    dtype = v.dtype
    b, s, h, dk = q.shape
    dv = v.shape[-1]
    f32 = jnp.float32
    q = q.astype(f32) * dk ** -0.5
    k = k.astype(f32)
    v = v.astype(f32)
    log_f = jax.nn.log_sigmoid(f_pre.astype(f32))
    log_i = i_pre.astype(f32)
    causal = jnp.tril(jnp.ones((CHUNK, CHUNK), dtype=bool))

    def step(carry, inp):
        c_st, n_st, m_st = carry
        qc, kc, vc, lf, li = inp
        bcum = jnp.cumsum(lf, axis=-1)
        d_log = bcum[..., :, None] - bcum[..., None, :] + li[..., None, :]
        d_log = jnp.where(causal, d_log, -jnp.inf)
        inter = bcum + m_st[..., None]
        m_row = jnp.maximum(inter, jnp.max(d_log, axis=-1))
        w_intra = jnp.exp(d_log - m_row[..., None])
        w_inter = jnp.exp(inter - m_row)
        sc = jnp.einsum('bhld,bhrd->bhlr', qc, kc) * w_intra
        num = (jnp.einsum('bhlr,bhrv->bhlv', sc, vc)
               + w_inter[..., None] * jnp.einsum('bhld,bhdv->bhlv', qc, c_st))
        den = jnp.sum(sc, axis=-1) + w_inter * jnp.einsum('bhld,bhd->bhl', qc, n_st)
        out = num / jnp.maximum(jnp.abs(den), jnp.exp(-m_row))[..., None]
        b_last = bcum[..., -1]
        g_log = b_last[..., None] - bcum + li
        m_new = jnp.maximum(b_last + m_st, jnp.max(g_log, axis=-1))
        wk = jnp.exp(g_log - m_new[..., None])
        decay = jnp.exp(b_last + m_st - m_new)
        c_new = decay[..., None, None] * c_st + jnp.einsum('bhl,bhld,bhlv->bhdv', wk, kc, vc)
        n_new = decay[..., None] * n_st + jnp.einsum('bhl,bhld->bhd', wk, kc)
        return (c_new, n_new, m_new), out

    init = (jnp.zeros((b, h, dk, dv), f32), jnp.zeros((b, h, dk), f32), jnp.zeros((b, h), f32))
    _, out = lax.scan(step, init, (to_chunks(q), to_chunks(k), to_chunks(v),
                                   to_chunks(log_f), to_chunks(log_i)))
    return from_chunks(out).astype(dtype)


def retention(q, k, v):
    dtype = v.dtype
    b, s, h, dk = q.shape
    dv = v.shape[-1]
    f32 = jnp.float32
    q = q.astype(f32)
    k = k.astype(f32) * dk ** -0.5
    v = v.astype(f32)
    log_gamma = jnp.log1p(-jnp.exp2(-5.0 - jnp.arange(h, dtype=f32)))
    pos = jnp.arange(CHUNK, dtype=f32)
    diff = pos[:, None] - pos[None, :]
    intra = jnp.where(diff >= 0, jnp.exp(jnp.maximum(diff, 0.0)[None] * log_gamma[:, None, None]), 0.0)
    q_decay = jnp.exp((pos[None, :] + 1.0) * log_gamma[:, None])
    k_decay = jnp.exp((CHUNK - 1.0 - pos[None, :]) * log_gamma[:, None])
    chunk_decay = jnp.exp(CHUNK * log_gamma)

    def step(state, inp):
        qc, kc, vc = inp
        sc = jnp.einsum('bhld,bhrd->bhlr', qc, kc) * intra
        out = (jnp.einsum('bhlr,bhrv->bhlv', sc, vc)
               + q_decay[..., None] * jnp.einsum('bhld,bhdv->bhlv', qc, state))
        state = chunk_decay[:, None, None] * state + jnp.einsum('bhrd,hr,bhrv->bhdv', kc, k_decay, vc)
        return state, out

    _, out = lax.scan(step, jnp.zeros((b, h, dk, dv), f32), (to_chunks(q), to_chunks(k), to_chunks(v)))
    return from_chunks(out).astype(dtype)


def dsa_attention(q_lat, c_kv, q_idx, k_idx, w_idx, rel_bias):
    b, s = c_kv.shape[:2]
    top_k = min(TOPK_MAX, s // 4)
    key_pos = jnp.arange(s, dtype=jnp.int32)
    f32 = jnp.float32

    def block(start):
        qb = lax.dynamic_slice_in_dim(q_lat, start, Q_BLOCK, axis=1)
        qib = lax.dynamic_slice_in_dim(q_idx, start, Q_BLOCK, axis=1)
        wb = lax.dynamic_slice_in_dim(w_idx, start, Q_BLOCK, axis=1)
        q_pos = start + jnp.arange(Q_BLOCK, dtype=jnp.int32)
        head_sc = jax.nn.relu(jnp.einsum('btgd,bsd->btgs', qib, k_idx).astype(f32))
        idx_sc = jnp.einsum('btg,btgs->bts', wb.astype(f32), head_sc)
        idx_sc = jnp.where(key_pos[None, None, :] <= q_pos[None, :, None], idx_sc, -jnp.inf)
        _, sel = lax.top_k(idx_sc, top_k)
        valid = sel <= q_pos[None, :, None]
        kv_sel = jax.vmap(lambda c, i: c[i])(c_kv, sel)
        logits = jnp.einsum('bthc,btkc->bthk', qb, kv_sel).astype(f32)
        bias = rel_bias.astype(f32)[t5_bucket(q_pos[None, :, None] - sel)]
        logits = logits + jnp.swapaxes(bias, -1, -2)
        logits = jnp.where(valid[:, :, None, :], logits, -jnp.inf)
        probs = jax.nn.softmax(logits, axis=-1).astype(c_kv.dtype)
        return jnp.einsum('bthk,btkc->bthc', probs, kv_sel)

    starts = jnp.arange(s // Q_BLOCK, dtype=jnp.int32) * Q_BLOCK
    out = lax.map(block, starts)
    return jnp.moveaxis(out, 0, 1).reshape(b, s, A_HEADS, KV_RANK)


def hybrid_layer(x, rel_bias, norm1_g, w_in, m_conv, m_ibias, m_fbias, m_norm_g,
                 a_qnorm_g, a_wuq, a_wuq_idx, a_kidx_g, a_kvnorm_g, a_wuk, a_wuv,
                 r_norm_g, p_m, p_a, p_r, w_out, norm2_g, w_gate, w_up, w_down):
    b, s, _ = x.shape
    h = rms_norm(x, norm1_g)
    z = h @ w_in
    split_points = [int(c) for c in np.cumsum(IN_SPLITS)[:-1]]
    (m_q, m_k, m_v, m_i, m_f, m_o, a_cq, a_kidx, a_widx, a_ckv,
     r_q, r_k, r_v, r_g, g_m, g_a, g_r) = jnp.split(z, split_points, axis=-1)

    qk = jax.nn.silu(causal_conv(jnp.concatenate([m_q, m_k], axis=-1), m_conv))
    mq, mk = jnp.split(qk, 2, axis=-1)
    hm = mlstm(mq.reshape(b, s, M_HEADS, M_QK), mk.reshape(b, s, M_HEADS, M_QK),
               m_v.reshape(b, s, M_HEADS, M_V), m_i + m_ibias, m_f + m_fbias)
    hm = head_layer_norm(hm, m_norm_g).reshape(b, s, M_WIDTH) * jax.nn.sigmoid(m_o)

    c_q = rms_norm(a_cq, a_qnorm_g)
    q = (c_q @ a_wuq).reshape(b, s, A_HEADS, A_HEAD_DIM)
    q_idx = (c_q @ a_wuq_idx).reshape(b, s, IDX_HEADS, IDX_DIM)
    k_idx = layer_norm(a_kidx, a_kidx_g)
    w_idx = a_widx * (IDX_HEADS ** -0.5 * IDX_DIM ** -0.5)
    c_kv = rms_norm(a_ckv, a_kvnorm_g)
    q_lat = jnp.einsum('bshd,hdc->bshc', q, a_wuk) * A_HEAD_DIM ** -0.5
    o_lat = dsa_attention(q_lat, c_kv, q_idx, k_idx, w_idx, rel_bias)
    ha = jnp.einsum('bshc,hcd->bshd', o_lat, a_wuv).reshape(b, s, A_WIDTH)

    pos = jnp.arange(s, dtype=jnp.float32)
    rq = rotary(r_q.reshape(b, s, R_HEADS, R_QK), pos)
    rk = rotary(r_k.reshape(b, s, R_HEADS, R_QK), pos)
    hr = retention(rq, rk, r_v.reshape(b, s, R_HEADS, R_V))
    hr = head_layer_norm(hr, r_norm_g).reshape(b, s, R_WIDTH) * jax.nn.silu(r_g)

    y = (jax.nn.sigmoid(g_m) * (hm @ p_m) + jax.nn.sigmoid(g_a) * (ha @ p_a)
         + jax.nn.sigmoid(g_r) * (hr @ p_r))
    x = x + y @ w_out

    h2 = rms_norm(x, norm2_g)
    return x + (jax.nn.silu(h2 @ w_gate) * (h2 @ w_up)) @ w_down


def setup_inputs(seed: int = 0) -> dict:
    key = jax.random.key(seed)
    keys = iter(jax.random.split(key, 32))
    f32 = jnp.float32

    def normal(shape, scale):
        return scale * jax.random.normal(next(keys), shape, f32)

    def gain(shape):
        return 1.0 + normal(shape, 0.02)

    L = DEPTH
    return {
        'x': normal((BATCH, SEQ, D_MODEL), 1.0),
        'rel_bias': normal((N_BUCKETS, A_HEADS), 0.2),
        'final_norm_g': gain((D_MODEL,)),
        'norm1_g': gain((L, D_MODEL)),
        'w_in': normal((L, D_MODEL, D_IN), D_MODEL ** -0.5),
        'm_conv': normal((L, CONV_W, 2 * M_HEADS * M_QK), CONV_W ** -0.5),
        'm_ibias': normal((L, M_HEADS), 0.1),
        'm_fbias': jnp.linspace(3.0, 6.0, M_HEADS, dtype=f32)[None, :] + normal((L, M_HEADS), 0.01),
        'm_norm_g': gain((L, M_WIDTH)),
        'a_qnorm_g': gain((L, Q_RANK)),
        'a_wuq': normal((L, Q_RANK, A_WIDTH), Q_RANK ** -0.5),
        'a_wuq_idx': normal((L, Q_RANK, IDX_HEADS * IDX_DIM), Q_RANK ** -0.5),
        'a_kidx_g': gain((L, IDX_DIM)),
        'a_kvnorm_g': gain((L, KV_RANK)),
        'a_wuk': normal((L, A_HEADS, A_HEAD_DIM, KV_RANK), A_HEAD_DIM ** -0.5),
        'a_wuv': normal((L, A_HEADS, KV_RANK, A_HEAD_DIM), KV_RANK ** -0.5),
        'r_norm_g': gain((L, R_WIDTH)),
        'p_m': normal((L, M_WIDTH, D_MODEL), M_WIDTH ** -0.5),
        'p_a': normal((L, A_WIDTH, D_MODEL), A_WIDTH ** -0.5),
        'p_r': normal((L, R_WIDTH, D_MODEL), R_WIDTH ** -0.5),
        'w_out': normal((L, D_MODEL, D_MODEL), D_MODEL ** -0.5),
        'norm2_g': gain((L, D_MODEL)),
        'w_gate': normal((L, D_MODEL, D_FF), D_MODEL ** -0.5),
        'w_up': normal((L, D_MODEL, D_FF), D_MODEL ** -0.5),
        'w_down': normal((L, D_FF, D_MODEL), D_FF ** -0.5),
    }


def reference(x, rel_bias, final_norm_g, norm1_g, w_in, m_conv, m_ibias, m_fbias, m_norm_g,
              a_qnorm_g, a_wuq, a_wuq_idx, a_kidx_g, a_kvnorm_g, a_wuk, a_wuv, r_norm_g,
              p_m, p_a, p_r, w_out, norm2_g, w_gate, w_up, w_down):
    for l in range(DEPTH):
        x = hybrid_layer(x, rel_bias, norm1_g[l], w_in[l], m_conv[l], m_ibias[l], m_fbias[l],
                         m_norm_g[l], a_qnorm_g[l], a_wuq[l], a_wuq_idx[l], a_kidx_g[l],
                         a_kvnorm_g[l], a_wuk[l], a_wuv[l], r_norm_g[l], p_m[l], p_a[l], p_r[l],
                         w_out[l], norm2_g[l], w_gate[l], w_up[l], w_down[l])
    return rms_norm(x, final_norm_g)
```

```python
import math
from contextlib import ExitStack
import numpy as np
import concourse.bass as bass
import concourse.mybir as mybir
from concourse.bass_utils import run_bass_kernel_spmd

F32 = mybir.dt.float32
BF16 = mybir.dt.bfloat16
AF = mybir.ActivationFunctionType
ALU = mybir.AluOpType

N_DMA_SEMS = 40
NCORES = 8
SEQ = 2048
NSEQ = 2
NTOK = NSEQ * SEQ
D = 1024
DFF = 2816
EPS = 1e-6
NEG = -1.0e30


def _key(x):
    if isinstance(x, tuple):
        return x[0].tensor.name + ":" + str(x[1])
    if isinstance(x, str):
        return x
    return x.tensor.name


def _ap(x):
    return x[0] if isinstance(x, tuple) else x


def _isnum(v):
    return isinstance(v, (int, float))


class Sched:
    ENG = ("pe", "act", "dve", "pool", "sp")

    def __init__(self, nc, es):
        self.nc = nc
        self.es = es
        self.obj = {"pe": nc.tensor, "act": nc.scalar, "dve": nc.vector, "pool": nc.gpsimd, "sp": nc.sync}
        self.count = {e: 0 for e in self.ENG}
        self.sem = {e: es.enter_context(nc.semaphore("s_" + e)) for e in ("pe", "act", "dve", "pool")}
        self.dsem = [es.enter_context(nc.semaphore("d%d" % i)) for i in range(N_DMA_SEMS)]
        self.dval = [0] * N_DMA_SEMS
        self.dma_n = 0
        self.last_w = {}
        self.readers = {}
        self.waited = {e: {} for e in self.ENG}
        self.n_inst = 0
        self.uid = 0

    def sb(self, name, shape, dt):
        self.uid += 1
        return self.es.enter_context(self.nc.sbuf_tensor("%s_u%d" % (name, self.uid), list(shape), dt))

    def ps(self, name, shape, dt):
        return self.es.enter_context(self.nc.psum_tensor(name, list(shape), dt))

    def _deps(self, eng, reads, writes):
        toks = set()
        for k in reads:
            t = self.last_w.get(k)
            if t is not None:
                toks.add(t)
        for k in writes:
            t = self.last_w.get(k)
            if t is not None:
                toks.add(t)
            for t in self.readers.get(k, ()):
                toks.add(t)
        best = {}
        for (sk, v) in toks:
            if sk == eng and eng == "pe":
                continue
            if v > best.get(sk, 0):
                best[sk] = v
        waits = []
        w = self.waited[eng]
        for sk, v in best.items():
            if w.get(sk, 0) >= v:
                continue
            w[sk] = v
            waits.append((sk, v))
        return waits

    def _commit(self, tok, reads, writes):
        for k in reads:
            self.readers.setdefault(k, []).append(tok)
        for k in writes:
            self.last_w[k] = tok
            self.readers[k] = []

    def _emit(self, eng, waits, fn, kind):
        engine = self.obj[eng]
        for sk, v in waits:
            engine.wait_ge(self._semof(sk), v)
        if fn is None:
            return
        ins = fn(engine)
        if kind[0] == "c":
            ins.then_inc(self.sem[kind[1]], 1)
        else:
            ins.then_inc(self.dsem[kind[1]], 16)

    def op(self, eng, fn, reads, writes):
        rk = [_key(r) for r in reads]
        wk = [_key(w) for w in writes]
        waits = self._deps(eng, rk, wk)
        self.count[eng] += 1
        tok = (eng, self.count[eng])
        self._emit(eng, waits, fn, ("c", eng))
        self._commit(tok, rk, wk)
        self.n_inst += 1

    def dma(self, out, in_, q="sp", **kw):
        rk = [_key(in_)]
        wk = [_key(out)]
        slot = self.dma_n % N_DMA_SEMS
        self.dma_n += 1
        waits = self._deps(q, rk, wk)
        sk = ("d", slot)
        prev = self.dval[slot]
        if prev > 0 and self.waited[q].get(sk, 0) < prev:
            self.waited[q][sk] = prev
            waits.append((sk, prev))
        self.dval[slot] = prev + 16
        tok = (sk, prev + 16)
        o, i = _ap(out), _ap(in_)
        self._emit(q, waits, (lambda e: e.dma_start(out=o, in_=i, **kw)), ("d", slot))
        self._commit(tok, rk, wk)
        self.n_inst += 1

    def barrier(self):
        for e in self.ENG:
            waits = []
            w = self.waited[e]
            for e2 in ("pe", "act", "dve", "pool"):
                v = self.count[e2]
                if v == 0 or w.get(e2, 0) >= v:
                    continue
                if e2 == e and e == "pe":
                    continue
                w[e2] = v
                waits.append((e2, v))
            for s in range(N_DMA_SEMS):
                v = self.dval[s]
                sk = ("d", s)
                if v == 0 or w.get(sk, 0) >= v:
                    continue
                w[sk] = v
                waits.append((sk, v))
            if waits:
                self._emit(e, waits, None, None)
        self.last_w = {}
        self.readers = {}

    def _semof(self, sk):
        if isinstance(sk, tuple):
            return self.dsem[sk[1]]
        return self.sem[sk]

    def mm(self, out, lhsT, rhs, start=True, stop=True):
        o, l, r = _ap(out), _ap(lhsT), _ap(rhs)
        self.op("pe", lambda e: e.matmul(o, l, r, start=start, stop=stop), [lhsT, rhs], [out])

    def tr(self, out, in_, ident):
        o, i, d = _ap(out), _ap(in_), _ap(ident)
        self.op("pe", lambda e: e.transpose(o, i, d), [in_, ident], [out])

    def act(self, out, in_, func, bias=None, scale=None, accum_out=None):
        o, i = _ap(out), _ap(in_)
        kw = {}
        reads = [in_]
        writes = [out]
        if bias is not None:
            if _isnum(bias):
                kw["bias"] = bias
            else:
                kw["bias"] = _ap(bias)
                reads.append(bias)
        if scale is not None:
            if _isnum(scale):
                kw["scale"] = scale
            else:
                kw["scale"] = _ap(scale)
                reads.append(scale)
        if accum_out is not None:
            kw["accum_out"] = _ap(accum_out)
            writes.append(accum_out)
        self.op("act", lambda e: e.activation(o, i, func, **kw), reads, writes)

    def ts(self, out, in0, s1, s2, op0, op1=None, eng="dve"):
        o, i = _ap(out), _ap(in0)
        reads = [in0]
        a1, a2 = s1, s2
        if s1 is not None and not _isnum(s1):
            reads.append(s1)
            a1 = _ap(s1)
        if s2 is not None and not _isnum(s2):
            reads.append(s2)
            a2 = _ap(s2)
        if op1 is None:
            self.op(eng, lambda e: e.tensor_scalar(o, i, a1, None, op0), reads, [out])
        else:
            self.op(eng, lambda e: e.tensor_scalar(o, i, a1, a2, op0, op1), reads, [out])

    def tt(self, out, in0, in1, op, eng="dve"):
        o, a, b = _ap(out), _ap(in0), _ap(in1)
        self.op(eng, lambda e: e.tensor_tensor(o, a, b, op), [in0, in1], [out])

    def stt(self, out, in0, scalar, in1, op0, op1):
        o, a, b = _ap(out), _ap(in0), _ap(in1)
        reads = [in0, in1]
        s = scalar
        if not _isnum(scalar):
            reads.append(scalar)
            s = _ap(scalar)
        self.op("dve", lambda e: e.scalar_tensor_tensor(o, a, s, b, op0, op1), reads, [out])

    def copy(self, out, in_, eng="dve"):
        o, i = _ap(out), _ap(in_)
        if eng == "act":
            self.op("act", lambda e: e.copy(o, i), [in_], [out])
        else:
            self.op(eng, lambda e: e.tensor_copy(o, i), [in_], [out])

    def memset(self, out, val, eng="dve"):
        o = _ap(out)
        self.op(eng, lambda e: e.memset(o, val), [], [out])

    def recip(self, out, in_):
        o, i = _ap(out), _ap(in_)
        self.op("dve", lambda e: e.reciprocal(o, i), [in_], [out])

    def max8(self, out, in_):
        o, i = _ap(out), _ap(in_)
        self.op("dve", lambda e: e.max(o, i), [in_], [out])

    def mrep(self, out, rep, vals, imm):
        o, r, v = _ap(out), _ap(rep), _ap(vals)
        self.op("dve", lambda e: e.match_replace(o, r, v, imm), [rep, vals], [out])

    def bnstats(self, out, in_):
        o, i = _ap(out), _ap(in_)
        self.op("dve", lambda e: e.bn_stats(o, i), [in_], [out])

    def bnaggr(self, out, in_):
        o, i = _ap(out), _ap(in_)
        self.op("dve", lambda e: e.bn_aggr(o, i), [in_], [out])


def _t5_bucket_np(dist):
    dist = np.asarray(dist, dtype=np.int64)
    max_exact = 16
    d_f = np.maximum(dist, 1).astype(np.float32)
    large = max_exact + (np.log(d_f / np.float32(max_exact)) / np.float32(math.log(128 / max_exact))
                         * np.float32(32 - max_exact)).astype(np.int32)
    large = np.minimum(large, 31)
    return np.where(dist < max_exact, dist, large).astype(np.int64)


def host_constants():
    c = {}
    r = np.arange(128)
    c["c_ident"] = np.eye(128, dtype=np.float32)
    c["c_tri"] = (r[:, None] <= r[None, :]).astype(np.float32)
    c["c_ones"] = np.ones((128, 128), np.float32)
    c["c_negd"] = np.where(r[None, :] <= r[:, None], 0.0, NEG).astype(np.float32)
    half = 64
    freqs = (10000.0 ** (-np.linspace(0.0, 1.0, half, dtype=np.float32))).astype(np.float32)
    pos = np.arange(SEQ, dtype=np.float32)
    ang = (pos[None, :] * freqs[:, None]).astype(np.float32)
    cos = np.cos(ang).astype(np.float32)
    sin = np.sin(ang).astype(np.float32)
    c["c_cos"] = np.concatenate([cos, cos], 0)
    c["c_sin"] = np.concatenate([sin, sin], 0)
    h = np.arange(4, dtype=np.float64)
    lg = np.log1p(-np.exp2(-5.0 - h))
    rr = np.arange(128, dtype=np.float64)
    c["c_rete"] = np.exp(-(rr[:, None] + 1.0) * lg[None, :]).astype(np.float32)
    c["c_retrow"] = (np.exp((rr[:, None] + 1.0) * lg[None, :]) * 128 ** -0.5).astype(np.float32)
    c["ret_g"] = [float(np.exp(128.0 * v)) for v in lg]
    return c


def bias_index():
    t = np.arange(128)
    out = np.zeros((128, 2, 128), np.int64)
    for off in range(2):
        d = t[:, None] - t[None, :] + 128 * off
        out[:, off, :] = _t5_bucket_np(np.maximum(d, 0))
    return out


OFF = dict(m_q=0, m_k=256, m_v=512, m_i=1024, m_f=1028, m_o=1032, a_cq=1544, a_kidx=1800,
           a_widx=1864, a_ckv=1872, r_q=2000, r_k=2512, r_v=3024, r_g=3536, g_m=4048, g_a=5072,
           g_r=6096)
NFM = 5632
NTM = 2512
ZS_CQ, ZS_CKV, ZS_KIDX, ZS_WIDX, ZS_MI, ZS_MF, ZS_N = 0, 256, 384, 448, 456, 460, 464


def _rot_pre(w):
    w4 = w.reshape(w.shape[0], 4, 2, 64)
    return np.ascontiguousarray(w4[:, :, ::-1, :]).reshape(w.shape[0], 512)


def prep_layer_weights(inp, l):
    w = inp["w_in"][l]
    sl = lambda name, n: w[:, OFF[name]:OFF[name] + n]
    rq, rk = sl("r_q", 512), sl("r_k", 512)
    w_fm = np.concatenate([sl("m_q", 256), sl("m_k", 256), rq, _rot_pre(rq), rk, _rot_pre(rk),
                           sl("g_m", 1024), sl("g_a", 1024), sl("g_r", 1024)], axis=1)
    w_tm = np.concatenate([sl("m_v", 512), sl("m_o", 512), sl("r_v", 512), sl("r_g", 512),
                           sl("a_cq", 256), sl("a_ckv", 128), sl("a_kidx", 64), sl("a_widx", 8),
                           sl("m_i", 4), sl("m_f", 4)], axis=1)
    assert w_fm.shape[1] == NFM and w_tm.shape[1] == NTM
    rep = lambda v: np.ascontiguousarray(np.broadcast_to(np.asarray(v, np.float32).reshape(1, -1), (128, np.asarray(v).size)))
    colv = lambda v: np.ascontiguousarray(np.asarray(v, np.float32).reshape(-1, 128).T)
    d = {
        "w_fm": np.ascontiguousarray(w_fm), "w_tm": np.ascontiguousarray(w_tm),
        "n1g": colv(inp["norm1_g"][l]), "n2g": colv(inp["norm2_g"][l]),
        "mconv": np.ascontiguousarray(inp["m_conv"][l].reshape(4, 4, 128).transpose(2, 1, 0)),
        "mbias": rep(np.tile(np.concatenate([inp["m_ibias"][l], inp["m_fbias"][l]]), 16)),
        "mng": rep(inp["m_norm_g"][l]), "rng": rep(inp["r_norm_g"][l]),
        "aqg": colv(inp["a_qnorm_g"][l]),
        "akg": colv(np.concatenate([inp["a_kidx_g"][l], inp["a_kidx_g"][l]])),
        "akvg": rep(inp["a_kvnorm_g"][l]),
        "wuq": np.ascontiguousarray(inp["a_wuq"][l]), "wuqi": np.ascontiguousarray(inp["a_wuq_idx"][l]),
        "wuk": np.ascontiguousarray(inp["a_wuk"][l].reshape(4, 2, 64, 128).transpose(1, 2, 0, 3).reshape(128, 4, 128)),
        "wuv": np.ascontiguousarray(inp["a_wuv"][l].transpose(1, 0, 2)),
        "p_m": np.ascontiguousarray(inp["p_m"][l]), "p_a": np.ascontiguousarray(inp["p_a"][l]),
        "p_r": np.ascontiguousarray(inp["p_r"][l]), "w_out": np.ascontiguousarray(inp["w_out"][l]),
        "w_gate": np.ascontiguousarray(inp["w_gate"][l]), "w_up": np.ascontiguousarray(inp["w_up"][l]),
        "w_down": np.ascontiguousarray(inp["w_down"][l]),
    }
    return {"%s_%d" % (k, l): v for k, v in d.items()}


LAYER_SHAPES = {
    "w_fm": [D, NFM], "w_tm": [D, NTM], "n1g": [128, 8], "n2g": [128, 8], "mconv": [128, 4, 4],
    "mbias": [128, 128], "mng": [128, 512], "rng": [128, 512], "aqg": [128, 2], "akg": [128, 1],
    "akvg": [128, 128], "wuq": [256, 512], "wuqi": [256, 512], "wuk": [128, 4, 128],
    "wuv": [128, 8, 64], "p_m": [512, D], "p_a": [512, D], "p_r": [512, D], "w_out": [D, D],
    "w_gate": [D, DFF], "w_up": [D, DFF], "w_down": [DFF, D],
}
CONST_SHAPES = {"c_ident": [128, 128], "c_tri": [128, 128], "c_ones": [128, 128], "c_negd": [128, 128],
                "c_cos": [128, SEQ], "c_sin": [128, SEQ], "c_rete": [128, 4], "c_retrow": [128, 4],
                "biasn": [128, 8, 2, 128], "rb31": [128, 8], "fng": [128, D]}


class Prog:
    pass


def _scope(S):
    class _Sc:
        def __enter__(self_):
            self_.old = S.es
            self_.es = ExitStack()
            self_.es.__enter__()
            S.es = self_.es
            return self_

        def __exit__(self_, *a):
            S.barrier()
            S.es = self_.old
            return self_.es.__exit__(*a)
    return _Sc()


def load_cast(P, dst, src, K, N, gain=None, stg=None, engs=("pool", "dve", "act")):
    S = P.S
    kc = K // 128
    srcv = src.rearrange("(c p) n -> p c n", p=128)
    CH = stg[0].shape[1]
    for c in range(kc):
        for n0 in range(0, N, CH):
            n1 = min(N, n0 + CH)
            sbuf = stg[P.stg_i % len(stg)]
            eng = engs[P.stg_i % len(engs)]
            P.stg_i += 1
            S.dma(sbuf[:, 0:n1 - n0], srcv[:, c, n0:n1])
            if gain is None:
                S.copy(dst[:, c, n0:n1], sbuf[:, 0:n1 - n0], eng=eng)
            elif eng == "act":
                S.act(dst[:, c, n0:n1], sbuf[:, 0:n1 - n0], AF.Copy, scale=gain[:, c:c + 1])
            else:
                S.ts(dst[:, c, n0:n1], sbuf[:, 0:n1 - n0], gain[:, c:c + 1], None, ALU.mult, eng=eng)


def dma_chunks(S, out, in_, n, step):
    for a in range(0, n, step):
        b = min(n, a + step)
        S.dma(out[:, a:b], in_[:, a:b])


def rms_rstd(P, rs, ss, n):
    S = P.S
    S.ts(rs, ss, 1.0 / n, EPS, ALU.mult, ALU.add)
    S.act(rs, rs, AF.Sqrt)
    S.recip(rs, rs)


def norm_transpose(P, xt, hb, junk, ss, rs, hT, col0, ptb):
    S = P.S
    S.act(junk[:], xt[:], AF.Square, accum_out=ss[:])
    rms_rstd(P, rs[:], ss[:], D)
    S.ts(hb[:], xt[:], rs[:], None, ALU.mult)
    for k in range(8):
        S.tr(ptb[:, k * 128:(k + 1) * 128], hb[:, k * 128:(k + 1) * 128], P.identb[:])
    S.copy(hT[:, :, col0:col0 + 128], ptb[:, :].rearrange("p (k t) -> p k t", k=8), eng="act")


def stage_A1(P, l, x_in):
    S, G = P.S, P.G
    with _scope(S):
        Wfm = S.sb("Wfm", [128, 8, NFM], BF16)
        stg = [S.sb("stgA%d" % i, [128, 2048], F32) for i in range(3)]
        cos = S.sb("cos", [128, SEQ], F32)
        sin = S.sb("sin", [128, SEQ], F32)
        n1g = S.sb("n1g", [128, 8], F32)
        xb = [S.sb("xbA%d" % i, [128, D], F32) for i in range(2)]
        hb = S.sb("hbA", [128, D], BF16)
        junk = S.sb("junkA", [128, D], BF16)
        ss = S.sb("ssA", [128, 1], F32)
        rs = S.sb("rsA", [128, 1], F32)
        hT = S.sb("hTA", [128, 8, 512], BF16)
        ev32 = [S.sb("ev32A%d" % i, [128, 512], F32) for i in range(4)]
        evb = [S.sb("evbA%d" % i, [128, 512], BF16) for i in range(4)]
        S.dma(n1g[:], G["n1g_%d" % l])
        S.dma(cos[:], G["c_cos"])
        S.dma(sin[:], G["c_sin"])
        load_cast(P, Wfm, G["w_fm_%d" % l], D, NFM, gain=n1g, stg=stg)
        for c in range(8):
            for base in (1024, 2048):
                v = Wfm[:, c, base:base + 512].rearrange("p (h t j) -> p h t j", h=4, t=2)[:, :, 0, :]
                S.ts(v, v, -1.0, None, ALU.mult, eng="pool")
        hTd = G["hT_d"].rearrange("(k p) t -> p k t", p=128)
        e32 = 0
        eb = 0
        bank = 0
        for st in range(NTOK // 512):
            t0 = st * 512
            p0 = t0 % SEQ
            for ti in range(4):
                xt = xb[ti % 2]
                S.dma(xt[:], x_in[t0 + ti * 128:t0 + (ti + 1) * 128, :])
                norm_transpose(P, xt, hb, junk, ss, rs, hT, ti * 128, P.pbB[ti % 2])
            dma_chunks(S, hTd[:, :, t0:t0 + 512], hT, 8, 4)

            def proj(c):
                nonlocal bank
                ps = P.pbF[bank % 6]
                bank += 1
                for k in range(8):
                    S.mm(ps[:], Wfm[:, k, c * 128:(c + 1) * 128], hT[:, k, :], start=(k == 0), stop=(k == 7))
                return ps
            for c in range(4):
                ps = proj(c)
                o = ev32[e32 % 4]
                e32 += 1
                S.copy(o[:], ps[:], eng="act")
                S.dma(G["qkT"][c * 128:(c + 1) * 128, t0:t0 + 512], o[:])
            for which, dst in ((0, "rqT"), (1, "rkT")):
                for hh in range(4):
                    ca = 4 + which * 8 + hh
                    pa = proj(ca)
                    pbk = proj(ca + 4)
                    t1 = ev32[e32 % 4]
                    t2 = ev32[(e32 + 1) % 4]
                    e32 += 2
                    S.tt(t1[:], pa[:], cos[:, p0:p0 + 512], ALU.mult)
                    S.tt(t2[:], pbk[:], sin[:, p0:p0 + 512], ALU.mult)
                    ob = evb[eb % 4]
                    eb += 1
                    S.tt(ob[:], t1[:], t2[:], ALU.add, eng="pool")
                    S.dma(G[dst][hh * 128:(hh + 1) * 128, t0:t0 + 512], ob[:])
            for c in range(24):
                ps = proj(20 + c)
                ob = evb[eb % 4]
                eb += 1
                S.act(ob[:], ps[:], AF.Sigmoid)
                S.dma(G["gT"][c * 128:(c + 1) * 128, t0:t0 + 512], ob[:])


def stage_A2(P, l):
    S, G = P.S, P.G
    with _scope(S):
        Wtm = S.sb("Wtm", [128, 8, NTM], BF16)
        stg = [S.sb("stgB%d" % i, [128, 2048], F32) for i in range(3)]
        n1g = S.sb("n1gB", [128, 8], F32)
        hTs = [S.sb("hTB%d" % i, [128, 8, 512], BF16) for i in range(2)]
        evb = [S.sb("evbB%d" % i, [128, 512], BF16) for i in range(6)]
        ev32 = [S.sb("ev32B%d" % i, [128, 512], F32) for i in range(2)]
        S.dma(n1g[:], G["n1g_%d" % l])
        load_cast(P, Wtm, G["w_tm_%d" % l], D, NTM, gain=n1g, stg=stg)
        hTd = G["hT_d"].rearrange("(k p) t -> p k t", p=128)
        groups = [(0, 512, "zv", "copy"), (512, 512, "zo", "sig"), (1024, 512, "rv", "copy"),
                  (1536, 512, "rg", "silu"), (2048, ZS_N, "zs", "f32")]
        bank = 0
        eb = 0
        e32 = 0
        for st in range(NTOK // 512):
            t0 = st * 512
            hT = hTs[st % 2]
            dma_chunks(S, hT, hTd[:, :, t0:t0 + 512], 8, 4)
            for ti in range(4):
                r0 = t0 + ti * 128
                for (c0, n, dst, kind) in groups:
                    ps = P.pbF[bank % 6]
                    bank += 1
                    for k in range(8):
                        S.mm(ps[:, 0:n], hT[:, k, ti * 128:(ti + 1) * 128], Wtm[:, k, c0:c0 + n],
                             start=(k == 0), stop=(k == 7))
                    if kind == "f32":
                        o = ev32[e32 % 2]
                        e32 += 1
                        S.copy(o[:, 0:n], ps[:, 0:n], eng="dve")
                    else:
                        o = evb[eb % 6]
                        eb += 1
                        if kind == "copy":
                            S.copy(o[:, 0:n], ps[:, 0:n], eng="dve")
                        elif kind == "sig":
                            S.act(o[:, 0:n], ps[:, 0:n], AF.Sigmoid)
                        else:
                            S.act(o[:, 0:n], ps[:, 0:n], AF.Silu)
                    S.dma(G[dst][r0:r0 + 128, :], o[:, 0:n])


def ln_gate_out(P, pN, s2, hp, n, o32, gain_t, gate_all, hm, tmp):
    S = P.S
    st6, mv, t2, re = tmp
    for hh in range(2):
        S.bnstats(st6[:, hh, :], pN[:, hh, 0:128])
        S.bnaggr(mv[:, hh, :], st6[:, hh, :])
    S.tt(t2[:], s2, s2, ALU.mult)
    S.tt(t2[:], t2[:], mv[:, :, 1], ALU.mult)
    S.ts(t2[:], t2[:], EPS, None, ALU.add)
    S.act(t2[:], t2[:], AF.Sqrt)
    S.recip(t2[:], t2[:])
    S.tt(re[:], t2[:], s2, ALU.mult)
    for hh in range(2):
        S.ts(o32[:, hh * 128:(hh + 1) * 128], pN[:, hh, 0:128], mv[:, hh, 0:1], re[:, hh:hh + 1],
             ALU.subtract, ALU.mult)
    S.tt(o32[:], o32[:], gain_t[:, hp * 256:(hp + 1) * 256], ALU.mult, eng="pool")
    S.tt(hm[:, hp * 256:(hp + 1) * 256], o32[:], gate_all[:, n, hp * 256:(hp + 1) * 256], ALU.mult, eng="pool")


def out_transpose(P, hm, hmTs, dstT, tg, ptb):
    S = P.S
    for c in range(4):
        S.tr(ptb[:, c * 128:(c + 1) * 128], hm[:, c * 128:(c + 1) * 128], P.identb[:])
    S.copy(hmTs[:], ptb[:, 0:512].rearrange("p (c t) -> p c t", c=4), eng="act")
    S.dma(dstT.rearrange("(c p) t -> p c t", p=128)[:, :, tg:tg + 128], hmTs[:])


def stage_B(P, l):
    S, G = P.S, P.G
    NCH = SEQ // 128
    with _scope(S):
        qkraw = S.sb("qkraw", [128, 4, SEQ + 3], F32)
        cacc = [S.sb("cacc%d" % i, [128, SEQ], F32) for i in range(2)]
        qkb = S.sb("qkb", [128, 4, SEQ], BF16)
        qA = S.sb("qA", [128, 2, SEQ], BF16)
        qB = S.sb("qB", [128, 2, SEQ], BF16)
        mconv = S.sb("mconv", [128, 4, 4], F32)
        mbias = S.sb("mbias", [128, 128], F32)
        mng = S.sb("mng", [128, 512], F32)
        gi = S.sb("gi", [128, NCH, 8], F32)
        ax = S.sb("ax", [128, NCH, 4], F32)
        lf = S.sb("lf", [128, NCH, 4], F32)
        lia = S.sb("lia", [128, NCH, 4], F32)
        e_all = S.sb("e_all", [128, NCH, 4], F32)
        rowsc = S.sb("rowsc", [128, NCH, 4], F32)
        g_all = S.sb("g_all", [128, NCH, 4], F32)
        v_all = S.sb("v_all", [128, NCH, 512], BF16)
        mo_all = S.sb("mo_all", [128, NCH, 512], BF16)
        vt = S.sb("vt", [128, NCH, 4, 130], BF16)
        ktok = [S.sb("ktok%d" % i, [128, 256], BF16) for i in range(2)]
        scTm = [S.sb("scTm%d" % i, [128, 256], BF16) for i in range(2)]
        P32 = S.sb("P32", [128, 2, 129], F32)
        Cb = S.sb("Cb", [128, 2, 130], BF16)
        hm = [S.sb("hm%d" % i, [128, 512], BF16) for i in range(2)]
        hmTs = [S.sb("hmTs%d" % i, [128, 4, 128], BF16) for i in range(2)]
        o32 = [S.sb("o32%d" % i, [128, 256], F32) for i in range(2)]
        d1 = S.sb("d1", [128, 2], F32)
        s2 = S.sb("s2", [128, 2], F32)
        tmp = (S.sb("st6", [128, 2, 6], F32), S.sb("mv", [128, 2, 2], F32),
               S.sb("t2", [128, 2], F32), S.sb("re", [128, 2], F32))
        S.dma(mconv[:], G["mconv_%d" % l])
        S.dma(mbias[:], G["mbias_%d" % l])
        S.dma(mng[:], G["mng_%d" % l])
        S.memset(qkraw[:, :, 0:3], 0.0)
        S.memset(qA[64:128, :, :], 0.0, eng="pool")
        S.memset(qB[0:64, :, :], 0.0, eng="pool")
        S.memset(vt[:], 0.0, eng="pool")
        bank = 0
        for s in range(NSEQ):
            tb = s * SEQ
            for c in range(4):
                S.dma(qkraw[:, c, 3:SEQ + 3], G["qkT"][c * 128:(c + 1) * 128, tb:tb + SEQ])
            for c in range(4):
                acc = cacc[c % 2]
                S.ts(acc[:], qkraw[:, c, 0:SEQ], mconv[:, c, 0:1], None, ALU.mult)
                for j in range(1, 4):
                    S.stt(acc[:], qkraw[:, c, j:j + SEQ], mconv[:, c, j:j + 1], acc[:], ALU.mult, ALU.add)
                if c < 2:
                    S.act(qA[0:64, c, :], acc[0:64, :], AF.Silu)
                    S.act(qB[64:128, c, :], acc[64:128, :], AF.Silu)
                else:
                    S.act(qkb[:, c, :], acc[:], AF.Silu)
            dma_chunks(S, gi, G["zs"][tb:tb + SEQ, ZS_MI:ZS_MI + 8].rearrange("(n p) c -> p n c", p=128), NCH, 4)
            gif = gi[:].rearrange("p n c -> p (n c)")
            S.tt(gif, gif, mbias[:], ALU.add)
            S.act(ax[:], gi[:, :, 4:8], AF.Abs)
            S.act(ax[:], ax[:], AF.Exp, scale=-1.0)
            S.act(ax[:], ax[:], AF.Ln, bias=1.0)
            S.ts(lf[:], gi[:, :, 4:8], 0.0, None, ALU.min)
            S.tt(lf[:], lf[:], ax[:], ALU.subtract)
            pg = P.pbF[bank % 6]
            bank += 1
            lff = lf[:].rearrange("p n c -> p (n c)")
            S.mm(pg[:, 0:64], P.tri32[:], lff)
            S.mm(pg[:, 64:128], P.ones32[:], lff)
            S.tt(lia[:], gi[:, :, 0:4], pg[:, 0:64].rearrange("p (n c) -> p n c", c=4), ALU.subtract)
            S.act(e_all[:], lia[:], AF.Exp)
            S.act(rowsc[:].rearrange("p n c -> p (n c)"), pg[:, 0:64], AF.Exp, bias=math.log(0.125))
            S.act(g_all[:].rearrange("p n c -> p (n c)"), pg[:, 64:128], AF.Exp)
            dma_chunks(S, v_all, G["zv"][tb:tb + SEQ, :].rearrange("(n p) c -> p n c", p=128), NCH, 4)
            dma_chunks(S, mo_all, G["zo"][tb:tb + SEQ, :].rearrange("(n p) c -> p n c", p=128), NCH, 4)
            for n in range(NCH):
                for h in range(4):
                    S.ts(vt[:, n, h, 0:128], v_all[:, n, h * 128:(h + 1) * 128], e_all[:, n, h:h + 1], None,
                         ALU.mult, eng="pool")
            S.copy(vt[:, :, :, 128], e_all[:], eng="pool")
            for n in range(NCH):
                cs = slice(n * 128, (n + 1) * 128)
                kt = ktok[n % 2]
                ptb = P.pbB[n % 2]
                for hc in range(2):
                    S.tr(ptb[:, hc * 128:(hc + 1) * 128], qkb[:, 2 + hc, cs], P.identb[:])
                S.copy(kt[:], ptb[:, 0:256], eng="act")
                hmc = hm[n % 2]
                for hp in range(2):
                    psc = P.pbF[bank % 6]
                    bank += 1
                    for hh in range(2):
                        qX = qA if hh == 0 else qB
                        S.mm(psc[:, hh * 128:(hh + 1) * 128], qkb[:, 2 + hp, cs], qX[:, hp, cs])
                    sm = scTm[hp]
                    S.tt(sm[:], psc[:, 0:256], P.trib2[:], ALU.mult)
                    pNb = P.pbF[bank % 6]
                    bank += 1
                    pN = pNb[:, 0:258].rearrange("p (a b) -> p a b", a=2)
                    for hh in range(2):
                        h = 2 * hp + hh
                        pr = slice(64 * hh, 64 * hh + 64)
                        S.mm(pN[:, hh, :], sm[:, hh * 128:(hh + 1) * 128], vt[:, n, h, 0:129], start=True, stop=(n == 0))
                        if n > 0:
                            qX = qA if hh == 0 else qB
                            S.mm(pN[:, hh, :], qX[:, hp, cs], Cb[:, hp, 0:129], start=False, stop=True)
                    if n < NCH - 1:
                        pUb = P.pbF[bank % 6]
                        bank += 1
                        pU = pUb[:, 0:260].rearrange("p (a b) -> p a b", a=2)
                        S.mm(pUb[:, 0:260], kt[:, hp * 128:(hp + 1) * 128],
                             vt[:, n, 2 * hp:2 * hp + 2, :].rearrange("p a b -> p (a b)"))
                        for hh in range(2):
                            h = 2 * hp + hh
                            pr = slice(64 * hh, 64 * hh + 64)
                            if n == 0:
                                S.copy(P32[pr, hp, :], pU[pr, hh, 0:129])
                            else:
                                S.stt(P32[pr, hp, :], P32[pr, hp, :], g_all[pr, n - 1, h:h + 1], pU[pr, hh, 0:129],
                                      ALU.mult, ALU.add)
                            S.ts(Cb[pr, hp, 0:129], P32[pr, hp, :], g_all[pr, n, h:h + 1], None, ALU.mult)
                    rsl = rowsc[:, n, 2 * hp:2 * hp + 2]
                    S.tt(d1[:], pN[:, :, 128], rsl, ALU.mult)
                    S.act(d1[:], d1[:], AF.Abs)
                    S.ts(d1[:], d1[:], 1.0, None, ALU.max)
                    S.recip(d1[:], d1[:])
                    S.tt(s2[:], d1[:], rsl, ALU.mult)
                    ln_gate_out(P, pN, s2[:], hp, n, o32[hp], mng, mo_all, hmc, tmp)
                out_transpose(P, hmc, hmTs[n % 2], G["hmT"], tb + n * 128, P.pbB[(n + 1) % 2])


def stage_D(P, l):
    S, G = P.S, P.G
    NCH = SEQ // 128
    gam = P.ret_g
    with _scope(S):
        rq = S.sb("rq", [128, 4, SEQ], BF16)
        rk = S.sb("rk", [128, 4, SEQ], BF16)
        rng = S.sb("rng", [128, 512], F32)
        rete = S.sb("rete", [128, 4], F32)
        retrow = S.sb("retrow", [128, 4], F32)
        v_all = S.sb("rv_all", [128, NCH, 512], BF16)
        rg_all = S.sb("rg_all", [128, NCH, 512], BF16)
        vt = S.sb("rvt", [128, NCH, 512], BF16)
        ktok = [S.sb("rktok%d" % i, [128, 512], BF16) for i in range(2)]
        scTm = [S.sb("rscTm%d" % i, [128, 256], BF16) for i in range(2)]
        P32 = S.sb("rP32", [128, 4, 128], F32)
        Sb = S.sb("rSb", [128, 4, 128], BF16)
        hm = [S.sb("rhm%d" % i, [128, 512], BF16) for i in range(2)]
        hmTs = [S.sb("rhmTs%d" % i, [128, 4, 128], BF16) for i in range(2)]
        o32 = [S.sb("ro32%d" % i, [128, 256], F32) for i in range(2)]
        tmp = (S.sb("rst6", [128, 2, 6], F32), S.sb("rmv", [128, 2, 2], F32),
               S.sb("rt2", [128, 2], F32), S.sb("rre", [128, 2], F32))
        S.dma(rng[:], G["rng_%d" % l])
        S.dma(rete[:], G["c_rete"])
        S.dma(retrow[:], G["c_retrow"])
        bank = 0
        for s in range(NSEQ):
            tb = s * SEQ
            S.dma(rq[:], G["rqT"].rearrange("(c p) t -> p c t", p=128)[:, :, tb:tb + SEQ])
            S.dma(rk[:], G["rkT"].rearrange("(c p) t -> p c t", p=128)[:, :, tb:tb + SEQ])
            dma_chunks(S, v_all, G["rv"][tb:tb + SEQ, :].rearrange("(n p) c -> p n c", p=128), NCH, 4)
            dma_chunks(S, rg_all, G["rg"][tb:tb + SEQ, :].rearrange("(n p) c -> p n c", p=128), NCH, 4)
            for h in range(4):
                S.ts(vt[:, :, h * 128:(h + 1) * 128], v_all[:, :, h * 128:(h + 1) * 128], rete[:, h:h + 1], None,
                     ALU.mult, eng="pool")
            for n in range(NCH):
                cs = slice(n * 128, (n + 1) * 128)
                kt = ktok[n % 2]
                ptb = P.pbB[n % 2]
                for h in range(4):
                    S.tr(ptb[:, h * 128:(h + 1) * 128], rk[:, h, cs], P.identb[:])
                S.copy(kt[:], ptb[:, 0:512], eng="act")
                hmc = hm[n % 2]
                for hp in range(2):
                    psc = P.pbF[bank % 6]
                    bank += 1
                    for hh in range(2):
                        h = 2 * hp + hh
                        S.mm(psc[:, hh * 128:(hh + 1) * 128], rk[:, h, cs], rq[:, h, cs])
                    sm = scTm[hp]
                    S.tt(sm[:], psc[:, 0:256], P.trib2[:], ALU.mult)
                    pNb = P.pbF[bank % 6]
                    bank += 1
                    pN = pNb[:, 0:256].rearrange("p (a b) -> p a b", a=2)
                    for hh in range(2):
                        h = 2 * hp + hh
                        S.mm(pN[:, hh, :], sm[:, hh * 128:(hh + 1) * 128], vt[:, n, h * 128:(h + 1) * 128],
                             start=True, stop=(n == 0))
                        if n > 0:
                            S.mm(pN[:, hh, :], rq[:, h, cs], Sb[:, h, :], start=False, stop=True)
                    if n < NCH - 1:
                        pUb = P.pbF[bank % 6]
                        bank += 1
                        for hh in range(2):
                            h = 2 * hp + hh
                            S.mm(pUb[:, hh * 128:(hh + 1) * 128], kt[:, h * 128:(h + 1) * 128],
                                 vt[:, n, h * 128:(h + 1) * 128])
                        for hh in range(2):
                            h = 2 * hp + hh
                            pu = pUb[:, hh * 128:(hh + 1) * 128]
                            if n == 0:
                                S.copy(P32[:, h, :], pu)
                            else:
                                S.stt(P32[:, h, :], P32[:, h, :], gam[h], pu, ALU.mult, ALU.add)
                            S.ts(Sb[:, h, :], P32[:, h, :], gam[h], None, ALU.mult)
                    ln_gate_out(P, pN, retrow[:, 2 * hp:2 * hp + 2], hp, n, o32[hp], rng, rg_all, hmc, tmp)
                out_transpose(P, hmc, hmTs[n % 2], G["hrT"], tb + n * 128, P.pbB[(n + 1) % 2])


def stage_C(P, l):
    import os
    CUT = int(os.environ.get("C_CUT", "99"))
    C1 = int(os.environ.get("C1_CUT", "99"))
    S, G = P.S, P.G
    NT = SEQ // 128
    WSC = (8 ** -0.5) * (64 ** -0.5)
    with _scope(S):
        stg = [S.sb("stgC%d" % i, [128, 2048], F32) for i in range(2)]
        aqg = S.sb("aqg", [128, 2], F32)
        akg = S.sb("akg", [128, 1], F32)
        akvg = S.sb("akvg", [128, 128], F32)
        wuq = S.sb("wuq", [128, 2, 512], BF16)
        wuqi = S.sb("wuqi", [128, 2, 512], BF16)
        wuk = S.sb("wuk", [128, 1, 512], BF16)
        wuvz = S.sb("wuvz", [128, 8, 128], BF16)
        zt = [S.sb("ztC%d" % i, [128, ZS_N], F32) for i in range(2)]
        junk = S.sb("junkC", [128, 256], F32)
        ss = S.sb("ssC", [128, 2], F32)
        rs = S.sb("rsC", [128, 2], F32)
        st6 = S.sb("st6C", [128, 6], F32)
        mv = S.sb("mvC", [128, 2], F32)
        cqn = S.sb("cqn", [128, 256], BF16)
        kin = S.sb("kin", [128, 128], BF16)
        ckn = S.sb("ckn", [128, 128], F32)
        cqT = S.sb("cqT", [128, 2, SEQ], BF16)
        kidxT = S.sb("kidxT", [128, SEQ], BF16)
        ckv1 = S.sb("ckv1", [128, NT, 130], BF16)
        ckvT = S.sb("ckvT", [128, SEQ], BF16)
        qTa = S.sb("qTCa", [128, 4, 512], BF16)
        qTb = S.sb("qTCb", [128, 4, 512], BF16)
        qidxA = S.sb("qidxA", [128, 4, SEQ], BF16)
        qidxB = S.sb("qidxB", [128, 4, SEQ], BF16)
        qlatT = S.sb("qlatT", [128, 8, SEQ], BF16)
        wabs = S.sb("wabs", [128, NT, 8], F32)
        wsgn = S.sb("wsgn", [128, NT, 8], F32)
        acc = S.sb("accC", [128, SEQ], F32)
        work = S.sb("workC", [128, SEQ], F32)
        rbuf = [S.sb("rbufC%d" % i, [128, 512], F32) for i in range(3)]
        m8 = S.sb("m8C", [128, 8], F32)
        madd = S.sb("maddC", [128, SEQ], BF16)
        Eb = [S.sb("EbC%d" % i, [128, 512], BF16) for i in range(3)]
        otok = S.sb("otok", [128, 8, 128], BF16)
        olT = S.sb("olT", [128, 8, 128], BF16)
        haTs = [S.sb("haTs%d" % i, [128, 4, 128], BF16) for i in range(2)]
        rec = S.sb("recC", [128, 1], F32)
        S.dma(aqg[:], G["aqg_%d" % l])
        S.dma(akg[:], G["akg_%d" % l])
        S.dma(akvg[:], G["akvg_%d" % l])
        load_cast(P, wuq, G["wuq_%d" % l], 256, 512, gain=aqg, stg=stg)
        load_cast(P, wuqi, G["wuqi_%d" % l], 256, 512, gain=aqg, stg=stg)
        load_cast(P, wuk, G["wuk_%d" % l].rearrange("p a b -> p (a b)"), 128, 512, stg=stg)
        S.memset(wuvz[:], 0.0, eng="pool")
        sv = stg[P.stg_i % 2]
        P.stg_i += 1
        S.dma(sv[:, 0:512], G["wuv_%d" % l].rearrange("p a b -> p (a b)"))
        for h in range(8):
            S.copy(wuvz[:, h, (h % 2) * 64:(h % 2) * 64 + 64], sv[:, h * 64:(h + 1) * 64], eng="pool")
        S.memset(ckv1[:, :, 128:130], 1.0, eng="pool")
        S.memset(qTa[64:128, :, :], 0.0, eng="pool")
        S.memset(qTb[0:64, :, :], 0.0, eng="pool")
        S.memset(qidxA[64:128, :, :], 0.0, eng="pool")
        S.memset(qidxB[0:64, :, :], 0.0, eng="pool")
        bank = 0
        for s in range(NSEQ):
            tb = s * SEQ
            for i in range(NT if CUT >= 0 else 0):
                z = zt[i % 2]
                S.dma(z[:], G["zs"][tb + i * 128:tb + (i + 1) * 128, :])
                S.act(junk[:, 0:256], z[:, ZS_CQ:ZS_CQ + 256], AF.Square, accum_out=ss[:, 0:1])
                S.act(junk[:, 0:128], z[:, ZS_CKV:ZS_CKV + 128], AF.Square, accum_out=ss[:, 1:2])
                S.ts(rs[:, 0:1], ss[:, 0:1], 1.0 / 256, EPS, ALU.mult, ALU.add)
                S.ts(rs[:, 1:2], ss[:, 1:2], 1.0 / 128, EPS, ALU.mult, ALU.add)
                S.act(rs[:], rs[:], AF.Sqrt)
                S.recip(rs[:], rs[:])
                S.ts(cqn[:], z[:, ZS_CQ:ZS_CQ + 256], rs[:, 0:1], None, ALU.mult)
                if C1 < 1:
                    continue
                S.stt(ckn[:], z[:, ZS_CKV:ZS_CKV + 128], rs[:, 1:2], akvg[:], ALU.mult, ALU.mult)
                S.copy(ckv1[:, i, 0:128], ckn[:], eng="pool")
                if C1 < 2:
                    continue
                S.bnstats(st6[:], z[:, ZS_KIDX:ZS_KIDX + 64])
                S.bnaggr(mv[:], st6[:])
                S.ts(mv[:, 1:2], mv[:, 1:2], EPS, None, ALU.add)
                S.act(mv[:, 1:2], mv[:, 1:2], AF.Sqrt)
                S.recip(mv[:, 1:2], mv[:, 1:2])
                S.ts(kin[:, 0:64], z[:, ZS_KIDX:ZS_KIDX + 64], mv[:, 0:1], mv[:, 1:2], ALU.subtract, ALU.mult)
                S.copy(kin[:, 64:128], kin[:, 0:64], eng="pool")
                if C1 < 3:
                    continue
                S.act(wabs[:, i, :], z[:, ZS_WIDX:ZS_WIDX + 8], AF.Abs, scale=WSC)
                S.ts(wsgn[:, i, :], z[:, ZS_WIDX:ZS_WIDX + 8], 0.0, None, ALU.is_gt)
                S.ts(wsgn[:, i, :], wsgn[:, i, :], 2.0, -1.0, ALU.mult, ALU.add)
                if C1 < 5:
                    continue
                ptb = P.pbB[i % 2]
                S.tr(ptb[:, 0:128], cqn[:, 0:128], P.identb[:])
                S.tr(ptb[:, 128:256], cqn[:, 128:256], P.identb[:])
                S.tr(ptb[:, 256:384], kin[:], P.identb[:])
                if C1 < 6:
                    continue
                S.tr(ptb[:, 384:512], ckv1[:, i, 0:128], P.identb[:])
                if C1 < 7:
                    continue
                cs = slice(i * 128, (i + 1) * 128)
                S.copy(cqT[:, :, cs], ptb[:, 0:256].rearrange("p (c t) -> p c t", c=2), eng="act")
                if C1 < 8:
                    continue
                S.copy(kidxT[:, cs], ptb[:, 256:384], eng="act")
                S.ts(kidxT[:, cs], kidxT[:, cs], akg[:, 0:1], None, ALU.mult, eng="pool")
                if C1 < 9:
                    continue
                S.copy(ckvT[:, cs], ptb[:, 384:512], eng="act")
            for b4 in range(SEQ // 512 if CUT >= 1 else 0):
                ts_ = slice(b4 * 512, (b4 + 1) * 512)
                for c in range(4):
                    ps = P.pbF[bank % 6]
                    bank += 1
                    for k in range(2):
                        S.mm(ps[:], wuq[:, k, c * 128:(c + 1) * 128], cqT[:, k, ts_], start=(k == 0), stop=(k == 1))
                    S.copy(qTa[0:64, c, :], ps[0:64, :], eng="act")
                    S.copy(qTb[64:128, c, :], ps[64:128, :], eng="act")
                for c in range(4):
                    ps = P.pbF[bank % 6]
                    bank += 1
                    for k in range(2):
                        S.mm(ps[:], wuqi[:, k, c * 128:(c + 1) * 128], cqT[:, k, ts_], start=(k == 0), stop=(k == 1))
                    S.copy(qidxA[0:64, c, ts_], ps[0:64, :], eng="dve")
                    S.copy(qidxB[64:128, c, ts_], ps[64:128, :], eng="dve")
                for h in range(8):
                    qTx = qTa if h % 2 == 0 else qTb
                    ps = P.pbF[bank % 6]
                    bank += 1
                    S.mm(ps[:], wuk[:, 0, (h // 2) * 128:(h // 2 + 1) * 128], qTx[:, h // 2, :])
                    S.act(qlatT[:, h, ts_], ps[:], AF.Copy, scale=0.125)
            for i in range(NT if CUT >= 2 else 0):
                W = (i + 1) * 128
                cs = slice(i * 128, (i + 1) * 128)
                nb = (W + 511) // 512
                for b in range(nb):
                    w = min(512, W - b * 512)
                    ks = slice(b * 512, b * 512 + w)
                    for g in range(8):
                        qix = qidxA if g % 2 == 0 else qidxB
                        ps = P.pbF[bank % 4]
                        bank += 1
                        S.mm(ps[:, 0:w], qix[:, g // 2, cs], kidxT[:, ks])
                        r = rbuf[g % 3]
                        S.act(r[:, 0:w], ps[:, 0:w], AF.Relu, scale=wabs[:, i, g:g + 1])
                        if g == 0:
                            S.ts(acc[:, ks], r[:, 0:w], wsgn[:, i, 0:1], None, ALU.mult)
                        else:
                            S.stt(acc[:, ks], r[:, 0:w], wsgn[:, i, g:g + 1], acc[:, ks], ALU.mult, ALU.add)
                S.tt(acc[:, cs], acc[:, cs], P.negd[:], ALU.add)
                if CUT < 3:
                    continue
                if i >= 2:
                    S.max8(m8[:], acc[:, 0:W])
                    S.mrep(work[:, 0:W], m8[:], acc[:, 0:W], NEG)
                    for it in range(1, 31):
                        S.max8(m8[:], work[:, 0:W])
                        S.mrep(work[:, 0:W], m8[:], work[:, 0:W], NEG)
                    S.max8(m8[:], work[:, 0:W])
                    S.ts(madd[:, 0:W], acc[:, 0:W], m8[:, 7:8], 1.0, ALU.is_ge, ALU.subtract)
                else:
                    S.ts(madd[:, 0:W], acc[:, 0:W], -1.0e29, 1.0, ALU.is_ge, ALU.subtract)
                if CUT < 4:
                    continue
                for h in range(8):
                    hh = h % 2
                    pvbank = P.pbF[4 + (h // 2) % 2]
                    pv = pvbank[:, hh * 129:(hh + 1) * 129]
                    for bg in range((i + 4) // 4):
                        j0 = bg * 4
                        nj = min(4, i + 1 - j0)
                        pl = P.pbF[bank % 4]
                        bank += 1
                        for jj in range(nj):
                            j = j0 + jj
                            near = j >= i - 1
                            blk = pl[:, jj * 128:(jj + 1) * 128]
                            S.mm(blk, ckvT[:, j * 128:(j + 1) * 128], qlatT[:, h, cs], start=True, stop=False)
                            S.mm(blk, madd[:, j * 128:(j + 1) * 128], P.i30k[:], start=False, stop=(not near))
                            if near:
                                S.mm(blk, P.biasb[:, h, i - j, :], P.identb[:], start=False, stop=True)
                        E = Eb[(h * 4 + bg) % 3]
                        S.act(E[:, 0:nj * 128], pl[:, 0:nj * 128], AF.Exp)
                        for jj in range(nj):
                            j = j0 + jj
                            S.mm(pv, E[:, jj * 128:(jj + 1) * 128], ckv1[:, j, 0:129], start=(j == 0), stop=(j == i))
                    S.recip(rec[:], pv[:, 128:129])
                    S.ts(otok[:, h, :], pv[:, 0:128], rec[:, 0:1], None, ALU.mult)
                if CUT < 5:
                    continue
                ptb = P.pbB[i % 2]
                for h in range(8):
                    S.tr(ptb[:, h * 128:(h + 1) * 128], otok[:, h, :], P.identb[:])
                S.copy(olT[:], ptb[:, :].rearrange("p (h t) -> p h t", h=8), eng="act")
                ph = P.pbF[bank % 4]
                bank += 1
                for hc in range(4):
                    S.mm(ph[:, hc * 128:(hc + 1) * 128], wuvz[:, 2 * hc, :], olT[:, 2 * hc, :], start=True, stop=False)
                    S.mm(ph[:, hc * 128:(hc + 1) * 128], wuvz[:, 2 * hc + 1, :], olT[:, 2 * hc + 1, :], start=False, stop=True)
                hs = haTs[i % 2]
                S.copy(hs[:], ph[:].rearrange("p (c t) -> p c t", c=4), eng="dve")
                S.dma(G["haT"].rearrange("(c p) t -> p c t", p=128)[:, :, tb + i * 128:tb + (i + 1) * 128], hs[:])


def stage_E(P, l, x_in):
    S, G = P.S, P.G
    with _scope(S):
        stg = [S.sb("stgE%d" % i, [128, 1024], F32) for i in range(3)]
        pw = [S.sb("pwE%d" % i, [128, 4, D], BF16) for i in range(3)]
        wo = S.sb("woE", [128, 8, D], BF16)
        hin = [[S.sb("hinE%d_%d" % (b, i), [128, 4, 512], BF16) for i in range(2)] for b in range(3)]
        gin = [S.sb("ginE%d" % i, [128, 24, 512], BF16) for i in range(2)]
        y32 = S.sb("y32E", [128, 512], F32)
        t32 = [S.sb("t32E%d" % i, [128, 512], F32) for i in range(2)]
        yT = S.sb("yTE", [128, 8, 512], BF16)
        xb = [S.sb("xbE%d" % i, [128, D], F32) for i in range(2)]
        for b, nm in enumerate(("p_m", "p_a", "p_r")):
            load_cast(P, pw[b], G["%s_%d" % (nm, l)], 512, D, stg=stg)
        load_cast(P, wo, G["w_out_%d" % l], D, D, stg=stg)
        srcs = [G["hmT"], G["haT"], G["hrT"]]
        bank = 0
        for st in range(NTOK // 512):
            t0 = st * 512
            for b in range(3):
                S.dma(hin[b][st % 2][:], srcs[b].rearrange("(c p) t -> p c t", p=128)[:, :, t0:t0 + 512])
            gt = gin[st % 2]
            dma_chunks(S, gt, G["gT"].rearrange("(c p) t -> p c t", p=128)[:, :, t0:t0 + 512], 24, 4)
            for c in range(8):
                pss = []
                for b in range(3):
                    ps = P.pbF[bank % 6]
                    bank += 1
                    for k in range(4):
                        S.mm(ps[:], pw[b][:, k, c * 128:(c + 1) * 128], hin[b][st % 2][:, k, :],
                             start=(k == 0), stop=(k == 3))
                    pss.append(ps)
                S.tt(y32[:], pss[0][:], gt[:, c, :], ALU.mult)
                S.tt(t32[0][:], pss[1][:], gt[:, 8 + c, :], ALU.mult)
                S.tt(t32[1][:], pss[2][:], gt[:, 16 + c, :], ALU.mult)
                S.tt(y32[:], y32[:], t32[0][:], ALU.add, eng="pool")
                S.tt(yT[:, c, :], y32[:], t32[1][:], ALU.add, eng="pool")
            for ti in range(4):
                r0 = t0 + ti * 128
                xt = xb[ti % 2]
                S.dma(xt[:], x_in[r0:r0 + 128, :])
                for hf in range(2):
                    ps = P.pbF[bank % 6]
                    bank += 1
                    for k in range(8):
                        S.mm(ps[:], yT[:, k, ti * 128:(ti + 1) * 128], wo[:, k, hf * 512:(hf + 1) * 512],
                             start=(k == 0), stop=(k == 7))
                    S.tt(xt[:, hf * 512:(hf + 1) * 512], xt[:, hf * 512:(hf + 1) * 512], ps[:], ALU.add)
                S.dma(G["x1"][r0:r0 + 128, :], xt[:])


def stage_F(P, l, x_out, final):
    S, G = P.S, P.G
    ST = 256
    NF = DFF // 128
    with _scope(S):
        stg = [S.sb("stgF%d" % i, [128, 1024], F32) for i in range(3)]
        n2g = S.sb("n2g", [128, 8], F32)
        wg = S.sb("wgF", [128, 8, DFF], BF16)
        wu = S.sb("wuF", [128, 8, DFF], BF16)
        wd = S.sb("wdF", [128, NF, D], BF16)
        xb = [S.sb("xbF%d" % i, [128, D], F32) for i in range(2)]
        hb = S.sb("hbF", [128, D], BF16)
        junk = S.sb("junkF", [128, D], BF16)
        ss = S.sb("ssF", [128, 1], F32)
        rs = S.sb("rsF", [128, 1], F32)
        hT = S.sb("hTF", [128, 8, ST], BF16)
        sg = [S.sb("sgF%d" % i, [128, ST], F32) for i in range(2)]
        aT = S.sb("aTF", [128, NF, ST], BF16)
        fng = None
        if final:
            fng = S.sb("fngF", [128, D], F32)
            S.dma(fng[:], G["fng"])
        S.dma(n2g[:], G["n2g_%d" % l])
        load_cast(P, wg, G["w_gate_%d" % l], D, DFF, gain=n2g, stg=stg)
        load_cast(P, wu, G["w_up_%d" % l], D, DFF, gain=n2g, stg=stg)
        load_cast(P, wd, G["w_down_%d" % l], DFF, D, stg=stg)
        bank = 0
        for st in range(NTOK // ST):
            t0 = st * ST
            for ti in range(ST // 128):
                xt = xb[ti % 2]
                S.dma(xt[:], G["x1"][t0 + ti * 128:t0 + (ti + 1) * 128, :])
                norm_transpose(P, xt, hb, junk, ss, rs, hT, ti * 128, P.pbB[ti % 2])
            for f in range(NF):
                pg = P.pbF[bank % 6]
                pu = P.pbF[(bank + 1) % 6]
                bank += 2
                for k in range(8):
                    S.mm(pg[:, 0:ST], wg[:, k, f * 128:(f + 1) * 128], hT[:, k, :], start=(k == 0), stop=(k == 7))
                for k in range(8):
                    S.mm(pu[:, 0:ST], wu[:, k, f * 128:(f + 1) * 128], hT[:, k, :], start=(k == 0), stop=(k == 7))
                sgt = sg[f % 2]
                S.act(sgt[:], pg[:, 0:ST], AF.Silu)
                S.tt(aT[:, f, :], sgt[:], pu[:, 0:ST], ALU.mult)
            for ti in range(ST // 128):
                r0 = t0 + ti * 128
                xt = xb[ti % 2]
                S.dma(xt[:], G["x1"][r0:r0 + 128, :])
                for hf in range(2):
                    ps = P.pbF[bank % 6]
                    bank += 1
                    for f in range(NF):
                        S.mm(ps[:], aT[:, f, ti * 128:(ti + 1) * 128], wd[:, f, hf * 512:(hf + 1) * 512],
                             start=(f == 0), stop=(f == NF - 1))
                    S.tt(xt[:, hf * 512:(hf + 1) * 512], xt[:, hf * 512:(hf + 1) * 512], ps[:], ALU.add)
                if final:
                    S.act(junk[:], xt[:], AF.Square, accum_out=ss[:])
                    rms_rstd(P, rs[:], ss[:], D)
                    S.stt(xt[:], xt[:], rs[:, 0:1], fng[:], ALU.mult, ALU.mult)
                S.dma(x_out[r0:r0 + 128, :], xt[:])


SCRATCH = {
    "hT_d": ([D, NTOK], BF16), "qkT": ([512, NTOK], F32), "rqT": ([512, NTOK], BF16), "rkT": ([512, NTOK], BF16),
    "gT": ([3072, NTOK], BF16), "zv": ([NTOK, 512], BF16), "zo": ([NTOK, 512], BF16), "rv": ([NTOK, 512], BF16),
    "rg": ([NTOK, 512], BF16), "zs": ([NTOK, ZS_N], F32), "hmT": ([512, NTOK], BF16), "haT": ([512, NTOK], BF16),
    "hrT": ([512, NTOK], BF16), "x1": ([NTOK, D], F32), "xl": ([NTOK, D], F32),
}


def build_program(n_layers=2, dbg=(), stages="A1,A2,B,C,D,E,F"):
    stages = stages.split(",")
    nc = bass.Bass("TRN2", target_bir_lowering=False)
    P = Prog()
    G = {}
    P.G = G
    P.stg_i = 0
    G["x"] = nc.dram_tensor("x", [NTOK, D], F32, kind="ExternalInput").ap()
    for k, shp in CONST_SHAPES.items():
        G[k] = nc.dram_tensor(k, shp, F32, kind="ExternalInput").ap()
    for l in range(n_layers):
        for k, shp in LAYER_SHAPES.items():
            nm = "%s_%d" % (k, l)
            G[nm] = nc.dram_tensor(nm, shp, F32, kind="ExternalInput").ap()
    G["out"] = nc.dram_tensor("out", [NTOK, D], F32, kind="ExternalOutput").ap()
    for k, (shp, dt) in SCRATCH.items():
        G[k] = nc.dram_tensor(k, shp, dt, kind=("ExternalOutput" if k in dbg else "Internal")).ap()
    P.ret_g = host_constants()["ret_g"]
    with ExitStack() as es:
        S = Sched(nc, es)
        P.S = S
        P.pbF = [S.ps("pbF%d" % i, [128, 512], F32) for i in range(6)]
        P.pbB = [S.ps("pbB%d" % i, [128, 1024], BF16) for i in range(2)]
        P.identb = S.sb("identb", [128, 128], BF16)
        P.i30k = S.sb("i30k", [128, 128], BF16)
        P.trib2 = S.sb("trib2", [128, 256], BF16)
        P.tri32 = S.sb("tri32", [128, 128], F32)
        P.ones32 = S.sb("ones32", [128, 128], F32)
        P.negd = S.sb("negd", [128, 128], F32)
        P.biasb = S.sb("biasb", [128, 8, 2, 128], BF16)
        with _scope(S):
            c32 = S.sb("c32", [128, 128], F32)
            b32 = S.sb("b32", [128, 8, 2, 128], F32)
            rb31 = S.sb("rb31", [128, 8], F32)
            S.dma(c32[:], G["c_ident"])
            S.copy(P.identb[:], c32[:])
            S.ts(P.i30k[:], c32[:], 30000.0, None, ALU.mult)
            S.dma(P.tri32[:], G["c_tri"])
            S.copy(P.trib2[:, 0:128], P.tri32[:])
            S.copy(P.trib2[:, 128:256], P.tri32[:])
            S.dma(P.ones32[:], G["c_ones"])
            S.dma(P.negd[:], G["c_negd"])
            S.dma(b32[:], G["biasn"])
            S.dma(rb31[:], G["rb31"])
            for h in range(8):
                S.ts(P.biasb[:, h, :, :], b32[:, h, :, :], rb31[:, h:h + 1], None, ALU.subtract)
        x_in = G["x"]
        for l in range(n_layers):
            last = (l == n_layers - 1)
            x_out = G["out"] if last else G["xl"]
            if "A1" in stages:
                stage_A1(P, l, x_in)
            if "A2" in stages:
                stage_A2(P, l)
            if "B" in stages:
                stage_B(P, l)
            if "D" in stages:
                stage_D(P, l)
            if "C" in stages:
                stage_C(P, l)
            if "E" in stages:
                stage_E(P, l, x_in)
            if "F" in stages:
                stage_F(P, l, x_out, last)
            x_in = x_out
        S.barrier()
        P.n_inst = S.n_inst
    return nc, P


_CACHE = {}


def make_in_maps(inputs, n_layers=2, cores=range(NCORES)):
    hc = host_constants()
    shared = {k: np.ascontiguousarray(v, dtype=np.float32) for k, v in hc.items() if k.startswith("c_")}
    rel_bias = np.asarray(inputs["rel_bias"], np.float32)
    bi = bias_index()
    biasn = rel_bias[bi]
    shared["biasn"] = np.ascontiguousarray(biasn.transpose(0, 3, 1, 2))
    shared["rb31"] = np.ascontiguousarray(np.broadcast_to(rel_bias[31][None, :], (128, 8)))
    shared["fng"] = np.ascontiguousarray(np.broadcast_to(np.asarray(inputs["final_norm_g"], np.float32)[None, :], (128, D)))
    npin = {k: np.asarray(v) for k, v in inputs.items()}
    for l in range(n_layers):
        shared.update(prep_layer_weights(npin, l))
    x = npin["x"].astype(np.float32, copy=False)
    maps = []
    for c in cores:
        m = dict(shared)
        m["x"] = np.ascontiguousarray(x[NSEQ * c:NSEQ * (c + 1)].reshape(NTOK, D))
        maps.append(m)
    return maps


def kernel(**inputs):
    if "nc" not in _CACHE:
        _CACHE["nc"] = build_program()[0]
    nc = _CACHE["nc"]
    maps = make_in_maps(inputs)
    res = run_bass_kernel_spmd(nc, maps, core_ids=list(range(NCORES)))
    outs = [np.asarray(r["out"], dtype=np.float32).reshape(NSEQ, SEQ, D) for r in res.results]
    return np.concatenate(outs, axis=0)
```

```python
import math
from contextlib import ExitStack
import numpy as np
import concourse.bass as bass
import concourse.mybir as mybir
from concourse.bass_utils import run_bass_kernel_spmd

F32 = mybir.dt.float32
BF16 = mybir.dt.bfloat16
AF = mybir.ActivationFunctionType
ALU = mybir.AluOpType

N_DMA_SEMS = 40
NCORES = 8
SEQ = 2048
NSEQ = 2
NTOK = NSEQ * SEQ
D = 1024
DFF = 2816
EPS = 1e-6
NEG = -1.0e30


def _key(x):
    if isinstance(x, tuple):
        return x[0].tensor.name + ":" + str(x[1])
    if isinstance(x, str):
        return x
    return x.tensor.name


def _ap(x):
    return x[0] if isinstance(x, tuple) else x


def _isnum(v):
    return isinstance(v, (int, float))


class Sched:
    ENG = ("pe", "act", "dve", "pool", "sp")

    def __init__(self, nc, es):
        self.nc = nc
        self.es = es
        self.obj = {"pe": nc.tensor, "act": nc.scalar, "dve": nc.vector, "pool": nc.gpsimd, "sp": nc.sync}
        self.count = {e: 0 for e in self.ENG}
        self.sem = {e: es.enter_context(nc.semaphore("s_" + e)) for e in ("pe", "act", "dve", "pool")}
        self.dsem = [es.enter_context(nc.semaphore("d%d" % i)) for i in range(N_DMA_SEMS)]
        self.dval = [0] * N_DMA_SEMS
        self.dma_n = 0
        self.last_w = {}
        self.readers = {}
        self.waited = {e: {} for e in self.ENG}
        self.n_inst = 0
        self.uid = 0

    def sb(self, name, shape, dt):
        self.uid += 1
        return self.es.enter_context(self.nc.sbuf_tensor("%s_u%d" % (name, self.uid), list(shape), dt))

    def ps(self, name, shape, dt):
        return self.es.enter_context(self.nc.psum_tensor(name, list(shape), dt))

    def _deps(self, eng, reads, writes):
        toks = set()
        for k in reads:
            t = self.last_w.get(k)
            if t is not None:
                toks.add(t)
        for k in writes:
            t = self.last_w.get(k)
            if t is not None:
                toks.add(t)
            for t in self.readers.get(k, ()):
                toks.add(t)
        best = {}
        for (sk, v) in toks:
            if sk == eng and eng == "pe":
                continue
            if v > best.get(sk, 0):
                best[sk] = v
        waits = []
        w = self.waited[eng]
        for sk, v in best.items():
            if w.get(sk, 0) >= v:
                continue
            w[sk] = v
            waits.append((sk, v))
        return waits

    def _commit(self, tok, reads, writes):
        for k in reads:
            self.readers.setdefault(k, []).append(tok)
        for k in writes:
            self.last_w[k] = tok
            self.readers[k] = []

    def _emit(self, eng, waits, fn, kind):
        engine = self.obj[eng]
        for sk, v in waits:
            engine.wait_ge(self._semof(sk), v)
        if fn is None:
            return
        ins = fn(engine)
        if kind[0] == "c":
            ins.then_inc(self.sem[kind[1]], 1)
        else:
            ins.then_inc(self.dsem[kind[1]], 16)

    def op(self, eng, fn, reads, writes):
        rk = [_key(r) for r in reads]
        wk = [_key(w) for w in writes]
        waits = self._deps(eng, rk, wk)
        self.count[eng] += 1
        tok = (eng, self.count[eng])
        self._emit(eng, waits, fn, ("c", eng))
        self._commit(tok, rk, wk)
        self.n_inst += 1

    def dma(self, out, in_, q="sp", **kw):
        rk = [_key(in_)]
        wk = [_key(out)]
        slot = self.dma_n % N_DMA_SEMS
        self.dma_n += 1
        waits = self._deps(q, rk, wk)
        sk = ("d", slot)
        prev = self.dval[slot]
        if prev > 0 and self.waited[q].get(sk, 0) < prev:
            self.waited[q][sk] = prev
            waits.append((sk, prev))
        self.dval[slot] = prev + 16
        tok = (sk, prev + 16)
        o, i = _ap(out), _ap(in_)
        self._emit(q, waits, (lambda e: e.dma_start(out=o, in_=i, **kw)), ("d", slot))
        self._commit(tok, rk, wk)
        self.n_inst += 1

    def barrier(self):
        for e in self.ENG:
            waits = []
            w = self.waited[e]
            for e2 in ("pe", "act", "dve", "pool"):
                v = self.count[e2]
                if v == 0 or w.get(e2, 0) >= v:
                    continue
                if e2 == e and e == "pe":
                    continue
                w[e2] = v
                waits.append((e2, v))
            for s in range(N_DMA_SEMS):
                v = self.dval[s]
                sk = ("d", s)
                if v == 0 or w.get(sk, 0) >= v:
                    continue
                w[sk] = v
                waits.append((sk, v))
            if waits:
                self._emit(e, waits, None, None)
        self.last_w = {}
        self.readers = {}

    def _semof(self, sk):
        if isinstance(sk, tuple):
            return self.dsem[sk[1]]
        return self.sem[sk]

    def mm(self, out, lhsT, rhs, start=True, stop=True):
        o, l, r = _ap(out), _ap(lhsT), _ap(rhs)
        self.op("pe", lambda e: e.matmul(o, l, r, start=start, stop=stop), [lhsT, rhs], [out])

    def tr(self, out, in_, ident):
        o, i, d = _ap(out), _ap(in_), _ap(ident)
        self.op("pe", lambda e: e.transpose(o, i, d), [in_, ident], [out])

    def act(self, out, in_, func, bias=None, scale=None, accum_out=None):
        o, i = _ap(out), _ap(in_)
        kw = {}
        reads = [in_]
        writes = [out]
        if bias is not None:
            if _isnum(bias):
                kw["bias"] = bias
            else:
                kw["bias"] = _ap(bias)
                reads.append(bias)
        if scale is not None:
            if _isnum(scale):
                kw["scale"] = scale
            else:
                kw["scale"] = _ap(scale)
                reads.append(scale)
        if accum_out is not None:
            kw["accum_out"] = _ap(accum_out)
            writes.append(accum_out)
        self.op("act", lambda e: e.activation(o, i, func, **kw), reads, writes)

    def ts(self, out, in0, s1, s2, op0, op1=None, eng="dve"):
        o, i = _ap(out), _ap(in0)
        reads = [in0]
        a1, a2 = s1, s2
        if s1 is not None and not _isnum(s1):
            reads.append(s1)
            a1 = _ap(s1)
        if s2 is not None and not _isnum(s2):
            reads.append(s2)
            a2 = _ap(s2)
        if op1 is None:
            self.op(eng, lambda e: e.tensor_scalar(o, i, a1, None, op0), reads, [out])
        else:
            self.op(eng, lambda e: e.tensor_scalar(o, i, a1, a2, op0, op1), reads, [out])

    def ts_acc(self, out, in0, s1, init, op0, red_op, accum_out):
        o, i, ac = _ap(out), _ap(in0), _ap(accum_out)
        reads = [in0]
        a1 = s1
        if not _isnum(s1):
            reads.append(s1)
            a1 = _ap(s1)
        self.op("dve", lambda e: e.tensor_scalar(o, i, a1, init, op0, red_op, accum_out=ac), reads, [out, accum_out])

    def treduce(self, out, in_, op):
        o, i = _ap(out), _ap(in_)
        self.op("dve", lambda e: e.tensor_reduce(o, i, mybir.AxisListType.X, op), [in_], [out])

    def tt(self, out, in0, in1, op, eng="dve"):
        o, a, b = _ap(out), _ap(in0), _ap(in1)
        self.op(eng, lambda e: e.tensor_tensor(o, a, b, op), [in0, in1], [out])

    def stt(self, out, in0, scalar, in1, op0, op1):
        o, a, b = _ap(out), _ap(in0), _ap(in1)
        reads = [in0, in1]
        s = scalar
        if not _isnum(scalar):
            reads.append(scalar)
            s = _ap(scalar)
        self.op("dve", lambda e: e.scalar_tensor_tensor(o, a, s, b, op0, op1), reads, [out])

    def copy(self, out, in_, eng="dve"):
        o, i = _ap(out), _ap(in_)
        if eng == "act":
            self.op("act", lambda e: e.copy(o, i), [in_], [out])
        else:
            self.op(eng, lambda e: e.tensor_copy(o, i), [in_], [out])

    def memset(self, out, val, eng="dve"):
        o = _ap(out)
        self.op(eng, lambda e: e.memset(o, val), [], [out])

    def recip(self, out, in_):
        o, i = _ap(out), _ap(in_)
        self.op("dve", lambda e: e.reciprocal(o, i), [in_], [out])

    def max8(self, out, in_):
        o, i = _ap(out), _ap(in_)
        self.op("dve", lambda e: e.max(o, i), [in_], [out])

    def mrep(self, out, rep, vals, imm):
        o, r, v = _ap(out), _ap(rep), _ap(vals)
        self.op("dve", lambda e: e.match_replace(o, r, v, imm), [rep, vals], [out])

    def bnstats(self, out, in_):
        o, i = _ap(out), _ap(in_)
        self.op("dve", lambda e: e.bn_stats(o, i), [in_], [out])

    def bnaggr(self, out, in_):
        o, i = _ap(out), _ap(in_)
        self.op("dve", lambda e: e.bn_aggr(o, i), [in_], [out])


def _t5_bucket_np(dist):
    dist = np.asarray(dist, dtype=np.int64)
    max_exact = 16
    d_f = np.maximum(dist, 1).astype(np.float32)
    large = max_exact + (np.log(d_f / np.float32(max_exact)) / np.float32(math.log(128 / max_exact))
                         * np.float32(32 - max_exact)).astype(np.int32)
    large = np.minimum(large, 31)
    return np.where(dist < max_exact, dist, large).astype(np.int64)


def host_constants():
    c = {}
    r = np.arange(128)
    c["c_ident"] = np.eye(128, dtype=np.float32)
    c["c_tri"] = (r[:, None] <= r[None, :]).astype(np.float32)
    c["c_ones"] = np.ones((128, 128), np.float32)
    c["c_negd"] = np.where(r[None, :] <= r[:, None], 0.0, NEG).astype(np.float32)
    half = 64
    freqs = (10000.0 ** (-np.linspace(0.0, 1.0, half, dtype=np.float32))).astype(np.float32)
    pos = np.arange(SEQ, dtype=np.float32)
    ang = (pos[None, :] * freqs[:, None]).astype(np.float32)
    cos = np.cos(ang).astype(np.float32)
    sin = np.sin(ang).astype(np.float32)
    c["c_cos"] = np.concatenate([cos, cos], 0)
    c["c_sin"] = np.concatenate([sin, sin], 0)
    h = np.arange(4, dtype=np.float64)
    lg = np.log1p(-np.exp2(-5.0 - h))
    rr = np.arange(128, dtype=np.float64)
    c["c_rete"] = np.exp(-(rr[:, None] + 1.0) * lg[None, :]).astype(np.float32)
    c["c_retrow"] = (np.exp((rr[:, None] + 1.0) * lg[None, :]) * 128 ** -0.5).astype(np.float32)
    c["ret_g"] = [float(np.exp(128.0 * v)) for v in lg]
    return c


def bias_index():
    t = np.arange(128)
    out = np.zeros((128, 2, 128), np.int64)
    for off in range(2):
        d = t[:, None] - t[None, :] + 128 * off
        out[:, off, :] = _t5_bucket_np(np.maximum(d, 0))
    return out


OFF = dict(m_q=0, m_k=256, m_v=512, m_i=1024, m_f=1028, m_o=1032, a_cq=1544, a_kidx=1800,
           a_widx=1864, a_ckv=1872, r_q=2000, r_k=2512, r_v=3024, r_g=3536, g_m=4048, g_a=5072,
           g_r=6096)
NFM = 5632
NTM = 2512
ZS_CQ, ZS_CKV, ZS_KIDX, ZS_WIDX, ZS_MI, ZS_MF, ZS_N = 0, 256, 384, 448, 456, 460, 464


def _rot_pre(w):
    w4 = w.reshape(w.shape[0], 4, 2, 64)
    return np.ascontiguousarray(w4[:, :, ::-1, :]).reshape(w.shape[0], 512)


def prep_layer_weights(inp, l):
    w = inp["w_in"][l]
    sl = lambda name, n: w[:, OFF[name]:OFF[name] + n]
    rq, rk = sl("r_q", 512), sl("r_k", 512)
    w_fm = np.concatenate([sl("m_q", 256), sl("m_k", 256), rq, _rot_pre(rq), rk, _rot_pre(rk),
                           sl("g_m", 1024), sl("g_a", 1024), sl("g_r", 1024)], axis=1)
    w_tm = np.concatenate([sl("m_v", 512), sl("m_o", 512), sl("r_v", 512), sl("r_g", 512),
                           sl("a_cq", 256), sl("a_ckv", 128), sl("a_kidx", 64), sl("a_widx", 8),
                           sl("m_i", 4), sl("m_f", 4)], axis=1)
    assert w_fm.shape[1] == NFM and w_tm.shape[1] == NTM
    rep = lambda v: np.ascontiguousarray(np.broadcast_to(np.asarray(v, np.float32).reshape(1, -1), (128, np.asarray(v).size)))
    colv = lambda v: np.ascontiguousarray(np.asarray(v, np.float32).reshape(-1, 128).T)
    d = {
        "w_fm": np.ascontiguousarray(w_fm), "w_tm": np.ascontiguousarray(w_tm),
        "n1g": colv(inp["norm1_g"][l]), "n2g": colv(inp["norm2_g"][l]),
        "mconv": np.ascontiguousarray(inp["m_conv"][l].reshape(4, 4, 128).transpose(2, 1, 0)),
        "mbias": rep(np.tile(np.concatenate([inp["m_ibias"][l], inp["m_fbias"][l]]), 16)),
        "mng": rep(inp["m_norm_g"][l]), "rng": rep(inp["r_norm_g"][l]),
        "aqg": colv(inp["a_qnorm_g"][l]),
        "akg": colv(np.concatenate([inp["a_kidx_g"][l], inp["a_kidx_g"][l]])),
        "akvg": rep(inp["a_kvnorm_g"][l]),
        "wuq": np.ascontiguousarray(inp["a_wuq"][l]), "wuqi": np.ascontiguousarray(inp["a_wuq_idx"][l]),
        "wuk": np.ascontiguousarray(inp["a_wuk"][l].reshape(4, 2, 64, 128).transpose(1, 2, 0, 3).reshape(128, 4, 128)),
        "wuv": np.ascontiguousarray(inp["a_wuv"][l].transpose(1, 0, 2)),
        "p_m": np.ascontiguousarray(inp["p_m"][l]), "p_a": np.ascontiguousarray(inp["p_a"][l]),
        "p_r": np.ascontiguousarray(inp["p_r"][l]), "w_out": np.ascontiguousarray(inp["w_out"][l]),
        "w_gate": np.ascontiguousarray(inp["w_gate"][l]), "w_up": np.ascontiguousarray(inp["w_up"][l]),
        "w_down": np.ascontiguousarray(inp["w_down"][l]),
    }
    return {"%s_%d" % (k, l): v for k, v in d.items()}


LAYER_SHAPES = {
    "w_fm": [D, NFM], "w_tm": [D, NTM], "n1g": [128, 8], "n2g": [128, 8], "mconv": [128, 4, 4],
    "mbias": [128, 128], "mng": [128, 512], "rng": [128, 512], "aqg": [128, 2], "akg": [128, 1],
    "akvg": [128, 128], "wuq": [256, 512], "wuqi": [256, 512], "wuk": [128, 4, 128],
    "wuv": [128, 8, 64], "p_m": [512, D], "p_a": [512, D], "p_r": [512, D], "w_out": [D, D],
    "w_gate": [D, DFF], "w_up": [D, DFF], "w_down": [DFF, D],
}
CONST_SHAPES = {"c_ident": [128, 128], "c_tri": [128, 128], "c_ones": [128, 128], "c_negd": [128, 128],
                "c_cos": [128, SEQ], "c_sin": [128, SEQ], "c_rete": [128, 4], "c_retrow": [128, 4],
                "biasn": [128, 8, 2, 128], "rb31": [128, 8], "fng": [128, D]}


class Prog:
    pass


def _scope(S):
    class _Sc:
        def __enter__(self_):
            self_.old = S.es
            self_.es = ExitStack()
            self_.es.__enter__()
            S.es = self_.es
            return self_

        def __exit__(self_, *a):
            S.barrier()
            S.es = self_.old
            return self_.es.__exit__(*a)
    return _Sc()


def load_cast(P, dst, src, K, N, gain=None, stg=None, engs=("pool", "dve", "act")):
    S = P.S
    kc = K // 128
    srcv = src.rearrange("(c p) n -> p c n", p=128)
    CH = stg[0].shape[1]
    for c in range(kc):
        for n0 in range(0, N, CH):
            n1 = min(N, n0 + CH)
            sbuf = stg[P.stg_i % len(stg)]
            eng = engs[P.stg_i % len(engs)]
            P.stg_i += 1
            S.dma(sbuf[:, 0:n1 - n0], srcv[:, c, n0:n1])
            if gain is None:
                S.copy(dst[:, c, n0:n1], sbuf[:, 0:n1 - n0], eng=eng)
            elif eng == "act":
                S.act(dst[:, c, n0:n1], sbuf[:, 0:n1 - n0], AF.Copy, scale=gain[:, c:c + 1])
            else:
                S.ts(dst[:, c, n0:n1], sbuf[:, 0:n1 - n0], gain[:, c:c + 1], None, ALU.mult, eng=eng)


def dma_chunks(S, out, in_, n, step):
    for a in range(0, n, step):
        b = min(n, a + step)
        S.dma(out[:, a:b], in_[:, a:b])


def rms_rstd(P, rs, ss, n):
    S = P.S
    S.ts(rs, ss, 1.0 / n, EPS, ALU.mult, ALU.add)
    S.act(rs, rs, AF.Sqrt)
    S.recip(rs, rs)


def norm_transpose(P, xt, hb, junk, ss, rs, hT, col0, ptb):
    S = P.S
    S.act(junk[:], xt[:], AF.Square, accum_out=ss[:])
    rms_rstd(P, rs[:], ss[:], D)
    S.ts(hb[:], xt[:], rs[:], None, ALU.mult)
    for k in range(8):
        S.tr(ptb[:, k * 128:(k + 1) * 128], hb[:, k * 128:(k + 1) * 128], P.identb[:])
    S.copy(hT[:, :, col0:col0 + 128], ptb[:, :].rearrange("p (k t) -> p k t", k=8), eng="act")


def stage_A1(P, l, x_in):
    S, G = P.S, P.G
    with _scope(S):
        Wfm = S.sb("Wfm", [128, 8, NFM], BF16)
        stg = [S.sb("stgA%d" % i, [128, 2048], F32) for i in range(3)]
        cos = S.sb("cos", [128, SEQ], F32)
        sin = S.sb("sin", [128, SEQ], F32)
        n1g = S.sb("n1g", [128, 8], F32)
        xb = [S.sb("xbA%d" % i, [128, D], F32) for i in range(2)]
        hb = S.sb("hbA", [128, D], BF16)
        junk = S.sb("junkA", [128, D], BF16)
        ss = S.sb("ssA", [128, 1], F32)
        rs = S.sb("rsA", [128, 1], F32)
        hT = S.sb("hTA", [128, 8, 512], BF16)
        ev32 = [S.sb("ev32A%d" % i, [128, 512], F32) for i in range(4)]
        evb = [S.sb("evbA%d" % i, [128, 512], BF16) for i in range(4)]
        S.dma(n1g[:], G["n1g_%d" % l])
        S.dma(cos[:], G["c_cos"])
        S.dma(sin[:], G["c_sin"])
        load_cast(P, Wfm, G["w_fm_%d" % l], D, NFM, gain=n1g, stg=stg)
        for c in range(8):
            for base in (1024, 2048):
                v = Wfm[:, c, base:base + 512].rearrange("p (h t j) -> p h t j", h=4, t=2)[:, :, 0, :]
                S.ts(v, v, -1.0, None, ALU.mult, eng="pool")
        hTd = G["hT_d"].rearrange("(k p) t -> p k t", p=128)
        e32 = 0
        eb = 0
        bank = 0
        for st in range(NTOK // 512):
            t0 = st * 512
            p0 = t0 % SEQ
            for ti in range(4):
                xt = xb[ti % 2]
                S.dma(xt[:], x_in[t0 + ti * 128:t0 + (ti + 1) * 128, :])
                norm_transpose(P, xt, hb, junk, ss, rs, hT, ti * 128, P.pbB[ti % 2])
            dma_chunks(S, hTd[:, :, t0:t0 + 512], hT, 8, 4)

            def proj(c):
                nonlocal bank
                ps = P.pbF[bank % 6]
                bank += 1
                for k in range(8):
                    S.mm(ps[:], Wfm[:, k, c * 128:(c + 1) * 128], hT[:, k, :], start=(k == 0), stop=(k == 7))
                return ps
            for c in range(4):
                ps = proj(c)
                o = ev32[e32 % 4]
                e32 += 1
                S.copy(o[:], ps[:], eng="act")
                S.dma(G["qkT"][c * 128:(c + 1) * 128, t0:t0 + 512], o[:])
            for which, dst in ((0, "rqT"), (1, "rkT")):
                for hh in range(4):
                    ca = 4 + which * 8 + hh
                    pa = proj(ca)
                    pbk = proj(ca + 4)
                    t1 = ev32[e32 % 4]
                    t2 = ev32[(e32 + 1) % 4]
                    e32 += 2
                    S.tt(t1[:], pa[:], cos[:, p0:p0 + 512], ALU.mult)
                    S.tt(t2[:], pbk[:], sin[:, p0:p0 + 512], ALU.mult)
                    ob = evb[eb % 4]
                    eb += 1
                    S.tt(ob[:], t1[:], t2[:], ALU.add, eng="pool")
                    S.dma(G[dst][hh * 128:(hh + 1) * 128, t0:t0 + 512], ob[:])
            for c in range(24):
                ps = proj(20 + c)
                ob = evb[eb % 4]
                eb += 1
                S.act(ob[:], ps[:], AF.Sigmoid)
                S.dma(G["gT"][c * 128:(c + 1) * 128, t0:t0 + 512], ob[:])


def stage_A2(P, l):
    S, G = P.S, P.G
    with _scope(S):
        Wtm = S.sb("Wtm", [128, 8, NTM], BF16)
        stg = [S.sb("stgB%d" % i, [128, 2048], F32) for i in range(3)]
        n1g = S.sb("n1gB", [128, 8], F32)
        hTs = [S.sb("hTB%d" % i, [128, 8, 512], BF16) for i in range(2)]
        evb = [S.sb("evbB%d" % i, [128, 512], BF16) for i in range(6)]
        ev32 = [S.sb("ev32B%d" % i, [128, 512], F32) for i in range(2)]
        S.dma(n1g[:], G["n1g_%d" % l])
        load_cast(P, Wtm, G["w_tm_%d" % l], D, NTM, gain=n1g, stg=stg)
        hTd = G["hT_d"].rearrange("(k p) t -> p k t", p=128)
        groups = [(0, 512, "zv", "copy"), (512, 512, "zo", "sig"), (1024, 512, "rv", "copy"),
                  (1536, 512, "rg", "silu"), (2048, ZS_N, "zs", "f32")]
        bank = 0
        eb = 0
        e32 = 0
        for st in range(NTOK // 512):
            t0 = st * 512
            hT = hTs[st % 2]
            dma_chunks(S, hT, hTd[:, :, t0:t0 + 512], 8, 4)
            for ti in range(4):
                r0 = t0 + ti * 128
                for (c0, n, dst, kind) in groups:
                    ps = P.pbF[bank % 6]
                    bank += 1
                    for k in range(8):
                        S.mm(ps[:, 0:n], hT[:, k, ti * 128:(ti + 1) * 128], Wtm[:, k, c0:c0 + n],
                             start=(k == 0), stop=(k == 7))
                    if kind == "f32":
                        o = ev32[e32 % 2]
                        e32 += 1
                        S.copy(o[:, 0:n], ps[:, 0:n], eng="dve")
                    else:
                        o = evb[eb % 6]
                        eb += 1
                        if kind == "copy":
                            S.copy(o[:, 0:n], ps[:, 0:n], eng="dve")
                        elif kind == "sig":
                            S.act(o[:, 0:n], ps[:, 0:n], AF.Sigmoid)
                        else:
                            S.act(o[:, 0:n], ps[:, 0:n], AF.Silu)
                    S.dma(G[dst][r0:r0 + 128, :], o[:, 0:n])


def ln_gate_out(P, pN, s2, hp, n, o32, gain_t, gate_all, hm, tmp):
    S = P.S
    st6, mv, t2, re = tmp
    for hh in range(2):
        S.bnstats(st6[:, hh, :], pN[:, hh, 0:128])
        S.bnaggr(mv[:, hh, :], st6[:, hh, :])
    S.tt(t2[:], s2, s2, ALU.mult)
    S.tt(t2[:], t2[:], mv[:, :, 1], ALU.mult)
    S.ts(t2[:], t2[:], EPS, None, ALU.add)
    S.act(t2[:], t2[:], AF.Sqrt)
    S.recip(t2[:], t2[:])
    S.tt(re[:], t2[:], s2, ALU.mult)
    for hh in range(2):
        S.ts(o32[:, hh * 128:(hh + 1) * 128], pN[:, hh, 0:128], mv[:, hh, 0:1], re[:, hh:hh + 1],
             ALU.subtract, ALU.mult)
    S.tt(o32[:], o32[:], gain_t[:, hp * 256:(hp + 1) * 256], ALU.mult, eng="pool")
    S.tt(hm[:, hp * 256:(hp + 1) * 256], o32[:], gate_all[:, n, hp * 256:(hp + 1) * 256], ALU.mult, eng="pool")


def out_transpose(P, hm, hmTs, dstT, tg, ptb):
    S = P.S
    for c in range(4):
        S.tr(ptb[:, c * 128:(c + 1) * 128], hm[:, c * 128:(c + 1) * 128], P.identb[:])
    S.copy(hmTs[:], ptb[:, 0:512].rearrange("p (c t) -> p c t", c=4), eng="act")
    S.dma(dstT.rearrange("(c p) t -> p c t", p=128)[:, :, tg:tg + 128], hmTs[:])


def stage_B(P, l):
    S, G = P.S, P.G
    NCH = SEQ // 128
    with _scope(S):
        qkraw = S.sb("qkraw", [128, 4, SEQ + 3], F32)
        cacc = [S.sb("cacc%d" % i, [128, SEQ], F32) for i in range(2)]
        qkb = S.sb("qkb", [128, 4, SEQ], BF16)
        qA = S.sb("qA", [128, 2, SEQ], BF16)
        qB = S.sb("qB", [128, 2, SEQ], BF16)
        mconv = S.sb("mconv", [128, 4, 4], F32)
        mbias = S.sb("mbias", [128, 128], F32)
        mng = S.sb("mng", [128, 512], F32)
        gi = S.sb("gi", [128, NCH, 8], F32)
        ax = S.sb("ax", [128, NCH, 4], F32)
        lf = S.sb("lf", [128, NCH, 4], F32)
        lia = S.sb("lia", [128, NCH, 4], F32)
        e_all = S.sb("e_all", [128, NCH, 4], F32)
        rowsc = S.sb("rowsc", [128, NCH, 4], F32)
        g_all = S.sb("g_all", [128, NCH, 4], F32)
        v_all = S.sb("v_all", [128, NCH, 512], BF16)
        mo_all = S.sb("mo_all", [128, NCH, 512], BF16)
        vt = S.sb("vt", [128, NCH, 4, 130], BF16)
        ktok = [S.sb("ktok%d" % i, [128, 256], BF16) for i in range(2)]
        scTm = [S.sb("scTm%d" % i, [128, 256], BF16) for i in range(2)]
        P32 = S.sb("P32", [128, 2, 129], F32)
        Cb = S.sb("Cb", [128, 2, 130], BF16)
        hm = [S.sb("hm%d" % i, [128, 512], BF16) for i in range(2)]
        hmTs = [S.sb("hmTs%d" % i, [128, 4, 128], BF16) for i in range(2)]
        o32 = [S.sb("o32%d" % i, [128, 256], F32) for i in range(4)]
        d1_r = [S.sb("d1_%d" % i, [128, 2], F32) for i in range(4)]
        s2_r = [S.sb("s2_%d" % i, [128, 2], F32) for i in range(4)]
        tmp_r = [(S.sb("st6_%d" % i, [128, 2, 6], F32), S.sb("mv_%d" % i, [128, 2, 2], F32),
                  S.sb("t2_%d" % i, [128, 2], F32), S.sb("re_%d" % i, [128, 2], F32)) for i in range(4)]
        S.dma(mconv[:], G["mconv_%d" % l])
        S.dma(mbias[:], G["mbias_%d" % l])
        S.dma(mng[:], G["mng_%d" % l])
        S.memset(qkraw[:, :, 0:3], 0.0)
        S.memset(qA[64:128, :, :], 0.0, eng="pool")
        S.memset(qB[0:64, :, :], 0.0, eng="pool")
        S.memset(vt[:], 0.0, eng="pool")
        bank = 0
        for s in range(NSEQ):
            tb = s * SEQ
            for c in range(4):
                S.dma(qkraw[:, c, 3:SEQ + 3], G["qkT"][c * 128:(c + 1) * 128, tb:tb + SEQ])
            for c in range(4):
                acc = cacc[c % 2]
                S.ts(acc[:], qkraw[:, c, 0:SEQ], mconv[:, c, 0:1], None, ALU.mult)
                for j in range(1, 4):
                    S.stt(acc[:], qkraw[:, c, j:j + SEQ], mconv[:, c, j:j + 1], acc[:], ALU.mult, ALU.add)
                if c < 2:
                    S.act(qA[0:64, c, :], acc[0:64, :], AF.Silu)
                    S.act(qB[64:128, c, :], acc[64:128, :], AF.Silu)
                else:
                    S.act(qkb[:, c, :], acc[:], AF.Silu)
            dma_chunks(S, gi, G["zs"][tb:tb + SEQ, ZS_MI:ZS_MI + 8].rearrange("(n p) c -> p n c", p=128), NCH, 4)
            gif = gi[:].rearrange("p n c -> p (n c)")
            S.tt(gif, gif, mbias[:], ALU.add)
            S.act(ax[:], gi[:, :, 4:8], AF.Abs)
            S.act(ax[:], ax[:], AF.Exp, scale=-1.0)
            S.act(ax[:], ax[:], AF.Ln, bias=1.0)
            S.ts(lf[:], gi[:, :, 4:8], 0.0, None, ALU.min)
            S.tt(lf[:], lf[:], ax[:], ALU.subtract)
            pg = P.pbF[bank % 6]
            bank += 1
            lff = lf[:].rearrange("p n c -> p (n c)")
            S.mm(pg[:, 0:64], P.tri32[:], lff)
            S.mm(pg[:, 64:128], P.ones32[:], lff)
            S.tt(lia[:], gi[:, :, 0:4], pg[:, 0:64].rearrange("p (n c) -> p n c", c=4), ALU.subtract)
            S.act(e_all[:], lia[:], AF.Exp)
            S.act(rowsc[:].rearrange("p n c -> p (n c)"), pg[:, 0:64], AF.Exp, bias=math.log(0.125))
            S.act(g_all[:].rearrange("p n c -> p (n c)"), pg[:, 64:128], AF.Exp)
            dma_chunks(S, v_all, G["zv"][tb:tb + SEQ, :].rearrange("(n p) c -> p n c", p=128), NCH, 4)
            dma_chunks(S, mo_all, G["zo"][tb:tb + SEQ, :].rearrange("(n p) c -> p n c", p=128), NCH, 4)
            for n in range(NCH):
                for h in range(4):
                    S.ts(vt[:, n, h, 0:128], v_all[:, n, h * 128:(h + 1) * 128], e_all[:, n, h:h + 1], None,
                         ALU.mult, eng="pool")
            S.copy(vt[:, :, :, 128], e_all[:], eng="pool")
            for n in range(NCH):
                cs = slice(n * 128, (n + 1) * 128)
                kt = ktok[n % 2]
                ptb = P.pbB[n % 2]
                for hc in range(2):
                    S.tr(ptb[:, hc * 128:(hc + 1) * 128], qkb[:, 2 + hc, cs], P.identb[:])
                S.copy(kt[:], ptb[:, 0:256], eng="act")
                hmc = hm[n % 2]
                for hp in range(2):
                    psc = P.pbF[bank % 6]
                    bank += 1
                    for hh in range(2):
                        qX = qA if hh == 0 else qB
                        S.mm(psc[:, hh * 128:(hh + 1) * 128], qkb[:, 2 + hp, cs], qX[:, hp, cs])
                    sm = scTm[hp]
                    S.tt(sm[:], psc[:, 0:256], P.trib2[:], ALU.mult)
                    pNb = P.pbF[bank % 6]
                    bank += 1
                    pN = pNb[:, 0:258].rearrange("p (a b) -> p a b", a=2)
                    for hh in range(2):
                        h = 2 * hp + hh
                        pr = slice(64 * hh, 64 * hh + 64)
                        S.mm(pN[:, hh, :], sm[:, hh * 128:(hh + 1) * 128], vt[:, n, h, 0:129], start=True, stop=(n == 0))
                        if n > 0:
                            qX = qA if hh == 0 else qB
                            S.mm(pN[:, hh, :], qX[:, hp, cs], Cb[:, hp, 0:129], start=False, stop=True)
                    if n < NCH - 1:
                        pUb = P.pbF[bank % 6]
                        bank += 1
                        pU = pUb[:, 0:260].rearrange("p (a b) -> p a b", a=2)
                        S.mm(pUb[:, 0:260], kt[:, hp * 128:(hp + 1) * 128],
                             vt[:, n, 2 * hp:2 * hp + 2, :].rearrange("p a b -> p (a b)"))
                        for hh in range(2):
                            h = 2 * hp + hh
                            pr = slice(64 * hh, 64 * hh + 64)
                            if n == 0:
                                S.copy(P32[pr, hp, :], pU[pr, hh, 0:129])
                            else:
                                S.stt(P32[pr, hp, :], P32[pr, hp, :], g_all[pr, n - 1, h:h + 1], pU[pr, hh, 0:129],
                                      ALU.mult, ALU.add)
                            S.ts(Cb[pr, hp, 0:129], P32[pr, hp, :], g_all[pr, n, h:h + 1], None, ALU.mult)
                    rsl = rowsc[:, n, 2 * hp:2 * hp + 2]
                    d1, s2, tmp = d1_r[(2 * n + hp) % 4], s2_r[(2 * n + hp) % 4], tmp_r[(2 * n + hp) % 4]
                    S.tt(d1[:], pN[:, :, 128], rsl, ALU.mult)
                    S.act(d1[:], d1[:], AF.Abs)
                    S.ts(d1[:], d1[:], 1.0, None, ALU.max)
                    S.recip(d1[:], d1[:])
                    S.tt(s2[:], d1[:], rsl, ALU.mult)
                    ln_gate_out(P, pN, s2[:], hp, n, o32[(2 * n + hp) % 4], mng, mo_all, hmc, tmp)
                out_transpose(P, hmc, hmTs[n % 2], G["hmT"], tb + n * 128, P.pbB[(n + 1) % 2])


def stage_D(P, l):
    S, G = P.S, P.G
    NCH = SEQ // 128
    gam = P.ret_g
    with _scope(S):
        rq = S.sb("rq", [128, 4, SEQ], BF16)
        rk = S.sb("rk", [128, 4, SEQ], BF16)
        rng = S.sb("rng", [128, 512], F32)
        rete = S.sb("rete", [128, 4], F32)
        retrow = S.sb("retrow", [128, 4], F32)
        v_all = S.sb("rv_all", [128, NCH, 512], BF16)
        rg_all = S.sb("rg_all", [128, NCH, 512], BF16)
        vt = S.sb("rvt", [128, NCH, 512], BF16)
        ktok = [S.sb("rktok%d" % i, [128, 512], BF16) for i in range(2)]
        scTm = [S.sb("rscTm%d" % i, [128, 256], BF16) for i in range(2)]
        P32 = S.sb("rP32", [128, 4, 128], F32)
        Sb = S.sb("rSb", [128, 4, 128], BF16)
        hm = [S.sb("rhm%d" % i, [128, 512], BF16) for i in range(2)]
        hmTs = [S.sb("rhmTs%d" % i, [128, 4, 128], BF16) for i in range(2)]
        o32 = [S.sb("ro32%d" % i, [128, 256], F32) for i in range(4)]
        tmp_r = [(S.sb("rst6_%d" % i, [128, 2, 6], F32), S.sb("rmv_%d" % i, [128, 2, 2], F32),
                  S.sb("rt2_%d" % i, [128, 2], F32), S.sb("rre_%d" % i, [128, 2], F32)) for i in range(4)]
        S.dma(rng[:], G["rng_%d" % l])
        S.dma(rete[:], G["c_rete"])
        S.dma(retrow[:], G["c_retrow"])
        bank = 0
        for s in range(NSEQ):
            tb = s * SEQ
            S.dma(rq[:], G["rqT"].rearrange("(c p) t -> p c t", p=128)[:, :, tb:tb + SEQ])
            S.dma(rk[:], G["rkT"].rearrange("(c p) t -> p c t", p=128)[:, :, tb:tb + SEQ])
            dma_chunks(S, v_all, G["rv"][tb:tb + SEQ, :].rearrange("(n p) c -> p n c", p=128), NCH, 4)
            dma_chunks(S, rg_all, G["rg"][tb:tb + SEQ, :].rearrange("(n p) c -> p n c", p=128), NCH, 4)
            for h in range(4):
                S.ts(vt[:, :, h * 128:(h + 1) * 128], v_all[:, :, h * 128:(h + 1) * 128], rete[:, h:h + 1], None,
                     ALU.mult, eng="pool")
            for n in range(NCH):
                cs = slice(n * 128, (n + 1) * 128)
                kt = ktok[n % 2]
                ptb = P.pbB[n % 2]
                for h in range(4):
                    S.tr(ptb[:, h * 128:(h + 1) * 128], rk[:, h, cs], P.identb[:])
                S.copy(kt[:], ptb[:, 0:512], eng="act")
                hmc = hm[n % 2]
                for hp in range(2):
                    psc = P.pbF[bank % 6]
                    bank += 1
                    for hh in range(2):
                        h = 2 * hp + hh
                        S.mm(psc[:, hh * 128:(hh + 1) * 128], rk[:, h, cs], rq[:, h, cs])
                    sm = scTm[hp]
                    S.tt(sm[:], psc[:, 0:256], P.trib2[:], ALU.mult)
                    pNb = P.pbF[bank % 6]
                    bank += 1
                    pN = pNb[:, 0:256].rearrange("p (a b) -> p a b", a=2)
                    for hh in range(2):
                        h = 2 * hp + hh
                        S.mm(pN[:, hh, :], sm[:, hh * 128:(hh + 1) * 128], vt[:, n, h * 128:(h + 1) * 128],
                             start=True, stop=(n == 0))
                        if n > 0:
                            S.mm(pN[:, hh, :], rq[:, h, cs], Sb[:, h, :], start=False, stop=True)
                    if n < NCH - 1:
                        pUb = P.pbF[bank % 6]
                        bank += 1
                        for hh in range(2):
                            h = 2 * hp + hh
                            S.mm(pUb[:, hh * 128:(hh + 1) * 128], kt[:, h * 128:(h + 1) * 128],
                                 vt[:, n, h * 128:(h + 1) * 128])
                        for hh in range(2):
                            h = 2 * hp + hh
                            pu = pUb[:, hh * 128:(hh + 1) * 128]
                            if n == 0:
                                S.copy(P32[:, h, :], pu)
                            else:
                                S.stt(P32[:, h, :], P32[:, h, :], gam[h], pu, ALU.mult, ALU.add)
                            S.ts(Sb[:, h, :], P32[:, h, :], gam[h], None, ALU.mult)
                    ln_gate_out(P, pN, retrow[:, 2 * hp:2 * hp + 2], hp, n, o32[(2 * n + hp) % 4], rng, rg_all, hmc,
                                tmp_r[(2 * n + hp) % 4])
                out_transpose(P, hmc, hmTs[n % 2], G["hrT"], tb + n * 128, P.pbB[(n + 1) % 2])


def stage_C(P, l):
    S, G = P.S, P.G
    NT = SEQ // 128
    WSC = (8 ** -0.5) * (64 ** -0.5)
    with _scope(S):
        stg = [S.sb("stgC%d" % i, [128, 2048], F32) for i in range(2)]
        aqg = S.sb("aqg", [128, 2], F32)
        akg = S.sb("akg", [128, 1], F32)
        akvg = S.sb("akvg", [128, 128], F32)
        wuq = S.sb("wuq", [128, 2, 512], BF16)
        wuqi = S.sb("wuqi", [128, 2, 512], BF16)
        wuk = S.sb("wuk", [128, 1, 512], BF16)
        wuvz = S.sb("wuvz", [128, 8, 128], BF16)
        zt = [S.sb("ztC%d" % i, [128, ZS_N], F32) for i in range(4)]
        NR = 4
        junk_r = [S.sb("junkC%d" % i, [128, 256], BF16) for i in range(NR)]
        junk2_r = [S.sb("junk2C%d" % i, [128, 128], BF16) for i in range(NR)]
        ss_r = [S.sb("ssC%d" % i, [128, 2], F32) for i in range(NR)]
        rs_r = [S.sb("rsC%d" % i, [128, 2], F32) for i in range(NR)]
        st6_r = [S.sb("st6C%d" % i, [128, 6], F32) for i in range(NR)]
        mv_r = [S.sb("mvC%d" % i, [128, 2], F32) for i in range(NR)]
        cqn_r = [S.sb("cqn%d" % i, [128, 256], BF16) for i in range(NR)]
        kin_r = [S.sb("kin%d" % i, [128, 128], BF16) for i in range(NR)]
        ckn_r = [S.sb("ckn%d" % i, [128, 128], F32) for i in range(NR)]
        cqT = S.sb("cqT", [128, 2, SEQ], BF16)
        kidxT = S.sb("kidxT", [128, SEQ], BF16)
        ckv1 = S.sb("ckv1", [128, NT, 130], BF16)
        ckvT = S.sb("ckvT", [128, SEQ], BF16)
        qTa = S.sb("qTCa", [128, 4, 512], BF16)
        qTb = S.sb("qTCb", [128, 4, 512], BF16)
        qidxA = S.sb("qidxA", [128, 4, SEQ], BF16)
        qidxB = S.sb("qidxB", [128, 4, SEQ], BF16)
        qlatT = S.sb("qlatT", [128, 8, SEQ], BF16)
        wabs = S.sb("wabs", [128, NT, 8], F32)
        wsgn = S.sb("wsgn", [128, NT, 8], F32)
        acc2 = [S.sb("accC%d" % i, [128, SEQ], F32) for i in range(2)]
        NBIS = 24
        junkb = S.sb("junkbC", [128, SEQ], BF16)
        bis = [S.sb("bisC%d" % i, [128, 8], F32) for i in range(2)]
        dk = [S.sb("dkC%d" % i, [128, NBIS], F32) for i in range(2)]
        pw2 = S.sb("pw2C", [128, NBIS], F32)
        for k in range(NBIS):
            S.memset(pw2[:, k:k + 1], 2.0 ** -k, eng="pool")
        rbuf = [S.sb("rbufC%d" % i, [128, 512], F32) for i in range(3)]
        m8 = S.sb("m8C", [128, 8], F32)
        madd2 = [S.sb("maddC%d" % i, [128, SEQ], BF16) for i in range(2)]
        Eb = [S.sb("EbC%d" % i, [128, 512], BF16) for i in range(3)]
        otok = S.sb("otok", [128, 8, 128], BF16)
        olT = S.sb("olT", [128, 8, 128], BF16)
        haTs = [S.sb("haTs%d" % i, [128, 4, 128], BF16) for i in range(2)]
        rec = S.sb("recC", [128, 8], F32)
        S.dma(aqg[:], G["aqg_%d" % l])
        S.dma(akg[:], G["akg_%d" % l])
        S.dma(akvg[:], G["akvg_%d" % l])
        load_cast(P, wuq, G["wuq_%d" % l], 256, 512, gain=aqg, stg=stg)
        load_cast(P, wuqi, G["wuqi_%d" % l], 256, 512, gain=aqg, stg=stg)
        load_cast(P, wuk, G["wuk_%d" % l].rearrange("p a b -> p (a b)"), 128, 512, stg=stg)
        S.memset(wuvz[:], 0.0, eng="pool")
        sv = stg[P.stg_i % 2]
        P.stg_i += 1
        S.dma(sv[:, 0:512], G["wuv_%d" % l].rearrange("p a b -> p (a b)"))
        for h in range(8):
            S.copy(wuvz[:, h, (h % 2) * 64:(h % 2) * 64 + 64], sv[:, h * 64:(h + 1) * 64], eng="pool")
        S.memset(ckv1[:, :, 128:130], 1.0, eng="pool")
        S.memset(qTa[64:128, :, :], 0.0, eng="pool")
        S.memset(qTb[0:64, :, :], 0.0, eng="pool")
        S.memset(qidxA[64:128, :, :], 0.0, eng="pool")
        S.memset(qidxB[0:64, :, :], 0.0, eng="pool")
        bank = 0
        for s in range(NSEQ):
            tb = s * SEQ
            for i in range(NT):
                z = zt[i % 4]
                ri = i % NR
                junk, junk2, ss, rs, st6, mv = junk_r[ri], junk2_r[ri], ss_r[ri], rs_r[ri], st6_r[ri], mv_r[ri]
                cqn, kin, ckn = cqn_r[ri], kin_r[ri], ckn_r[ri]
                S.dma(z[:], G["zs"][tb + i * 128:tb + (i + 1) * 128, :])
                S.act(junk[:, 0:256], z[:, ZS_CQ:ZS_CQ + 256], AF.Square, accum_out=ss[:, 0:1])
                S.act(junk2[:, 0:128], z[:, ZS_CKV:ZS_CKV + 128], AF.Square, accum_out=ss[:, 1:2])
                S.ts(rs[:, 0:1], ss[:, 0:1], 1.0 / 256, EPS, ALU.mult, ALU.add)
                S.ts(rs[:, 1:2], ss[:, 1:2], 1.0 / 128, EPS, ALU.mult, ALU.add)
                S.act(rs[:], rs[:], AF.Sqrt)
                S.recip(rs[:], rs[:])
                S.ts(cqn[:], z[:, ZS_CQ:ZS_CQ + 256], rs[:, 0:1], None, ALU.mult)
                S.stt(ckn[:], z[:, ZS_CKV:ZS_CKV + 128], rs[:, 1:2], akvg[:], ALU.mult, ALU.mult)
                S.copy(ckv1[:, i, 0:128], ckn[:], eng="pool")
                S.bnstats(st6[:], z[:, ZS_KIDX:ZS_KIDX + 64])
                S.bnaggr(mv[:], st6[:])
                S.ts(mv[:, 1:2], mv[:, 1:2], EPS, None, ALU.add)
                S.act(mv[:, 1:2], mv[:, 1:2], AF.Sqrt)
                S.recip(mv[:, 1:2], mv[:, 1:2])
                S.ts(kin[:, 0:64], z[:, ZS_KIDX:ZS_KIDX + 64], mv[:, 0:1], mv[:, 1:2], ALU.subtract, ALU.mult)
                S.copy(kin[:, 64:128], kin[:, 0:64], eng="pool")
                S.act(wabs[:, i, :], z[:, ZS_WIDX:ZS_WIDX + 8], AF.Abs, scale=WSC)
                S.ts(wsgn[:, i, :], z[:, ZS_WIDX:ZS_WIDX + 8], 0.0, None, ALU.is_gt)
                S.ts(wsgn[:, i, :], wsgn[:, i, :], 2.0, -1.0, ALU.mult, ALU.add)
                ptb = P.pbB[i % 2]
                S.tr(ptb[:, 0:128], cqn[:, 0:128], P.identb[:])
                S.tr(ptb[:, 128:256], cqn[:, 128:256], P.identb[:])
                S.tr(ptb[:, 256:384], kin[:], P.identb[:])
                S.tr(ptb[:, 384:512], ckv1[:, i, 0:128], P.identb[:])
                cs = slice(i * 128, (i + 1) * 128)
                S.copy(cqT[:, :, cs], ptb[:, 0:256].rearrange("p (c t) -> p c t", c=2), eng="act")
                S.copy(kidxT[:, cs], ptb[:, 256:384], eng="act")
                S.ts(kidxT[:, cs], kidxT[:, cs], akg[:, 0:1], None, ALU.mult, eng="pool")
                S.copy(ckvT[:, cs], ptb[:, 384:512], eng="act")
            for b4 in range(SEQ // 512):
                ts_ = slice(b4 * 512, (b4 + 1) * 512)
                for c in range(4):
                    ps = P.pbF[bank % 6]
                    bank += 1
                    for k in range(2):
                        S.mm(ps[:], wuq[:, k, c * 128:(c + 1) * 128], cqT[:, k, ts_], start=(k == 0), stop=(k == 1))
                    S.copy(qTa[0:64, c, :], ps[0:64, :], eng="act")
                    S.copy(qTb[64:128, c, :], ps[64:128, :], eng="act")
                for c in range(4):
                    ps = P.pbF[bank % 6]
                    bank += 1
                    for k in range(2):
                        S.mm(ps[:], wuqi[:, k, c * 128:(c + 1) * 128], cqT[:, k, ts_], start=(k == 0), stop=(k == 1))
                    S.copy(qidxA[0:64, c, ts_], ps[0:64, :], eng="dve")
                    S.copy(qidxB[64:128, c, ts_], ps[64:128, :], eng="dve")
                for h in range(8):
                    qTx = qTa if h % 2 == 0 else qTb
                    ps = P.pbF[bank % 6]
                    bank += 1
                    S.mm(ps[:], wuk[:, 0, (h // 2) * 128:(h // 2 + 1) * 128], qTx[:, h // 2, :])
                    S.act(qlatT[:, h, ts_], ps[:], AF.Copy, scale=0.125)
            def idx_topk(i):
                nonlocal bank
                W = (i + 1) * 128
                cs = slice(i * 128, (i + 1) * 128)
                nb = (W + 511) // 512
                acc = acc2[i % 2]
                madd = madd2[i % 2]
                for b in range(nb):
                    w = min(512, W - b * 512)
                    ks = slice(b * 512, b * 512 + w)
                    for g in range(8):
                        qix = qidxA if g % 2 == 0 else qidxB
                        ps = P.pbF[bank % 4]
                        bank += 1
                        S.mm(ps[:, 0:w], qix[:, g // 2, cs], kidxT[:, ks])
                        r = rbuf[g % 3]
                        S.act(r[:, 0:w], ps[:, 0:w], AF.Relu, scale=wabs[:, i, g:g + 1])
                        if g == 0:
                            S.ts(acc[:, ks], r[:, 0:w], wsgn[:, i, 0:1], None, ALU.mult)
                        else:
                            S.stt(acc[:, ks], r[:, 0:w], wsgn[:, i, g:g + 1], acc[:, ks], ALU.mult, ALU.add)
                S.tt(acc[:, cs], acc[:, cs], P.negd[:], ALU.add)
                if i >= 2:
                    bs = bis[i % 2]
                    S.max8(m8[:], acc[:, 0:W])
                    S.treduce(bs[:, 0:1], acc[:, 0:i * 128], ALU.min)
                    S.ts(bs[:, 1:2], m8[:, 0:1], bs[:, 0:1], 0.5, ALU.subtract, ALU.mult)
                    S.ts(bs[:, 2:3], m8[:, 0:1], bs[:, 0:1], 0.5, ALU.add, ALU.mult)
                    S.ts(dk[i % 2][:], pw2[:], bs[:, 1:2], None, ALU.mult)
                    for k in range(NBIS):
                        S.ts_acc(junkb[:, 0:W], acc[:, 0:W], bs[:, 2:3], 0.0, ALU.is_ge, ALU.add, bs[:, 3:4])
                        S.ts(bs[:, 4:5], bs[:, 3:4], 255.5, -0.5, ALU.is_ge, ALU.add)
                        S.stt(bs[:, 2:3], bs[:, 4:5], dk[i % 2][:, k:k + 1], bs[:, 2:3], ALU.mult, ALU.add)
                    S.tt(bs[:, 5:6], bs[:, 2:3], dk[i % 2][:, NBIS - 1:NBIS], ALU.subtract)
                    S.ts(madd[:, 0:W], acc[:, 0:W], bs[:, 5:6], 1.0, ALU.is_ge, ALU.subtract)
                else:
                    S.ts(madd[:, 0:W], acc[:, 0:W], -1.0e29, 1.0, ALU.is_ge, ALU.subtract)

            def attn(i):
                nonlocal bank
                cs = slice(i * 128, (i + 1) * 128)
                madd = madd2[i % 2]
                for h in range(8):
                    hh = h % 2
                    pvbank = P.pbF[4 + (h // 2) % 2]
                    pv = pvbank[:, hh * 129:(hh + 1) * 129]
                    for bg in range((i + 4) // 4):
                        j0 = bg * 4
                        nj = min(4, i + 1 - j0)
                        pl = P.pbF[bank % 4]
                        bank += 1
                        for jj in range(nj):
                            j = j0 + jj
                            near = j >= i - 1
                            blk = pl[:, jj * 128:(jj + 1) * 128]
                            S.mm(blk, ckvT[:, j * 128:(j + 1) * 128], qlatT[:, h, cs], start=True, stop=False)
                            S.mm(blk, madd[:, j * 128:(j + 1) * 128], P.i30k[:], start=False, stop=(not near))
                            if near:
                                S.mm(blk, P.biasb[:, h, i - j, :], P.identb[:], start=False, stop=True)
                        E = Eb[(h * 4 + bg) % 3]
                        S.act(E[:, 0:nj * 128], pl[:, 0:nj * 128], AF.Exp)
                        for jj in range(nj):
                            j = j0 + jj
                            S.mm(pv, E[:, jj * 128:(jj + 1) * 128], ckv1[:, j, 0:129], start=(j == 0), stop=(j == i))
                    S.recip(rec[:, h:h + 1], pv[:, 128:129])
                    S.act(otok[:, h, :], pv[:, 0:128], AF.Copy, scale=rec[:, h:h + 1])
                ptb = P.pbB[i % 2]
                for h in range(8):
                    S.tr(ptb[:, h * 128:(h + 1) * 128], otok[:, h, :], P.identb[:])
                S.copy(olT[:], ptb[:, :].rearrange("p (h t) -> p h t", h=8), eng="act")
                ph = P.pbF[bank % 4]
                bank += 1
                for hc in range(4):
                    S.mm(ph[:, hc * 128:(hc + 1) * 128], wuvz[:, 2 * hc, :], olT[:, 2 * hc, :], start=True, stop=False)
                    S.mm(ph[:, hc * 128:(hc + 1) * 128], wuvz[:, 2 * hc + 1, :], olT[:, 2 * hc + 1, :], start=False, stop=True)
                hs = haTs[i % 2]
                S.copy(hs[:], ph[:].rearrange("p (c t) -> p c t", c=4), eng="act")
                S.dma(G["haT"].rearrange("(c p) t -> p c t", p=128)[:, :, tb + i * 128:tb + (i + 1) * 128], hs[:])

            idx_topk(0)
            for i in range(NT):
                if i + 1 < NT:
                    idx_topk(i + 1)
                attn(i)


def stage_E(P, l, x_in):
    S, G = P.S, P.G
    with _scope(S):
        stg = [S.sb("stgE%d" % i, [128, 1024], F32) for i in range(3)]
        pw = [S.sb("pwE%d" % i, [128, 4, D], BF16) for i in range(3)]
        wo = S.sb("woE", [128, 8, D], BF16)
        hin = [[S.sb("hinE%d_%d" % (b, i), [128, 4, 512], BF16) for i in range(2)] for b in range(3)]
        gin = [S.sb("ginE%d" % i, [128, 24, 512], BF16) for i in range(2)]
        y32 = S.sb("y32E", [128, 512], F32)
        t32 = [S.sb("t32E%d" % i, [128, 512], F32) for i in range(2)]
        yT = S.sb("yTE", [128, 8, 512], BF16)
        xb = [S.sb("xbE%d" % i, [128, D], F32) for i in range(2)]
        for b, nm in enumerate(("p_m", "p_a", "p_r")):
            load_cast(P, pw[b], G["%s_%d" % (nm, l)], 512, D, stg=stg)
        load_cast(P, wo, G["w_out_%d" % l], D, D, stg=stg)
        srcs = [G["hmT"], G["haT"], G["hrT"]]
        bank = 0
        for st in range(NTOK // 512):
            t0 = st * 512
            for b in range(3):
                S.dma(hin[b][st % 2][:], srcs[b].rearrange("(c p) t -> p c t", p=128)[:, :, t0:t0 + 512])
            gt = gin[st % 2]
            dma_chunks(S, gt, G["gT"].rearrange("(c p) t -> p c t", p=128)[:, :, t0:t0 + 512], 24, 4)
            for c in range(8):
                pss = []
                for b in range(3):
                    ps = P.pbF[bank % 6]
                    bank += 1
                    for k in range(4):
                        S.mm(ps[:], pw[b][:, k, c * 128:(c + 1) * 128], hin[b][st % 2][:, k, :],
                             start=(k == 0), stop=(k == 3))
                    pss.append(ps)
                S.tt(y32[:], pss[0][:], gt[:, c, :], ALU.mult)
                S.tt(t32[0][:], pss[1][:], gt[:, 8 + c, :], ALU.mult)
                S.tt(t32[1][:], pss[2][:], gt[:, 16 + c, :], ALU.mult)
                S.tt(y32[:], y32[:], t32[0][:], ALU.add, eng="pool")
                S.tt(yT[:, c, :], y32[:], t32[1][:], ALU.add, eng="pool")
            for ti in range(4):
                r0 = t0 + ti * 128
                xt = xb[ti % 2]
                S.dma(xt[:], x_in[r0:r0 + 128, :])
                for hf in range(2):
                    ps = P.pbF[bank % 6]
                    bank += 1
                    for k in range(8):
                        S.mm(ps[:], yT[:, k, ti * 128:(ti + 1) * 128], wo[:, k, hf * 512:(hf + 1) * 512],
                             start=(k == 0), stop=(k == 7))
                    S.tt(xt[:, hf * 512:(hf + 1) * 512], xt[:, hf * 512:(hf + 1) * 512], ps[:], ALU.add)
                S.dma(G["x1"][r0:r0 + 128, :], xt[:])


def stage_F(P, l, x_out, final):
    S, G = P.S, P.G
    ST = 256
    NF = DFF // 128
    with _scope(S):
        stg = [S.sb("stgF%d" % i, [128, 1024], F32) for i in range(3)]
        n2g = S.sb("n2g", [128, 8], F32)
        wg = S.sb("wgF", [128, 8, DFF], BF16)
        wu = S.sb("wuF", [128, 8, DFF], BF16)
        wd = S.sb("wdF", [128, NF, D], BF16)
        xb = [S.sb("xbF%d" % i, [128, D], F32) for i in range(2)]
        hb = S.sb("hbF", [128, D], BF16)
        junk = S.sb("junkF", [128, D], BF16)
        ss = S.sb("ssF", [128, 1], F32)
        rs = S.sb("rsF", [128, 1], F32)
        hT = S.sb("hTF", [128, 8, ST], BF16)
        sg = [S.sb("sgF%d" % i, [128, ST], F32) for i in range(2)]
        aT = S.sb("aTF", [128, NF, ST], BF16)
        fng = None
        if final:
            fng = S.sb("fngF", [128, D], F32)
            S.dma(fng[:], G["fng"])
        S.dma(n2g[:], G["n2g_%d" % l])
        load_cast(P, wg, G["w_gate_%d" % l], D, DFF, gain=n2g, stg=stg)
        load_cast(P, wu, G["w_up_%d" % l], D, DFF, gain=n2g, stg=stg)
        load_cast(P, wd, G["w_down_%d" % l], DFF, D, stg=stg)
        bank = 0
        for st in range(NTOK // ST):
            t0 = st * ST
            for ti in range(ST // 128):
                xt = xb[ti % 2]
                S.dma(xt[:], G["x1"][t0 + ti * 128:t0 + (ti + 1) * 128, :])
                norm_transpose(P, xt, hb, junk, ss, rs, hT, ti * 128, P.pbB[ti % 2])
            for f in range(NF):
                pg = P.pbF[bank % 6]
                pu = P.pbF[(bank + 1) % 6]
                bank += 2
                for k in range(8):
                    S.mm(pg[:, 0:ST], wg[:, k, f * 128:(f + 1) * 128], hT[:, k, :], start=(k == 0), stop=(k == 7))
                for k in range(8):
                    S.mm(pu[:, 0:ST], wu[:, k, f * 128:(f + 1) * 128], hT[:, k, :], start=(k == 0), stop=(k == 7))
                sgt = sg[f % 2]
                S.act(sgt[:], pg[:, 0:ST], AF.Silu)
                S.tt(aT[:, f, :], sgt[:], pu[:, 0:ST], ALU.mult)
            for ti in range(ST // 128):
                r0 = t0 + ti * 128
                xt = xb[ti % 2]
                S.dma(xt[:], G["x1"][r0:r0 + 128, :])
                for hf in range(2):
                    ps = P.pbF[bank % 6]
                    bank += 1
                    for f in range(NF):
                        S.mm(ps[:], aT[:, f, ti * 128:(ti + 1) * 128], wd[:, f, hf * 512:(hf + 1) * 512],
                             start=(f == 0), stop=(f == NF - 1))
                    S.tt(xt[:, hf * 512:(hf + 1) * 512], xt[:, hf * 512:(hf + 1) * 512], ps[:], ALU.add)
                if final:
                    S.act(junk[:], xt[:], AF.Square, accum_out=ss[:])
                    rms_rstd(P, rs[:], ss[:], D)
                    S.stt(xt[:], xt[:], rs[:, 0:1], fng[:], ALU.mult, ALU.mult)
                S.dma(x_out[r0:r0 + 128, :], xt[:])


SCRATCH = {
    "hT_d": ([D, NTOK], BF16), "qkT": ([512, NTOK], F32), "rqT": ([512, NTOK], BF16), "rkT": ([512, NTOK], BF16),
    "gT": ([3072, NTOK], BF16), "zv": ([NTOK, 512], BF16), "zo": ([NTOK, 512], BF16), "rv": ([NTOK, 512], BF16),
    "rg": ([NTOK, 512], BF16), "zs": ([NTOK, ZS_N], F32), "hmT": ([512, NTOK], BF16), "haT": ([512, NTOK], BF16),
    "hrT": ([512, NTOK], BF16), "x1": ([NTOK, D], F32), "xl": ([NTOK, D], F32),
}


def build_program(n_layers=2, dbg=(), stages="A1,A2,B,C,D,E,F"):
    stages = stages.split(",")
    nc = bass.Bass("TRN2", target_bir_lowering=False)
    P = Prog()
    G = {}
    P.G = G
    P.stg_i = 0
    G["x"] = nc.dram_tensor("x", [NTOK, D], F32, kind="ExternalInput").ap()
    for k, shp in CONST_SHAPES.items():
        G[k] = nc.dram_tensor(k, shp, F32, kind="ExternalInput").ap()
    for l in range(n_layers):
        for k, shp in LAYER_SHAPES.items():
            nm = "%s_%d" % (k, l)
            G[nm] = nc.dram_tensor(nm, shp, F32, kind="ExternalInput").ap()
    G["out"] = nc.dram_tensor("out", [NTOK, D], F32, kind="ExternalOutput").ap()
    for k, (shp, dt) in SCRATCH.items():
        G[k] = nc.dram_tensor(k, shp, dt, kind=("ExternalOutput" if k in dbg else "Internal")).ap()
    P.ret_g = host_constants()["ret_g"]
    with ExitStack() as es:
        S = Sched(nc, es)
        P.S = S
        P.pbF = [S.ps("pbF%d" % i, [128, 512], F32) for i in range(6)]
        P.pbB = [S.ps("pbB%d" % i, [128, 1024], BF16) for i in range(2)]
        P.identb = S.sb("identb", [128, 128], BF16)
        P.i30k = S.sb("i30k", [128, 128], BF16)
        P.trib2 = S.sb("trib2", [128, 256], BF16)
        P.tri32 = S.sb("tri32", [128, 128], F32)
        P.ones32 = S.sb("ones32", [128, 128], F32)
        P.negd = S.sb("negd", [128, 128], F32)
        P.biasb = S.sb("biasb", [128, 8, 2, 128], BF16)
        with _scope(S):
            c32 = S.sb("c32", [128, 128], F32)
            b32 = S.sb("b32", [128, 8, 2, 128], F32)
            rb31 = S.sb("rb31", [128, 8], F32)
            S.dma(c32[:], G["c_ident"])
            S.copy(P.identb[:], c32[:])
            S.ts(P.i30k[:], c32[:], 30000.0, None, ALU.mult)
            S.dma(P.tri32[:], G["c_tri"])
            S.copy(P.trib2[:, 0:128], P.tri32[:])
            S.copy(P.trib2[:, 128:256], P.tri32[:])
            S.dma(P.ones32[:], G["c_ones"])
            S.dma(P.negd[:], G["c_negd"])
            S.dma(b32[:], G["biasn"])
            S.dma(rb31[:], G["rb31"])
            for h in range(8):
                S.ts(P.biasb[:, h, :, :], b32[:, h, :, :], rb31[:, h:h + 1], None, ALU.subtract)
        x_in = G["x"]
        for l in range(n_layers):
            last = (l == n_layers - 1)
            x_out = G["out"] if last else G["xl"]
            if "A1" in stages:
                stage_A1(P, l, x_in)
            if "A2" in stages:
                stage_A2(P, l)
            if "B" in stages:
                stage_B(P, l)
            if "D" in stages:
                stage_D(P, l)
            if "C" in stages:
                stage_C(P, l)
            if "E" in stages:
                stage_E(P, l, x_in)
            if "F" in stages:
                stage_F(P, l, x_out, last)
            x_in = x_out
        S.barrier()
        P.n_inst = S.n_inst
    return nc, P


_CACHE = {}


def make_in_maps(inputs, n_layers=2, cores=range(NCORES)):
    hc = host_constants()
    shared = {k: np.ascontiguousarray(v, dtype=np.float32) for k, v in hc.items() if k.startswith("c_")}
    rel_bias = np.asarray(inputs["rel_bias"], np.float32)
    bi = bias_index()
    biasn = rel_bias[bi]
    shared["biasn"] = np.ascontiguousarray(biasn.transpose(0, 3, 1, 2))
    shared["rb31"] = np.ascontiguousarray(np.broadcast_to(rel_bias[31][None, :], (128, 8)))
    shared["fng"] = np.ascontiguousarray(np.broadcast_to(np.asarray(inputs["final_norm_g"], np.float32)[None, :], (128, D)))
    npin = {k: np.asarray(v) for k, v in inputs.items()}
    for l in range(n_layers):
        shared.update(prep_layer_weights(npin, l))
    x = npin["x"].astype(np.float32, copy=False)
    maps = []
    for c in cores:
        m = dict(shared)
        m["x"] = np.ascontiguousarray(x[NSEQ * c:NSEQ * (c + 1)].reshape(NTOK, D))
        maps.append(m)
    return maps


def kernel(**inputs):
    if "nc" not in _CACHE:
        _CACHE["nc"] = build_program()[0]
    nc = _CACHE["nc"]
    maps = make_in_maps(inputs)
    res = run_bass_kernel_spmd(nc, maps, core_ids=list(range(NCORES)))
    outs = [np.asarray(r["out"], dtype=np.float32).reshape(NSEQ, SEQ, D) for r in res.results]
    return np.concatenate(outs, axis=0)
```

```python
import math
from contextlib import ExitStack
import numpy as np
import concourse.bass as bass
import concourse.mybir as mybir
from concourse.bass_utils import run_bass_kernel_spmd

F32 = mybir.dt.float32
BF16 = mybir.dt.bfloat16
AF = mybir.ActivationFunctionType
ALU = mybir.AluOpType

N_DMA_SEMS = 40
NCORES = 8
SEQ = 2048
NSEQ = 2
NTOK = NSEQ * SEQ
D = 1024
DFF = 2816
EPS = 1e-6
NEG = -1.0e30


def _key(x):
    if isinstance(x, tuple):
        return x[0].tensor.name + ":" + str(x[1])
    if isinstance(x, str):
        return x
    return x.tensor.name


def _ap(x):
    return x[0] if isinstance(x, tuple) else x


def _isnum(v):
    return isinstance(v, (int, float))


class Sched:
    ENG = ("pe", "act", "dve", "pool", "sp")

    def __init__(self, nc, es):
        self.nc = nc
        self.es = es
        self.obj = {"pe": nc.tensor, "act": nc.scalar, "dve": nc.vector, "pool": nc.gpsimd, "sp": nc.sync}
        self.count = {e: 0 for e in self.ENG}
        self.sem = {e: es.enter_context(nc.semaphore("s_" + e)) for e in ("pe", "act", "dve", "pool")}
        self.dsem = [es.enter_context(nc.semaphore("d%d" % i)) for i in range(N_DMA_SEMS)]
        self.dval = [0] * N_DMA_SEMS
        self.dma_n = 0
        self.last_w = {}
        self.readers = {}
        self.waited = {e: {} for e in self.ENG}
        self.n_inst = 0
        self.uid = 0

    def sb(self, name, shape, dt):
        self.uid += 1
        return self.es.enter_context(self.nc.sbuf_tensor("%s_u%d" % (name, self.uid), list(shape), dt))

    def ps(self, name, shape, dt):
        return self.es.enter_context(self.nc.psum_tensor(name, list(shape), dt))

    def _deps(self, eng, reads, writes):
        toks = set()
        for k in reads:
            t = self.last_w.get(k)
            if t is not None:
                toks.add(t)
        for k in writes:
            t = self.last_w.get(k)
            if t is not None:
                toks.add(t)
            for t in self.readers.get(k, ()):
                toks.add(t)
        best = {}
        for (sk, v) in toks:
            if sk == eng and eng == "pe":
                continue
            if v > best.get(sk, 0):
                best[sk] = v
        waits = []
        w = self.waited[eng]
        for sk, v in best.items():
            if w.get(sk, 0) >= v:
                continue
            w[sk] = v
            waits.append((sk, v))
        return waits

    def _commit(self, tok, reads, writes):
        for k in reads:
            self.readers.setdefault(k, []).append(tok)
        for k in writes:
            self.last_w[k] = tok
            self.readers[k] = []

    def _emit(self, eng, waits, fn, kind):
        engine = self.obj[eng]
        for sk, v in waits:
            engine.wait_ge(self._semof(sk), v)
        if fn is None:
            return
        ins = fn(engine)
        if kind[0] == "c":
            ins.then_inc(self.sem[kind[1]], 1)
        else:
            ins.then_inc(self.dsem[kind[1]], 16)

    def op(self, eng, fn, reads, writes):
        rk = [_key(r) for r in reads]
        wk = [_key(w) for w in writes]
        waits = self._deps(eng, rk, wk)
        self.count[eng] += 1
        tok = (eng, self.count[eng])
        self._emit(eng, waits, fn, ("c", eng))
        self._commit(tok, rk, wk)
        self.n_inst += 1

    def dma(self, out, in_, q="sp", **kw):
        rk = [_key(in_)]
        wk = [_key(out)]
        slot = self.dma_n % N_DMA_SEMS
        self.dma_n += 1
        waits = self._deps(q, rk, wk)
        sk = ("d", slot)
        prev = self.dval[slot]
        if prev > 0 and self.waited[q].get(sk, 0) < prev:
            self.waited[q][sk] = prev
            waits.append((sk, prev))
        self.dval[slot] = prev + 16
        tok = (sk, prev + 16)
        o, i = _ap(out), _ap(in_)
        self._emit(q, waits, (lambda e: e.dma_start(out=o, in_=i, **kw)), ("d", slot))
        self._commit(tok, rk, wk)
        self.n_inst += 1

    def barrier(self):
        for e in self.ENG:
            waits = []
            w = self.waited[e]
            for e2 in ("pe", "act", "dve", "pool"):
                v = self.count[e2]
                if v == 0 or w.get(e2, 0) >= v:
                    continue
                if e2 == e and e == "pe":
                    continue
                w[e2] = v
                waits.append((e2, v))
            for s in range(N_DMA_SEMS):
                v = self.dval[s]
                sk = ("d", s)
                if v == 0 or w.get(sk, 0) >= v:
                    continue
                w[sk] = v
                waits.append((sk, v))
            if waits:
                self._emit(e, waits, None, None)
        self.last_w = {}
        self.readers = {}

    def _semof(self, sk):
        if isinstance(sk, tuple):
            return self.dsem[sk[1]]
        return self.sem[sk]

    def mm(self, out, lhsT, rhs, start=True, stop=True):
        o, l, r = _ap(out), _ap(lhsT), _ap(rhs)
        self.op("pe", lambda e: e.matmul(o, l, r, start=start, stop=stop), [lhsT, rhs], [out])

    def tr(self, out, in_, ident):
        o, i, d = _ap(out), _ap(in_), _ap(ident)
        self.op("pe", lambda e: e.transpose(o, i, d), [in_, ident], [out])

    def act(self, out, in_, func, bias=None, scale=None, accum_out=None):
        o, i = _ap(out), _ap(in_)
        kw = {}
        reads = [in_]
        writes = [out]
        if bias is not None:
            if _isnum(bias):
                kw["bias"] = bias
            else:
                kw["bias"] = _ap(bias)
                reads.append(bias)
        if scale is not None:
            if _isnum(scale):
                kw["scale"] = scale
            else:
                kw["scale"] = _ap(scale)
                reads.append(scale)
        if accum_out is not None:
            kw["accum_out"] = _ap(accum_out)
            writes.append(accum_out)
        self.op("act", lambda e: e.activation(o, i, func, **kw), reads, writes)

    def ts(self, out, in0, s1, s2, op0, op1=None, eng="dve"):
        o, i = _ap(out), _ap(in0)
        reads = [in0]
        a1, a2 = s1, s2
        if s1 is not None and not _isnum(s1):
            reads.append(s1)
            a1 = _ap(s1)
        if s2 is not None and not _isnum(s2):
            reads.append(s2)
            a2 = _ap(s2)
        if op1 is None:
            self.op(eng, lambda e: e.tensor_scalar(o, i, a1, None, op0), reads, [out])
        else:
            self.op(eng, lambda e: e.tensor_scalar(o, i, a1, a2, op0, op1), reads, [out])

    def ts_acc(self, out, in0, s1, init, op0, red_op, accum_out):
        o, i, ac = _ap(out), _ap(in0), _ap(accum_out)
        reads = [in0]
        a1 = s1
        if not _isnum(s1):
            reads.append(s1)
            a1 = _ap(s1)
        self.op("dve", lambda e: e.tensor_scalar(o, i, a1, init, op0, red_op, accum_out=ac), reads, [out, accum_out])

    def treduce(self, out, in_, op):
        o, i = _ap(out), _ap(in_)
        self.op("dve", lambda e: e.tensor_reduce(o, i, mybir.AxisListType.X, op), [in_], [out])

    def tt(self, out, in0, in1, op, eng="dve"):
        o, a, b = _ap(out), _ap(in0), _ap(in1)
        self.op(eng, lambda e: e.tensor_tensor(o, a, b, op), [in0, in1], [out])

    def stt(self, out, in0, scalar, in1, op0, op1):
        o, a, b = _ap(out), _ap(in0), _ap(in1)
        reads = [in0, in1]
        s = scalar
        if not _isnum(scalar):
            reads.append(scalar)
            s = _ap(scalar)
        self.op("dve", lambda e: e.scalar_tensor_tensor(o, a, s, b, op0, op1), reads, [out])

    def copy(self, out, in_, eng="dve"):
        o, i = _ap(out), _ap(in_)
        if eng == "act":
            self.op("act", lambda e: e.copy(o, i), [in_], [out])
        else:
            self.op(eng, lambda e: e.tensor_copy(o, i), [in_], [out])

    def memset(self, out, val, eng="dve"):
        o = _ap(out)
        self.op(eng, lambda e: e.memset(o, val), [], [out])

    def recip(self, out, in_):
        o, i = _ap(out), _ap(in_)
        self.op("dve", lambda e: e.reciprocal(o, i), [in_], [out])

    def max8(self, out, in_):
        o, i = _ap(out), _ap(in_)
        self.op("dve", lambda e: e.max(o, i), [in_], [out])

    def mrep(self, out, rep, vals, imm):
        o, r, v = _ap(out), _ap(rep), _ap(vals)
        self.op("dve", lambda e: e.match_replace(o, r, v, imm), [rep, vals], [out])

    def bnstats(self, out, in_):
        o, i = _ap(out), _ap(in_)
        self.op("dve", lambda e: e.bn_stats(o, i), [in_], [out])

    def bnaggr(self, out, in_):
        o, i = _ap(out), _ap(in_)
        self.op("dve", lambda e: e.bn_aggr(o, i), [in_], [out])


def _t5_bucket_np(dist):
    dist = np.asarray(dist, dtype=np.int64)
    max_exact = 16
    d_f = np.maximum(dist, 1).astype(np.float32)
    large = max_exact + (np.log(d_f / np.float32(max_exact)) / np.float32(math.log(128 / max_exact))
                         * np.float32(32 - max_exact)).astype(np.int32)
    large = np.minimum(large, 31)
    return np.where(dist < max_exact, dist, large).astype(np.int64)


def host_constants():
    c = {}
    r = np.arange(128)
    c["c_ident"] = np.eye(128, dtype=np.float32)
    c["c_tri"] = (r[:, None] <= r[None, :]).astype(np.float32)
    c["c_ones"] = np.ones((128, 128), np.float32)
    c["c_negd"] = np.where(r[None, :] <= r[:, None], 0.0, NEG).astype(np.float32)
    half = 64
    freqs = (10000.0 ** (-np.linspace(0.0, 1.0, half, dtype=np.float32))).astype(np.float32)
    pos = np.arange(SEQ, dtype=np.float32)
    ang = (pos[None, :] * freqs[:, None]).astype(np.float32)
    cos = np.cos(ang).astype(np.float32)
    sin = np.sin(ang).astype(np.float32)
    c["c_cos"] = np.concatenate([cos, cos], 0)
    c["c_sin"] = np.concatenate([sin, sin], 0)
    h = np.arange(4, dtype=np.float64)
    lg = np.log1p(-np.exp2(-5.0 - h))
    rr = np.arange(128, dtype=np.float64)
    c["c_rete"] = np.exp(-(rr[:, None] + 1.0) * lg[None, :]).astype(np.float32)
    c["c_retrow"] = (np.exp((rr[:, None] + 1.0) * lg[None, :]) * 128 ** -0.5).astype(np.float32)
    c["ret_g"] = [float(np.exp(128.0 * v)) for v in lg]
    return c


def bias_index():
    t = np.arange(128)
    out = np.zeros((128, 2, 128), np.int64)
    for off in range(2):
        d = t[:, None] - t[None, :] + 128 * off
        out[:, off, :] = _t5_bucket_np(np.maximum(d, 0))
    return out


OFF = dict(m_q=0, m_k=256, m_v=512, m_i=1024, m_f=1028, m_o=1032, a_cq=1544, a_kidx=1800,
           a_widx=1864, a_ckv=1872, r_q=2000, r_k=2512, r_v=3024, r_g=3536, g_m=4048, g_a=5072,
           g_r=6096)
NFM = 5632
NTM = 2512
ZS_CQ, ZS_CKV, ZS_KIDX, ZS_WIDX, ZS_MI, ZS_MF, ZS_N = 0, 256, 384, 448, 456, 460, 464


def _rot_pre(w):
    w4 = w.reshape(w.shape[0], 4, 2, 64)
    return np.ascontiguousarray(w4[:, :, ::-1, :]).reshape(w.shape[0], 512)


def prep_layer_weights(inp, l):
    w = inp["w_in"][l]
    sl = lambda name, n: w[:, OFF[name]:OFF[name] + n]
    rq, rk = sl("r_q", 512), sl("r_k", 512)
    w_fm = np.concatenate([sl("m_q", 256), sl("m_k", 256), rq, _rot_pre(rq), rk, _rot_pre(rk),
                           sl("g_m", 1024), sl("g_a", 1024), sl("g_r", 1024)], axis=1)
    w_tm = np.concatenate([sl("m_v", 512), sl("m_o", 512), sl("r_v", 512), sl("r_g", 512),
                           sl("a_cq", 256), sl("a_ckv", 128), sl("a_kidx", 64), sl("a_widx", 8),
                           sl("m_i", 4), sl("m_f", 4)], axis=1)
    assert w_fm.shape[1] == NFM and w_tm.shape[1] == NTM
    rep = lambda v: np.ascontiguousarray(np.broadcast_to(np.asarray(v, np.float32).reshape(1, -1), (128, np.asarray(v).size)))
    colv = lambda v: np.ascontiguousarray(np.asarray(v, np.float32).reshape(-1, 128).T)
    d = {
        "w_fm": np.ascontiguousarray(w_fm), "w_tm": np.ascontiguousarray(w_tm),
        "n1g": colv(inp["norm1_g"][l]), "n2g": colv(inp["norm2_g"][l]),
        "mconv": np.ascontiguousarray(inp["m_conv"][l].reshape(4, 4, 128).transpose(2, 1, 0)),
        "mbias": rep(np.tile(np.concatenate([inp["m_ibias"][l], inp["m_fbias"][l]]), 16)),
        "mng": rep(inp["m_norm_g"][l]), "rng": rep(inp["r_norm_g"][l]),
        "aqg": colv(inp["a_qnorm_g"][l]),
        "akg": colv(np.concatenate([inp["a_kidx_g"][l], inp["a_kidx_g"][l]])),
        "akvg": rep(inp["a_kvnorm_g"][l]),
        "wuq": np.ascontiguousarray(inp["a_wuq"][l]), "wuqi": np.ascontiguousarray(inp["a_wuq_idx"][l]),
        "wuk": np.ascontiguousarray(inp["a_wuk"][l].reshape(4, 2, 64, 128).transpose(1, 2, 0, 3).reshape(128, 4, 128)),
        "wuv": np.ascontiguousarray(inp["a_wuv"][l].transpose(1, 0, 2)),
        "p_m": np.ascontiguousarray(inp["p_m"][l]), "p_a": np.ascontiguousarray(inp["p_a"][l]),
        "p_r": np.ascontiguousarray(inp["p_r"][l]), "w_out": np.ascontiguousarray(inp["w_out"][l]),
        "w_gate": np.ascontiguousarray(inp["w_gate"][l]), "w_up": np.ascontiguousarray(inp["w_up"][l]),
        "w_down": np.ascontiguousarray(inp["w_down"][l]),
    }
    return {"%s_%d" % (k, l): v for k, v in d.items()}


LAYER_SHAPES = {
    "w_fm": [D, NFM], "w_tm": [D, NTM], "n1g": [128, 8], "n2g": [128, 8], "mconv": [128, 4, 4],
    "mbias": [128, 128], "mng": [128, 512], "rng": [128, 512], "aqg": [128, 2], "akg": [128, 1],
    "akvg": [128, 128], "wuq": [256, 512], "wuqi": [256, 512], "wuk": [128, 4, 128],
    "wuv": [128, 8, 64], "p_m": [512, D], "p_a": [512, D], "p_r": [512, D], "w_out": [D, D],
    "w_gate": [D, DFF], "w_up": [D, DFF], "w_down": [DFF, D],
}
CONST_SHAPES = {"c_ident": [128, 128], "c_tri": [128, 128], "c_ones": [128, 128], "c_negd": [128, 128],
                "c_cos": [128, SEQ], "c_sin": [128, SEQ], "c_rete": [128, 4], "c_retrow": [128, 4],
                "biasn": [128, 8, 2, 128], "rb31": [128, 8], "fng": [128, D]}


class Prog:
    pass


def _scope(S):
    class _Sc:
        def __enter__(self_):
            self_.old = S.es
            self_.es = ExitStack()
            self_.es.__enter__()
            S.es = self_.es
            return self_

        def __exit__(self_, *a):
            S.barrier()
            S.es = self_.old
            return self_.es.__exit__(*a)
    return _Sc()


def load_cast(P, dst, src, K, N, gain=None, stg=None, engs=("pool", "dve", "act")):
    S = P.S
    kc = K // 128
    srcv = src.rearrange("(c p) n -> p c n", p=128)
    CH = stg[0].shape[1]
    for c in range(kc):
        for n0 in range(0, N, CH):
            n1 = min(N, n0 + CH)
            sbuf = stg[P.stg_i % len(stg)]
            eng = engs[P.stg_i % len(engs)]
            P.stg_i += 1
            S.dma(sbuf[:, 0:n1 - n0], srcv[:, c, n0:n1])
            if gain is None:
                S.copy(dst[:, c, n0:n1], sbuf[:, 0:n1 - n0], eng=eng)
            elif eng == "act":
                S.act(dst[:, c, n0:n1], sbuf[:, 0:n1 - n0], AF.Copy, scale=gain[:, c:c + 1])
            else:
                S.ts(dst[:, c, n0:n1], sbuf[:, 0:n1 - n0], gain[:, c:c + 1], None, ALU.mult, eng=eng)


def dma_chunks(S, out, in_, n, step):
    for a in range(0, n, step):
        b = min(n, a + step)
        S.dma(out[:, a:b], in_[:, a:b])


def rms_rstd(P, rs, ss, n):
    S = P.S
    S.ts(rs, ss, 1.0 / n, EPS, ALU.mult, ALU.add)
    S.act(rs, rs, AF.Sqrt)
    S.recip(rs, rs)


def norm_transpose(P, xt, hb, junk, ss, rs, hT, col0, ptb):
    S = P.S
    S.act(junk[:], xt[:], AF.Square, accum_out=ss[:])
    rms_rstd(P, rs[:], ss[:], D)
    S.ts(hb[:], xt[:], rs[:], None, ALU.mult)
    for k in range(8):
        S.tr(ptb[:, k * 128:(k + 1) * 128], hb[:, k * 128:(k + 1) * 128], P.identb[:])
    S.copy(hT[:, :, col0:col0 + 128], ptb[:, :].rearrange("p (k t) -> p k t", k=8), eng="act")


def stage_A1(P, l, x_in):
    S, G = P.S, P.G
    with _scope(S):
        Wfm = S.sb("Wfm", [128, 8, NFM], BF16)
        stg = [S.sb("stgA%d" % i, [128, 2048], F32) for i in range(3)]
        cos = S.sb("cos", [128, SEQ], F32)
        sin = S.sb("sin", [128, SEQ], F32)
        n1g = S.sb("n1g", [128, 8], F32)
        xb = [S.sb("xbA%d" % i, [128, D], F32) for i in range(2)]
        hb = S.sb("hbA", [128, D], BF16)
        junk = S.sb("junkA", [128, D], BF16)
        ss = S.sb("ssA", [128, 1], F32)
        rs = S.sb("rsA", [128, 1], F32)
        hT = S.sb("hTA", [128, 8, 512], BF16)
        ev32 = [S.sb("ev32A%d" % i, [128, 512], F32) for i in range(4)]
        evb = [S.sb("evbA%d" % i, [128, 512], BF16) for i in range(4)]
        S.dma(n1g[:], G["n1g_%d" % l])
        S.dma(cos[:], G["c_cos"])
        S.dma(sin[:], G["c_sin"])
        load_cast(P, Wfm, G["w_fm_%d" % l], D, NFM, gain=n1g, stg=stg)
        for c in range(8):
            for base in (1024, 2048):
                v = Wfm[:, c, base:base + 512].rearrange("p (h t j) -> p h t j", h=4, t=2)[:, :, 0, :]
                S.ts(v, v, -1.0, None, ALU.mult, eng="pool")
        hTd = G["hT_d"].rearrange("(k p) t -> p k t", p=128)
        e32 = 0
        eb = 0
        bank = 0
        for st in range(NTOK // 512):
            t0 = st * 512
            p0 = t0 % SEQ
            for ti in range(4):
                xt = xb[ti % 2]
                S.dma(xt[:], x_in[t0 + ti * 128:t0 + (ti + 1) * 128, :])
                norm_transpose(P, xt, hb, junk, ss, rs, hT, ti * 128, P.pbB[ti % 2])
            dma_chunks(S, hTd[:, :, t0:t0 + 512], hT, 8, 4)

            def proj(c):
                nonlocal bank
                ps = P.pbF[bank % 6]
                bank += 1
                for k in range(8):
                    S.mm(ps[:], Wfm[:, k, c * 128:(c + 1) * 128], hT[:, k, :], start=(k == 0), stop=(k == 7))
                return ps
            for c in range(4):
                ps = proj(c)
                o = ev32[e32 % 4]
                e32 += 1
                S.copy(o[:], ps[:], eng="act")
                S.dma(G["qkT"][c * 128:(c + 1) * 128, t0:t0 + 512], o[:])
            for which, dst in ((0, "rqT"), (1, "rkT")):
                for hh in range(4):
                    ca = 4 + which * 8 + hh
                    pa = proj(ca)
                    pbk = proj(ca + 4)
                    t1 = ev32[e32 % 4]
                    t2 = ev32[(e32 + 1) % 4]
                    e32 += 2
                    S.tt(t1[:], pa[:], cos[:, p0:p0 + 512], ALU.mult)
                    S.tt(t2[:], pbk[:], sin[:, p0:p0 + 512], ALU.mult)
                    ob = evb[eb % 4]
                    eb += 1
                    S.tt(ob[:], t1[:], t2[:], ALU.add, eng="pool")
                    S.dma(G[dst][hh * 128:(hh + 1) * 128, t0:t0 + 512], ob[:])
            for c in range(24):
                ps = proj(20 + c)
                ob = evb[eb % 4]
                eb += 1
                S.act(ob[:], ps[:], AF.Sigmoid)
                S.dma(G["gT"][c * 128:(c + 1) * 128, t0:t0 + 512], ob[:])


def stage_A2(P, l):
    S, G = P.S, P.G
    with _scope(S):
        Wtm = S.sb("Wtm", [128, 8, NTM], BF16)
        stg = [S.sb("stgB%d" % i, [128, 2048], F32) for i in range(3)]
        n1g = S.sb("n1gB", [128, 8], F32)
        hTs = [S.sb("hTB%d" % i, [128, 8, 512], BF16) for i in range(2)]
        evb = [S.sb("evbB%d" % i, [128, 512], BF16) for i in range(6)]
        ev32 = [S.sb("ev32B%d" % i, [128, 512], F32) for i in range(2)]
        S.dma(n1g[:], G["n1g_%d" % l])
        load_cast(P, Wtm, G["w_tm_%d" % l], D, NTM, gain=n1g, stg=stg)
        hTd = G["hT_d"].rearrange("(k p) t -> p k t", p=128)
        groups = [(0, 512, "zv", "copy"), (512, 512, "zo", "sig"), (1024, 512, "rv", "copy"),
                  (1536, 512, "rg", "silu"), (2048, ZS_N, "zs", "f32")]
        bank = 0
        eb = 0
        e32 = 0
        for st in range(NTOK // 512):
            t0 = st * 512
            hT = hTs[st % 2]
            dma_chunks(S, hT, hTd[:, :, t0:t0 + 512], 8, 4)
            for ti in range(4):
                r0 = t0 + ti * 128
                for (c0, n, dst, kind) in groups:
                    ps = P.pbF[bank % 6]
                    bank += 1
                    for k in range(8):
                        S.mm(ps[:, 0:n], hT[:, k, ti * 128:(ti + 1) * 128], Wtm[:, k, c0:c0 + n],
                             start=(k == 0), stop=(k == 7))
                    if kind == "f32":
                        o = ev32[e32 % 2]
                        e32 += 1
                        S.copy(o[:, 0:n], ps[:, 0:n], eng="dve")
                    else:
                        o = evb[eb % 6]
                        eb += 1
                        if kind == "copy":
                            S.copy(o[:, 0:n], ps[:, 0:n], eng="dve")
                        elif kind == "sig":
                            S.act(o[:, 0:n], ps[:, 0:n], AF.Sigmoid)
                        else:
                            S.act(o[:, 0:n], ps[:, 0:n], AF.Silu)
                    S.dma(G[dst][r0:r0 + 128, :], o[:, 0:n])


def ln_gate_out(P, pN, s2, hp, n, o32, gain_t, gate_all, hm, tmp):
    S = P.S
    st6, mv, t2, re = tmp
    for hh in range(2):
        S.bnstats(st6[:, hh, :], pN[:, hh, 0:128])
        S.bnaggr(mv[:, hh, :], st6[:, hh, :])
    S.tt(t2[:], s2, s2, ALU.mult)
    S.tt(t2[:], t2[:], mv[:, :, 1], ALU.mult)
    S.ts(t2[:], t2[:], EPS, None, ALU.add)
    S.act(t2[:], t2[:], AF.Sqrt)
    S.recip(t2[:], t2[:])
    S.tt(re[:], t2[:], s2, ALU.mult)
    for hh in range(2):
        S.ts(o32[:, hh * 128:(hh + 1) * 128], pN[:, hh, 0:128], mv[:, hh, 0:1], re[:, hh:hh + 1],
             ALU.subtract, ALU.mult)
    S.tt(o32[:], o32[:], gain_t[:, hp * 256:(hp + 1) * 256], ALU.mult, eng="pool")
    S.tt(hm[:, hp * 256:(hp + 1) * 256], o32[:], gate_all[:, n, hp * 256:(hp + 1) * 256], ALU.mult, eng="pool")


def out_transpose(P, hm, hmTs, dstT, tg, ptb):
    S = P.S
    for c in range(4):
        S.tr(ptb[:, c * 128:(c + 1) * 128], hm[:, c * 128:(c + 1) * 128], P.identb[:])
    S.copy(hmTs[:], ptb[:, 0:512].rearrange("p (c t) -> p c t", c=4), eng="act")
    S.dma(dstT.rearrange("(c p) t -> p c t", p=128)[:, :, tg:tg + 128], hmTs[:])


def stage_B(P, l):
    S, G = P.S, P.G
    NCH = SEQ // 128
    with _scope(S):
        qkraw = S.sb("qkraw", [128, 4, SEQ + 3], F32)
        cacc = [S.sb("cacc%d" % i, [128, SEQ], F32) for i in range(2)]
        qkb = S.sb("qkb", [128, 4, SEQ], BF16)
        qA = S.sb("qA", [128, 2, SEQ], BF16)
        qB = S.sb("qB", [128, 2, SEQ], BF16)
        mconv = S.sb("mconv", [128, 4, 4], F32)
        mbias = S.sb("mbias", [128, 128], F32)
        mng = S.sb("mng", [128, 512], F32)
        gi = S.sb("gi", [128, NCH, 8], F32)
        ax = S.sb("ax", [128, NCH, 4], F32)
        lf = S.sb("lf", [128, NCH, 4], F32)
        lia = S.sb("lia", [128, NCH, 4], F32)
        e_all = S.sb("e_all", [128, NCH, 4], F32)
        rowsc = S.sb("rowsc", [128, NCH, 4], F32)
        g_all = S.sb("g_all", [128, NCH, 4], F32)
        v_all = S.sb("v_all", [128, NCH, 512], BF16)
        mo_all = S.sb("mo_all", [128, NCH, 512], BF16)
        vt = S.sb("vt", [128, NCH, 4, 130], BF16)
        ktok = [S.sb("ktok%d" % i, [128, 256], BF16) for i in range(2)]
        scTm = [S.sb("scTm%d" % i, [128, 256], BF16) for i in range(2)]
        P32 = S.sb("P32", [128, 2, 129], F32)
        Cb = S.sb("Cb", [128, 2, 130], BF16)
        hm = [S.sb("hm%d" % i, [128, 512], BF16) for i in range(2)]
        hmTs = [S.sb("hmTs%d" % i, [128, 4, 128], BF16) for i in range(2)]
        o32 = [S.sb("o32%d" % i, [128, 256], F32) for i in range(4)]
        d1_r = [S.sb("d1_%d" % i, [128, 2], F32) for i in range(4)]
        s2_r = [S.sb("s2_%d" % i, [128, 2], F32) for i in range(4)]
        tmp_r = [(S.sb("st6_%d" % i, [128, 2, 6], F32), S.sb("mv_%d" % i, [128, 2, 2], F32),
                  S.sb("t2_%d" % i, [128, 2], F32), S.sb("re_%d" % i, [128, 2], F32)) for i in range(4)]
        S.dma(mconv[:], G["mconv_%d" % l])
        S.dma(mbias[:], G["mbias_%d" % l])
        S.dma(mng[:], G["mng_%d" % l])
        S.memset(qkraw[:, :, 0:3], 0.0)
        S.memset(qA[64:128, :, :], 0.0, eng="pool")
        S.memset(qB[0:64, :, :], 0.0, eng="pool")
        S.memset(vt[:], 0.0, eng="pool")
        bank = 0
        for s in range(NSEQ):
            tb = s * SEQ
            for c in range(4):
                S.dma(qkraw[:, c, 3:SEQ + 3], G["qkT"][c * 128:(c + 1) * 128, tb:tb + SEQ])
            for c in range(4):
                acc = cacc[c % 2]
                S.ts(acc[:], qkraw[:, c, 0:SEQ], mconv[:, c, 0:1], None, ALU.mult)
                for j in range(1, 4):
                    S.stt(acc[:], qkraw[:, c, j:j + SEQ], mconv[:, c, j:j + 1], acc[:], ALU.mult, ALU.add)
                if c < 2:
                    S.act(qA[0:64, c, :], acc[0:64, :], AF.Silu)
                    S.act(qB[64:128, c, :], acc[64:128, :], AF.Silu)
                else:
                    S.act(qkb[:, c, :], acc[:], AF.Silu)
            dma_chunks(S, gi, G["zs"][tb:tb + SEQ, ZS_MI:ZS_MI + 8].rearrange("(n p) c -> p n c", p=128), NCH, 4)
            gif = gi[:].rearrange("p n c -> p (n c)")
            S.tt(gif, gif, mbias[:], ALU.add)
            S.act(ax[:], gi[:, :, 4:8], AF.Abs)
            S.act(ax[:], ax[:], AF.Exp, scale=-1.0)
            S.act(ax[:], ax[:], AF.Ln, bias=1.0)
            S.ts(lf[:], gi[:, :, 4:8], 0.0, None, ALU.min)
            S.tt(lf[:], lf[:], ax[:], ALU.subtract)
            pg = P.pbF[bank % 6]
            bank += 1
            lff = lf[:].rearrange("p n c -> p (n c)")
            S.mm(pg[:, 0:64], P.tri32[:], lff)
            S.mm(pg[:, 64:128], P.ones32[:], lff)
            S.tt(lia[:], gi[:, :, 0:4], pg[:, 0:64].rearrange("p (n c) -> p n c", c=4), ALU.subtract)
            S.act(e_all[:], lia[:], AF.Exp)
            S.act(rowsc[:].rearrange("p n c -> p (n c)"), pg[:, 0:64], AF.Exp, bias=math.log(0.125))
            S.act(g_all[:].rearrange("p n c -> p (n c)"), pg[:, 64:128], AF.Exp)
            dma_chunks(S, v_all, G["zv"][tb:tb + SEQ, :].rearrange("(n p) c -> p n c", p=128), NCH, 4)
            dma_chunks(S, mo_all, G["zo"][tb:tb + SEQ, :].rearrange("(n p) c -> p n c", p=128), NCH, 4)
            for n in range(NCH):
                for h in range(4):
                    S.ts(vt[:, n, h, 0:128], v_all[:, n, h * 128:(h + 1) * 128], e_all[:, n, h:h + 1], None,
                         ALU.mult, eng="pool")
            S.copy(vt[:, :, :, 128], e_all[:], eng="pool")
            for n in range(NCH):
                cs = slice(n * 128, (n + 1) * 128)
                kt = ktok[n % 2]
                ptb = P.pbB[n % 2]
                for hc in range(2):
                    S.tr(ptb[:, hc * 128:(hc + 1) * 128], qkb[:, 2 + hc, cs], P.identb[:])
                S.copy(kt[:], ptb[:, 0:256], eng="act")
                hmc = hm[n % 2]
                for hp in range(2):
                    psc = P.pbF[bank % 6]
                    bank += 1
                    for hh in range(2):
                        qX = qA if hh == 0 else qB
                        S.mm(psc[:, hh * 128:(hh + 1) * 128], qkb[:, 2 + hp, cs], qX[:, hp, cs])
                    sm = scTm[hp]
                    S.tt(sm[:], psc[:, 0:256], P.trib2[:], ALU.mult)
                    pNb = P.pbF[bank % 6]
                    bank += 1
                    pN = pNb[:, 0:258].rearrange("p (a b) -> p a b", a=2)
                    for hh in range(2):
                        h = 2 * hp + hh
                        pr = slice(64 * hh, 64 * hh + 64)
                        S.mm(pN[:, hh, :], sm[:, hh * 128:(hh + 1) * 128], vt[:, n, h, 0:129], start=True, stop=(n == 0))
                        if n > 0:
                            qX = qA if hh == 0 else qB
                            S.mm(pN[:, hh, :], qX[:, hp, cs], Cb[:, hp, 0:129], start=False, stop=True)
                    if n < NCH - 1:
                        pUb = P.pbF[bank % 6]
                        bank += 1
                        pU = pUb[:, 0:260].rearrange("p (a b) -> p a b", a=2)
                        S.mm(pUb[:, 0:260], kt[:, hp * 128:(hp + 1) * 128],
                             vt[:, n, 2 * hp:2 * hp + 2, :].rearrange("p a b -> p (a b)"))
                        for hh in range(2):
                            h = 2 * hp + hh
                            pr = slice(64 * hh, 64 * hh + 64)
                            if n == 0:
                                S.copy(P32[pr, hp, :], pU[pr, hh, 0:129])
                            else:
                                S.stt(P32[pr, hp, :], P32[pr, hp, :], g_all[pr, n - 1, h:h + 1], pU[pr, hh, 0:129],
                                      ALU.mult, ALU.add)
                            S.ts(Cb[pr, hp, 0:129], P32[pr, hp, :], g_all[pr, n, h:h + 1], None, ALU.mult)
                    rsl = rowsc[:, n, 2 * hp:2 * hp + 2]
                    d1, s2, tmp = d1_r[(2 * n + hp) % 4], s2_r[(2 * n + hp) % 4], tmp_r[(2 * n + hp) % 4]
                    S.tt(d1[:], pN[:, :, 128], rsl, ALU.mult)
                    S.act(d1[:], d1[:], AF.Abs)
                    S.ts(d1[:], d1[:], 1.0, None, ALU.max)
                    S.recip(d1[:], d1[:])
                    S.tt(s2[:], d1[:], rsl, ALU.mult)
                    ln_gate_out(P, pN, s2[:], hp, n, o32[(2 * n + hp) % 4], mng, mo_all, hmc, tmp)
                out_transpose(P, hmc, hmTs[n % 2], G["hmT"], tb + n * 128, P.pbB[(n + 1) % 2])


def stage_D(P, l):
    S, G = P.S, P.G
    NCH = SEQ // 128
    gam = P.ret_g
    with _scope(S):
        rq = S.sb("rq", [128, 4, SEQ], BF16)
        rk = S.sb("rk", [128, 4, SEQ], BF16)
        rng = S.sb("rng", [128, 512], F32)
        rete = S.sb("rete", [128, 4], F32)
        retrow = S.sb("retrow", [128, 4], F32)
        v_all = S.sb("rv_all", [128, NCH, 512], BF16)
        rg_all = S.sb("rg_all", [128, NCH, 512], BF16)
        vt = S.sb("rvt", [128, NCH, 512], BF16)
        ktok = [S.sb("rktok%d" % i, [128, 512], BF16) for i in range(2)]
        scTm = [S.sb("rscTm%d" % i, [128, 256], BF16) for i in range(2)]
        P32 = S.sb("rP32", [128, 4, 128], F32)
        Sb = S.sb("rSb", [128, 4, 128], BF16)
        hm = [S.sb("rhm%d" % i, [128, 512], BF16) for i in range(2)]
        hmTs = [S.sb("rhmTs%d" % i, [128, 4, 128], BF16) for i in range(2)]
        o32 = [S.sb("ro32%d" % i, [128, 256], F32) for i in range(4)]
        tmp_r = [(S.sb("rst6_%d" % i, [128, 2, 6], F32), S.sb("rmv_%d" % i, [128, 2, 2], F32),
                  S.sb("rt2_%d" % i, [128, 2], F32), S.sb("rre_%d" % i, [128, 2], F32)) for i in range(4)]
        S.dma(rng[:], G["rng_%d" % l])
        S.dma(rete[:], G["c_rete"])
        S.dma(retrow[:], G["c_retrow"])
        bank = 0
        for s in range(NSEQ):
            tb = s * SEQ
            S.dma(rq[:], G["rqT"].rearrange("(c p) t -> p c t", p=128)[:, :, tb:tb + SEQ])
            S.dma(rk[:], G["rkT"].rearrange("(c p) t -> p c t", p=128)[:, :, tb:tb + SEQ])
            dma_chunks(S, v_all, G["rv"][tb:tb + SEQ, :].rearrange("(n p) c -> p n c", p=128), NCH, 4)
            dma_chunks(S, rg_all, G["rg"][tb:tb + SEQ, :].rearrange("(n p) c -> p n c", p=128), NCH, 4)
            for h in range(4):
                S.ts(vt[:, :, h * 128:(h + 1) * 128], v_all[:, :, h * 128:(h + 1) * 128], rete[:, h:h + 1], None,
                     ALU.mult, eng="pool")
            for n in range(NCH):
                cs = slice(n * 128, (n + 1) * 128)
                kt = ktok[n % 2]
                ptb = P.pbB[n % 2]
                for h in range(4):
                    S.tr(ptb[:, h * 128:(h + 1) * 128], rk[:, h, cs], P.identb[:])
                S.copy(kt[:], ptb[:, 0:512], eng="act")
                hmc = hm[n % 2]
                for hp in range(2):
                    psc = P.pbF[bank % 6]
                    bank += 1
                    for hh in range(2):
                        h = 2 * hp + hh
                        S.mm(psc[:, hh * 128:(hh + 1) * 128], rk[:, h, cs], rq[:, h, cs])
                    sm = scTm[hp]
                    S.tt(sm[:], psc[:, 0:256], P.trib2[:], ALU.mult)
                    pNb = P.pbF[bank % 6]
                    bank += 1
                    pN = pNb[:, 0:256].rearrange("p (a b) -> p a b", a=2)
                    for hh in range(2):
                        h = 2 * hp + hh
                        S.mm(pN[:, hh, :], sm[:, hh * 128:(hh + 1) * 128], vt[:, n, h * 128:(h + 1) * 128],
                             start=True, stop=(n == 0))
                        if n > 0:
                            S.mm(pN[:, hh, :], rq[:, h, cs], Sb[:, h, :], start=False, stop=True)
                    if n < NCH - 1:
                        pUb = P.pbF[bank % 6]
                        bank += 1
                        for hh in range(2):
                            h = 2 * hp + hh
                            S.mm(pUb[:, hh * 128:(hh + 1) * 128], kt[:, h * 128:(h + 1) * 128],
                                 vt[:, n, h * 128:(h + 1) * 128])
                        for hh in range(2):
                            h = 2 * hp + hh
                            pu = pUb[:, hh * 128:(hh + 1) * 128]
                            if n == 0:
                                S.copy(P32[:, h, :], pu)
                            else:
                                S.stt(P32[:, h, :], P32[:, h, :], gam[h], pu, ALU.mult, ALU.add)
                            S.ts(Sb[:, h, :], P32[:, h, :], gam[h], None, ALU.mult)
                    ln_gate_out(P, pN, retrow[:, 2 * hp:2 * hp + 2], hp, n, o32[(2 * n + hp) % 4], rng, rg_all, hmc,
                                tmp_r[(2 * n + hp) % 4])
                out_transpose(P, hmc, hmTs[n % 2], G["hrT"], tb + n * 128, P.pbB[(n + 1) % 2])


def stage_C(P, l):
    S, G = P.S, P.G
    NT = SEQ // 128
    WSC = (8 ** -0.5) * (64 ** -0.5)
    with _scope(S):
        stg = [S.sb("stgC%d" % i, [128, 2048], F32) for i in range(2)]
        aqg = S.sb("aqg", [128, 2], F32)
        akg = S.sb("akg", [128, 1], F32)
        akvg = S.sb("akvg", [128, 128], F32)
        wuq = S.sb("wuq", [128, 2, 512], BF16)
        wuqi = S.sb("wuqi", [128, 2, 512], BF16)
        wuk = S.sb("wuk", [128, 1, 512], BF16)
        wuvz = S.sb("wuvz", [128, 8, 128], BF16)
        zt = [S.sb("ztC%d" % i, [128, ZS_N], F32) for i in range(4)]
        NR = 4
        junk_r = [S.sb("junkC%d" % i, [128, 256], BF16) for i in range(NR)]
        junk2_r = [S.sb("junk2C%d" % i, [128, 128], BF16) for i in range(NR)]
        ss_r = [S.sb("ssC%d" % i, [128, 2], F32) for i in range(NR)]
        rs_r = [S.sb("rsC%d" % i, [128, 2], F32) for i in range(NR)]
        st6_r = [S.sb("st6C%d" % i, [128, 6], F32) for i in range(NR)]
        mv_r = [S.sb("mvC%d" % i, [128, 2], F32) for i in range(NR)]
        cqn_r = [S.sb("cqn%d" % i, [128, 256], BF16) for i in range(NR)]
        kin_r = [S.sb("kin%d" % i, [128, 128], BF16) for i in range(NR)]
        ckn_r = [S.sb("ckn%d" % i, [128, 128], F32) for i in range(NR)]
        cqT = S.sb("cqT", [128, 2, SEQ], BF16)
        kidxT = S.sb("kidxT", [128, SEQ], BF16)
        ckv1 = S.sb("ckv1", [128, NT, 130], BF16)
        ckvT = S.sb("ckvT", [128, SEQ], BF16)
        qTa = S.sb("qTCa", [128, 4, 512], BF16)
        qTb = S.sb("qTCb", [128, 4, 512], BF16)
        qidxA = S.sb("qidxA", [128, 4, SEQ], BF16)
        qidxB = S.sb("qidxB", [128, 4, SEQ], BF16)
        qlatT = S.sb("qlatT", [128, 8, SEQ], BF16)
        wabs = S.sb("wabs", [128, NT, 8], F32)
        wsgn = S.sb("wsgn", [128, NT, 8], F32)
        acc2 = [S.sb("accC%d" % i, [128, SEQ], F32) for i in range(2)]
        NBIS = 20
        junkb = [S.sb("junkbC%d" % i, [128, SEQ], BF16) for i in range(2)]
        bis = [S.sb("bisC%d" % i, [128, 8], F32) for i in range(2)]
        dk = [S.sb("dkC%d" % i, [128, NBIS], F32) for i in range(2)]
        pw2 = S.sb("pw2C", [128, NBIS], F32)
        for k in range(NBIS):
            S.memset(pw2[:, k:k + 1], 2.0 ** -k, eng="pool")
        rbuf = [S.sb("rbufC%d" % i, [128, 512], F32) for i in range(3)]
        m8 = [S.sb("m8C%d" % i, [128, 8], F32) for i in range(2)]
        madd4 = [S.sb("maddC%d" % i, [128, SEQ], BF16) for i in range(4)]
        Eb = [S.sb("EbC%d" % i, [128, 512], BF16) for i in range(3)]
        otok = S.sb("otok", [128, 8, 128], BF16)
        olT = S.sb("olT", [128, 8, 128], BF16)
        haTs = [S.sb("haTs%d" % i, [128, 4, 128], BF16) for i in range(2)]
        rec = S.sb("recC", [128, 8], F32)
        S.dma(aqg[:], G["aqg_%d" % l])
        S.dma(akg[:], G["akg_%d" % l])
        S.dma(akvg[:], G["akvg_%d" % l])
        load_cast(P, wuq, G["wuq_%d" % l], 256, 512, gain=aqg, stg=stg)
        load_cast(P, wuqi, G["wuqi_%d" % l], 256, 512, gain=aqg, stg=stg)
        load_cast(P, wuk, G["wuk_%d" % l].rearrange("p a b -> p (a b)"), 128, 512, stg=stg)
        S.memset(wuvz[:], 0.0, eng="pool")
        sv = stg[P.stg_i % 2]
        P.stg_i += 1
        S.dma(sv[:, 0:512], G["wuv_%d" % l].rearrange("p a b -> p (a b)"))
        for h in range(8):
            S.copy(wuvz[:, h, (h % 2) * 64:(h % 2) * 64 + 64], sv[:, h * 64:(h + 1) * 64], eng="pool")
        S.memset(ckv1[:, :, 128:130], 1.0, eng="pool")
        S.memset(qTa[64:128, :, :], 0.0, eng="pool")
        S.memset(qTb[0:64, :, :], 0.0, eng="pool")
        S.memset(qidxA[64:128, :, :], 0.0, eng="pool")
        S.memset(qidxB[0:64, :, :], 0.0, eng="pool")
        bank = 0
        for s in range(NSEQ):
            tb = s * SEQ
            for i in range(NT):
                z = zt[i % 4]
                ri = i % NR
                junk, junk2, ss, rs, st6, mv = junk_r[ri], junk2_r[ri], ss_r[ri], rs_r[ri], st6_r[ri], mv_r[ri]
                cqn, kin, ckn = cqn_r[ri], kin_r[ri], ckn_r[ri]
                S.dma(z[:], G["zs"][tb + i * 128:tb + (i + 1) * 128, :])
                S.act(junk[:, 0:256], z[:, ZS_CQ:ZS_CQ + 256], AF.Square, accum_out=ss[:, 0:1])
                S.act(junk2[:, 0:128], z[:, ZS_CKV:ZS_CKV + 128], AF.Square, accum_out=ss[:, 1:2])
                S.ts(rs[:, 0:1], ss[:, 0:1], 1.0 / 256, EPS, ALU.mult, ALU.add)
                S.ts(rs[:, 1:2], ss[:, 1:2], 1.0 / 128, EPS, ALU.mult, ALU.add)
                S.act(rs[:], rs[:], AF.Sqrt)
                S.recip(rs[:], rs[:])
                S.ts(cqn[:], z[:, ZS_CQ:ZS_CQ + 256], rs[:, 0:1], None, ALU.mult)
                S.stt(ckn[:], z[:, ZS_CKV:ZS_CKV + 128], rs[:, 1:2], akvg[:], ALU.mult, ALU.mult)
                S.copy(ckv1[:, i, 0:128], ckn[:], eng="pool")
                S.bnstats(st6[:], z[:, ZS_KIDX:ZS_KIDX + 64])
                S.bnaggr(mv[:], st6[:])
                S.ts(mv[:, 1:2], mv[:, 1:2], EPS, None, ALU.add)
                S.act(mv[:, 1:2], mv[:, 1:2], AF.Sqrt)
                S.recip(mv[:, 1:2], mv[:, 1:2])
                S.ts(kin[:, 0:64], z[:, ZS_KIDX:ZS_KIDX + 64], mv[:, 0:1], mv[:, 1:2], ALU.subtract, ALU.mult)
                S.copy(kin[:, 64:128], kin[:, 0:64], eng="pool")
                S.act(wabs[:, i, :], z[:, ZS_WIDX:ZS_WIDX + 8], AF.Abs, scale=WSC)
                S.ts(wsgn[:, i, :], z[:, ZS_WIDX:ZS_WIDX + 8], 0.0, None, ALU.is_gt)
                S.ts(wsgn[:, i, :], wsgn[:, i, :], 2.0, -1.0, ALU.mult, ALU.add)
                ptb = P.pbB[i % 2]
                S.tr(ptb[:, 0:128], cqn[:, 0:128], P.identb[:])
                S.tr(ptb[:, 128:256], cqn[:, 128:256], P.identb[:])
                S.tr(ptb[:, 256:384], kin[:], P.identb[:])
                S.tr(ptb[:, 384:512], ckv1[:, i, 0:128], P.identb[:])
                cs = slice(i * 128, (i + 1) * 128)
                S.copy(cqT[:, :, cs], ptb[:, 0:256].rearrange("p (c t) -> p c t", c=2), eng="act")
                S.copy(kidxT[:, cs], ptb[:, 256:384], eng="act")
                S.ts(kidxT[:, cs], kidxT[:, cs], akg[:, 0:1], None, ALU.mult, eng="pool")
                S.copy(ckvT[:, cs], ptb[:, 384:512], eng="act")
            for b4 in range(SEQ // 512):
                ts_ = slice(b4 * 512, (b4 + 1) * 512)
                for c in range(4):
                    ps = P.pbF[bank % 6]
                    bank += 1
                    for k in range(2):
                        S.mm(ps[:], wuq[:, k, c * 128:(c + 1) * 128], cqT[:, k, ts_], start=(k == 0), stop=(k == 1))
                    S.copy(qTa[0:64, c, :], ps[0:64, :], eng="act")
                    S.copy(qTb[64:128, c, :], ps[64:128, :], eng="act")
                for c in range(4):
                    ps = P.pbF[bank % 6]
                    bank += 1
                    for k in range(2):
                        S.mm(ps[:], wuqi[:, k, c * 128:(c + 1) * 128], cqT[:, k, ts_], start=(k == 0), stop=(k == 1))
                    S.copy(qidxA[0:64, c, ts_], ps[0:64, :], eng="dve")
                    S.copy(qidxB[64:128, c, ts_], ps[64:128, :], eng="dve")
                for h in range(8):
                    qTx = qTa if h % 2 == 0 else qTb
                    ps = P.pbF[bank % 6]
                    bank += 1
                    S.mm(ps[:], wuk[:, 0, (h // 2) * 128:(h // 2 + 1) * 128], qTx[:, h // 2, :])
                    S.act(qlatT[:, h, ts_], ps[:], AF.Copy, scale=0.125)
            def idx_topk_pair(ia):
                nonlocal bank
                tiles = (ia, ia + 1)
                for i in tiles:
                    W = (i + 1) * 128
                    cs = slice(i * 128, (i + 1) * 128)
                    nb = (W + 511) // 512
                    acc = acc2[i % 2]
                    for b in range(nb):
                        w = min(512, W - b * 512)
                        ks = slice(b * 512, b * 512 + w)
                        for g in range(8):
                            qix = qidxA if g % 2 == 0 else qidxB
                            ps = P.pbF[bank % 4]
                            bank += 1
                            S.mm(ps[:, 0:w], qix[:, g // 2, cs], kidxT[:, ks])
                            r = rbuf[g % 3]
                            S.act(r[:, 0:w], ps[:, 0:w], AF.Relu, scale=wabs[:, i, g:g + 1])
                            if g == 0:
                                S.ts(acc[:, ks], r[:, 0:w], wsgn[:, i, 0:1], None, ALU.mult)
                            else:
                                S.stt(acc[:, ks], r[:, 0:w], wsgn[:, i, g:g + 1], acc[:, ks], ALU.mult, ALU.add)
                    S.tt(acc[:, cs], acc[:, cs], P.negd[:], ALU.add)
                if ia >= 2:
                    for i in tiles:
                        W = (i + 1) * 128
                        acc, bs = acc2[i % 2], bis[i % 2]
                        S.max8(m8[i % 2][:], acc[:, 0:W])
                        S.treduce(bs[:, 0:1], acc[:, 0:i * 128], ALU.min)
                        S.ts(bs[:, 1:2], m8[i % 2][:, 0:1], bs[:, 0:1], 0.5, ALU.subtract, ALU.mult)
                        S.ts(bs[:, 2:3], m8[i % 2][:, 0:1], bs[:, 0:1], 0.5, ALU.add, ALU.mult)
                        S.ts(dk[i % 2][:], pw2[:], bs[:, 1:2], None, ALU.mult)
                    for k in range(NBIS):
                        for i in tiles:
                            W = (i + 1) * 128
                            S.ts_acc(junkb[i % 2][:, 0:W], acc2[i % 2][:, 0:W], bis[i % 2][:, 2:3], 0.0,
                                     ALU.is_ge, ALU.add, bis[i % 2][:, 3:4])
                        for i in tiles:
                            bs = bis[i % 2]
                            S.ts(bs[:, 4:5], bs[:, 3:4], 255.5, -0.5, ALU.is_ge, ALU.add)
                        for i in tiles:
                            bs = bis[i % 2]
                            S.stt(bs[:, 2:3], bs[:, 4:5], dk[i % 2][:, k:k + 1], bs[:, 2:3], ALU.mult, ALU.add)
                    for i in tiles:
                        W = (i + 1) * 128
                        bs = bis[i % 2]
                        S.tt(bs[:, 5:6], bs[:, 2:3], dk[i % 2][:, NBIS - 1:NBIS], ALU.subtract)
                        S.ts(madd4[i % 4][:, 0:W], acc2[i % 2][:, 0:W], bs[:, 5:6], 1.0, ALU.is_ge, ALU.subtract)
                else:
                    for i in tiles:
                        W = (i + 1) * 128
                        S.ts(madd4[i % 4][:, 0:W], acc2[i % 2][:, 0:W], -1.0e29, 1.0, ALU.is_ge, ALU.subtract)

            def attn(i):
                nonlocal bank
                cs = slice(i * 128, (i + 1) * 128)
                madd = madd4[i % 4]
                for h in range(8):
                    hh = h % 2
                    pvbank = P.pbF[4 + (h // 2) % 2]
                    pv = pvbank[:, hh * 129:(hh + 1) * 129]
                    for bg in range((i + 4) // 4):
                        j0 = bg * 4
                        nj = min(4, i + 1 - j0)
                        pl = P.pbF[bank % 4]
                        bank += 1
                        for jj in range(nj):
                            j = j0 + jj
                            near = j >= i - 1
                            blk = pl[:, jj * 128:(jj + 1) * 128]
                            S.mm(blk, ckvT[:, j * 128:(j + 1) * 128], qlatT[:, h, cs], start=True, stop=False)
                            S.mm(blk, madd[:, j * 128:(j + 1) * 128], P.i30k[:], start=False, stop=(not near))
                            if near:
                                S.mm(blk, P.biasb[:, h, i - j, :], P.identb[:], start=False, stop=True)
                        E = Eb[(h * 4 + bg) % 3]
                        S.act(E[:, 0:nj * 128], pl[:, 0:nj * 128], AF.Exp)
                        for jj in range(nj):
                            j = j0 + jj
                            S.mm(pv, E[:, jj * 128:(jj + 1) * 128], ckv1[:, j, 0:129], start=(j == 0), stop=(j == i))
                    S.recip(rec[:, h:h + 1], pv[:, 128:129])
                    S.act(otok[:, h, :], pv[:, 0:128], AF.Copy, scale=rec[:, h:h + 1])
                ptb = P.pbB[i % 2]
                for h in range(8):
                    S.tr(ptb[:, h * 128:(h + 1) * 128], otok[:, h, :], P.identb[:])
                S.copy(olT[:], ptb[:, :].rearrange("p (h t) -> p h t", h=8), eng="act")
                ph = P.pbF[bank % 4]
                bank += 1
                for hc in range(4):
                    S.mm(ph[:, hc * 128:(hc + 1) * 128], wuvz[:, 2 * hc, :], olT[:, 2 * hc, :], start=True, stop=False)
                    S.mm(ph[:, hc * 128:(hc + 1) * 128], wuvz[:, 2 * hc + 1, :], olT[:, 2 * hc + 1, :], start=False, stop=True)
                hs = haTs[i % 2]
                S.copy(hs[:], ph[:].rearrange("p (c t) -> p c t", c=4), eng="act")
                S.dma(G["haT"].rearrange("(c p) t -> p c t", p=128)[:, :, tb + i * 128:tb + (i + 1) * 128], hs[:])

            idx_topk_pair(0)
            for pp in range(NT // 2):
                if pp + 1 < NT // 2:
                    idx_topk_pair(2 * pp + 2)
                attn(2 * pp)
                attn(2 * pp + 1)


def stage_E(P, l, x_in):
    S, G = P.S, P.G
    with _scope(S):
        stg = [S.sb("stgE%d" % i, [128, 1024], F32) for i in range(3)]
        pw = [S.sb("pwE%d" % i, [128, 4, D], BF16) for i in range(3)]
        wo = S.sb("woE", [128, 8, D], BF16)
        hin = [[S.sb("hinE%d_%d" % (b, i), [128, 4, 512], BF16) for i in range(2)] for b in range(3)]
        gin = [S.sb("ginE%d" % i, [128, 24, 512], BF16) for i in range(2)]
        y32 = S.sb("y32E", [128, 512], F32)
        t32 = [S.sb("t32E%d" % i, [128, 512], F32) for i in range(2)]
        yT = S.sb("yTE", [128, 8, 512], BF16)
        xb = [S.sb("xbE%d" % i, [128, D], F32) for i in range(2)]
        for b, nm in enumerate(("p_m", "p_a", "p_r")):
            load_cast(P, pw[b], G["%s_%d" % (nm, l)], 512, D, stg=stg)
        load_cast(P, wo, G["w_out_%d" % l], D, D, stg=stg)
        srcs = [G["hmT"], G["haT"], G["hrT"]]
        bank = 0
        for st in range(NTOK // 512):
            t0 = st * 512
            for b in range(3):
                S.dma(hin[b][st % 2][:], srcs[b].rearrange("(c p) t -> p c t", p=128)[:, :, t0:t0 + 512])
            gt = gin[st % 2]
            dma_chunks(S, gt, G["gT"].rearrange("(c p) t -> p c t", p=128)[:, :, t0:t0 + 512], 24, 4)
            for c in range(8):
                pss = []
                for b in range(3):
                    ps = P.pbF[bank % 6]
                    bank += 1
                    for k in range(4):
                        S.mm(ps[:], pw[b][:, k, c * 128:(c + 1) * 128], hin[b][st % 2][:, k, :],
                             start=(k == 0), stop=(k == 3))
                    pss.append(ps)
                S.tt(y32[:], pss[0][:], gt[:, c, :], ALU.mult)
                S.tt(t32[0][:], pss[1][:], gt[:, 8 + c, :], ALU.mult)
                S.tt(t32[1][:], pss[2][:], gt[:, 16 + c, :], ALU.mult)
                S.tt(y32[:], y32[:], t32[0][:], ALU.add, eng="pool")
                S.tt(yT[:, c, :], y32[:], t32[1][:], ALU.add, eng="pool")
            for ti in range(4):
                r0 = t0 + ti * 128
                xt = xb[ti % 2]
                S.dma(xt[:], x_in[r0:r0 + 128, :])
                for hf in range(2):
                    ps = P.pbF[bank % 6]
                    bank += 1
                    for k in range(8):
                        S.mm(ps[:], yT[:, k, ti * 128:(ti + 1) * 128], wo[:, k, hf * 512:(hf + 1) * 512],
                             start=(k == 0), stop=(k == 7))
                    S.tt(xt[:, hf * 512:(hf + 1) * 512], xt[:, hf * 512:(hf + 1) * 512], ps[:], ALU.add)
                S.dma(G["x1"][r0:r0 + 128, :], xt[:])


def stage_F(P, l, x_out, final):
    S, G = P.S, P.G
    ST = 256
    NF = DFF // 128
    with _scope(S):
        stg = [S.sb("stgF%d" % i, [128, 1024], F32) for i in range(3)]
        n2g = S.sb("n2g", [128, 8], F32)
        wg = S.sb("wgF", [128, 8, DFF], BF16)
        wu = S.sb("wuF", [128, 8, DFF], BF16)
        wd = S.sb("wdF", [128, NF, D], BF16)
        xb = [S.sb("xbF%d" % i, [128, D], F32) for i in range(2)]
        hb = S.sb("hbF", [128, D], BF16)
        junk = S.sb("junkF", [128, D], BF16)
        ss = S.sb("ssF", [128, 1], F32)
        rs = S.sb("rsF", [128, 1], F32)
        hT = S.sb("hTF", [128, 8, ST], BF16)
        sg = [S.sb("sgF%d" % i, [128, ST], F32) for i in range(2)]
        aT = S.sb("aTF", [128, NF, ST], BF16)
        fng = None
        if final:
            fng = S.sb("fngF", [128, D], F32)
            S.dma(fng[:], G["fng"])
        S.dma(n2g[:], G["n2g_%d" % l])
        load_cast(P, wg, G["w_gate_%d" % l], D, DFF, gain=n2g, stg=stg)
        load_cast(P, wu, G["w_up_%d" % l], D, DFF, gain=n2g, stg=stg)
        load_cast(P, wd, G["w_down_%d" % l], DFF, D, stg=stg)
        bank = 0
        for st in range(NTOK // ST):
            t0 = st * ST
            for ti in range(ST // 128):
                xt = xb[ti % 2]
                S.dma(xt[:], G["x1"][t0 + ti * 128:t0 + (ti + 1) * 128, :])
                norm_transpose(P, xt, hb, junk, ss, rs, hT, ti * 128, P.pbB[ti % 2])
            for f in range(NF):
                pg = P.pbF[bank % 6]
                pu = P.pbF[(bank + 1) % 6]
                bank += 2
                for k in range(8):
                    S.mm(pg[:, 0:ST], wg[:, k, f * 128:(f + 1) * 128], hT[:, k, :], start=(k == 0), stop=(k == 7))
                for k in range(8):
                    S.mm(pu[:, 0:ST], wu[:, k, f * 128:(f + 1) * 128], hT[:, k, :], start=(k == 0), stop=(k == 7))
                sgt = sg[f % 2]
                S.act(sgt[:], pg[:, 0:ST], AF.Silu)
                S.tt(aT[:, f, :], sgt[:], pu[:, 0:ST], ALU.mult)
            for ti in range(ST // 128):
                r0 = t0 + ti * 128
                xt = xb[ti % 2]
                S.dma(xt[:], G["x1"][r0:r0 + 128, :])
                for hf in range(2):
                    ps = P.pbF[bank % 6]
                    bank += 1
                    for f in range(NF):
                        S.mm(ps[:], aT[:, f, ti * 128:(ti + 1) * 128], wd[:, f, hf * 512:(hf + 1) * 512],
                             start=(f == 0), stop=(f == NF - 1))
                    S.tt(xt[:, hf * 512:(hf + 1) * 512], xt[:, hf * 512:(hf + 1) * 512], ps[:], ALU.add)
                if final:
                    S.act(junk[:], xt[:], AF.Square, accum_out=ss[:])
                    rms_rstd(P, rs[:], ss[:], D)
                    S.stt(xt[:], xt[:], rs[:, 0:1], fng[:], ALU.mult, ALU.mult)
                S.dma(x_out[r0:r0 + 128, :], xt[:])


SCRATCH = {
    "hT_d": ([D, NTOK], BF16), "qkT": ([512, NTOK], F32), "rqT": ([512, NTOK], BF16), "rkT": ([512, NTOK], BF16),
    "gT": ([3072, NTOK], BF16), "zv": ([NTOK, 512], BF16), "zo": ([NTOK, 512], BF16), "rv": ([NTOK, 512], BF16),
    "rg": ([NTOK, 512], BF16), "zs": ([NTOK, ZS_N], F32), "hmT": ([512, NTOK], BF16), "haT": ([512, NTOK], BF16),
    "hrT": ([512, NTOK], BF16), "x1": ([NTOK, D], F32), "xl": ([NTOK, D], F32),
}


def build_program(n_layers=2, dbg=(), stages="A1,A2,B,C,D,E,F"):
    stages = stages.split(",")
    nc = bass.Bass("TRN2", target_bir_lowering=False)
    P = Prog()
    G = {}
    P.G = G
    P.stg_i = 0
    G["x"] = nc.dram_tensor("x", [NTOK, D], F32, kind="ExternalInput").ap()
    for k, shp in CONST_SHAPES.items():
        G[k] = nc.dram_tensor(k, shp, F32, kind="ExternalInput").ap()
    for l in range(n_layers):
        for k, shp in LAYER_SHAPES.items():
            nm = "%s_%d" % (k, l)
            G[nm] = nc.dram_tensor(nm, shp, F32, kind="ExternalInput").ap()
    G["out"] = nc.dram_tensor("out", [NTOK, D], F32, kind="ExternalOutput").ap()
    for k, (shp, dt) in SCRATCH.items():
        G[k] = nc.dram_tensor(k, shp, dt, kind=("ExternalOutput" if k in dbg else "Internal")).ap()
    P.ret_g = host_constants()["ret_g"]
    with ExitStack() as es:
        S = Sched(nc, es)
        P.S = S
        P.pbF = [S.ps("pbF%d" % i, [128, 512], F32) for i in range(6)]
        P.pbB = [S.ps("pbB%d" % i, [128, 1024], BF16) for i in range(2)]
        P.identb = S.sb("identb", [128, 128], BF16)
        P.i30k = S.sb("i30k", [128, 128], BF16)
        P.trib2 = S.sb("trib2", [128, 256], BF16)
        P.tri32 = S.sb("tri32", [128, 128], F32)
        P.ones32 = S.sb("ones32", [128, 128], F32)
        P.negd = S.sb("negd", [128, 128], F32)
        P.biasb = S.sb("biasb", [128, 8, 2, 128], BF16)
        with _scope(S):
            c32 = S.sb("c32", [128, 128], F32)
            b32 = S.sb("b32", [128, 8, 2, 128], F32)
            rb31 = S.sb("rb31", [128, 8], F32)
            S.dma(c32[:], G["c_ident"])
            S.copy(P.identb[:], c32[:])
            S.ts(P.i30k[:], c32[:], 30000.0, None, ALU.mult)
            S.dma(P.tri32[:], G["c_tri"])
            S.copy(P.trib2[:, 0:128], P.tri32[:])
            S.copy(P.trib2[:, 128:256], P.tri32[:])
            S.dma(P.ones32[:], G["c_ones"])
            S.dma(P.negd[:], G["c_negd"])
            S.dma(b32[:], G["biasn"])
            S.dma(rb31[:], G["rb31"])
            for h in range(8):
                S.ts(P.biasb[:, h, :, :], b32[:, h, :, :], rb31[:, h:h + 1], None, ALU.subtract)
        x_in = G["x"]
        for l in range(n_layers):
            last = (l == n_layers - 1)
            x_out = G["out"] if last else G["xl"]
            if "A1" in stages:
                stage_A1(P, l, x_in)
            if "A2" in stages:
                stage_A2(P, l)
            if "B" in stages:
                stage_B(P, l)
            if "D" in stages:
                stage_D(P, l)
            if "C" in stages:
                stage_C(P, l)
            if "E" in stages:
                stage_E(P, l, x_in)
            if "F" in stages:
                stage_F(P, l, x_out, last)
            x_in = x_out
        S.barrier()
        P.n_inst = S.n_inst
    return nc, P


_CACHE = {}


def make_in_maps(inputs, n_layers=2, cores=range(NCORES)):
    hc = host_constants()
    shared = {k: np.ascontiguousarray(v, dtype=np.float32) for k, v in hc.items() if k.startswith("c_")}
    rel_bias = np.asarray(inputs["rel_bias"], np.float32)
    bi = bias_index()
    biasn = rel_bias[bi]
    shared["biasn"] = np.ascontiguousarray(biasn.transpose(0, 3, 1, 2))
    shared["rb31"] = np.ascontiguousarray(np.broadcast_to(rel_bias[31][None, :], (128, 8)))
    shared["fng"] = np.ascontiguousarray(np.broadcast_to(np.asarray(inputs["final_norm_g"], np.float32)[None, :], (128, D)))
    npin = {k: np.asarray(v) for k, v in inputs.items()}
    for l in range(n_layers):
        shared.update(prep_layer_weights(npin, l))
    x = npin["x"].astype(np.float32, copy=False)
    maps = []
    for c in cores:
        m = dict(shared)
        m["x"] = np.ascontiguousarray(x[NSEQ * c:NSEQ * (c + 1)].reshape(NTOK, D))
        maps.append(m)
    return maps


def kernel(**inputs):
    if "nc" not in _CACHE:
        _CACHE["nc"] = build_program()[0]
    nc = _CACHE["nc"]
    maps = make_in_maps(inputs)
    res = run_bass_kernel_spmd(nc, maps, core_ids=list(range(NCORES)))
    outs = [np.asarray(r["out"], dtype=np.float32).reshape(NSEQ, SEQ, D) for r in res.results]
    return np.concatenate(outs, axis=0)
```

```python
import math
from contextlib import ExitStack
import numpy as np
import concourse.bass as bass
import concourse.mybir as mybir
from concourse.bass_utils import run_bass_kernel_spmd

F32 = mybir.dt.float32
BF16 = mybir.dt.bfloat16
AF = mybir.ActivationFunctionType
ALU = mybir.AluOpType

N_DMA_SEMS = 40
NCORES = 8
SEQ = 2048
NSEQ = 2
NTOK = NSEQ * SEQ
D = 1024
DFF = 2816
EPS = 1e-6
NEG = -1.0e30


def _key(x):
    if isinstance(x, tuple):
        return x[0].tensor.name + ":" + str(x[1])
    if isinstance(x, str):
        return x
    return x.tensor.name


def _ap(x):
    return x[0] if isinstance(x, tuple) else x


def _isnum(v):
    return isinstance(v, (int, float))


class Sched:
    ENG = ("pe", "act", "dve", "pool", "sp")

    def __init__(self, nc, es):
        self.nc = nc
        self.es = es
        self.obj = {"pe": nc.tensor, "act": nc.scalar, "dve": nc.vector, "pool": nc.gpsimd, "sp": nc.sync}
        self.count = {e: 0 for e in self.ENG}
        self.sem = {e: es.enter_context(nc.semaphore("s_" + e)) for e in ("pe", "act", "dve", "pool")}
        self.dsem = [es.enter_context(nc.semaphore("d%d" % i)) for i in range(N_DMA_SEMS)]
        self.dval = [0] * N_DMA_SEMS
        self.dma_n = 0
        self.last_w = {}
        self.readers = {}
        self.waited = {e: {} for e in self.ENG}
        self.n_inst = 0
        self.uid = 0

    def sb(self, name, shape, dt):
        self.uid += 1
        return self.es.enter_context(self.nc.sbuf_tensor("%s_u%d" % (name, self.uid), list(shape), dt))

    def ps(self, name, shape, dt):
        return self.es.enter_context(self.nc.psum_tensor(name, list(shape), dt))

    def _deps(self, eng, reads, writes):
        toks = set()
        for k in reads:
            t = self.last_w.get(k)
            if t is not None:
                toks.add(t)
        for k in writes:
            t = self.last_w.get(k)
            if t is not None:
                toks.add(t)
            for t in self.readers.get(k, ()):
                toks.add(t)
        best = {}
        for (sk, v) in toks:
            if sk == eng and eng == "pe":
                continue
            if v > best.get(sk, 0):
                best[sk] = v
        waits = []
        w = self.waited[eng]
        for sk, v in best.items():
            if w.get(sk, 0) >= v:
                continue
            w[sk] = v
            waits.append((sk, v))
        return waits

    def _commit(self, tok, reads, writes):
        for k in reads:
            self.readers.setdefault(k, []).append(tok)
        for k in writes:
            self.last_w[k] = tok
            self.readers[k] = []

    def _emit(self, eng, waits, fn, kind):
        engine = self.obj[eng]
        for sk, v in waits:
            engine.wait_ge(self._semof(sk), v)
        if fn is None:
            return
        ins = fn(engine)
        if kind[0] == "c":
            ins.then_inc(self.sem[kind[1]], 1)
        else:
            ins.then_inc(self.dsem[kind[1]], 16)

    def op(self, eng, fn, reads, writes):
        rk = [_key(r) for r in reads]
        wk = [_key(w) for w in writes]
        waits = self._deps(eng, rk, wk)
        self.count[eng] += 1
        tok = (eng, self.count[eng])
        self._emit(eng, waits, fn, ("c", eng))
        self._commit(tok, rk, wk)
        self.n_inst += 1

    def dma(self, out, in_, q="sp", **kw):
        rk = [_key(in_)]
        wk = [_key(out)]
        slot = self.dma_n % N_DMA_SEMS
        self.dma_n += 1
        waits = self._deps(q, rk, wk)
        sk = ("d", slot)
        prev = self.dval[slot]
        if prev > 0 and self.waited[q].get(sk, 0) < prev:
            self.waited[q][sk] = prev
            waits.append((sk, prev))
        self.dval[slot] = prev + 16
        tok = (sk, prev + 16)
        o, i = _ap(out), _ap(in_)
        self._emit(q, waits, (lambda e: e.dma_start(out=o, in_=i, **kw)), ("d", slot))
        self._commit(tok, rk, wk)
        self.n_inst += 1

    def barrier(self):
        for e in self.ENG:
            waits = []
            w = self.waited[e]
            for e2 in ("pe", "act", "dve", "pool"):
                v = self.count[e2]
                if v == 0 or w.get(e2, 0) >= v:
                    continue
                if e2 == e and e == "pe":
                    continue
                w[e2] = v
                waits.append((e2, v))
            for s in range(N_DMA_SEMS):
                v = self.dval[s]
                sk = ("d", s)
                if v == 0 or w.get(sk, 0) >= v:
                    continue
                w[sk] = v
                waits.append((sk, v))
            if waits:
                self._emit(e, waits, None, None)
        self.last_w = {}
        self.readers = {}

    def _semof(self, sk):
        if isinstance(sk, tuple):
            return self.dsem[sk[1]]
        return self.sem[sk]

    def mm(self, out, lhsT, rhs, start=True, stop=True):
        o, l, r = _ap(out), _ap(lhsT), _ap(rhs)
        self.op("pe", lambda e: e.matmul(o, l, r, start=start, stop=stop), [lhsT, rhs], [out])

    def tr(self, out, in_, ident):
        o, i, d = _ap(out), _ap(in_), _ap(ident)
        self.op("pe", lambda e: e.transpose(o, i, d), [in_, ident], [out])

    def act(self, out, in_, func, bias=None, scale=None, accum_out=None):
        o, i = _ap(out), _ap(in_)
        kw = {}
        reads = [in_]
        writes = [out]
        if bias is not None:
            if _isnum(bias):
                kw["bias"] = bias
            else:
                kw["bias"] = _ap(bias)
                reads.append(bias)
        if scale is not None:
            if _isnum(scale):
                kw["scale"] = scale
            else:
                kw["scale"] = _ap(scale)
                reads.append(scale)
        if accum_out is not None:
            kw["accum_out"] = _ap(accum_out)
            writes.append(accum_out)
        self.op("act", lambda e: e.activation(o, i, func, **kw), reads, writes)

    def ts(self, out, in0, s1, s2, op0, op1=None, eng="dve"):
        o, i = _ap(out), _ap(in0)
        reads = [in0]
        a1, a2 = s1, s2
        if s1 is not None and not _isnum(s1):
            reads.append(s1)
            a1 = _ap(s1)
        if s2 is not None and not _isnum(s2):
            reads.append(s2)
            a2 = _ap(s2)
        if op1 is None:
            self.op(eng, lambda e: e.tensor_scalar(o, i, a1, None, op0), reads, [out])
        else:
            self.op(eng, lambda e: e.tensor_scalar(o, i, a1, a2, op0, op1), reads, [out])

    def ts_acc(self, out, in0, s1, init, op0, red_op, accum_out):
        o, i, ac = _ap(out), _ap(in0), _ap(accum_out)
        reads = [in0]
        a1 = s1
        if not _isnum(s1):
            reads.append(s1)
            a1 = _ap(s1)
        self.op("dve", lambda e: e.tensor_scalar(o, i, a1, init, op0, red_op, accum_out=ac), reads, [out, accum_out])

    def treduce(self, out, in_, op):
        o, i = _ap(out), _ap(in_)
        self.op("dve", lambda e: e.tensor_reduce(o, i, mybir.AxisListType.X, op), [in_], [out])

    def tt(self, out, in0, in1, op, eng="dve"):
        o, a, b = _ap(out), _ap(in0), _ap(in1)
        self.op(eng, lambda e: e.tensor_tensor(o, a, b, op), [in0, in1], [out])

    def stt(self, out, in0, scalar, in1, op0, op1):
        o, a, b = _ap(out), _ap(in0), _ap(in1)
        reads = [in0, in1]
        s = scalar
        if not _isnum(scalar):
            reads.append(scalar)
            s = _ap(scalar)
        self.op("dve", lambda e: e.scalar_tensor_tensor(o, a, s, b, op0, op1), reads, [out])

    def copy(self, out, in_, eng="dve"):
        o, i = _ap(out), _ap(in_)
        if eng == "act":
            self.op("act", lambda e: e.copy(o, i), [in_], [out])
        else:
            self.op(eng, lambda e: e.tensor_copy(o, i), [in_], [out])

    def memset(self, out, val, eng="dve"):
        o = _ap(out)
        self.op(eng, lambda e: e.memset(o, val), [], [out])

    def recip(self, out, in_):
        o, i = _ap(out), _ap(in_)
        self.op("dve", lambda e: e.reciprocal(o, i), [in_], [out])

    def max8(self, out, in_):
        o, i = _ap(out), _ap(in_)
        self.op("dve", lambda e: e.max(o, i), [in_], [out])

    def mrep(self, out, rep, vals, imm):
        o, r, v = _ap(out), _ap(rep), _ap(vals)
        self.op("dve", lambda e: e.match_replace(o, r, v, imm), [rep, vals], [out])

    def bnstats(self, out, in_):
        o, i = _ap(out), _ap(in_)
        self.op("dve", lambda e: e.bn_stats(o, i), [in_], [out])

    def bnaggr(self, out, in_):
        o, i = _ap(out), _ap(in_)
        self.op("dve", lambda e: e.bn_aggr(o, i), [in_], [out])


def _t5_bucket_np(dist):
    dist = np.asarray(dist, dtype=np.int64)
    max_exact = 16
    d_f = np.maximum(dist, 1).astype(np.float32)
    large = max_exact + (np.log(d_f / np.float32(max_exact)) / np.float32(math.log(128 / max_exact))
                         * np.float32(32 - max_exact)).astype(np.int32)
    large = np.minimum(large, 31)
    return np.where(dist < max_exact, dist, large).astype(np.int64)


def host_constants():
    c = {}
    r = np.arange(128)
    c["c_ident"] = np.eye(128, dtype=np.float32)
    c["c_tri"] = (r[:, None] <= r[None, :]).astype(np.float32)
    c["c_ones"] = np.ones((128, 128), np.float32)
    c["c_negd"] = np.where(r[None, :] <= r[:, None], 0.0, NEG).astype(np.float32)
    half = 64
    freqs = (10000.0 ** (-np.linspace(0.0, 1.0, half, dtype=np.float32))).astype(np.float32)
    pos = np.arange(SEQ, dtype=np.float32)
    ang = (pos[None, :] * freqs[:, None]).astype(np.float32)
    cos = np.cos(ang).astype(np.float32)
    sin = np.sin(ang).astype(np.float32)
    c["c_cos"] = np.concatenate([cos, cos], 0)
    c["c_sin"] = np.concatenate([sin, sin], 0)
    h = np.arange(4, dtype=np.float64)
    lg = np.log1p(-np.exp2(-5.0 - h))
    rr = np.arange(128, dtype=np.float64)
    c["c_rete"] = np.exp(-(rr[:, None] + 1.0) * lg[None, :]).astype(np.float32)
    c["c_retrow"] = (np.exp((rr[:, None] + 1.0) * lg[None, :]) * 128 ** -0.5).astype(np.float32)
    c["ret_g"] = [float(np.exp(128.0 * v)) for v in lg]
    return c


def bias_index():
    t = np.arange(128)
    out = np.zeros((128, 2, 128), np.int64)
    for off in range(2):
        d = t[:, None] - t[None, :] + 128 * off
        out[:, off, :] = _t5_bucket_np(np.maximum(d, 0))
    return out


OFF = dict(m_q=0, m_k=256, m_v=512, m_i=1024, m_f=1028, m_o=1032, a_cq=1544, a_kidx=1800,
           a_widx=1864, a_ckv=1872, r_q=2000, r_k=2512, r_v=3024, r_g=3536, g_m=4048, g_a=5072,
           g_r=6096)
NFM = 5632
NTM = 2512
ZS_CQ, ZS_CKV, ZS_KIDX, ZS_WIDX, ZS_MI, ZS_MF, ZS_N = 0, 256, 384, 448, 456, 460, 464


def _rot_pre(w):
    w4 = w.reshape(w.shape[0], 4, 2, 64)
    return np.ascontiguousarray(w4[:, :, ::-1, :]).reshape(w.shape[0], 512)


def prep_layer_weights(inp, l):
    w = inp["w_in"][l]
    sl = lambda name, n: w[:, OFF[name]:OFF[name] + n]
    rq, rk = sl("r_q", 512), sl("r_k", 512)
    w_fm = np.concatenate([sl("m_q", 256), sl("m_k", 256), rq, _rot_pre(rq), rk, _rot_pre(rk),
                           sl("g_m", 1024), sl("g_a", 1024), sl("g_r", 1024)], axis=1)
    w_tm = np.concatenate([sl("m_v", 512), sl("m_o", 512), sl("r_v", 512), sl("r_g", 512),
                           sl("a_cq", 256), sl("a_ckv", 128), sl("a_kidx", 64), sl("a_widx", 8),
                           sl("m_i", 4), sl("m_f", 4)], axis=1)
    assert w_fm.shape[1] == NFM and w_tm.shape[1] == NTM
    rep = lambda v: np.ascontiguousarray(np.broadcast_to(np.asarray(v, np.float32).reshape(1, -1), (128, np.asarray(v).size)))
    colv = lambda v: np.ascontiguousarray(np.asarray(v, np.float32).reshape(-1, 128).T)
    d = {
        "w_fm": np.ascontiguousarray(w_fm), "w_tm": np.ascontiguousarray(w_tm),
        "n1g": colv(inp["norm1_g"][l]), "n2g": colv(inp["norm2_g"][l]),
        "mconv": np.ascontiguousarray(inp["m_conv"][l].reshape(4, 4, 128).transpose(2, 1, 0)),
        "mbias": rep(np.tile(np.concatenate([inp["m_ibias"][l], inp["m_fbias"][l]]), 16)),
        "mng": rep(inp["m_norm_g"][l]), "rng": rep(inp["r_norm_g"][l]),
        "aqg": colv(inp["a_qnorm_g"][l]),
        "akg": colv(np.concatenate([inp["a_kidx_g"][l], inp["a_kidx_g"][l]])),
        "akvg": rep(inp["a_kvnorm_g"][l]),
        "wuq": np.ascontiguousarray(inp["a_wuq"][l]), "wuqi": np.ascontiguousarray(inp["a_wuq_idx"][l]),
        "wuk": np.ascontiguousarray(inp["a_wuk"][l].reshape(4, 2, 64, 128).transpose(1, 2, 0, 3).reshape(128, 4, 128)),
        "wuv": np.ascontiguousarray(inp["a_wuv"][l].transpose(1, 0, 2)),
        "p_m": np.ascontiguousarray(inp["p_m"][l]), "p_a": np.ascontiguousarray(inp["p_a"][l]),
        "p_r": np.ascontiguousarray(inp["p_r"][l]), "w_out": np.ascontiguousarray(inp["w_out"][l]),
        "w_gate": np.ascontiguousarray(inp["w_gate"][l]), "w_up": np.ascontiguousarray(inp["w_up"][l]),
        "w_down": np.ascontiguousarray(inp["w_down"][l]),
    }
    return {"%s_%d" % (k, l): v for k, v in d.items()}


LAYER_SHAPES = {
    "w_fm": [D, NFM], "w_tm": [D, NTM], "n1g": [128, 8], "n2g": [128, 8], "mconv": [128, 4, 4],
    "mbias": [128, 128], "mng": [128, 512], "rng": [128, 512], "aqg": [128, 2], "akg": [128, 1],
    "akvg": [128, 128], "wuq": [256, 512], "wuqi": [256, 512], "wuk": [128, 4, 128],
    "wuv": [128, 8, 64], "p_m": [512, D], "p_a": [512, D], "p_r": [512, D], "w_out": [D, D],
    "w_gate": [D, DFF], "w_up": [D, DFF], "w_down": [DFF, D],
}
CONST_SHAPES = {"c_ident": [128, 128], "c_tri": [128, 128], "c_ones": [128, 128], "c_negd": [128, 128],
                "c_cos": [128, SEQ], "c_sin": [128, SEQ], "c_rete": [128, 4], "c_retrow": [128, 4],
                "biasn": [128, 8, 2, 128], "rb31": [128, 8], "fng": [128, D]}


class Prog:
    pass


def _scope(S):
    class _Sc:
        def __enter__(self_):
            self_.old = S.es
            self_.es = ExitStack()
            self_.es.__enter__()
            S.es = self_.es
            return self_

        def __exit__(self_, *a):
            S.barrier()
            S.es = self_.old
            return self_.es.__exit__(*a)
    return _Sc()


def load_cast(P, dst, src, K, N, gain=None, stg=None, engs=("pool", "dve", "act")):
    S = P.S
    kc = K // 128
    srcv = src.rearrange("(c p) n -> p c n", p=128)
    CH = stg[0].shape[1]
    for c in range(kc):
        for n0 in range(0, N, CH):
            n1 = min(N, n0 + CH)
            sbuf = stg[P.stg_i % len(stg)]
            eng = engs[P.stg_i % len(engs)]
            P.stg_i += 1
            S.dma(sbuf[:, 0:n1 - n0], srcv[:, c, n0:n1])
            if gain is None:
                S.copy(dst[:, c, n0:n1], sbuf[:, 0:n1 - n0], eng=eng)
            elif eng == "act":
                S.act(dst[:, c, n0:n1], sbuf[:, 0:n1 - n0], AF.Copy, scale=gain[:, c:c + 1])
            else:
                S.ts(dst[:, c, n0:n1], sbuf[:, 0:n1 - n0], gain[:, c:c + 1], None, ALU.mult, eng=eng)


def dma_chunks(S, out, in_, n, step):
    for a in range(0, n, step):
        b = min(n, a + step)
        S.dma(out[:, a:b], in_[:, a:b])


def rms_rstd(P, rs, ss, n):
    S = P.S
    S.ts(rs, ss, 1.0 / n, EPS, ALU.mult, ALU.add)
    S.act(rs, rs, AF.Sqrt)
    S.recip(rs, rs)


def norm_transpose(P, xt, hb, junk, ss, rs, hT, col0, ptb):
    S = P.S
    S.act(junk[:], xt[:], AF.Square, accum_out=ss[:])
    rms_rstd(P, rs[:], ss[:], D)
    S.ts(hb[:], xt[:], rs[:], None, ALU.mult)
    for k in range(8):
        S.tr(ptb[:, k * 128:(k + 1) * 128], hb[:, k * 128:(k + 1) * 128], P.identb[:])
    S.copy(hT[:, :, col0:col0 + 128], ptb[:, :].rearrange("p (k t) -> p k t", k=8), eng="act")


def stage_A1(P, l, x_in):
    S, G = P.S, P.G
    with _scope(S):
        Wfm = S.sb("Wfm", [128, 8, NFM], BF16)
        stg = [S.sb("stgA%d" % i, [128, 2048], F32) for i in range(3)]
        cos = S.sb("cos", [128, SEQ], F32)
        sin = S.sb("sin", [128, SEQ], F32)
        n1g = S.sb("n1g", [128, 8], F32)
        xb = [S.sb("xbA%d" % i, [128, D], F32) for i in range(2)]
        hb = S.sb("hbA", [128, D], BF16)
        junk = S.sb("junkA", [128, D], BF16)
        ss = S.sb("ssA", [128, 1], F32)
        rs = S.sb("rsA", [128, 1], F32)
        hT = S.sb("hTA", [128, 8, 512], BF16)
        ev32 = [S.sb("ev32A%d" % i, [128, 512], F32) for i in range(4)]
        evb = [S.sb("evbA%d" % i, [128, 512], BF16) for i in range(4)]
        S.dma(n1g[:], G["n1g_%d" % l])
        S.dma(cos[:], G["c_cos"])
        S.dma(sin[:], G["c_sin"])
        load_cast(P, Wfm, G["w_fm_%d" % l], D, NFM, gain=n1g, stg=stg)
        for c in range(8):
            for base in (1024, 2048):
                v = Wfm[:, c, base:base + 512].rearrange("p (h t j) -> p h t j", h=4, t=2)[:, :, 0, :]
                S.ts(v, v, -1.0, None, ALU.mult, eng="pool")
        hTd = G["hT_d"].rearrange("(k p) t -> p k t", p=128)
        e32 = 0
        eb = 0
        bank = 0
        for st in range(NTOK // 512):
            t0 = st * 512
            p0 = t0 % SEQ
            for ti in range(4):
                xt = xb[ti % 2]
                S.dma(xt[:], x_in[t0 + ti * 128:t0 + (ti + 1) * 128, :])
                norm_transpose(P, xt, hb, junk, ss, rs, hT, ti * 128, P.pbB[ti % 2])
            dma_chunks(S, hTd[:, :, t0:t0 + 512], hT, 8, 4)

            def proj(c):
                nonlocal bank
                ps = P.pbF[bank % 6]
                bank += 1
                for k in range(8):
                    S.mm(ps[:], Wfm[:, k, c * 128:(c + 1) * 128], hT[:, k, :], start=(k == 0), stop=(k == 7))
                return ps
            for c in range(4):
                ps = proj(c)
                o = ev32[e32 % 4]
                e32 += 1
                S.copy(o[:], ps[:], eng="act")
                S.dma(G["qkT"][c * 128:(c + 1) * 128, t0:t0 + 512], o[:])
            for which, dst in ((0, "rqT"), (1, "rkT")):
                for hh in range(4):
                    ca = 4 + which * 8 + hh
                    pa = proj(ca)
                    pbk = proj(ca + 4)
                    t1 = ev32[e32 % 4]
                    t2 = ev32[(e32 + 1) % 4]
                    e32 += 2
                    S.tt(t1[:], pa[:], cos[:, p0:p0 + 512], ALU.mult)
                    S.tt(t2[:], pbk[:], sin[:, p0:p0 + 512], ALU.mult)
                    ob = evb[eb % 4]
                    eb += 1
                    S.tt(ob[:], t1[:], t2[:], ALU.add, eng="pool")
                    S.dma(G[dst][hh * 128:(hh + 1) * 128, t0:t0 + 512], ob[:])
            for c in range(24):
                ps = proj(20 + c)
                ob = evb[eb % 4]
                eb += 1
                S.act(ob[:], ps[:], AF.Sigmoid)
                S.dma(G["gT"][c * 128:(c + 1) * 128, t0:t0 + 512], ob[:])


def stage_A2(P, l):
    S, G = P.S, P.G
    with _scope(S):
        Wtm = S.sb("Wtm", [128, 8, NTM], BF16)
        stg = [S.sb("stgB%d" % i, [128, 2048], F32) for i in range(3)]
        n1g = S.sb("n1gB", [128, 8], F32)
        hTs = [S.sb("hTB%d" % i, [128, 8, 512], BF16) for i in range(2)]
        evb = [S.sb("evbB%d" % i, [128, 512], BF16) for i in range(6)]
        ev32 = [S.sb("ev32B%d" % i, [128, 512], F32) for i in range(2)]
        S.dma(n1g[:], G["n1g_%d" % l])
        load_cast(P, Wtm, G["w_tm_%d" % l], D, NTM, gain=n1g, stg=stg)
        hTd = G["hT_d"].rearrange("(k p) t -> p k t", p=128)
        groups = [(0, 512, "zv", "copy"), (512, 512, "zo", "sig"), (1024, 512, "rv", "copy"),
                  (1536, 512, "rg", "silu"), (2048, ZS_N, "zs", "f32")]
        bank = 0
        eb = 0
        e32 = 0
        for st in range(NTOK // 512):
            t0 = st * 512
            hT = hTs[st % 2]
            dma_chunks(S, hT, hTd[:, :, t0:t0 + 512], 8, 4)
            for ti in range(4):
                r0 = t0 + ti * 128
                for (c0, n, dst, kind) in groups:
                    ps = P.pbF[bank % 6]
                    bank += 1
                    for k in range(8):
                        S.mm(ps[:, 0:n], hT[:, k, ti * 128:(ti + 1) * 128], Wtm[:, k, c0:c0 + n],
                             start=(k == 0), stop=(k == 7))
                    if kind == "f32":
                        o = ev32[e32 % 2]
                        e32 += 1
                        S.copy(o[:, 0:n], ps[:, 0:n], eng="dve")
                    else:
                        o = evb[eb % 6]
                        eb += 1
                        if kind == "copy":
                            S.copy(o[:, 0:n], ps[:, 0:n], eng="dve")
                        elif kind == "sig":
                            S.act(o[:, 0:n], ps[:, 0:n], AF.Sigmoid)
                        else:
                            S.act(o[:, 0:n], ps[:, 0:n], AF.Silu)
                    S.dma(G[dst][r0:r0 + 128, :], o[:, 0:n])


def ln_gate_out(P, pN, s2, hp, n, o32, gain_t, gate_all, hm, tmp):
    S = P.S
    st6, mv, t2, re = tmp
    for hh in range(2):
        S.bnstats(st6[:, hh, :], pN[:, hh, 0:128])
        S.bnaggr(mv[:, hh, :], st6[:, hh, :])
    S.tt(t2[:], s2, s2, ALU.mult)
    S.tt(t2[:], t2[:], mv[:, :, 1], ALU.mult)
    S.ts(t2[:], t2[:], EPS, None, ALU.add)
    S.act(t2[:], t2[:], AF.Sqrt)
    S.recip(t2[:], t2[:])
    S.tt(re[:], t2[:], s2, ALU.mult)
    for hh in range(2):
        S.ts(o32[:, hh * 128:(hh + 1) * 128], pN[:, hh, 0:128], mv[:, hh, 0:1], re[:, hh:hh + 1],
             ALU.subtract, ALU.mult)
    S.tt(o32[:], o32[:], gain_t[:, hp * 256:(hp + 1) * 256], ALU.mult, eng="pool")
    S.tt(hm[:, hp * 256:(hp + 1) * 256], o32[:], gate_all[:, n, hp * 256:(hp + 1) * 256], ALU.mult, eng="pool")


def out_transpose(P, hm, hmTs, dstT, tg, ptb):
    S = P.S
    for c in range(4):
        S.tr(ptb[:, c * 128:(c + 1) * 128], hm[:, c * 128:(c + 1) * 128], P.identb[:])
    S.copy(hmTs[:], ptb[:, 0:512].rearrange("p (c t) -> p c t", c=4), eng="act")
    S.dma(dstT.rearrange("(c p) t -> p c t", p=128)[:, :, tg:tg + 128], hmTs[:])


def stage_B(P, l):
    S, G = P.S, P.G
    NCH = SEQ // 128
    with _scope(S):
        qkraw = S.sb("qkraw", [128, 4, SEQ + 3], F32)
        cacc = [S.sb("cacc%d" % i, [128, SEQ], F32) for i in range(2)]
        qkb = S.sb("qkb", [128, 4, SEQ], BF16)
        qA = S.sb("qA", [128, 2, SEQ], BF16)
        qB = S.sb("qB", [128, 2, SEQ], BF16)
        mconv = S.sb("mconv", [128, 4, 4], F32)
        mbias = S.sb("mbias", [128, 128], F32)
        mng = S.sb("mng", [128, 512], F32)
        gi = S.sb("gi", [128, NCH, 8], F32)
        ax = S.sb("ax", [128, NCH, 4], F32)
        lf = S.sb("lf", [128, NCH, 4], F32)
        lia = S.sb("lia", [128, NCH, 4], F32)
        e_all = S.sb("e_all", [128, NCH, 4], F32)
        rowsc = S.sb("rowsc", [128, NCH, 4], F32)
        g_all = S.sb("g_all", [128, NCH, 4], F32)
        v_all = S.sb("v_all", [128, NCH, 512], BF16)
        mo_all = S.sb("mo_all", [128, NCH, 512], BF16)
        vt = S.sb("vt", [128, NCH, 4, 130], BF16)
        ktok = [S.sb("ktok%d" % i, [128, 256], BF16) for i in range(2)]
        scTm = [S.sb("scTm%d" % i, [128, 256], BF16) for i in range(2)]
        P32 = S.sb("P32", [128, 2, 129], F32)
        Cb = S.sb("Cb", [128, 2, 130], BF16)
        hm = [S.sb("hm%d" % i, [128, 512], BF16) for i in range(2)]
        hmTs = [S.sb("hmTs%d" % i, [128, 4, 128], BF16) for i in range(2)]
        o32 = [S.sb("o32%d" % i, [128, 256], F32) for i in range(4)]
        d1_r = [S.sb("d1_%d" % i, [128, 2], F32) for i in range(4)]
        s2_r = [S.sb("s2_%d" % i, [128, 2], F32) for i in range(4)]
        tmp_r = [(S.sb("st6_%d" % i, [128, 2, 6], F32), S.sb("mv_%d" % i, [128, 2, 2], F32),
                  S.sb("t2_%d" % i, [128, 2], F32), S.sb("re_%d" % i, [128, 2], F32)) for i in range(4)]
        S.dma(mconv[:], G["mconv_%d" % l])
        S.dma(mbias[:], G["mbias_%d" % l])
        S.dma(mng[:], G["mng_%d" % l])
        S.memset(qkraw[:, :, 0:3], 0.0)
        S.memset(qA[64:128, :, :], 0.0, eng="pool")
        S.memset(qB[0:64, :, :], 0.0, eng="pool")
        S.memset(vt[:], 0.0, eng="pool")
        bank = 0
        for s in range(NSEQ):
            tb = s * SEQ
            for c in range(4):
                S.dma(qkraw[:, c, 3:SEQ + 3], G["qkT"][c * 128:(c + 1) * 128, tb:tb + SEQ])
            for c in range(4):
                acc = cacc[c % 2]
                S.ts(acc[:], qkraw[:, c, 0:SEQ], mconv[:, c, 0:1], None, ALU.mult)
                for j in range(1, 4):
                    S.stt(acc[:], qkraw[:, c, j:j + SEQ], mconv[:, c, j:j + 1], acc[:], ALU.mult, ALU.add)
                if c < 2:
                    S.act(qA[0:64, c, :], acc[0:64, :], AF.Silu)
                    S.act(qB[64:128, c, :], acc[64:128, :], AF.Silu)
                else:
                    S.act(qkb[:, c, :], acc[:], AF.Silu)
            dma_chunks(S, gi, G["zs"][tb:tb + SEQ, ZS_MI:ZS_MI + 8].rearrange("(n p) c -> p n c", p=128), NCH, 4)
            gif = gi[:].rearrange("p n c -> p (n c)")
            S.tt(gif, gif, mbias[:], ALU.add)
            S.act(ax[:], gi[:, :, 4:8], AF.Abs)
            S.act(ax[:], ax[:], AF.Exp, scale=-1.0)
            S.act(ax[:], ax[:], AF.Ln, bias=1.0)
            S.ts(lf[:], gi[:, :, 4:8], 0.0, None, ALU.min)
            S.tt(lf[:], lf[:], ax[:], ALU.subtract)
            pg = P.pbF[bank % 6]
            bank += 1
            lff = lf[:].rearrange("p n c -> p (n c)")
            S.mm(pg[:, 0:64], P.tri32[:], lff)
            S.mm(pg[:, 64:128], P.ones32[:], lff)
            S.tt(lia[:], gi[:, :, 0:4], pg[:, 0:64].rearrange("p (n c) -> p n c", c=4), ALU.subtract)
            S.act(e_all[:], lia[:], AF.Exp)
            S.act(rowsc[:].rearrange("p n c -> p (n c)"), pg[:, 0:64], AF.Exp, bias=math.log(0.125))
            S.act(g_all[:].rearrange("p n c -> p (n c)"), pg[:, 64:128], AF.Exp)
            dma_chunks(S, v_all, G["zv"][tb:tb + SEQ, :].rearrange("(n p) c -> p n c", p=128), NCH, 4)
            dma_chunks(S, mo_all, G["zo"][tb:tb + SEQ, :].rearrange("(n p) c -> p n c", p=128), NCH, 4)
            for n in range(NCH):
                for h in range(4):
                    S.ts(vt[:, n, h, 0:128], v_all[:, n, h * 128:(h + 1) * 128], e_all[:, n, h:h + 1], None,
                         ALU.mult, eng="pool")
            S.copy(vt[:, :, :, 128], e_all[:], eng="pool")
            for n in range(NCH):
                cs = slice(n * 128, (n + 1) * 128)
                kt = ktok[n % 2]
                ptb = P.pbB[n % 2]
                for hc in range(2):
                    S.tr(ptb[:, hc * 128:(hc + 1) * 128], qkb[:, 2 + hc, cs], P.identb[:])
                S.copy(kt[:], ptb[:, 0:256], eng="act")
                hmc = hm[n % 2]
                for hp in range(2):
                    psc = P.pbF[bank % 6]
                    bank += 1
                    for hh in range(2):
                        qX = qA if hh == 0 else qB
                        S.mm(psc[:, hh * 128:(hh + 1) * 128], qkb[:, 2 + hp, cs], qX[:, hp, cs])
                    sm = scTm[hp]
                    S.tt(sm[:], psc[:, 0:256], P.trib2[:], ALU.mult)
                    pNb = P.pbF[bank % 6]
                    bank += 1
                    pN = pNb[:, 0:258].rearrange("p (a b) -> p a b", a=2)
                    for hh in range(2):
                        h = 2 * hp + hh
                        pr = slice(64 * hh, 64 * hh + 64)
                        S.mm(pN[:, hh, :], sm[:, hh * 128:(hh + 1) * 128], vt[:, n, h, 0:129], start=True, stop=(n == 0))
                        if n > 0:
                            qX = qA if hh == 0 else qB
                            S.mm(pN[:, hh, :], qX[:, hp, cs], Cb[:, hp, 0:129], start=False, stop=True)
                    if n < NCH - 1:
                        pUb = P.pbF[bank % 6]
                        bank += 1
                        pU = pUb[:, 0:260].rearrange("p (a b) -> p a b", a=2)
                        S.mm(pUb[:, 0:260], kt[:, hp * 128:(hp + 1) * 128],
                             vt[:, n, 2 * hp:2 * hp + 2, :].rearrange("p a b -> p (a b)"))
                        for hh in range(2):
                            h = 2 * hp + hh
                            pr = slice(64 * hh, 64 * hh + 64)
                            if n == 0:
                                S.copy(P32[pr, hp, :], pU[pr, hh, 0:129])
                            else:
                                S.stt(P32[pr, hp, :], P32[pr, hp, :], g_all[pr, n - 1, h:h + 1], pU[pr, hh, 0:129],
                                      ALU.mult, ALU.add)
                            S.ts(Cb[pr, hp, 0:129], P32[pr, hp, :], g_all[pr, n, h:h + 1], None, ALU.mult)
                    rsl = rowsc[:, n, 2 * hp:2 * hp + 2]
                    d1, s2, tmp = d1_r[(2 * n + hp) % 4], s2_r[(2 * n + hp) % 4], tmp_r[(2 * n + hp) % 4]
                    S.tt(d1[:], pN[:, :, 128], rsl, ALU.mult)
                    S.act(d1[:], d1[:], AF.Abs)
                    S.ts(d1[:], d1[:], 1.0, None, ALU.max)
                    S.recip(d1[:], d1[:])
                    S.tt(s2[:], d1[:], rsl, ALU.mult)
                    ln_gate_out(P, pN, s2[:], hp, n, o32[(2 * n + hp) % 4], mng, mo_all, hmc, tmp)
                out_transpose(P, hmc, hmTs[n % 2], G["hmT"], tb + n * 128, P.pbB[(n + 1) % 2])


def stage_D(P, l):
    S, G = P.S, P.G
    NCH = SEQ // 128
    gam = P.ret_g
    with _scope(S):
        rq = S.sb("rq", [128, 4, NTOK], BF16)
        rk = S.sb("rk", [128, 4, NTOK], BF16)
        rng = S.sb("rng", [128, 512], F32)
        rete = S.sb("rete", [128, 4], F32)
        retrow = S.sb("retrow", [128, 4], F32)
        v_all = S.sb("rv_all", [128, NCH, 512], BF16)
        rg_all = [S.sb("rg_all%d" % s, [128, NCH, 512], BF16) for s in range(NSEQ)]
        vt = [S.sb("rvt%d" % s, [128, NCH, 512], BF16) for s in range(NSEQ)]
        ktok = [[S.sb("rktok%d_%d" % (s, i), [128, 512], BF16) for i in range(2)] for s in range(NSEQ)]
        scTm = [[S.sb("rscTm%d_%d" % (s, i), [128, 256], BF16) for i in range(2)] for s in range(NSEQ)]
        P32 = [S.sb("rP32_%d" % s, [128, 4, 128], F32) for s in range(NSEQ)]
        Sb = [S.sb("rSb_%d" % s, [128, 4, 128], BF16) for s in range(NSEQ)]
        hm = [[S.sb("rhm%d_%d" % (s, i), [128, 512], BF16) for i in range(2)] for s in range(NSEQ)]
        hmTs = [[S.sb("rhmTs%d_%d" % (s, i), [128, 4, 128], BF16) for i in range(2)] for s in range(NSEQ)]
        o32 = [S.sb("ro32%d" % i, [128, 256], F32) for i in range(4)]
        tmp_r = [(S.sb("rst6_%d" % i, [128, 2, 6], F32), S.sb("rmv_%d" % i, [128, 2, 2], F32),
                  S.sb("rt2_%d" % i, [128, 2], F32), S.sb("rre_%d" % i, [128, 2], F32)) for i in range(4)]
        S.dma(rng[:], G["rng_%d" % l])
        S.dma(rete[:], G["c_rete"])
        S.dma(retrow[:], G["c_retrow"])
        bank = 0
        tcnt = 0
        ocnt = 0
        for s in range(NSEQ):
            tb = s * SEQ
            S.dma(rq[:, :, tb:tb + SEQ], G["rqT"].rearrange("(c p) t -> p c t", p=128)[:, :, tb:tb + SEQ])
            S.dma(rk[:, :, tb:tb + SEQ], G["rkT"].rearrange("(c p) t -> p c t", p=128)[:, :, tb:tb + SEQ])
            dma_chunks(S, v_all, G["rv"][tb:tb + SEQ, :].rearrange("(n p) c -> p n c", p=128), NCH, 4)
            dma_chunks(S, rg_all[s], G["rg"][tb:tb + SEQ, :].rearrange("(n p) c -> p n c", p=128), NCH, 4)
            for h in range(4):
                S.ts(vt[s][:, :, h * 128:(h + 1) * 128], v_all[:, :, h * 128:(h + 1) * 128], rete[:, h:h + 1], None,
                     ALU.mult, eng="pool")
        for n in range(NCH):
            for s in range(NSEQ):
                tb = s * SEQ
                cs = slice(tb + n * 128, tb + (n + 1) * 128)
                kt = ktok[s][n % 2]
                ptb = P.pbB[tcnt % 2]
                tcnt += 1
                for h in range(4):
                    S.tr(ptb[:, h * 128:(h + 1) * 128], rk[:, h, cs], P.identb[:])
                S.copy(kt[:], ptb[:, 0:512], eng="act")
                hmc = hm[s][n % 2]
                for hp in range(2):
                    psc = P.pbF[bank % 6]
                    bank += 1
                    for hh in range(2):
                        h = 2 * hp + hh
                        S.mm(psc[:, hh * 128:(hh + 1) * 128], rk[:, h, cs], rq[:, h, cs])
                    sm = scTm[s][hp]
                    S.tt(sm[:], psc[:, 0:256], P.trib2[:], ALU.mult)
                    pNb = P.pbF[bank % 6]
                    bank += 1
                    pN = pNb[:, 0:256].rearrange("p (a b) -> p a b", a=2)
                    for hh in range(2):
                        h = 2 * hp + hh
                        S.mm(pN[:, hh, :], sm[:, hh * 128:(hh + 1) * 128], vt[s][:, n, h * 128:(h + 1) * 128],
                             start=True, stop=(n == 0))
                        if n > 0:
                            S.mm(pN[:, hh, :], rq[:, h, cs], Sb[s][:, h, :], start=False, stop=True)
                    if n < NCH - 1:
                        pUb = P.pbF[bank % 6]
                        bank += 1
                        for hh in range(2):
                            h = 2 * hp + hh
                            S.mm(pUb[:, hh * 128:(hh + 1) * 128], kt[:, h * 128:(h + 1) * 128],
                                 vt[s][:, n, h * 128:(h + 1) * 128])
                        for hh in range(2):
                            h = 2 * hp + hh
                            pu = pUb[:, hh * 128:(hh + 1) * 128]
                            if n == 0:
                                S.copy(P32[s][:, h, :], pu)
                            else:
                                S.stt(P32[s][:, h, :], P32[s][:, h, :], gam[h], pu, ALU.mult, ALU.add)
                            S.ts(Sb[s][:, h, :], P32[s][:, h, :], gam[h], None, ALU.mult)
                    ri = ocnt % 4
                    ocnt += 1
                    ln_gate_out(P, pN, retrow[:, 2 * hp:2 * hp + 2], hp, n, o32[ri], rng, rg_all[s], hmc, tmp_r[ri])
                out_transpose(P, hmc, hmTs[s][n % 2], G["hrT"], tb + n * 128, P.pbB[tcnt % 2])
                tcnt += 1


def stage_C(P, l):
    S, G = P.S, P.G
    NT = SEQ // 128
    WSC = (8 ** -0.5) * (64 ** -0.5)
    with _scope(S):
        stg = [S.sb("stgC%d" % i, [128, 2048], F32) for i in range(2)]
        aqg = S.sb("aqg", [128, 2], F32)
        akg = S.sb("akg", [128, 1], F32)
        akvg = S.sb("akvg", [128, 128], F32)
        wuq = S.sb("wuq", [128, 2, 512], BF16)
        wuqi = S.sb("wuqi", [128, 2, 512], BF16)
        wuk = S.sb("wuk", [128, 1, 512], BF16)
        wuvz = S.sb("wuvz", [128, 8, 128], BF16)
        zt = [S.sb("ztC%d" % i, [128, ZS_N], F32) for i in range(4)]
        NR = 4
        junk_r = [S.sb("junkC%d" % i, [128, 256], BF16) for i in range(NR)]
        junk2_r = [S.sb("junk2C%d" % i, [128, 128], BF16) for i in range(NR)]
        ss_r = [S.sb("ssC%d" % i, [128, 2], F32) for i in range(NR)]
        rs_r = [S.sb("rsC%d" % i, [128, 2], F32) for i in range(NR)]
        st6_r = [S.sb("st6C%d" % i, [128, 6], F32) for i in range(NR)]
        mv_r = [S.sb("mvC%d" % i, [128, 2], F32) for i in range(NR)]
        cqn_r = [S.sb("cqn%d" % i, [128, 256], BF16) for i in range(NR)]
        kin_r = [S.sb("kin%d" % i, [128, 128], BF16) for i in range(NR)]
        ckn_r = [S.sb("ckn%d" % i, [128, 128], F32) for i in range(NR)]
        cqT = S.sb("cqT", [128, 2, SEQ], BF16)
        kidxT = S.sb("kidxT", [128, SEQ], BF16)
        ckv1 = S.sb("ckv1", [128, NT, 130], BF16)
        ckvT = S.sb("ckvT", [128, SEQ], BF16)
        qTa = S.sb("qTCa", [128, 4, 512], BF16)
        qTb = S.sb("qTCb", [128, 4, 512], BF16)
        qidxA = S.sb("qidxA", [128, 4, SEQ], BF16)
        qidxB = S.sb("qidxB", [128, 4, SEQ], BF16)
        qlatT = S.sb("qlatT", [128, 8, SEQ], BF16)
        wabs = S.sb("wabs", [128, NT, 8], F32)
        wsgn = S.sb("wsgn", [128, NT, 8], F32)
        acc2 = [S.sb("accC%d" % i, [128, SEQ], F32) for i in range(2)]
        NBIS = 20
        junkb = [S.sb("junkbC%d" % i, [128, SEQ], BF16) for i in range(2)]
        bis = [S.sb("bisC%d" % i, [128, 8], F32) for i in range(2)]
        dk = [S.sb("dkC%d" % i, [128, NBIS], F32) for i in range(2)]
        pw2 = S.sb("pw2C", [128, NBIS], F32)
        for k in range(NBIS):
            S.memset(pw2[:, k:k + 1], 2.0 ** -k, eng="pool")
        rbuf = [S.sb("rbufC%d" % i, [128, 512], F32) for i in range(3)]
        m8 = [S.sb("m8C%d" % i, [128, 8], F32) for i in range(2)]
        madd4 = [S.sb("maddC%d" % i, [128, SEQ], BF16) for i in range(4)]
        Eb = [S.sb("EbC%d" % i, [128, 512], BF16) for i in range(3)]
        otok = S.sb("otok", [128, 8, 128], BF16)
        olT = S.sb("olT", [128, 8, 128], BF16)
        haTs = [S.sb("haTs%d" % i, [128, 4, 128], BF16) for i in range(2)]
        rec = S.sb("recC", [128, 8], F32)
        S.dma(aqg[:], G["aqg_%d" % l])
        S.dma(akg[:], G["akg_%d" % l])
        S.dma(akvg[:], G["akvg_%d" % l])
        load_cast(P, wuq, G["wuq_%d" % l], 256, 512, gain=aqg, stg=stg)
        load_cast(P, wuqi, G["wuqi_%d" % l], 256, 512, gain=aqg, stg=stg)
        load_cast(P, wuk, G["wuk_%d" % l].rearrange("p a b -> p (a b)"), 128, 512, stg=stg)
        S.memset(wuvz[:], 0.0, eng="pool")
        sv = stg[P.stg_i % 2]
        P.stg_i += 1
        S.dma(sv[:, 0:512], G["wuv_%d" % l].rearrange("p a b -> p (a b)"))
        for h in range(8):
            S.copy(wuvz[:, h, (h % 2) * 64:(h % 2) * 64 + 64], sv[:, h * 64:(h + 1) * 64], eng="pool")
        S.memset(ckv1[:, :, 128:130], 1.0, eng="pool")
        S.memset(qTa[64:128, :, :], 0.0, eng="pool")
        S.memset(qTb[0:64, :, :], 0.0, eng="pool")
        S.memset(qidxA[64:128, :, :], 0.0, eng="pool")
        S.memset(qidxB[0:64, :, :], 0.0, eng="pool")
        bank = 0
        for s in range(NSEQ):
            tb = s * SEQ
            for i in range(NT):
                z = zt[i % 4]
                ri = i % NR
                junk, junk2, ss, rs, st6, mv = junk_r[ri], junk2_r[ri], ss_r[ri], rs_r[ri], st6_r[ri], mv_r[ri]
                cqn, kin, ckn = cqn_r[ri], kin_r[ri], ckn_r[ri]
                S.dma(z[:], G["zs"][tb + i * 128:tb + (i + 1) * 128, :])
                S.act(junk[:, 0:256], z[:, ZS_CQ:ZS_CQ + 256], AF.Square, accum_out=ss[:, 0:1])
                S.act(junk2[:, 0:128], z[:, ZS_CKV:ZS_CKV + 128], AF.Square, accum_out=ss[:, 1:2])
                S.ts(rs[:, 0:1], ss[:, 0:1], 1.0 / 256, EPS, ALU.mult, ALU.add)
                S.ts(rs[:, 1:2], ss[:, 1:2], 1.0 / 128, EPS, ALU.mult, ALU.add)
                S.act(rs[:], rs[:], AF.Sqrt)
                S.recip(rs[:], rs[:])
                S.ts(cqn[:], z[:, ZS_CQ:ZS_CQ + 256], rs[:, 0:1], None, ALU.mult)
                S.stt(ckn[:], z[:, ZS_CKV:ZS_CKV + 128], rs[:, 1:2], akvg[:], ALU.mult, ALU.mult)
                S.copy(ckv1[:, i, 0:128], ckn[:], eng="pool")
                S.bnstats(st6[:], z[:, ZS_KIDX:ZS_KIDX + 64])
                S.bnaggr(mv[:], st6[:])
                S.ts(mv[:, 1:2], mv[:, 1:2], EPS, None, ALU.add)
                S.act(mv[:, 1:2], mv[:, 1:2], AF.Sqrt)
                S.recip(mv[:, 1:2], mv[:, 1:2])
                S.ts(kin[:, 0:64], z[:, ZS_KIDX:ZS_KIDX + 64], mv[:, 0:1], mv[:, 1:2], ALU.subtract, ALU.mult)
                S.copy(kin[:, 64:128], kin[:, 0:64], eng="pool")
                S.act(wabs[:, i, :], z[:, ZS_WIDX:ZS_WIDX + 8], AF.Abs, scale=WSC)
                S.ts(wsgn[:, i, :], z[:, ZS_WIDX:ZS_WIDX + 8], 0.0, None, ALU.is_gt)
                S.ts(wsgn[:, i, :], wsgn[:, i, :], 2.0, -1.0, ALU.mult, ALU.add)
                ptb = P.pbB[i % 2]
                S.tr(ptb[:, 0:128], cqn[:, 0:128], P.identb[:])
                S.tr(ptb[:, 128:256], cqn[:, 128:256], P.identb[:])
                S.tr(ptb[:, 256:384], kin[:], P.identb[:])
                S.tr(ptb[:, 384:512], ckv1[:, i, 0:128], P.identb[:])
                cs = slice(i * 128, (i + 1) * 128)
                S.copy(cqT[:, :, cs], ptb[:, 0:256].rearrange("p (c t) -> p c t", c=2), eng="act")
                S.copy(kidxT[:, cs], ptb[:, 256:384], eng="act")
                S.ts(kidxT[:, cs], kidxT[:, cs], akg[:, 0:1], None, ALU.mult, eng="pool")
                S.copy(ckvT[:, cs], ptb[:, 384:512], eng="act")
            for b4 in range(SEQ // 512):
                ts_ = slice(b4 * 512, (b4 + 1) * 512)
                for c in range(4):
                    ps = P.pbF[bank % 6]
                    bank += 1
                    for k in range(2):
                        S.mm(ps[:], wuq[:, k, c * 128:(c + 1) * 128], cqT[:, k, ts_], start=(k == 0), stop=(k == 1))
                    S.copy(qTa[0:64, c, :], ps[0:64, :], eng="act")
                    S.copy(qTb[64:128, c, :], ps[64:128, :], eng="act")
                for c in range(4):
                    ps = P.pbF[bank % 6]
                    bank += 1
                    for k in range(2):
                        S.mm(ps[:], wuqi[:, k, c * 128:(c + 1) * 128], cqT[:, k, ts_], start=(k == 0), stop=(k == 1))
                    S.copy(qidxA[0:64, c, ts_], ps[0:64, :], eng="dve")
                    S.copy(qidxB[64:128, c, ts_], ps[64:128, :], eng="dve")
                for h in range(8):
                    qTx = qTa if h % 2 == 0 else qTb
                    ps = P.pbF[bank % 6]
                    bank += 1
                    S.mm(ps[:], wuk[:, 0, (h // 2) * 128:(h // 2 + 1) * 128], qTx[:, h // 2, :])
                    S.act(qlatT[:, h, ts_], ps[:], AF.Copy, scale=0.125)
            def idx_topk_pair(ia):
                nonlocal bank
                tiles = (ia, ia + 1)
                for i in tiles:
                    W = (i + 1) * 128
                    cs = slice(i * 128, (i + 1) * 128)
                    nb = (W + 511) // 512
                    acc = acc2[i % 2]
                    for b in range(nb):
                        w = min(512, W - b * 512)
                        ks = slice(b * 512, b * 512 + w)
                        for g in range(8):
                            qix = qidxA if g % 2 == 0 else qidxB
                            ps = P.pbF[bank % 4]
                            bank += 1
                            S.mm(ps[:, 0:w], qix[:, g // 2, cs], kidxT[:, ks])
                            r = rbuf[g % 3]
                            S.act(r[:, 0:w], ps[:, 0:w], AF.Relu, scale=wabs[:, i, g:g + 1])
                            if g == 0:
                                S.ts(acc[:, ks], r[:, 0:w], wsgn[:, i, 0:1], None, ALU.mult)
                            else:
                                S.stt(acc[:, ks], r[:, 0:w], wsgn[:, i, g:g + 1], acc[:, ks], ALU.mult, ALU.add)
                    S.tt(acc[:, cs], acc[:, cs], P.negd[:], ALU.add)
                if ia >= 2:
                    for i in tiles:
                        W = (i + 1) * 128
                        acc, bs = acc2[i % 2], bis[i % 2]
                        S.max8(m8[i % 2][:], acc[:, 0:W])
                        S.treduce(bs[:, 0:1], acc[:, 0:i * 128], ALU.min)
                        S.ts(bs[:, 1:2], m8[i % 2][:, 0:1], bs[:, 0:1], 0.5, ALU.subtract, ALU.mult)
                        S.ts(bs[:, 2:3], m8[i % 2][:, 0:1], bs[:, 0:1], 0.5, ALU.add, ALU.mult)
                        S.ts(dk[i % 2][:], pw2[:], bs[:, 1:2], None, ALU.mult)
                    for k in range(NBIS):
                        for i in tiles:
                            W = (i + 1) * 128
                            S.ts_acc(junkb[i % 2][:, 0:W], acc2[i % 2][:, 0:W], bis[i % 2][:, 2:3], 0.0,
                                     ALU.is_ge, ALU.add, bis[i % 2][:, 3:4])
                        for i in tiles:
                            bs = bis[i % 2]
                            S.ts(bs[:, 4:5], bs[:, 3:4], 255.5, -0.5, ALU.is_ge, ALU.add)
                        for i in tiles:
                            bs = bis[i % 2]
                            S.stt(bs[:, 2:3], bs[:, 4:5], dk[i % 2][:, k:k + 1], bs[:, 2:3], ALU.mult, ALU.add)
                    for i in tiles:
                        W = (i + 1) * 128
                        bs = bis[i % 2]
                        S.tt(bs[:, 5:6], bs[:, 2:3], dk[i % 2][:, NBIS - 1:NBIS], ALU.subtract)
                        S.ts(madd4[i % 4][:, 0:W], acc2[i % 2][:, 0:W], bs[:, 5:6], 1.0, ALU.is_ge, ALU.subtract)
                else:
                    for i in tiles:
                        W = (i + 1) * 128
                        S.ts(madd4[i % 4][:, 0:W], acc2[i % 2][:, 0:W], -1.0e29, 1.0, ALU.is_ge, ALU.subtract)

            def attn(i):
                nonlocal bank
                cs = slice(i * 128, (i + 1) * 128)
                madd = madd4[i % 4]
                for h in range(8):
                    hh = h % 2
                    pvbank = P.pbF[4 + (h // 2) % 2]
                    pv = pvbank[:, hh * 129:(hh + 1) * 129]
                    for bg in range((i + 4) // 4):
                        j0 = bg * 4
                        nj = min(4, i + 1 - j0)
                        pl = P.pbF[bank % 4]
                        bank += 1
                        for jj in range(nj):
                            j = j0 + jj
                            near = j >= i - 1
                            blk = pl[:, jj * 128:(jj + 1) * 128]
                            S.mm(blk, ckvT[:, j * 128:(j + 1) * 128], qlatT[:, h, cs], start=True, stop=False)
                            S.mm(blk, madd[:, j * 128:(j + 1) * 128], P.i30k[:], start=False, stop=(not near))
                            if near:
                                S.mm(blk, P.biasb[:, h, i - j, :], P.identb[:], start=False, stop=True)
                        E = Eb[(h * 4 + bg) % 3]
                        S.act(E[:, 0:nj * 128], pl[:, 0:nj * 128], AF.Exp)
                        for jj in range(nj):
                            j = j0 + jj
                            S.mm(pv, E[:, jj * 128:(jj + 1) * 128], ckv1[:, j, 0:129], start=(j == 0), stop=(j == i))
                    S.recip(rec[:, h:h + 1], pv[:, 128:129])
                    S.act(otok[:, h, :], pv[:, 0:128], AF.Copy, scale=rec[:, h:h + 1])
                ptb = P.pbB[i % 2]
                for h in range(8):
                    S.tr(ptb[:, h * 128:(h + 1) * 128], otok[:, h, :], P.identb[:])
                S.copy(olT[:], ptb[:, :].rearrange("p (h t) -> p h t", h=8), eng="act")
                ph = P.pbF[bank % 4]
                bank += 1
                for hc in range(4):
                    S.mm(ph[:, hc * 128:(hc + 1) * 128], wuvz[:, 2 * hc, :], olT[:, 2 * hc, :], start=True, stop=False)
                    S.mm(ph[:, hc * 128:(hc + 1) * 128], wuvz[:, 2 * hc + 1, :], olT[:, 2 * hc + 1, :], start=False, stop=True)
                hs = haTs[i % 2]
                S.copy(hs[:], ph[:].rearrange("p (c t) -> p c t", c=4), eng="act")
                S.dma(G["haT"].rearrange("(c p) t -> p c t", p=128)[:, :, tb + i * 128:tb + (i + 1) * 128], hs[:])

            idx_topk_pair(0)
            for pp in range(NT // 2):
                if pp + 1 < NT // 2:
                    idx_topk_pair(2 * pp + 2)
                attn(2 * pp)
                attn(2 * pp + 1)


def stage_E(P, l, x_in):
    S, G = P.S, P.G
    with _scope(S):
        stg = [S.sb("stgE%d" % i, [128, 1024], F32) for i in range(3)]
        pw = [S.sb("pwE%d" % i, [128, 4, D], BF16) for i in range(3)]
        wo = S.sb("woE", [128, 8, D], BF16)
        hin = [[S.sb("hinE%d_%d" % (b, i), [128, 4, 512], BF16) for i in range(2)] for b in range(3)]
        gin = [S.sb("ginE%d" % i, [128, 24, 512], BF16) for i in range(2)]
        y32 = S.sb("y32E", [128, 512], F32)
        t32 = [S.sb("t32E%d" % i, [128, 512], F32) for i in range(2)]
        yT = S.sb("yTE", [128, 8, 512], BF16)
        xb = [S.sb("xbE%d" % i, [128, D], F32) for i in range(2)]
        for b, nm in enumerate(("p_m", "p_a", "p_r")):
            load_cast(P, pw[b], G["%s_%d" % (nm, l)], 512, D, stg=stg)
        load_cast(P, wo, G["w_out_%d" % l], D, D, stg=stg)
        srcs = [G["hmT"], G["haT"], G["hrT"]]
        bank = 0
        for st in range(NTOK // 512):
            t0 = st * 512
            for b in range(3):
                S.dma(hin[b][st % 2][:], srcs[b].rearrange("(c p) t -> p c t", p=128)[:, :, t0:t0 + 512])
            gt = gin[st % 2]
            dma_chunks(S, gt, G["gT"].rearrange("(c p) t -> p c t", p=128)[:, :, t0:t0 + 512], 24, 4)
            for c in range(8):
                pss = []
                for b in range(3):
                    ps = P.pbF[bank % 6]
                    bank += 1
                    for k in range(4):
                        S.mm(ps[:], pw[b][:, k, c * 128:(c + 1) * 128], hin[b][st % 2][:, k, :],
                             start=(k == 0), stop=(k == 3))
                    pss.append(ps)
                S.tt(y32[:], pss[0][:], gt[:, c, :], ALU.mult)
                S.tt(t32[0][:], pss[1][:], gt[:, 8 + c, :], ALU.mult)
                S.tt(t32[1][:], pss[2][:], gt[:, 16 + c, :], ALU.mult)
                S.tt(y32[:], y32[:], t32[0][:], ALU.add, eng="pool")
                S.tt(yT[:, c, :], y32[:], t32[1][:], ALU.add, eng="pool")
            for ti in range(4):
                r0 = t0 + ti * 128
                xt = xb[ti % 2]
                S.dma(xt[:], x_in[r0:r0 + 128, :])
                for hf in range(2):
                    ps = P.pbF[bank % 6]
                    bank += 1
                    for k in range(8):
                        S.mm(ps[:], yT[:, k, ti * 128:(ti + 1) * 128], wo[:, k, hf * 512:(hf + 1) * 512],
                             start=(k == 0), stop=(k == 7))
                    S.tt(xt[:, hf * 512:(hf + 1) * 512], xt[:, hf * 512:(hf + 1) * 512], ps[:], ALU.add)
                S.dma(G["x1"][r0:r0 + 128, :], xt[:])


def stage_F(P, l, x_out, final):
    S, G = P.S, P.G
    ST = 256
    NF = DFF // 128
    with _scope(S):
        stg = [S.sb("stgF%d" % i, [128, 1024], F32) for i in range(3)]
        n2g = S.sb("n2g", [128, 8], F32)
        wg = S.sb("wgF", [128, 8, DFF], BF16)
        wu = S.sb("wuF", [128, 8, DFF], BF16)
        wd = S.sb("wdF", [128, NF, D], BF16)
        xb = [S.sb("xbF%d" % i, [128, D], F32) for i in range(2)]
        hb = S.sb("hbF", [128, D], BF16)
        junk = S.sb("junkF", [128, D], BF16)
        ss = S.sb("ssF", [128, 1], F32)
        rs = S.sb("rsF", [128, 1], F32)
        hT = S.sb("hTF", [128, 8, ST], BF16)
        sg = [S.sb("sgF%d" % i, [128, ST], F32) for i in range(2)]
        aT = S.sb("aTF", [128, NF, ST], BF16)
        fng = None
        if final:
            fng = S.sb("fngF", [128, D], F32)
            S.dma(fng[:], G["fng"])
        S.dma(n2g[:], G["n2g_%d" % l])
        load_cast(P, wg, G["w_gate_%d" % l], D, DFF, gain=n2g, stg=stg)
        load_cast(P, wu, G["w_up_%d" % l], D, DFF, gain=n2g, stg=stg)
        load_cast(P, wd, G["w_down_%d" % l], DFF, D, stg=stg)
        bank = 0
        for st in range(NTOK // ST):
            t0 = st * ST
            for ti in range(ST // 128):
                xt = xb[ti % 2]
                S.dma(xt[:], G["x1"][t0 + ti * 128:t0 + (ti + 1) * 128, :])
                norm_transpose(P, xt, hb, junk, ss, rs, hT, ti * 128, P.pbB[ti % 2])
            for f in range(NF):
                pg = P.pbF[bank % 6]
                pu = P.pbF[(bank + 1) % 6]
                bank += 2
                for k in range(8):
                    S.mm(pg[:, 0:ST], wg[:, k, f * 128:(f + 1) * 128], hT[:, k, :], start=(k == 0), stop=(k == 7))
                for k in range(8):
                    S.mm(pu[:, 0:ST], wu[:, k, f * 128:(f + 1) * 128], hT[:, k, :], start=(k == 0), stop=(k == 7))
                sgt = sg[f % 2]
                S.act(sgt[:], pg[:, 0:ST], AF.Silu)
                S.tt(aT[:, f, :], sgt[:], pu[:, 0:ST], ALU.mult)
            for ti in range(ST // 128):
                r0 = t0 + ti * 128
                xt = xb[ti % 2]
                S.dma(xt[:], G["x1"][r0:r0 + 128, :])
                for hf in range(2):
                    ps = P.pbF[bank % 6]
                    bank += 1
                    for f in range(NF):
                        S.mm(ps[:], aT[:, f, ti * 128:(ti + 1) * 128], wd[:, f, hf * 512:(hf + 1) * 512],
                             start=(f == 0), stop=(f == NF - 1))
                    S.tt(xt[:, hf * 512:(hf + 1) * 512], xt[:, hf * 512:(hf + 1) * 512], ps[:], ALU.add)
                if final:
                    S.act(junk[:], xt[:], AF.Square, accum_out=ss[:])
                    rms_rstd(P, rs[:], ss[:], D)
                    S.stt(xt[:], xt[:], rs[:, 0:1], fng[:], ALU.mult, ALU.mult)
                S.dma(x_out[r0:r0 + 128, :], xt[:])


SCRATCH = {
    "hT_d": ([D, NTOK], BF16), "qkT": ([512, NTOK], F32), "rqT": ([512, NTOK], BF16), "rkT": ([512, NTOK], BF16),
    "gT": ([3072, NTOK], BF16), "zv": ([NTOK, 512], BF16), "zo": ([NTOK, 512], BF16), "rv": ([NTOK, 512], BF16),
    "rg": ([NTOK, 512], BF16), "zs": ([NTOK, ZS_N], F32), "hmT": ([512, NTOK], BF16), "haT": ([512, NTOK], BF16),
    "hrT": ([512, NTOK], BF16), "x1": ([NTOK, D], F32), "xl": ([NTOK, D], F32),
}


def build_program(n_layers=2, dbg=(), stages="A1,A2,B,C,D,E,F"):
    stages = stages.split(",")
    nc = bass.Bass("TRN2", target_bir_lowering=False)
    P = Prog()
    G = {}
    P.G = G
    P.stg_i = 0
    G["x"] = nc.dram_tensor("x", [NTOK, D], F32, kind="ExternalInput").ap()
    for k, shp in CONST_SHAPES.items():
        G[k] = nc.dram_tensor(k, shp, F32, kind="ExternalInput").ap()
    for l in range(n_layers):
        for k, shp in LAYER_SHAPES.items():
            nm = "%s_%d" % (k, l)
            G[nm] = nc.dram_tensor(nm, shp, F32, kind="ExternalInput").ap()
    G["out"] = nc.dram_tensor("out", [NTOK, D], F32, kind="ExternalOutput").ap()
    for k, (shp, dt) in SCRATCH.items():
        G[k] = nc.dram_tensor(k, shp, dt, kind=("ExternalOutput" if k in dbg else "Internal")).ap()
    P.ret_g = host_constants()["ret_g"]
    with ExitStack() as es:
        S = Sched(nc, es)
        P.S = S
        P.pbF = [S.ps("pbF%d" % i, [128, 512], F32) for i in range(6)]
        P.pbB = [S.ps("pbB%d" % i, [128, 1024], BF16) for i in range(2)]
        P.identb = S.sb("identb", [128, 128], BF16)
        P.i30k = S.sb("i30k", [128, 128], BF16)
        P.trib2 = S.sb("trib2", [128, 256], BF16)
        P.tri32 = S.sb("tri32", [128, 128], F32)
        P.ones32 = S.sb("ones32", [128, 128], F32)
        P.negd = S.sb("negd", [128, 128], F32)
        P.biasb = S.sb("biasb", [128, 8, 2, 128], BF16)
        with _scope(S):
            c32 = S.sb("c32", [128, 128], F32)
            b32 = S.sb("b32", [128, 8, 2, 128], F32)
            rb31 = S.sb("rb31", [128, 8], F32)
            S.dma(c32[:], G["c_ident"])
            S.copy(P.identb[:], c32[:])
            S.ts(P.i30k[:], c32[:], 30000.0, None, ALU.mult)
            S.dma(P.tri32[:], G["c_tri"])
            S.copy(P.trib2[:, 0:128], P.tri32[:])
            S.copy(P.trib2[:, 128:256], P.tri32[:])
            S.dma(P.ones32[:], G["c_ones"])
            S.dma(P.negd[:], G["c_negd"])
            S.dma(b32[:], G["biasn"])
            S.dma(rb31[:], G["rb31"])
            for h in range(8):
                S.ts(P.biasb[:, h, :, :], b32[:, h, :, :], rb31[:, h:h + 1], None, ALU.subtract)
        x_in = G["x"]
        for l in range(n_layers):
            last = (l == n_layers - 1)
            x_out = G["out"] if last else G["xl"]
            if "A1" in stages:
                stage_A1(P, l, x_in)
            if "A2" in stages:
                stage_A2(P, l)
            if "B" in stages:
                stage_B(P, l)
            if "D" in stages:
                stage_D(P, l)
            if "C" in stages:
                stage_C(P, l)
            if "E" in stages:
                stage_E(P, l, x_in)
            if "F" in stages:
                stage_F(P, l, x_out, last)
            x_in = x_out
        S.barrier()
        P.n_inst = S.n_inst
    return nc, P


_CACHE = {}


def make_in_maps(inputs, n_layers=2, cores=range(NCORES)):
    hc = host_constants()
    shared = {k: np.ascontiguousarray(v, dtype=np.float32) for k, v in hc.items() if k.startswith("c_")}
    rel_bias = np.asarray(inputs["rel_bias"], np.float32)
    bi = bias_index()
    biasn = rel_bias[bi]
    shared["biasn"] = np.ascontiguousarray(biasn.transpose(0, 3, 1, 2))
    shared["rb31"] = np.ascontiguousarray(np.broadcast_to(rel_bias[31][None, :], (128, 8)))
    shared["fng"] = np.ascontiguousarray(np.broadcast_to(np.asarray(inputs["final_norm_g"], np.float32)[None, :], (128, D)))
    npin = {k: np.asarray(v) for k, v in inputs.items()}
    for l in range(n_layers):
        shared.update(prep_layer_weights(npin, l))
    x = npin["x"].astype(np.float32, copy=False)
    maps = []
    for c in cores:
        m = dict(shared)
        m["x"] = np.ascontiguousarray(x[NSEQ * c:NSEQ * (c + 1)].reshape(NTOK, D))
        maps.append(m)
    return maps


def kernel(**inputs):
    if "nc" not in _CACHE:
        _CACHE["nc"] = build_program()[0]
    nc = _CACHE["nc"]
    maps = make_in_maps(inputs)
    res = run_bass_kernel_spmd(nc, maps, core_ids=list(range(NCORES)))
    outs = [np.asarray(r["out"], dtype=np.float32).reshape(NSEQ, SEQ, D) for r in res.results]
    return np.concatenate(outs, axis=0)
```

```python
import math
from contextlib import ExitStack
import numpy as np
import concourse.bass as bass
import concourse.mybir as mybir
from concourse.bass_utils import run_bass_kernel_spmd

F32 = mybir.dt.float32
BF16 = mybir.dt.bfloat16
AF = mybir.ActivationFunctionType
ALU = mybir.AluOpType

N_DMA_SEMS = 40
NCORES = 8
SEQ = 2048
NSEQ = 2
NTOK = NSEQ * SEQ
D = 1024
DFF = 2816
EPS = 1e-6
NEG = -1.0e30


def _key(x):
    if isinstance(x, tuple):
        return x[0].tensor.name + ":" + str(x[1])
    if isinstance(x, str):
        return x
    return x.tensor.name


def _ap(x):
    return x[0] if isinstance(x, tuple) else x


def _isnum(v):
    return isinstance(v, (int, float))


class Sched:
    ENG = ("pe", "act", "dve", "pool", "sp")

    def __init__(self, nc, es):
        self.nc = nc
        self.es = es
        self.obj = {"pe": nc.tensor, "act": nc.scalar, "dve": nc.vector, "pool": nc.gpsimd, "sp": nc.sync}
        self.count = {e: 0 for e in self.ENG}
        self.sem = {e: es.enter_context(nc.semaphore("s_" + e)) for e in ("pe", "act", "dve", "pool")}
        self.dsem = [es.enter_context(nc.semaphore("d%d" % i)) for i in range(N_DMA_SEMS)]
        self.dval = [0] * N_DMA_SEMS
        self.dma_n = 0
        self.last_w = {}
        self.readers = {}
        self.waited = {e: {} for e in self.ENG}
        self.n_inst = 0
        self.uid = 0

    def sb(self, name, shape, dt):
        self.uid += 1
        return self.es.enter_context(self.nc.sbuf_tensor("%s_u%d" % (name, self.uid), list(shape), dt))

    def ps(self, name, shape, dt):
        return self.es.enter_context(self.nc.psum_tensor(name, list(shape), dt))

    def _deps(self, eng, reads, writes):
        toks = set()
        for k in reads:
            t = self.last_w.get(k)
            if t is not None:
                toks.add(t)
        for k in writes:
            t = self.last_w.get(k)
            if t is not None:
                toks.add(t)
            for t in self.readers.get(k, ()):
                toks.add(t)
        best = {}
        for (sk, v) in toks:
            if sk == eng and eng == "pe":
                continue
            if v > best.get(sk, 0):
                best[sk] = v
        waits = []
        w = self.waited[eng]
        for sk, v in best.items():
            if w.get(sk, 0) >= v:
                continue
            w[sk] = v
            waits.append((sk, v))
        return waits

    def _commit(self, tok, reads, writes):
        for k in reads:
            self.readers.setdefault(k, []).append(tok)
        for k in writes:
            self.last_w[k] = tok
            self.readers[k] = []

    def _emit(self, eng, waits, fn, kind):
        engine = self.obj[eng]
        for sk, v in waits:
            engine.wait_ge(self._semof(sk), v)
        if fn is None:
            return
        ins = fn(engine)
        if kind[0] == "c":
            ins.then_inc(self.sem[kind[1]], 1)
        else:
            ins.then_inc(self.dsem[kind[1]], 16)

    def op(self, eng, fn, reads, writes):
        rk = [_key(r) for r in reads]
        wk = [_key(w) for w in writes]
        waits = self._deps(eng, rk, wk)
        self.count[eng] += 1
        tok = (eng, self.count[eng])
        self._emit(eng, waits, fn, ("c", eng))
        self._commit(tok, rk, wk)
        self.n_inst += 1

    def dma(self, out, in_, q="sp", **kw):
        rk = [_key(in_)]
        wk = [_key(out)]
        slot = self.dma_n % N_DMA_SEMS
        self.dma_n += 1
        waits = self._deps(q, rk, wk)
        sk = ("d", slot)
        prev = self.dval[slot]
        if prev > 0 and self.waited[q].get(sk, 0) < prev:
            self.waited[q][sk] = prev
            waits.append((sk, prev))
        self.dval[slot] = prev + 16
        tok = (sk, prev + 16)
        o, i = _ap(out), _ap(in_)
        self._emit(q, waits, (lambda e: e.dma_start(out=o, in_=i, **kw)), ("d", slot))
        self._commit(tok, rk, wk)
        self.n_inst += 1

    def barrier(self):
        for e in self.ENG:
            waits = []
            w = self.waited[e]
            for e2 in ("pe", "act", "dve", "pool"):
                v = self.count[e2]
                if v == 0 or w.get(e2, 0) >= v:
                    continue
                if e2 == e and e == "pe":
                    continue
                w[e2] = v
                waits.append((e2, v))
            for s in range(N_DMA_SEMS):
                v = self.dval[s]
                sk = ("d", s)
                if v == 0 or w.get(sk, 0) >= v:
                    continue
                w[sk] = v
                waits.append((sk, v))
            if waits:
                self._emit(e, waits, None, None)
        self.last_w = {}
        self.readers = {}

    def _semof(self, sk):
        if isinstance(sk, tuple):
            return self.dsem[sk[1]]
        return self.sem[sk]

    def mm(self, out, lhsT, rhs, start=True, stop=True):
        o, l, r = _ap(out), _ap(lhsT), _ap(rhs)
        self.op("pe", lambda e: e.matmul(o, l, r, start=start, stop=stop), [lhsT, rhs], [out])

    def tr(self, out, in_, ident):
        o, i, d = _ap(out), _ap(in_), _ap(ident)
        self.op("pe", lambda e: e.transpose(o, i, d), [in_, ident], [out])

    def act(self, out, in_, func, bias=None, scale=None, accum_out=None):
        o, i = _ap(out), _ap(in_)
        kw = {}
        reads = [in_]
        writes = [out]
        if bias is not None:
            if _isnum(bias):
                kw["bias"] = bias
            else:
                kw["bias"] = _ap(bias)
                reads.append(bias)
        if scale is not None:
            if _isnum(scale):
                kw["scale"] = scale
            else:
                kw["scale"] = _ap(scale)
                reads.append(scale)
        if accum_out is not None:
            kw["accum_out"] = _ap(accum_out)
            writes.append(accum_out)
        self.op("act", lambda e: e.activation(o, i, func, **kw), reads, writes)

    def ts(self, out, in0, s1, s2, op0, op1=None, eng="dve"):
        o, i = _ap(out), _ap(in0)
        reads = [in0]
        a1, a2 = s1, s2
        if s1 is not None and not _isnum(s1):
            reads.append(s1)
            a1 = _ap(s1)
        if s2 is not None and not _isnum(s2):
            reads.append(s2)
            a2 = _ap(s2)
        if op1 is None:
            self.op(eng, lambda e: e.tensor_scalar(o, i, a1, None, op0), reads, [out])
        else:
            self.op(eng, lambda e: e.tensor_scalar(o, i, a1, a2, op0, op1), reads, [out])

    def ts_acc(self, out, in0, s1, init, op0, red_op, accum_out):
        o, i, ac = _ap(out), _ap(in0), _ap(accum_out)
        reads = [in0]
        a1 = s1
        if not _isnum(s1):
            reads.append(s1)
            a1 = _ap(s1)
        self.op("dve", lambda e: e.tensor_scalar(o, i, a1, init, op0, red_op, accum_out=ac), reads, [out, accum_out])

    def treduce(self, out, in_, op):
        o, i = _ap(out), _ap(in_)
        self.op("dve", lambda e: e.tensor_reduce(o, i, mybir.AxisListType.X, op), [in_], [out])

    def tt(self, out, in0, in1, op, eng="dve"):
        o, a, b = _ap(out), _ap(in0), _ap(in1)
        self.op(eng, lambda e: e.tensor_tensor(o, a, b, op), [in0, in1], [out])

    def stt(self, out, in0, scalar, in1, op0, op1):
        o, a, b = _ap(out), _ap(in0), _ap(in1)
        reads = [in0, in1]
        s = scalar
        if not _isnum(scalar):
            reads.append(scalar)
            s = _ap(scalar)
        self.op("dve", lambda e: e.scalar_tensor_tensor(o, a, s, b, op0, op1), reads, [out])

    def copy(self, out, in_, eng="dve"):
        o, i = _ap(out), _ap(in_)
        if eng == "act":
            self.op("act", lambda e: e.copy(o, i), [in_], [out])
        else:
            self.op(eng, lambda e: e.tensor_copy(o, i), [in_], [out])

    def memset(self, out, val, eng="dve"):
        o = _ap(out)
        self.op(eng, lambda e: e.memset(o, val), [], [out])

    def recip(self, out, in_):
        o, i = _ap(out), _ap(in_)
        self.op("dve", lambda e: e.reciprocal(o, i), [in_], [out])

    def max8(self, out, in_):
        o, i = _ap(out), _ap(in_)
        self.op("dve", lambda e: e.max(o, i), [in_], [out])

    def mrep(self, out, rep, vals, imm):
        o, r, v = _ap(out), _ap(rep), _ap(vals)
        self.op("dve", lambda e: e.match_replace(o, r, v, imm), [rep, vals], [out])

    def bnstats(self, out, in_):
        o, i = _ap(out), _ap(in_)
        self.op("dve", lambda e: e.bn_stats(o, i), [in_], [out])

    def bnaggr(self, out, in_):
        o, i = _ap(out), _ap(in_)
        self.op("dve", lambda e: e.bn_aggr(o, i), [in_], [out])


def _t5_bucket_np(dist):
    dist = np.asarray(dist, dtype=np.int64)
    max_exact = 16
    d_f = np.maximum(dist, 1).astype(np.float32)
    large = max_exact + (np.log(d_f / np.float32(max_exact)) / np.float32(math.log(128 / max_exact))
                         * np.float32(32 - max_exact)).astype(np.int32)
    large = np.minimum(large, 31)
    return np.where(dist < max_exact, dist, large).astype(np.int64)


def host_constants():
    c = {}
    r = np.arange(128)
    c["c_ident"] = np.eye(128, dtype=np.float32)
    c["c_tri"] = (r[:, None] <= r[None, :]).astype(np.float32)
    c["c_ones"] = np.ones((128, 128), np.float32)
    c["c_negd"] = np.where(r[None, :] <= r[:, None], 0.0, NEG).astype(np.float32)
    half = 64
    freqs = (10000.0 ** (-np.linspace(0.0, 1.0, half, dtype=np.float32))).astype(np.float32)
    pos = np.arange(SEQ, dtype=np.float32)
    ang = (pos[None, :] * freqs[:, None]).astype(np.float32)
    cos = np.cos(ang).astype(np.float32)
    sin = np.sin(ang).astype(np.float32)
    c["c_cos"] = np.concatenate([cos, cos], 0)
    c["c_sin"] = np.concatenate([sin, sin], 0)
    h = np.arange(4, dtype=np.float64)
    lg = np.log1p(-np.exp2(-5.0 - h))
    rr = np.arange(128, dtype=np.float64)
    c["c_rete"] = np.exp(-(rr[:, None] + 1.0) * lg[None, :]).astype(np.float32)
    c["c_retrow"] = (np.exp((rr[:, None] + 1.0) * lg[None, :]) * 128 ** -0.5).astype(np.float32)
    c["ret_g"] = [float(np.exp(128.0 * v)) for v in lg]
    return c


def bias_index():
    t = np.arange(128)
    out = np.zeros((128, 2, 128), np.int64)
    for off in range(2):
        d = t[:, None] - t[None, :] + 128 * off
        out[:, off, :] = _t5_bucket_np(np.maximum(d, 0))
    return out


OFF = dict(m_q=0, m_k=256, m_v=512, m_i=1024, m_f=1028, m_o=1032, a_cq=1544, a_kidx=1800,
           a_widx=1864, a_ckv=1872, r_q=2000, r_k=2512, r_v=3024, r_g=3536, g_m=4048, g_a=5072,
           g_r=6096)
NFM = 5632
NTM = 2512
ZS_CQ, ZS_CKV, ZS_KIDX, ZS_WIDX, ZS_MI, ZS_MF, ZS_N = 0, 256, 384, 448, 456, 460, 464


def _rot_pre(w):
    w4 = w.reshape(w.shape[0], 4, 2, 64)
    return np.ascontiguousarray(w4[:, :, ::-1, :]).reshape(w.shape[0], 512)


def prep_layer_weights(inp, l):
    w = inp["w_in"][l]
    sl = lambda name, n: w[:, OFF[name]:OFF[name] + n]
    rq, rk = sl("r_q", 512), sl("r_k", 512)
    w_fm = np.concatenate([sl("m_q", 256), sl("m_k", 256), rq, _rot_pre(rq), rk, _rot_pre(rk),
                           sl("g_m", 1024), sl("g_a", 1024), sl("g_r", 1024)], axis=1)
    w_tm = np.concatenate([sl("m_v", 512), sl("m_o", 512), sl("r_v", 512), sl("r_g", 512),
                           sl("a_cq", 256), sl("a_ckv", 128), sl("a_kidx", 64), sl("a_widx", 8),
                           sl("m_i", 4), sl("m_f", 4)], axis=1)
    assert w_fm.shape[1] == NFM and w_tm.shape[1] == NTM
    rep = lambda v: np.ascontiguousarray(np.broadcast_to(np.asarray(v, np.float32).reshape(1, -1), (128, np.asarray(v).size)))
    colv = lambda v: np.ascontiguousarray(np.asarray(v, np.float32).reshape(-1, 128).T)
    d = {
        "w_fm": np.ascontiguousarray(w_fm), "w_tm": np.ascontiguousarray(w_tm),
        "n1g": colv(inp["norm1_g"][l]), "n2g": colv(inp["norm2_g"][l]),
        "mconv": np.ascontiguousarray(inp["m_conv"][l].reshape(4, 4, 128).transpose(2, 1, 0)),
        "mbias": rep(np.tile(np.concatenate([inp["m_ibias"][l], inp["m_fbias"][l]]), 16)),
        "mng": rep(inp["m_norm_g"][l]), "rng": rep(inp["r_norm_g"][l]),
        "aqg": colv(inp["a_qnorm_g"][l]),
        "akg": colv(np.concatenate([inp["a_kidx_g"][l], inp["a_kidx_g"][l]])),
        "akvg": rep(inp["a_kvnorm_g"][l]),
        "wuq": np.ascontiguousarray(inp["a_wuq"][l]), "wuqi": np.ascontiguousarray(inp["a_wuq_idx"][l]),
        "wuk": np.ascontiguousarray(inp["a_wuk"][l].reshape(4, 2, 64, 128).transpose(1, 2, 0, 3).reshape(128, 4, 128)),
        "wuv": np.ascontiguousarray(inp["a_wuv"][l].transpose(1, 0, 2)),
        "p_m": np.ascontiguousarray(inp["p_m"][l]), "p_a": np.ascontiguousarray(inp["p_a"][l]),
        "p_r": np.ascontiguousarray(inp["p_r"][l]), "w_out": np.ascontiguousarray(inp["w_out"][l]),
        "w_gate": np.ascontiguousarray(inp["w_gate"][l]), "w_up": np.ascontiguousarray(inp["w_up"][l]),
        "w_down": np.ascontiguousarray(inp["w_down"][l]),
    }
    return {"%s_%d" % (k, l): v for k, v in d.items()}


LAYER_SHAPES = {
    "w_fm": [D, NFM], "w_tm": [D, NTM], "n1g": [128, 8], "n2g": [128, 8], "mconv": [128, 4, 4],
    "mbias": [128, 128], "mng": [128, 512], "rng": [128, 512], "aqg": [128, 2], "akg": [128, 1],
    "akvg": [128, 128], "wuq": [256, 512], "wuqi": [256, 512], "wuk": [128, 4, 128],
    "wuv": [128, 8, 64], "p_m": [512, D], "p_a": [512, D], "p_r": [512, D], "w_out": [D, D],
    "w_gate": [D, DFF], "w_up": [D, DFF], "w_down": [DFF, D],
}
CONST_SHAPES = {"c_ident": [128, 128], "c_tri": [128, 128], "c_ones": [128, 128], "c_negd": [128, 128],
                "c_cos": [128, SEQ], "c_sin": [128, SEQ], "c_rete": [128, 4], "c_retrow": [128, 4],
                "biasn": [128, 8, 2, 128], "rb31": [128, 8], "fng": [128, D]}


class Prog:
    pass


def _scope(S):
    class _Sc:
        def __enter__(self_):
            self_.old = S.es
            self_.es = ExitStack()
            self_.es.__enter__()
            S.es = self_.es
            return self_

        def __exit__(self_, *a):
            S.barrier()
            S.es = self_.old
            return self_.es.__exit__(*a)
    return _Sc()


def load_cast(P, dst, src, K, N, gain=None, stg=None, engs=("dve", "act")):
    S = P.S
    kc = K // 128
    srcv = src.rearrange("(c p) n -> p c n", p=128)
    CH = stg[0].shape[1]
    for c in range(kc):
        for n0 in range(0, N, CH):
            n1 = min(N, n0 + CH)
            sbuf = stg[P.stg_i % len(stg)]
            eng = engs[P.stg_i % len(engs)]
            P.stg_i += 1
            S.dma(sbuf[:, 0:n1 - n0], srcv[:, c, n0:n1])
            if gain is None:
                S.copy(dst[:, c, n0:n1], sbuf[:, 0:n1 - n0], eng=eng)
            elif eng == "act":
                S.act(dst[:, c, n0:n1], sbuf[:, 0:n1 - n0], AF.Copy, scale=gain[:, c:c + 1])
            else:
                S.ts(dst[:, c, n0:n1], sbuf[:, 0:n1 - n0], gain[:, c:c + 1], None, ALU.mult, eng=eng)


def dma_chunks(S, out, in_, n, step):
    for a in range(0, n, step):
        b = min(n, a + step)
        S.dma(out[:, a:b], in_[:, a:b])


def rms_rstd(P, rs, ss, n):
    S = P.S
    S.ts(rs, ss, 1.0 / n, EPS, ALU.mult, ALU.add)
    S.act(rs, rs, AF.Sqrt)
    S.recip(rs, rs)


def norm_transpose(P, xt, hb, junk, ss, rs, hT, col0, ptb):
    S = P.S
    S.act(junk[:], xt[:], AF.Square, accum_out=ss[:])
    rms_rstd(P, rs[:], ss[:], D)
    S.ts(hb[:], xt[:], rs[:], None, ALU.mult)
    for k in range(8):
        S.tr(ptb[:, k * 128:(k + 1) * 128], hb[:, k * 128:(k + 1) * 128], P.identb[:])
    S.copy(hT[:, :, col0:col0 + 128], ptb[:, :].rearrange("p (k t) -> p k t", k=8), eng="act")


def stage_A1(P, l, x_in):
    S, G = P.S, P.G
    with _scope(S):
        Wfm = S.sb("Wfm", [128, 8, NFM], BF16)
        stg = [S.sb("stgA%d" % i, [128, 2048], F32) for i in range(3)]
        cos = S.sb("cos", [128, SEQ], F32)
        sin = S.sb("sin", [128, SEQ], F32)
        n1g = S.sb("n1g", [128, 8], F32)
        xb = [S.sb("xbA%d" % i, [128, D], F32) for i in range(2)]
        hb = S.sb("hbA", [128, D], BF16)
        junk = S.sb("junkA", [128, D], BF16)
        ss = S.sb("ssA", [128, 1], F32)
        rs = S.sb("rsA", [128, 1], F32)
        hT = S.sb("hTA", [128, 8, 512], BF16)
        ev32 = [S.sb("ev32A%d" % i, [128, 512], F32) for i in range(4)]
        evb = [S.sb("evbA%d" % i, [128, 512], BF16) for i in range(4)]
        S.dma(n1g[:], G["n1g_%d" % l])
        S.dma(cos[:], G["c_cos"])
        S.dma(sin[:], G["c_sin"])
        load_cast(P, Wfm, G["w_fm_%d" % l], D, NFM, gain=n1g, stg=stg)
        for c in range(8):
            for base in (1024, 2048):
                v = Wfm[:, c, base:base + 512].rearrange("p (h t j) -> p h t j", h=4, t=2)[:, :, 0, :]
                S.ts(v, v, -1.0, None, ALU.mult)
        hTd = G["hT_d"].rearrange("(k p) t -> p k t", p=128)
        e32 = 0
        eb = 0
        bank = 0
        for st in range(NTOK // 512):
            t0 = st * 512
            p0 = t0 % SEQ
            for ti in range(4):
                xt = xb[ti % 2]
                S.dma(xt[:], x_in[t0 + ti * 128:t0 + (ti + 1) * 128, :])
                norm_transpose(P, xt, hb, junk, ss, rs, hT, ti * 128, P.pbB[ti % 2])
            dma_chunks(S, hTd[:, :, t0:t0 + 512], hT, 8, 4)

            def proj(c):
                nonlocal bank
                ps = P.pbF[bank % 6]
                bank += 1
                for k in range(8):
                    S.mm(ps[:], Wfm[:, k, c * 128:(c + 1) * 128], hT[:, k, :], start=(k == 0), stop=(k == 7))
                return ps
            for c in range(4):
                ps = proj(c)
                o = ev32[e32 % 4]
                e32 += 1
                S.copy(o[:], ps[:], eng="act")
                S.dma(G["qkT"][c * 128:(c + 1) * 128, t0:t0 + 512], o[:])
            for which, dst in ((0, "rqT"), (1, "rkT")):
                for hh in range(4):
                    ca = 4 + which * 8 + hh
                    pa = proj(ca)
                    pbk = proj(ca + 4)
                    t1 = ev32[e32 % 4]
                    t2 = ev32[(e32 + 1) % 4]
                    e32 += 2
                    S.tt(t1[:], pa[:], cos[:, p0:p0 + 512], ALU.mult)
                    S.tt(t2[:], pbk[:], sin[:, p0:p0 + 512], ALU.mult)
                    ob = evb[eb % 4]
                    eb += 1
                    S.tt(ob[:], t1[:], t2[:], ALU.add, eng="pool")
                    S.dma(G[dst][hh * 128:(hh + 1) * 128, t0:t0 + 512], ob[:])
            for c in range(24):
                ps = proj(20 + c)
                ob = evb[eb % 4]
                eb += 1
                S.act(ob[:], ps[:], AF.Sigmoid)
                S.dma(G["gT"][c * 128:(c + 1) * 128, t0:t0 + 512], ob[:])


def stage_A2(P, l):
    S, G = P.S, P.G
    with _scope(S):
        Wtm = S.sb("Wtm", [128, 8, NTM], BF16)
        stg = [S.sb("stgB%d" % i, [128, 2048], F32) for i in range(3)]
        n1g = S.sb("n1gB", [128, 8], F32)
        hTs = [S.sb("hTB%d" % i, [128, 8, 512], BF16) for i in range(2)]
        evb = [S.sb("evbB%d" % i, [128, 512], BF16) for i in range(6)]
        ev32 = [S.sb("ev32B%d" % i, [128, 512], F32) for i in range(2)]
        S.dma(n1g[:], G["n1g_%d" % l])
        load_cast(P, Wtm, G["w_tm_%d" % l], D, NTM, gain=n1g, stg=stg)
        hTd = G["hT_d"].rearrange("(k p) t -> p k t", p=128)
        groups = [(0, 512, "zv", "copy"), (512, 512, "zo", "sig"), (1024, 512, "rv", "copy"),
                  (1536, 512, "rg", "silu"), (2048, ZS_N, "zs", "f32")]
        bank = 0
        eb = 0
        e32 = 0
        for st in range(NTOK // 512):
            t0 = st * 512
            hT = hTs[st % 2]
            dma_chunks(S, hT, hTd[:, :, t0:t0 + 512], 8, 4)
            for ti in range(4):
                r0 = t0 + ti * 128
                for (c0, n, dst, kind) in groups:
                    ps = P.pbF[bank % 6]
                    bank += 1
                    for k in range(8):
                        S.mm(ps[:, 0:n], hT[:, k, ti * 128:(ti + 1) * 128], Wtm[:, k, c0:c0 + n],
                             start=(k == 0), stop=(k == 7))
                    if kind == "f32":
                        o = ev32[e32 % 2]
                        e32 += 1
                        S.copy(o[:, 0:n], ps[:, 0:n], eng="dve")
                    else:
                        o = evb[eb % 6]
                        eb += 1
                        if kind == "copy":
                            S.copy(o[:, 0:n], ps[:, 0:n], eng="dve")
                        elif kind == "sig":
                            S.act(o[:, 0:n], ps[:, 0:n], AF.Sigmoid)
                        else:
                            S.act(o[:, 0:n], ps[:, 0:n], AF.Silu)
                    S.dma(G[dst][r0:r0 + 128, :], o[:, 0:n])


def ln_gate_out(P, pN, s2, hp, n, o32, gain_t, gate_all, hm, tmp):
    S = P.S
    st6, mv, t2, re = tmp
    for hh in range(2):
        S.bnstats(st6[:, hh, :], pN[:, hh, 0:128])
        S.bnaggr(mv[:, hh, :], st6[:, hh, :])
    S.tt(t2[:], s2, s2, ALU.mult)
    S.tt(t2[:], t2[:], mv[:, :, 1], ALU.mult)
    S.ts(t2[:], t2[:], EPS, None, ALU.add)
    S.act(t2[:], t2[:], AF.Sqrt)
    S.recip(t2[:], t2[:])
    S.tt(re[:], t2[:], s2, ALU.mult)
    for hh in range(2):
        S.ts(o32[:, hh * 128:(hh + 1) * 128], pN[:, hh, 0:128], mv[:, hh, 0:1], re[:, hh:hh + 1],
             ALU.subtract, ALU.mult)
    S.tt(o32[:], o32[:], gain_t[:, hp * 256:(hp + 1) * 256], ALU.mult, eng="pool")
    S.tt(hm[:, hp * 256:(hp + 1) * 256], o32[:], gate_all[:, n, hp * 256:(hp + 1) * 256], ALU.mult, eng="pool")


def out_transpose(P, hm, hmTs, dstT, tg, ptb):
    S = P.S
    for c in range(4):
        S.tr(ptb[:, c * 128:(c + 1) * 128], hm[:, c * 128:(c + 1) * 128], P.identb[:])
    S.copy(hmTs[:], ptb[:, 0:512].rearrange("p (c t) -> p c t", c=4), eng="act")
    S.dma(dstT.rearrange("(c p) t -> p c t", p=128)[:, :, tg:tg + 128], hmTs[:])


def stage_B(P, l):
    S, G = P.S, P.G
    NCH = SEQ // 128
    with _scope(S):
        qkraw = S.sb("qkraw", [128, 4, SEQ + 3], F32)
        cacc = [S.sb("cacc%d" % i, [128, SEQ], F32) for i in range(2)]
        qkb = S.sb("qkb", [128, 4, SEQ], BF16)
        qA = S.sb("qA", [128, 2, SEQ], BF16)
        qB = S.sb("qB", [128, 2, SEQ], BF16)
        mconv = S.sb("mconv", [128, 4, 4], F32)
        mbias = S.sb("mbias", [128, 128], F32)
        mng = S.sb("mng", [128, 512], F32)
        gi = S.sb("gi", [128, NCH, 8], F32)
        ax = S.sb("ax", [128, NCH, 4], F32)
        lf = S.sb("lf", [128, NCH, 4], F32)
        lia = S.sb("lia", [128, NCH, 4], F32)
        e_all = S.sb("e_all", [128, NCH, 4], F32)
        rowsc = S.sb("rowsc", [128, NCH, 4], F32)
        g_all = S.sb("g_all", [128, NCH, 4], F32)
        v_all = S.sb("v_all", [128, NCH, 512], BF16)
        mo_all = S.sb("mo_all", [128, NCH, 512], BF16)
        vt = S.sb("vt", [128, NCH, 4, 130], BF16)
        ktok = [S.sb("ktok%d" % i, [128, 256], BF16) for i in range(2)]
        scTm = [S.sb("scTm%d" % i, [128, 256], BF16) for i in range(2)]
        P32 = S.sb("P32", [128, 2, 129], F32)
        Cb = S.sb("Cb", [128, 2, 130], BF16)
        hm = [S.sb("hm%d" % i, [128, 512], BF16) for i in range(2)]
        hmTs = [S.sb("hmTs%d" % i, [128, 4, 128], BF16) for i in range(2)]
        o32 = [S.sb("o32%d" % i, [128, 256], F32) for i in range(4)]
        d1_r = [S.sb("d1_%d" % i, [128, 2], F32) for i in range(4)]
        s2_r = [S.sb("s2_%d" % i, [128, 2], F32) for i in range(4)]
        tmp_r = [(S.sb("st6_%d" % i, [128, 2, 6], F32), S.sb("mv_%d" % i, [128, 2, 2], F32),
                  S.sb("t2_%d" % i, [128, 2], F32), S.sb("re_%d" % i, [128, 2], F32)) for i in range(4)]
        S.dma(mconv[:], G["mconv_%d" % l])
        S.dma(mbias[:], G["mbias_%d" % l])
        S.dma(mng[:], G["mng_%d" % l])
        S.memset(qkraw[:, :, 0:3], 0.0)
        S.memset(qA[64:128, :, :], 0.0)
        S.memset(qB[0:64, :, :], 0.0)
        S.memset(vt[:], 0.0)
        bank = 0
        for s in range(NSEQ):
            tb = s * SEQ
            for c in range(4):
                S.dma(qkraw[:, c, 3:SEQ + 3], G["qkT"][c * 128:(c + 1) * 128, tb:tb + SEQ])
            for c in range(4):
                acc = cacc[c % 2]
                S.ts(acc[:], qkraw[:, c, 0:SEQ], mconv[:, c, 0:1], None, ALU.mult)
                for j in range(1, 4):
                    S.stt(acc[:], qkraw[:, c, j:j + SEQ], mconv[:, c, j:j + 1], acc[:], ALU.mult, ALU.add)
                if c < 2:
                    S.act(qA[0:64, c, :], acc[0:64, :], AF.Silu)
                    S.act(qB[64:128, c, :], acc[64:128, :], AF.Silu)
                else:
                    S.act(qkb[:, c, :], acc[:], AF.Silu)
            dma_chunks(S, gi, G["zs"][tb:tb + SEQ, ZS_MI:ZS_MI + 8].rearrange("(n p) c -> p n c", p=128), NCH, 4)
            gif = gi[:].rearrange("p n c -> p (n c)")
            S.tt(gif, gif, mbias[:], ALU.add)
            S.act(ax[:], gi[:, :, 4:8], AF.Abs)
            S.act(ax[:], ax[:], AF.Exp, scale=-1.0)
            S.act(ax[:], ax[:], AF.Ln, bias=1.0)
            S.ts(lf[:], gi[:, :, 4:8], 0.0, None, ALU.min)
            S.tt(lf[:], lf[:], ax[:], ALU.subtract)
            pg = P.pbF[bank % 6]
            bank += 1
            lff = lf[:].rearrange("p n c -> p (n c)")
            S.mm(pg[:, 0:64], P.tri32[:], lff)
            S.mm(pg[:, 64:128], P.ones32[:], lff)
            S.tt(lia[:], gi[:, :, 0:4], pg[:, 0:64].rearrange("p (n c) -> p n c", c=4), ALU.subtract)
            S.act(e_all[:], lia[:], AF.Exp)
            S.act(rowsc[:].rearrange("p n c -> p (n c)"), pg[:, 0:64], AF.Exp, bias=math.log(0.125))
            S.act(g_all[:].rearrange("p n c -> p (n c)"), pg[:, 64:128], AF.Exp)
            dma_chunks(S, v_all, G["zv"][tb:tb + SEQ, :].rearrange("(n p) c -> p n c", p=128), NCH, 4)
            dma_chunks(S, mo_all, G["zo"][tb:tb + SEQ, :].rearrange("(n p) c -> p n c", p=128), NCH, 4)
            for n in range(NCH):
                for h in range(4):
                    S.ts(vt[:, n, h, 0:128], v_all[:, n, h * 128:(h + 1) * 128], e_all[:, n, h:h + 1], None,
                         ALU.mult)
            S.copy(vt[:, :, :, 128], e_all[:])
            for n in range(NCH):
                cs = slice(n * 128, (n + 1) * 128)
                kt = ktok[n % 2]
                ptb = P.pbB[n % 2]
                for hc in range(2):
                    S.tr(ptb[:, hc * 128:(hc + 1) * 128], qkb[:, 2 + hc, cs], P.identb[:])
                S.copy(kt[:], ptb[:, 0:256], eng="act")
                hmc = hm[n % 2]
                for hp in range(2):
                    psc = P.pbF[bank % 6]
                    bank += 1
                    for hh in range(2):
                        qX = qA if hh == 0 else qB
                        S.mm(psc[:, hh * 128:(hh + 1) * 128], qkb[:, 2 + hp, cs], qX[:, hp, cs])
                    sm = scTm[hp]
                    S.tt(sm[:], psc[:, 0:256], P.trib2[:], ALU.mult)
                    pNb = P.pbF[bank % 6]
                    bank += 1
                    pN = pNb[:, 0:258].rearrange("p (a b) -> p a b", a=2)
                    for hh in range(2):
                        h = 2 * hp + hh
                        pr = slice(64 * hh, 64 * hh + 64)
                        S.mm(pN[:, hh, :], sm[:, hh * 128:(hh + 1) * 128], vt[:, n, h, 0:129], start=True, stop=(n == 0))
                        if n > 0:
                            qX = qA if hh == 0 else qB
                            S.mm(pN[:, hh, :], qX[:, hp, cs], Cb[:, hp, 0:129], start=False, stop=True)
                    if n < NCH - 1:
                        pUb = P.pbF[bank % 6]
                        bank += 1
                        pU = pUb[:, 0:260].rearrange("p (a b) -> p a b", a=2)
                        S.mm(pUb[:, 0:260], kt[:, hp * 128:(hp + 1) * 128],
                             vt[:, n, 2 * hp:2 * hp + 2, :].rearrange("p a b -> p (a b)"))
                        for hh in range(2):
                            h = 2 * hp + hh
                            pr = slice(64 * hh, 64 * hh + 64)
                            if n == 0:
                                S.copy(P32[pr, hp, :], pU[pr, hh, 0:129])
                            else:
                                S.stt(P32[pr, hp, :], P32[pr, hp, :], g_all[pr, n - 1, h:h + 1], pU[pr, hh, 0:129],
                                      ALU.mult, ALU.add)
                            S.ts(Cb[pr, hp, 0:129], P32[pr, hp, :], g_all[pr, n, h:h + 1], None, ALU.mult)
                    rsl = rowsc[:, n, 2 * hp:2 * hp + 2]
                    d1, s2, tmp = d1_r[(2 * n + hp) % 4], s2_r[(2 * n + hp) % 4], tmp_r[(2 * n + hp) % 4]
                    S.tt(d1[:], pN[:, :, 128], rsl, ALU.mult)
                    S.act(d1[:], d1[:], AF.Abs)
                    S.ts(d1[:], d1[:], 1.0, None, ALU.max)
                    S.recip(d1[:], d1[:])
                    S.tt(s2[:], d1[:], rsl, ALU.mult)
                    ln_gate_out(P, pN, s2[:], hp, n, o32[(2 * n + hp) % 4], mng, mo_all, hmc, tmp)
                out_transpose(P, hmc, hmTs[n % 2], G["hmT"], tb + n * 128, P.pbB[(n + 1) % 2])


def stage_D(P, l):
    S, G = P.S, P.G
    NCH = SEQ // 128
    gam = P.ret_g
    with _scope(S):
        rq = S.sb("rq", [128, 4, NTOK], BF16)
        rk = S.sb("rk", [128, 4, NTOK], BF16)
        rng = S.sb("rng", [128, 512], F32)
        rete = S.sb("rete", [128, 4], F32)
        retrow = S.sb("retrow", [128, 4], F32)
        v_all = S.sb("rv_all", [128, NCH, 512], BF16)
        rg_all = [S.sb("rg_all%d" % s, [128, NCH, 512], BF16) for s in range(NSEQ)]
        vt = [S.sb("rvt%d" % s, [128, NCH, 512], BF16) for s in range(NSEQ)]
        ktok = [[S.sb("rktok%d_%d" % (s, i), [128, 512], BF16) for i in range(2)] for s in range(NSEQ)]
        scTm = [[S.sb("rscTm%d_%d" % (s, i), [128, 256], BF16) for i in range(2)] for s in range(NSEQ)]
        P32 = [S.sb("rP32_%d" % s, [128, 4, 128], F32) for s in range(NSEQ)]
        Sb = [S.sb("rSb_%d" % s, [128, 4, 128], BF16) for s in range(NSEQ)]
        hm = [[S.sb("rhm%d_%d" % (s, i), [128, 512], BF16) for i in range(2)] for s in range(NSEQ)]
        hmTs = [[S.sb("rhmTs%d_%d" % (s, i), [128, 4, 128], BF16) for i in range(2)] for s in range(NSEQ)]
        o32 = [S.sb("ro32%d" % i, [128, 256], F32) for i in range(4)]
        tmp_r = [(S.sb("rst6_%d" % i, [128, 2, 6], F32), S.sb("rmv_%d" % i, [128, 2, 2], F32),
                  S.sb("rt2_%d" % i, [128, 2], F32), S.sb("rre_%d" % i, [128, 2], F32)) for i in range(4)]
        S.dma(rng[:], G["rng_%d" % l])
        S.dma(rete[:], G["c_rete"])
        S.dma(retrow[:], G["c_retrow"])
        bank = 0
        tcnt = 0
        ocnt = 0
        for s in range(NSEQ):
            tb = s * SEQ
            S.dma(rq[:, :, tb:tb + SEQ], G["rqT"].rearrange("(c p) t -> p c t", p=128)[:, :, tb:tb + SEQ])
            S.dma(rk[:, :, tb:tb + SEQ], G["rkT"].rearrange("(c p) t -> p c t", p=128)[:, :, tb:tb + SEQ])
            dma_chunks(S, v_all, G["rv"][tb:tb + SEQ, :].rearrange("(n p) c -> p n c", p=128), NCH, 4)
            dma_chunks(S, rg_all[s], G["rg"][tb:tb + SEQ, :].rearrange("(n p) c -> p n c", p=128), NCH, 4)
            for h in range(4):
                S.ts(vt[s][:, :, h * 128:(h + 1) * 128], v_all[:, :, h * 128:(h + 1) * 128], rete[:, h:h + 1], None,
                     ALU.mult)
        for n in range(NCH):
            for s in range(NSEQ):
                tb = s * SEQ
                cs = slice(tb + n * 128, tb + (n + 1) * 128)
                kt = ktok[s][n % 2]
                ptb = P.pbB[tcnt % 2]
                tcnt += 1
                for h in range(4):
                    S.tr(ptb[:, h * 128:(h + 1) * 128], rk[:, h, cs], P.identb[:])
                S.copy(kt[:], ptb[:, 0:512], eng="act")
                hmc = hm[s][n % 2]
                for hp in range(2):
                    psc = P.pbF[bank % 6]
                    bank += 1
                    for hh in range(2):
                        h = 2 * hp + hh
                        S.mm(psc[:, hh * 128:(hh + 1) * 128], rk[:, h, cs], rq[:, h, cs])
                    sm = scTm[s][hp]
                    S.tt(sm[:], psc[:, 0:256], P.trib2[:], ALU.mult)
                    pNb = P.pbF[bank % 6]
                    bank += 1
                    pN = pNb[:, 0:256].rearrange("p (a b) -> p a b", a=2)
                    for hh in range(2):
                        h = 2 * hp + hh
                        S.mm(pN[:, hh, :], sm[:, hh * 128:(hh + 1) * 128], vt[s][:, n, h * 128:(h + 1) * 128],
                             start=True, stop=(n == 0))
                        if n > 0:
                            S.mm(pN[:, hh, :], rq[:, h, cs], Sb[s][:, h, :], start=False, stop=True)
                    if n < NCH - 1:
                        pUb = P.pbF[bank % 6]
                        bank += 1
                        for hh in range(2):
                            h = 2 * hp + hh
                            S.mm(pUb[:, hh * 128:(hh + 1) * 128], kt[:, h * 128:(h + 1) * 128],
                                 vt[s][:, n, h * 128:(h + 1) * 128])
                        for hh in range(2):
                            h = 2 * hp + hh
                            pu = pUb[:, hh * 128:(hh + 1) * 128]
                            if n == 0:
                                S.copy(P32[s][:, h, :], pu)
                            else:
                                S.stt(P32[s][:, h, :], P32[s][:, h, :], gam[h], pu, ALU.mult, ALU.add)
                            S.ts(Sb[s][:, h, :], P32[s][:, h, :], gam[h], None, ALU.mult)
                    ri = ocnt % 4
                    ocnt += 1
                    ln_gate_out(P, pN, retrow[:, 2 * hp:2 * hp + 2], hp, n, o32[ri], rng, rg_all[s], hmc, tmp_r[ri])
                out_transpose(P, hmc, hmTs[s][n % 2], G["hrT"], tb + n * 128, P.pbB[tcnt % 2])
                tcnt += 1


def stage_C(P, l):
    S, G = P.S, P.G
    NT = SEQ // 128
    WSC = (8 ** -0.5) * (64 ** -0.5)
    with _scope(S):
        stg = [S.sb("stgC%d" % i, [128, 2048], F32) for i in range(2)]
        aqg = S.sb("aqg", [128, 2], F32)
        akg = S.sb("akg", [128, 1], F32)
        akvg = S.sb("akvg", [128, 128], F32)
        wuq = S.sb("wuq", [128, 2, 512], BF16)
        wuqi = S.sb("wuqi", [128, 2, 512], BF16)
        wuk = S.sb("wuk", [128, 1, 512], BF16)
        wuvz = S.sb("wuvz", [128, 8, 128], BF16)
        zt = [S.sb("ztC%d" % i, [128, ZS_N], F32) for i in range(4)]
        NR = 4
        junk_r = [S.sb("junkC%d" % i, [128, 256], BF16) for i in range(NR)]
        junk2_r = [S.sb("junk2C%d" % i, [128, 128], BF16) for i in range(NR)]
        ss_r = [S.sb("ssC%d" % i, [128, 2], F32) for i in range(NR)]
        rs_r = [S.sb("rsC%d" % i, [128, 2], F32) for i in range(NR)]
        st6_r = [S.sb("st6C%d" % i, [128, 6], F32) for i in range(NR)]
        mv_r = [S.sb("mvC%d" % i, [128, 2], F32) for i in range(NR)]
        cqn_r = [S.sb("cqn%d" % i, [128, 256], BF16) for i in range(NR)]
        kin_r = [S.sb("kin%d" % i, [128, 128], BF16) for i in range(NR)]
        ckn_r = [S.sb("ckn%d" % i, [128, 128], F32) for i in range(NR)]
        cqT = S.sb("cqT", [128, 2, SEQ], BF16)
        kidxT = S.sb("kidxT", [128, SEQ], BF16)
        ckv1 = S.sb("ckv1", [128, NT, 130], BF16)
        ckvT = S.sb("ckvT", [128, SEQ], BF16)
        qTa = S.sb("qTCa", [128, 4, 512], BF16)
        qTb = S.sb("qTCb", [128, 4, 512], BF16)
        qidxA = S.sb("qidxA", [128, 4, SEQ], BF16)
        qidxB = S.sb("qidxB", [128, 4, SEQ], BF16)
        qlatT = S.sb("qlatT", [128, 8, SEQ], BF16)
        wabs = S.sb("wabs", [128, NT, 8], F32)
        wsgn = S.sb("wsgn", [128, NT, 8], F32)
        acc2 = [S.sb("accC%d" % i, [128, SEQ], F32) for i in range(2)]
        NBIS = 20
        junkb = [S.sb("junkbC%d" % i, [128, SEQ], BF16) for i in range(2)]
        bis = [S.sb("bisC%d" % i, [128, 8], F32) for i in range(2)]
        dk = [S.sb("dkC%d" % i, [128, NBIS], F32) for i in range(2)]
        pw2 = S.sb("pw2C", [128, NBIS], F32)
        for k in range(NBIS):
            S.memset(pw2[:, k:k + 1], 2.0 ** -k, eng="pool")
        rbuf = [S.sb("rbufC%d" % i, [128, 512], F32) for i in range(3)]
        m8 = [S.sb("m8C%d" % i, [128, 8], F32) for i in range(2)]
        madd4 = [S.sb("maddC%d" % i, [128, SEQ], BF16) for i in range(4)]
        Eb = [S.sb("EbC%d" % i, [128, 512], BF16) for i in range(3)]
        otok = S.sb("otok", [128, 8, 128], BF16)
        olT = S.sb("olT", [128, 8, 128], BF16)
        haTs = [S.sb("haTs%d" % i, [128, 4, 128], BF16) for i in range(2)]
        rec = S.sb("recC", [128, 8], F32)
        S.dma(aqg[:], G["aqg_%d" % l])
        S.dma(akg[:], G["akg_%d" % l])
        S.dma(akvg[:], G["akvg_%d" % l])
        load_cast(P, wuq, G["wuq_%d" % l], 256, 512, gain=aqg, stg=stg)
        load_cast(P, wuqi, G["wuqi_%d" % l], 256, 512, gain=aqg, stg=stg)
        load_cast(P, wuk, G["wuk_%d" % l].rearrange("p a b -> p (a b)"), 128, 512, stg=stg)
        S.memset(wuvz[:], 0.0)
        sv = stg[P.stg_i % 2]
        P.stg_i += 1
        S.dma(sv[:, 0:512], G["wuv_%d" % l].rearrange("p a b -> p (a b)"))
        for h in range(8):
            S.copy(wuvz[:, h, (h % 2) * 64:(h % 2) * 64 + 64], sv[:, h * 64:(h + 1) * 64], eng="pool")
        S.memset(ckv1[:, :, 128:130], 1.0)
        S.memset(qTa[64:128, :, :], 0.0)
        S.memset(qTb[0:64, :, :], 0.0)
        S.memset(qidxA[64:128, :, :], 0.0)
        S.memset(qidxB[0:64, :, :], 0.0)
        bank = 0
        for s in range(NSEQ):
            tb = s * SEQ
            for i in range(NT):
                z = zt[i % 4]
                ri = i % NR
                junk, junk2, ss, rs, st6, mv = junk_r[ri], junk2_r[ri], ss_r[ri], rs_r[ri], st6_r[ri], mv_r[ri]
                cqn, kin, ckn = cqn_r[ri], kin_r[ri], ckn_r[ri]
                S.dma(z[:], G["zs"][tb + i * 128:tb + (i + 1) * 128, :])
                S.act(junk[:, 0:256], z[:, ZS_CQ:ZS_CQ + 256], AF.Square, accum_out=ss[:, 0:1])
                S.act(junk2[:, 0:128], z[:, ZS_CKV:ZS_CKV + 128], AF.Square, accum_out=ss[:, 1:2])
                S.ts(rs[:, 0:1], ss[:, 0:1], 1.0 / 256, EPS, ALU.mult, ALU.add)
                S.ts(rs[:, 1:2], ss[:, 1:2], 1.0 / 128, EPS, ALU.mult, ALU.add)
                S.act(rs[:], rs[:], AF.Sqrt)
                S.recip(rs[:], rs[:])
                S.ts(cqn[:], z[:, ZS_CQ:ZS_CQ + 256], rs[:, 0:1], None, ALU.mult)
                S.stt(ckn[:], z[:, ZS_CKV:ZS_CKV + 128], rs[:, 1:2], akvg[:], ALU.mult, ALU.mult)
                S.copy(ckv1[:, i, 0:128], ckn[:], eng="pool")
                S.bnstats(st6[:], z[:, ZS_KIDX:ZS_KIDX + 64])
                S.bnaggr(mv[:], st6[:])
                S.ts(mv[:, 1:2], mv[:, 1:2], EPS, None, ALU.add)
                S.act(mv[:, 1:2], mv[:, 1:2], AF.Sqrt)
                S.recip(mv[:, 1:2], mv[:, 1:2])
                S.ts(kin[:, 0:64], z[:, ZS_KIDX:ZS_KIDX + 64], mv[:, 0:1], mv[:, 1:2], ALU.subtract, ALU.mult)
                S.copy(kin[:, 64:128], kin[:, 0:64], eng="pool")
                S.act(wabs[:, i, :], z[:, ZS_WIDX:ZS_WIDX + 8], AF.Abs, scale=WSC)
                S.ts(wsgn[:, i, :], z[:, ZS_WIDX:ZS_WIDX + 8], 0.0, None, ALU.is_gt)
                S.ts(wsgn[:, i, :], wsgn[:, i, :], 2.0, -1.0, ALU.mult, ALU.add)
                ptb = P.pbB[i % 2]
                S.tr(ptb[:, 0:128], cqn[:, 0:128], P.identb[:])
                S.tr(ptb[:, 128:256], cqn[:, 128:256], P.identb[:])
                S.tr(ptb[:, 256:384], kin[:], P.identb[:])
                S.tr(ptb[:, 384:512], ckv1[:, i, 0:128], P.identb[:])
                cs = slice(i * 128, (i + 1) * 128)
                S.copy(cqT[:, :, cs], ptb[:, 0:256].rearrange("p (c t) -> p c t", c=2), eng="act")
                S.copy(kidxT[:, cs], ptb[:, 256:384], eng="act")
                S.ts(kidxT[:, cs], kidxT[:, cs], akg[:, 0:1], None, ALU.mult, eng="pool")
                S.copy(ckvT[:, cs], ptb[:, 384:512], eng="act")
            for b4 in range(SEQ // 512):
                ts_ = slice(b4 * 512, (b4 + 1) * 512)
                for c in range(4):
                    ps = P.pbF[bank % 6]
                    bank += 1
                    for k in range(2):
                        S.mm(ps[:], wuq[:, k, c * 128:(c + 1) * 128], cqT[:, k, ts_], start=(k == 0), stop=(k == 1))
                    S.copy(qTa[0:64, c, :], ps[0:64, :], eng="act")
                    S.copy(qTb[64:128, c, :], ps[64:128, :], eng="act")
                for c in range(4):
                    ps = P.pbF[bank % 6]
                    bank += 1
                    for k in range(2):
                        S.mm(ps[:], wuqi[:, k, c * 128:(c + 1) * 128], cqT[:, k, ts_], start=(k == 0), stop=(k == 1))
                    S.copy(qidxA[0:64, c, ts_], ps[0:64, :], eng="dve")
                    S.copy(qidxB[64:128, c, ts_], ps[64:128, :], eng="dve")
                for h in range(8):
                    qTx = qTa if h % 2 == 0 else qTb
                    ps = P.pbF[bank % 6]
                    bank += 1
                    S.mm(ps[:], wuk[:, 0, (h // 2) * 128:(h // 2 + 1) * 128], qTx[:, h // 2, :])
                    S.act(qlatT[:, h, ts_], ps[:], AF.Copy, scale=0.125)
            def idx_topk_pair(ia):
                nonlocal bank
                tiles = (ia, ia + 1)
                for i in tiles:
                    W = (i + 1) * 128
                    cs = slice(i * 128, (i + 1) * 128)
                    nb = (W + 511) // 512
                    acc = acc2[i % 2]
                    for b in range(nb):
                        w = min(512, W - b * 512)
                        ks = slice(b * 512, b * 512 + w)
                        for g in range(8):
                            qix = qidxA if g % 2 == 0 else qidxB
                            ps = P.pbF[bank % 4]
                            bank += 1
                            S.mm(ps[:, 0:w], qix[:, g // 2, cs], kidxT[:, ks])
                            r = rbuf[g % 3]
                            S.act(r[:, 0:w], ps[:, 0:w], AF.Relu, scale=wabs[:, i, g:g + 1])
                            if g == 0:
                                S.ts(acc[:, ks], r[:, 0:w], wsgn[:, i, 0:1], None, ALU.mult)
                            else:
                                S.stt(acc[:, ks], r[:, 0:w], wsgn[:, i, g:g + 1], acc[:, ks], ALU.mult, ALU.add)
                    S.tt(acc[:, cs], acc[:, cs], P.negd[:], ALU.add)
                if ia >= 2:
                    for i in tiles:
                        W = (i + 1) * 128
                        acc, bs = acc2[i % 2], bis[i % 2]
                        S.max8(m8[i % 2][:], acc[:, 0:W])
                        S.treduce(bs[:, 0:1], acc[:, 0:i * 128], ALU.min)
                        S.ts(bs[:, 1:2], m8[i % 2][:, 0:1], bs[:, 0:1], 0.5, ALU.subtract, ALU.mult)
                        S.ts(bs[:, 2:3], m8[i % 2][:, 0:1], bs[:, 0:1], 0.5, ALU.add, ALU.mult)
                        S.ts(dk[i % 2][:], pw2[:], bs[:, 1:2], None, ALU.mult)
                    for k in range(NBIS):
                        for i in tiles:
                            W = (i + 1) * 128
                            S.ts_acc(junkb[i % 2][:, 0:W], acc2[i % 2][:, 0:W], bis[i % 2][:, 2:3], 0.0,
                                     ALU.is_ge, ALU.add, bis[i % 2][:, 3:4])
                        for i in tiles:
                            bs = bis[i % 2]
                            S.ts(bs[:, 4:5], bs[:, 3:4], 255.5, -0.5, ALU.is_ge, ALU.add)
                        for i in tiles:
                            bs = bis[i % 2]
                            S.stt(bs[:, 2:3], bs[:, 4:5], dk[i % 2][:, k:k + 1], bs[:, 2:3], ALU.mult, ALU.add)
                    for i in tiles:
                        W = (i + 1) * 128
                        bs = bis[i % 2]
                        S.tt(bs[:, 5:6], bs[:, 2:3], dk[i % 2][:, NBIS - 1:NBIS], ALU.subtract)
                        S.ts(madd4[i % 4][:, 0:W], acc2[i % 2][:, 0:W], bs[:, 5:6], 1.0, ALU.is_ge, ALU.subtract)
                else:
                    for i in tiles:
                        W = (i + 1) * 128
                        S.ts(madd4[i % 4][:, 0:W], acc2[i % 2][:, 0:W], -1.0e29, 1.0, ALU.is_ge, ALU.subtract)

            def attn(i):
                nonlocal bank
                cs = slice(i * 128, (i + 1) * 128)
                madd = madd4[i % 4]
                for h in range(8):
                    hh = h % 2
                    pvbank = P.pbF[4 + (h // 2) % 2]
                    pv = pvbank[:, hh * 129:(hh + 1) * 129]
                    for bg in range((i + 4) // 4):
                        j0 = bg * 4
                        nj = min(4, i + 1 - j0)
                        pl = P.pbF[bank % 4]
                        bank += 1
                        for jj in range(nj):
                            j = j0 + jj
                            near = j >= i - 1
                            blk = pl[:, jj * 128:(jj + 1) * 128]
                            S.mm(blk, ckvT[:, j * 128:(j + 1) * 128], qlatT[:, h, cs], start=True, stop=False)
                            S.mm(blk, madd[:, j * 128:(j + 1) * 128], P.i30k[:], start=False, stop=(not near))
                            if near:
                                S.mm(blk, P.biasb[:, h, i - j, :], P.identb[:], start=False, stop=True)
                        E = Eb[(h * 4 + bg) % 3]
                        S.act(E[:, 0:nj * 128], pl[:, 0:nj * 128], AF.Exp)
                        for jj in range(nj):
                            j = j0 + jj
                            S.mm(pv, E[:, jj * 128:(jj + 1) * 128], ckv1[:, j, 0:129], start=(j == 0), stop=(j == i))
                    S.recip(rec[:, h:h + 1], pv[:, 128:129])
                    S.act(otok[:, h, :], pv[:, 0:128], AF.Copy, scale=rec[:, h:h + 1])
                ptb = P.pbB[i % 2]
                for h in range(8):
                    S.tr(ptb[:, h * 128:(h + 1) * 128], otok[:, h, :], P.identb[:])
                S.copy(olT[:], ptb[:, :].rearrange("p (h t) -> p h t", h=8), eng="act")
                ph = P.pbF[bank % 4]
                bank += 1
                for hc in range(4):
                    S.mm(ph[:, hc * 128:(hc + 1) * 128], wuvz[:, 2 * hc, :], olT[:, 2 * hc, :], start=True, stop=False)
                    S.mm(ph[:, hc * 128:(hc + 1) * 128], wuvz[:, 2 * hc + 1, :], olT[:, 2 * hc + 1, :], start=False, stop=True)
                hs = haTs[i % 2]
                S.copy(hs[:], ph[:].rearrange("p (c t) -> p c t", c=4), eng="act")
                S.dma(G["haT"].rearrange("(c p) t -> p c t", p=128)[:, :, tb + i * 128:tb + (i + 1) * 128], hs[:])

            idx_topk_pair(0)
            for pp in range(NT // 2):
                if pp + 1 < NT // 2:
                    idx_topk_pair(2 * pp + 2)
                attn(2 * pp)
                attn(2 * pp + 1)


def stage_E(P, l, x_in):
    S, G = P.S, P.G
    with _scope(S):
        stg = [S.sb("stgE%d" % i, [128, 1024], F32) for i in range(3)]
        pw = [S.sb("pwE%d" % i, [128, 4, D], BF16) for i in range(3)]
        wo = S.sb("woE", [128, 8, D], BF16)
        hin = [[S.sb("hinE%d_%d" % (b, i), [128, 4, 512], BF16) for i in range(2)] for b in range(3)]
        gin = [S.sb("ginE%d" % i, [128, 24, 512], BF16) for i in range(2)]
        y32 = S.sb("y32E", [128, 512], F32)
        t32 = [S.sb("t32E%d" % i, [128, 512], F32) for i in range(2)]
        yT = S.sb("yTE", [128, 8, 512], BF16)
        xb = [S.sb("xbE%d" % i, [128, D], F32) for i in range(2)]
        for b, nm in enumerate(("p_m", "p_a", "p_r")):
            load_cast(P, pw[b], G["%s_%d" % (nm, l)], 512, D, stg=stg)
        load_cast(P, wo, G["w_out_%d" % l], D, D, stg=stg)
        srcs = [G["hmT"], G["haT"], G["hrT"]]
        bank = 0
        for st in range(NTOK // 512):
            t0 = st * 512
            for b in range(3):
                S.dma(hin[b][st % 2][:], srcs[b].rearrange("(c p) t -> p c t", p=128)[:, :, t0:t0 + 512])
            gt = gin[st % 2]
            dma_chunks(S, gt, G["gT"].rearrange("(c p) t -> p c t", p=128)[:, :, t0:t0 + 512], 24, 4)
            for c in range(8):
                pss = []
                for b in range(3):
                    ps = P.pbF[bank % 6]
                    bank += 1
                    for k in range(4):
                        S.mm(ps[:], pw[b][:, k, c * 128:(c + 1) * 128], hin[b][st % 2][:, k, :],
                             start=(k == 0), stop=(k == 3))
                    pss.append(ps)
                S.tt(y32[:], pss[0][:], gt[:, c, :], ALU.mult)
                S.tt(t32[0][:], pss[1][:], gt[:, 8 + c, :], ALU.mult)
                S.tt(t32[1][:], pss[2][:], gt[:, 16 + c, :], ALU.mult)
                S.tt(y32[:], y32[:], t32[0][:], ALU.add, eng="pool")
                S.tt(yT[:, c, :], y32[:], t32[1][:], ALU.add, eng="pool")
            for ti in range(4):
                r0 = t0 + ti * 128
                xt = xb[ti % 2]
                S.dma(xt[:], x_in[r0:r0 + 128, :])
                for hf in range(2):
                    ps = P.pbF[bank % 6]
                    bank += 1
                    for k in range(8):
                        S.mm(ps[:], yT[:, k, ti * 128:(ti + 1) * 128], wo[:, k, hf * 512:(hf + 1) * 512],
                             start=(k == 0), stop=(k == 7))
                    S.tt(xt[:, hf * 512:(hf + 1) * 512], xt[:, hf * 512:(hf + 1) * 512], ps[:], ALU.add)
                S.dma(G["x1"][r0:r0 + 128, :], xt[:])


def stage_F(P, l, x_out, final):
    S, G = P.S, P.G
    ST = 256
    NF = DFF // 128
    with _scope(S):
        stg = [S.sb("stgF%d" % i, [128, 1024], F32) for i in range(3)]
        n2g = S.sb("n2g", [128, 8], F32)
        wg = S.sb("wgF", [128, 8, DFF], BF16)
        wu = S.sb("wuF", [128, 8, DFF], BF16)
        wd = S.sb("wdF", [128, NF, D], BF16)
        xb = [S.sb("xbF%d" % i, [128, D], F32) for i in range(2)]
        hb = S.sb("hbF", [128, D], BF16)
        junk = S.sb("junkF", [128, D], BF16)
        ss = S.sb("ssF", [128, 1], F32)
        rs = S.sb("rsF", [128, 1], F32)
        hT = S.sb("hTF", [128, 8, ST], BF16)
        sg = [S.sb("sgF%d" % i, [128, ST], F32) for i in range(2)]
        aT = S.sb("aTF", [128, NF, ST], BF16)
        fng = None
        if final:
            fng = S.sb("fngF", [128, D], F32)
            S.dma(fng[:], G["fng"])
        S.dma(n2g[:], G["n2g_%d" % l])
        load_cast(P, wg, G["w_gate_%d" % l], D, DFF, gain=n2g, stg=stg)
        load_cast(P, wu, G["w_up_%d" % l], D, DFF, gain=n2g, stg=stg)
        load_cast(P, wd, G["w_down_%d" % l], DFF, D, stg=stg)
        bank = 0
        for st in range(NTOK // ST):
            t0 = st * ST
            for ti in range(ST // 128):
                xt = xb[ti % 2]
                S.dma(xt[:], G["x1"][t0 + ti * 128:t0 + (ti + 1) * 128, :])
                norm_transpose(P, xt, hb, junk, ss, rs, hT, ti * 128, P.pbB[ti % 2])
            for f in range(NF):
                pg = P.pbF[bank % 6]
                pu = P.pbF[(bank + 1) % 6]
                bank += 2
                for k in range(8):
                    S.mm(pg[:, 0:ST], wg[:, k, f * 128:(f + 1) * 128], hT[:, k, :], start=(k == 0), stop=(k == 7))
                for k in range(8):
                    S.mm(pu[:, 0:ST], wu[:, k, f * 128:(f + 1) * 128], hT[:, k, :], start=(k == 0), stop=(k == 7))
                sgt = sg[f % 2]
                S.act(sgt[:], pg[:, 0:ST], AF.Silu)
                S.tt(aT[:, f, :], sgt[:], pu[:, 0:ST], ALU.mult)
            for ti in range(ST // 128):
                r0 = t0 + ti * 128
                xt = xb[ti % 2]
                S.dma(xt[:], G["x1"][r0:r0 + 128, :])
                for hf in range(2):
                    ps = P.pbF[bank % 6]
                    bank += 1
                    for f in range(NF):
                        S.mm(ps[:], aT[:, f, ti * 128:(ti + 1) * 128], wd[:, f, hf * 512:(hf + 1) * 512],
                             start=(f == 0), stop=(f == NF - 1))
                    S.tt(xt[:, hf * 512:(hf + 1) * 512], xt[:, hf * 512:(hf + 1) * 512], ps[:], ALU.add)
                if final:
                    S.act(junk[:], xt[:], AF.Square, accum_out=ss[:])
                    rms_rstd(P, rs[:], ss[:], D)
                    S.stt(xt[:], xt[:], rs[:, 0:1], fng[:], ALU.mult, ALU.mult)
                S.dma(x_out[r0:r0 + 128, :], xt[:])


SCRATCH = {
    "hT_d": ([D, NTOK], BF16), "qkT": ([512, NTOK], F32), "rqT": ([512, NTOK], BF16), "rkT": ([512, NTOK], BF16),
    "gT": ([3072, NTOK], BF16), "zv": ([NTOK, 512], BF16), "zo": ([NTOK, 512], BF16), "rv": ([NTOK, 512], BF16),
    "rg": ([NTOK, 512], BF16), "zs": ([NTOK, ZS_N], F32), "hmT": ([512, NTOK], BF16), "haT": ([512, NTOK], BF16),
    "hrT": ([512, NTOK], BF16), "x1": ([NTOK, D], F32), "xl": ([NTOK, D], F32),
}


def build_program(n_layers=2, dbg=(), stages="A1,A2,B,C,D,E,F"):
    stages = stages.split(",")
    nc = bass.Bass("TRN2", target_bir_lowering=False)
    P = Prog()
    G = {}
    P.G = G
    P.stg_i = 0
    G["x"] = nc.dram_tensor("x", [NTOK, D], F32, kind="ExternalInput").ap()
    for k, shp in CONST_SHAPES.items():
        G[k] = nc.dram_tensor(k, shp, F32, kind="ExternalInput").ap()
    for l in range(n_layers):
        for k, shp in LAYER_SHAPES.items():
            nm = "%s_%d" % (k, l)
            G[nm] = nc.dram_tensor(nm, shp, F32, kind="ExternalInput").ap()
    G["out"] = nc.dram_tensor("out", [NTOK, D], F32, kind="ExternalOutput").ap()
    for k, (shp, dt) in SCRATCH.items():
        G[k] = nc.dram_tensor(k, shp, dt, kind=("ExternalOutput" if k in dbg else "Internal")).ap()
    P.ret_g = host_constants()["ret_g"]
    with ExitStack() as es:
        S = Sched(nc, es)
        P.S = S
        P.pbF = [S.ps("pbF%d" % i, [128, 512], F32) for i in range(6)]
        P.pbB = [S.ps("pbB%d" % i, [128, 1024], BF16) for i in range(2)]
        P.identb = S.sb("identb", [128, 128], BF16)
        P.i30k = S.sb("i30k", [128, 128], BF16)
        P.trib2 = S.sb("trib2", [128, 256], BF16)
        P.tri32 = S.sb("tri32", [128, 128], F32)
        P.ones32 = S.sb("ones32", [128, 128], F32)
        P.negd = S.sb("negd", [128, 128], F32)
        P.biasb = S.sb("biasb", [128, 8, 2, 128], BF16)
        with _scope(S):
            c32 = S.sb("c32", [128, 128], F32)
            b32 = S.sb("b32", [128, 8, 2, 128], F32)
            rb31 = S.sb("rb31", [128, 8], F32)
            S.dma(c32[:], G["c_ident"])
            S.copy(P.identb[:], c32[:])
            S.ts(P.i30k[:], c32[:], 30000.0, None, ALU.mult)
            S.dma(P.tri32[:], G["c_tri"])
            S.copy(P.trib2[:, 0:128], P.tri32[:])
            S.copy(P.trib2[:, 128:256], P.tri32[:])
            S.dma(P.ones32[:], G["c_ones"])
            S.dma(P.negd[:], G["c_negd"])
            S.dma(b32[:], G["biasn"])
            S.dma(rb31[:], G["rb31"])
            for h in range(8):
                S.ts(P.biasb[:, h, :, :], b32[:, h, :, :], rb31[:, h:h + 1], None, ALU.subtract)
        x_in = G["x"]
        for l in range(n_layers):
            last = (l == n_layers - 1)
            x_out = G["out"] if last else G["xl"]
            if "A1" in stages:
                stage_A1(P, l, x_in)
            if "A2" in stages:
                stage_A2(P, l)
            if "B" in stages:
                stage_B(P, l)
            if "D" in stages:
                stage_D(P, l)
            if "C" in stages:
                stage_C(P, l)
            if "E" in stages:
                stage_E(P, l, x_in)
            if "F" in stages:
                stage_F(P, l, x_out, last)
            x_in = x_out
        S.barrier()
        P.n_inst = S.n_inst
    return nc, P


_CACHE = {}


def make_in_maps(inputs, n_layers=2, cores=range(NCORES)):
    hc = host_constants()
    shared = {k: np.ascontiguousarray(v, dtype=np.float32) for k, v in hc.items() if k.startswith("c_")}
    rel_bias = np.asarray(inputs["rel_bias"], np.float32)
    bi = bias_index()
    biasn = rel_bias[bi]
    shared["biasn"] = np.ascontiguousarray(biasn.transpose(0, 3, 1, 2))
    shared["rb31"] = np.ascontiguousarray(np.broadcast_to(rel_bias[31][None, :], (128, 8)))
    shared["fng"] = np.ascontiguousarray(np.broadcast_to(np.asarray(inputs["final_norm_g"], np.float32)[None, :], (128, D)))
    npin = {k: np.asarray(v) for k, v in inputs.items()}
    for l in range(n_layers):
        shared.update(prep_layer_weights(npin, l))
    x = npin["x"].astype(np.float32, copy=False)
    maps = []
    for c in cores:
        m = dict(shared)
        m["x"] = np.ascontiguousarray(x[NSEQ * c:NSEQ * (c + 1)].reshape(NTOK, D))
        maps.append(m)
    return maps


def kernel(**inputs):
    if "nc" not in _CACHE:
        _CACHE["nc"] = build_program()[0]
    nc = _CACHE["nc"]
    maps = make_in_maps(inputs)
    res = run_bass_kernel_spmd(nc, maps, core_ids=list(range(NCORES)))
    outs = [np.asarray(r["out"], dtype=np.float32).reshape(NSEQ, SEQ, D) for r in res.results]
    return np.concatenate(outs, axis=0)
```

```python
import math
from contextlib import ExitStack
import numpy as np
import concourse.bass as bass
import concourse.mybir as mybir
from concourse.bass_utils import run_bass_kernel_spmd

F32 = mybir.dt.float32
BF16 = mybir.dt.bfloat16
AF = mybir.ActivationFunctionType
ALU = mybir.AluOpType

N_DMA_SEMS = 40
NCORES = 8
SEQ = 2048
NSEQ = 2
NTOK = NSEQ * SEQ
D = 1024
DFF = 2816
EPS = 1e-6
NEG = -1.0e30


def _key(x):
    if isinstance(x, tuple):
        return x[0].tensor.name + ":" + str(x[1])
    if isinstance(x, str):
        return x
    return x.tensor.name


def _ap(x):
    return x[0] if isinstance(x, tuple) else x


def _isnum(v):
    return isinstance(v, (int, float))


class Sched:
    ENG = ("pe", "act", "dve", "pool", "sp")

    def __init__(self, nc, es):
        self.nc = nc
        self.es = es
        self.obj = {"pe": nc.tensor, "act": nc.scalar, "dve": nc.vector, "pool": nc.gpsimd, "sp": nc.sync}
        self.count = {e: 0 for e in self.ENG}
        self.sem = {e: es.enter_context(nc.semaphore("s_" + e)) for e in ("pe", "act", "dve", "pool")}
        self.dsem = [es.enter_context(nc.semaphore("d%d" % i)) for i in range(N_DMA_SEMS)]
        self.dval = [0] * N_DMA_SEMS
        self.dma_n = 0
        self.last_w = {}
        self.readers = {}
        self.waited = {e: {} for e in self.ENG}
        self.n_inst = 0
        self.uid = 0

    def sb(self, name, shape, dt):
        self.uid += 1
        return self.es.enter_context(self.nc.sbuf_tensor("%s_u%d" % (name, self.uid), list(shape), dt))

    def ps(self, name, shape, dt):
        return self.es.enter_context(self.nc.psum_tensor(name, list(shape), dt))

    def _deps(self, eng, reads, writes):
        toks = set()
        for k in reads:
            t = self.last_w.get(k)
            if t is not None:
                toks.add(t)
        for k in writes:
            t = self.last_w.get(k)
            if t is not None:
                toks.add(t)
            for t in self.readers.get(k, ()):
                toks.add(t)
        best = {}
        for (sk, v) in toks:
            if sk == eng and eng == "pe":
                continue
            if v > best.get(sk, 0):
                best[sk] = v
        waits = []
        w = self.waited[eng]
        for sk, v in best.items():
            if w.get(sk, 0) >= v:
                continue
            w[sk] = v
            waits.append((sk, v))
        return waits

    def _commit(self, tok, reads, writes):
        for k in reads:
            self.readers.setdefault(k, []).append(tok)
        for k in writes:
            self.last_w[k] = tok
            self.readers[k] = []

    def _emit(self, eng, waits, fn, kind):
        engine = self.obj[eng]
        for sk, v in waits:
            engine.wait_ge(self._semof(sk), v)
        if fn is None:
            return
        ins = fn(engine)
        if kind[0] == "c":
            ins.then_inc(self.sem[kind[1]], 1)
        else:
            ins.then_inc(self.dsem[kind[1]], 16)

    def op(self, eng, fn, reads, writes):
        rk = [_key(r) for r in reads]
        wk = [_key(w) for w in writes]
        waits = self._deps(eng, rk, wk)
        self.count[eng] += 1
        tok = (eng, self.count[eng])
        self._emit(eng, waits, fn, ("c", eng))
        self._commit(tok, rk, wk)
        self.n_inst += 1

    def dma(self, out, in_, q="sp", **kw):
        rk = [_key(in_)]
        wk = [_key(out)]
        slot = self.dma_n % N_DMA_SEMS
        self.dma_n += 1
        waits = self._deps(q, rk, wk)
        sk = ("d", slot)
        prev = self.dval[slot]
        if prev > 0 and self.waited[q].get(sk, 0) < prev:
            self.waited[q][sk] = prev
            waits.append((sk, prev))
        self.dval[slot] = prev + 16
        tok = (sk, prev + 16)
        o, i = _ap(out), _ap(in_)
        self._emit(q, waits, (lambda e: e.dma_start(out=o, in_=i, **kw)), ("d", slot))
        self._commit(tok, rk, wk)
        self.n_inst += 1

    def barrier(self):
        for e in self.ENG:
            waits = []
            w = self.waited[e]
            for e2 in ("pe", "act", "dve", "pool"):
                v = self.count[e2]
                if v == 0 or w.get(e2, 0) >= v:
                    continue
                if e2 == e and e == "pe":
                    continue
                w[e2] = v
                waits.append((e2, v))
            for s in range(N_DMA_SEMS):
                v = self.dval[s]
                sk = ("d", s)
                if v == 0 or w.get(sk, 0) >= v:
                    continue
                w[sk] = v
                waits.append((sk, v))
            if waits:
                self._emit(e, waits, None, None)
        self.last_w = {}
        self.readers = {}

    def _semof(self, sk):
        if isinstance(sk, tuple):
            return self.dsem[sk[1]]
        return self.sem[sk]

    def mm(self, out, lhsT, rhs, start=True, stop=True):
        o, l, r = _ap(out), _ap(lhsT), _ap(rhs)
        self.op("pe", lambda e: e.matmul(o, l, r, start=start, stop=stop), [lhsT, rhs], [out])

    def tr(self, out, in_, ident):
        o, i, d = _ap(out), _ap(in_), _ap(ident)
        self.op("pe", lambda e: e.transpose(o, i, d), [in_, ident], [out])

    def act(self, out, in_, func, bias=None, scale=None, accum_out=None):
        o, i = _ap(out), _ap(in_)
        kw = {}
        reads = [in_]
        writes = [out]
        if bias is not None:
            if _isnum(bias):
                kw["bias"] = bias
            else:
                kw["bias"] = _ap(bias)
                reads.append(bias)
        if scale is not None:
            if _isnum(scale):
                kw["scale"] = scale
            else:
                kw["scale"] = _ap(scale)
                reads.append(scale)
        if accum_out is not None:
            kw["accum_out"] = _ap(accum_out)
            writes.append(accum_out)
        self.op("act", lambda e: e.activation(o, i, func, **kw), reads, writes)

    def ts(self, out, in0, s1, s2, op0, op1=None, eng="dve"):
        o, i = _ap(out), _ap(in0)
        reads = [in0]
        a1, a2 = s1, s2
        if s1 is not None and not _isnum(s1):
            reads.append(s1)
            a1 = _ap(s1)
        if s2 is not None and not _isnum(s2):
            reads.append(s2)
            a2 = _ap(s2)
        if op1 is None:
            self.op(eng, lambda e: e.tensor_scalar(o, i, a1, None, op0), reads, [out])
        else:
            self.op(eng, lambda e: e.tensor_scalar(o, i, a1, a2, op0, op1), reads, [out])

    def ts_acc(self, out, in0, s1, init, op0, red_op, accum_out):
        o, i, ac = _ap(out), _ap(in0), _ap(accum_out)
        reads = [in0]
        a1 = s1
        if not _isnum(s1):
            reads.append(s1)
            a1 = _ap(s1)
        self.op("dve", lambda e: e.tensor_scalar(o, i, a1, init, op0, red_op, accum_out=ac), reads, [out, accum_out])

    def treduce(self, out, in_, op):
        o, i = _ap(out), _ap(in_)
        self.op("dve", lambda e: e.tensor_reduce(o, i, mybir.AxisListType.X, op), [in_], [out])

    def tt(self, out, in0, in1, op, eng="dve"):
        o, a, b = _ap(out), _ap(in0), _ap(in1)
        self.op(eng, lambda e: e.tensor_tensor(o, a, b, op), [in0, in1], [out])

    def stt(self, out, in0, scalar, in1, op0, op1):
        o, a, b = _ap(out), _ap(in0), _ap(in1)
        reads = [in0, in1]
        s = scalar
        if not _isnum(scalar):
            reads.append(scalar)
            s = _ap(scalar)
        self.op("dve", lambda e: e.scalar_tensor_tensor(o, a, s, b, op0, op1), reads, [out])

    def copy(self, out, in_, eng="dve"):
        o, i = _ap(out), _ap(in_)
        if eng == "act":
            self.op("act", lambda e: e.copy(o, i), [in_], [out])
        else:
            self.op(eng, lambda e: e.tensor_copy(o, i), [in_], [out])

    def memset(self, out, val, eng="dve"):
        o = _ap(out)
        self.op(eng, lambda e: e.memset(o, val), [], [out])

    def recip(self, out, in_):
        o, i = _ap(out), _ap(in_)
        self.op("dve", lambda e: e.reciprocal(o, i), [in_], [out])

    def max8(self, out, in_):
        o, i = _ap(out), _ap(in_)
        self.op("dve", lambda e: e.max(o, i), [in_], [out])

    def mrep(self, out, rep, vals, imm):
        o, r, v = _ap(out), _ap(rep), _ap(vals)
        self.op("dve", lambda e: e.match_replace(o, r, v, imm), [rep, vals], [out])

    def bnstats(self, out, in_):
        o, i = _ap(out), _ap(in_)
        self.op("dve", lambda e: e.bn_stats(o, i), [in_], [out])

    def bnaggr(self, out, in_):
        o, i = _ap(out), _ap(in_)
        self.op("dve", lambda e: e.bn_aggr(o, i), [in_], [out])


def _t5_bucket_np(dist):
    dist = np.asarray(dist, dtype=np.int64)
    max_exact = 16
    d_f = np.maximum(dist, 1).astype(np.float32)
    large = max_exact + (np.log(d_f / np.float32(max_exact)) / np.float32(math.log(128 / max_exact))
                         * np.float32(32 - max_exact)).astype(np.int32)
    large = np.minimum(large, 31)
    return np.where(dist < max_exact, dist, large).astype(np.int64)


def host_constants():
    c = {}
    r = np.arange(128)
    c["c_ident"] = np.eye(128, dtype=np.float32)
    c["c_tri"] = (r[:, None] <= r[None, :]).astype(np.float32)
    c["c_ones"] = np.ones((128, 128), np.float32)
    c["c_negd"] = np.where(r[None, :] <= r[:, None], 0.0, NEG).astype(np.float32)
    half = 64
    freqs = (10000.0 ** (-np.linspace(0.0, 1.0, half, dtype=np.float32))).astype(np.float32)
    pos = np.arange(SEQ, dtype=np.float32)
    ang = (pos[None, :] * freqs[:, None]).astype(np.float32)
    cos = np.cos(ang).astype(np.float32)
    sin = np.sin(ang).astype(np.float32)
    c["c_cos"] = np.concatenate([cos, cos], 0)
    c["c_sin"] = np.concatenate([sin, sin], 0)
    h = np.arange(4, dtype=np.float64)
    lg = np.log1p(-np.exp2(-5.0 - h))
    rr = np.arange(128, dtype=np.float64)
    c["c_rete"] = np.exp(-(rr[:, None] + 1.0) * lg[None, :]).astype(np.float32)
    c["c_retrow"] = (np.exp((rr[:, None] + 1.0) * lg[None, :]) * 128 ** -0.5).astype(np.float32)
    c["ret_g"] = [float(np.exp(128.0 * v)) for v in lg]
    return c


def bias_index():
    t = np.arange(128)
    out = np.zeros((128, 2, 128), np.int64)
    for off in range(2):
        d = t[:, None] - t[None, :] + 128 * off
        out[:, off, :] = _t5_bucket_np(np.maximum(d, 0))
    return out


OFF = dict(m_q=0, m_k=256, m_v=512, m_i=1024, m_f=1028, m_o=1032, a_cq=1544, a_kidx=1800,
           a_widx=1864, a_ckv=1872, r_q=2000, r_k=2512, r_v=3024, r_g=3536, g_m=4048, g_a=5072,
           g_r=6096)
NFM = 5632
NTM = 2512
ZS_CQ, ZS_CKV, ZS_KIDX, ZS_WIDX, ZS_MI, ZS_MF, ZS_N = 0, 256, 384, 448, 456, 460, 464


def _rot_pre(w):
    w4 = w.reshape(w.shape[0], 4, 2, 64)
    return np.ascontiguousarray(w4[:, :, ::-1, :]).reshape(w.shape[0], 512)


def prep_layer_weights(inp, l):
    w = inp["w_in"][l]
    sl = lambda name, n: w[:, OFF[name]:OFF[name] + n]
    rq, rk = sl("r_q", 512), sl("r_k", 512)
    w_fm = np.concatenate([sl("m_q", 256), sl("m_k", 256), rq, _rot_pre(rq), rk, _rot_pre(rk),
                           sl("g_m", 1024), sl("g_a", 1024), sl("g_r", 1024)], axis=1)
    w_tm = np.concatenate([sl("m_v", 512), sl("m_o", 512), sl("r_v", 512), sl("r_g", 512),
                           sl("a_cq", 256), sl("a_ckv", 128), sl("a_kidx", 64), sl("a_widx", 8),
                           sl("m_i", 4), sl("m_f", 4)], axis=1)
    assert w_fm.shape[1] == NFM and w_tm.shape[1] == NTM
    rep = lambda v: np.ascontiguousarray(np.broadcast_to(np.asarray(v, np.float32).reshape(1, -1), (128, np.asarray(v).size)))
    colv = lambda v: np.ascontiguousarray(np.asarray(v, np.float32).reshape(-1, 128).T)
    d = {
        "w_fm": np.ascontiguousarray(w_fm), "w_tm": np.ascontiguousarray(w_tm),
        "n1g": colv(inp["norm1_g"][l]), "n2g": colv(inp["norm2_g"][l]),
        "mconv": np.ascontiguousarray(inp["m_conv"][l].reshape(4, 4, 128).transpose(2, 1, 0)),
        "mbias": rep(np.tile(np.concatenate([inp["m_ibias"][l], inp["m_fbias"][l]]), 16)),
        "mng": rep(inp["m_norm_g"][l]), "rng": rep(inp["r_norm_g"][l]),
        "aqg": colv(inp["a_qnorm_g"][l]),
        "akg": colv(np.concatenate([inp["a_kidx_g"][l], inp["a_kidx_g"][l]])),
        "akvg": rep(inp["a_kvnorm_g"][l]),
        "wuq": np.ascontiguousarray(inp["a_wuq"][l]), "wuqi": np.ascontiguousarray(inp["a_wuq_idx"][l]),
        "wuk": np.ascontiguousarray(inp["a_wuk"][l].reshape(4, 2, 64, 128).transpose(1, 2, 0, 3).reshape(128, 4, 128)),
        "wuv": np.ascontiguousarray(inp["a_wuv"][l].transpose(1, 0, 2)),
        "p_m": np.ascontiguousarray(inp["p_m"][l]), "p_a": np.ascontiguousarray(inp["p_a"][l]),
        "p_r": np.ascontiguousarray(inp["p_r"][l]), "w_out": np.ascontiguousarray(inp["w_out"][l]),
        "w_gate": np.ascontiguousarray(inp["w_gate"][l]), "w_up": np.ascontiguousarray(inp["w_up"][l]),
        "w_down": np.ascontiguousarray(inp["w_down"][l]),
    }
    return {"%s_%d" % (k, l): v for k, v in d.items()}


LAYER_SHAPES = {
    "w_fm": [D, NFM], "w_tm": [D, NTM], "n1g": [128, 8], "n2g": [128, 8], "mconv": [128, 4, 4],
    "mbias": [128, 128], "mng": [128, 512], "rng": [128, 512], "aqg": [128, 2], "akg": [128, 1],
    "akvg": [128, 128], "wuq": [256, 512], "wuqi": [256, 512], "wuk": [128, 4, 128],
    "wuv": [128, 8, 64], "p_m": [512, D], "p_a": [512, D], "p_r": [512, D], "w_out": [D, D],
    "w_gate": [D, DFF], "w_up": [D, DFF], "w_down": [DFF, D],
}
CONST_SHAPES = {"c_ident": [128, 128], "c_tri": [128, 128], "c_ones": [128, 128], "c_negd": [128, 128],
                "c_cos": [128, SEQ], "c_sin": [128, SEQ], "c_rete": [128, 4], "c_retrow": [128, 4],
                "biasn": [128, 8, 2, 128], "rb31": [128, 8], "fng": [128, D]}


class Prog:
    pass


def _scope(S):
    class _Sc:
        def __enter__(self_):
            self_.old = S.es
            self_.es = ExitStack()
            self_.es.__enter__()
            S.es = self_.es
            return self_

        def __exit__(self_, *a):
            S.barrier()
            S.es = self_.old
            return self_.es.__exit__(*a)
    return _Sc()


def load_cast(P, dst, src, K, N, gain=None, stg=None, engs=("dve", "act"), colkey=None, cols=None):
    S = P.S
    kc = K // 128
    srcv = src.rearrange("(c p) n -> p c n", p=128)
    CH = stg[0].shape[1]
    lo, hi = (0, N) if cols is None else cols
    for n0 in range(lo, hi, CH):
        n1 = min(hi, n0 + CH)
        for c in range(kc):
            sbuf = stg[P.stg_i % len(stg)]
            eng = engs[P.stg_i % len(engs)]
            P.stg_i += 1
            S.dma(sbuf[:, 0:n1 - n0], srcv[:, c, n0:n1])
            d = dst[:, c, n0:n1]
            if colkey is not None:
                d = (d, "cb%d" % (n0 // colkey))
            if gain is None:
                S.copy(d, sbuf[:, 0:n1 - n0], eng=eng)
            elif eng == "act":
                S.act(d, sbuf[:, 0:n1 - n0], AF.Copy, scale=gain[:, c:c + 1])
            else:
                S.ts(d, sbuf[:, 0:n1 - n0], gain[:, c:c + 1], None, ALU.mult, eng=eng)


def dma_chunks(S, out, in_, n, step):
    for a in range(0, n, step):
        b = min(n, a + step)
        S.dma(out[:, a:b], in_[:, a:b])


def rms_rstd(P, rs, ss, n):
    S = P.S
    S.ts(rs, ss, 1.0 / n, EPS, ALU.mult, ALU.add)
    S.act(rs, rs, AF.Sqrt)
    S.recip(rs, rs)


def norm_transpose(P, xt, hb, junk, ss, rs, hT, col0, ptb):
    S = P.S
    S.act(junk[:], xt[:], AF.Square, accum_out=ss[:])
    rms_rstd(P, rs[:], ss[:], D)
    S.ts(hb[:], xt[:], rs[:], None, ALU.mult)
    for k in range(8):
        S.tr(ptb[:, k * 128:(k + 1) * 128], hb[:, k * 128:(k + 1) * 128], P.identb[:])
    S.copy(hT[:, :, col0:col0 + 128], ptb[:, :].rearrange("p (k t) -> p k t", k=8), eng="act")


def stage_A1(P, l, x_in):
    S, G = P.S, P.G
    with _scope(S):
        Wfm = S.sb("Wfm", [128, 8, NFM], BF16)
        stg = [S.sb("stgA%d" % i, [128, 2048], F32) for i in range(3)]
        cos = S.sb("cos", [128, SEQ], F32)
        sin = S.sb("sin", [128, SEQ], F32)
        n1g = S.sb("n1g", [128, 8], F32)
        xb = [S.sb("xbA%d" % i, [128, D], F32) for i in range(2)]
        hb = S.sb("hbA", [128, D], BF16)
        junk = S.sb("junkA", [128, D], BF16)
        ss = S.sb("ssA", [128, 1], F32)
        rs = S.sb("rsA", [128, 1], F32)
        hT = S.sb("hTA", [128, 8, 512], BF16)
        ev32 = [S.sb("ev32A%d" % i, [128, 512], F32) for i in range(4)]
        evb = [S.sb("evbA%d" % i, [128, 512], BF16) for i in range(4)]
        S.dma(n1g[:], G["n1g_%d" % l])
        S.dma(cos[:], G["c_cos"])
        S.dma(sin[:], G["c_sin"])
        hTd = G["hT_d"].rearrange("(k p) t -> p k t", p=128)

        def prep(st):
            t0 = st * 512
            for ti in range(4):
                xt = xb[ti % 2]
                S.dma(xt[:], x_in[t0 + ti * 128:t0 + (ti + 1) * 128, :])
                norm_transpose(P, xt, hb, junk, ss, rs, hT, ti * 128, P.pbB[ti % 2])
            dma_chunks(S, hTd[:, :, t0:t0 + 512], hT, 8, 4)

        prep(0)
        load_cast(P, Wfm, G["w_fm_%d" % l], D, NFM, gain=n1g, stg=stg, colkey=2048)
        for c in range(8):
            for base in (1024, 2048):
                v = Wfm[:, c, base:base + 512].rearrange("p (h t j) -> p h t j", h=4, t=2)[:, :, 0, :]
                vk = (v, "cb%d" % (base // 2048))
                S.ts(vk, vk, -1.0, None, ALU.mult)
        e32 = 0
        eb = 0
        bank = 0
        for st in range(NTOK // 512):
            t0 = st * 512
            p0 = t0 % SEQ
            if st > 0:
                prep(st)

            def proj(c):
                nonlocal bank
                ps = P.pbF[bank % 6]
                bank += 1
                for k in range(8):
                    S.mm(ps[:], (Wfm[:, k, c * 128:(c + 1) * 128], "cb%d" % (c * 128 // 2048)), hT[:, k, :],
                         start=(k == 0), stop=(k == 7))
                return ps
            for c in range(4):
                ps = proj(c)
                o = ev32[e32 % 4]
                e32 += 1
                S.copy(o[:], ps[:], eng="act")
                S.dma(G["qkT"][c * 128:(c + 1) * 128, t0:t0 + 512], o[:])
            for which, dst in ((0, "rqT"), (1, "rkT")):
                for hh in range(4):
                    ca = 4 + which * 8 + hh
                    pa = proj(ca)
                    pbk = proj(ca + 4)
                    t1 = ev32[e32 % 4]
                    t2 = ev32[(e32 + 1) % 4]
                    e32 += 2
                    S.tt(t1[:], pa[:], cos[:, p0:p0 + 512], ALU.mult)
                    S.tt(t2[:], pbk[:], sin[:, p0:p0 + 512], ALU.mult)
                    ob = evb[eb % 4]
                    eb += 1
                    S.tt(ob[:], t1[:], t2[:], ALU.add, eng="pool")
                    S.dma(G[dst][hh * 128:(hh + 1) * 128, t0:t0 + 512], ob[:])
            for c in range(24):
                ps = proj(20 + c)
                ob = evb[eb % 4]
                eb += 1
                S.act(ob[:], ps[:], AF.Sigmoid)
                S.dma(G["gT"][c * 128:(c + 1) * 128, t0:t0 + 512], ob[:])


def stage_A2(P, l):
    S, G = P.S, P.G
    with _scope(S):
        Wtm = S.sb("Wtm", [128, 8, NTM], BF16)
        stg = [S.sb("stgB%d" % i, [128, 2048], F32) for i in range(3)]
        n1g = S.sb("n1gB", [128, 8], F32)
        hTs = [S.sb("hTB%d" % i, [128, 8, 512], BF16) for i in range(2)]
        evb = [S.sb("evbB%d" % i, [128, 512], BF16) for i in range(6)]
        ev32 = [S.sb("ev32B%d" % i, [128, 512], F32) for i in range(2)]
        S.dma(n1g[:], G["n1g_%d" % l])
        load_cast(P, Wtm, G["w_tm_%d" % l], D, NTM, gain=n1g, stg=stg)
        hTd = G["hT_d"].rearrange("(k p) t -> p k t", p=128)
        groups = [(0, 512, "zv", "copy"), (512, 512, "zo", "sig"), (1024, 512, "rv", "copy"),
                  (1536, 512, "rg", "silu"), (2048, ZS_N, "zs", "f32")]
        bank = 0
        eb = 0
        e32 = 0
        for st in range(NTOK // 512):
            t0 = st * 512
            hT = hTs[st % 2]
            dma_chunks(S, hT, hTd[:, :, t0:t0 + 512], 8, 4)
            for ti in range(4):
                r0 = t0 + ti * 128
                for (c0, n, dst, kind) in groups:
                    ps = P.pbF[bank % 6]
                    bank += 1
                    for k in range(8):
                        S.mm(ps[:, 0:n], hT[:, k, ti * 128:(ti + 1) * 128], Wtm[:, k, c0:c0 + n],
                             start=(k == 0), stop=(k == 7))
                    if kind == "f32":
                        o = ev32[e32 % 2]
                        e32 += 1
                        S.copy(o[:, 0:n], ps[:, 0:n], eng="dve")
                    else:
                        o = evb[eb % 6]
                        eb += 1
                        if kind == "copy":
                            S.copy(o[:, 0:n], ps[:, 0:n], eng="dve")
                        elif kind == "sig":
                            S.act(o[:, 0:n], ps[:, 0:n], AF.Sigmoid)
                        else:
                            S.act(o[:, 0:n], ps[:, 0:n], AF.Silu)
                    S.dma(G[dst][r0:r0 + 128, :], o[:, 0:n])


def ln_gate_out(P, pN, s2, hp, n, o32, gain_t, gate_all, hm, tmp):
    S = P.S
    st6, mv, t2, re = tmp
    for hh in range(2):
        S.bnstats(st6[:, hh, :], pN[:, hh, 0:128])
        S.bnaggr(mv[:, hh, :], st6[:, hh, :])
    S.tt(t2[:], s2, s2, ALU.mult)
    S.tt(t2[:], t2[:], mv[:, :, 1], ALU.mult)
    S.ts(t2[:], t2[:], EPS, None, ALU.add)
    S.act(t2[:], t2[:], AF.Sqrt)
    S.recip(t2[:], t2[:])
    S.tt(re[:], t2[:], s2, ALU.mult)
    for hh in range(2):
        S.ts(o32[:, hh * 128:(hh + 1) * 128], pN[:, hh, 0:128], mv[:, hh, 0:1], re[:, hh:hh + 1],
             ALU.subtract, ALU.mult)
    S.tt(o32[:], o32[:], gain_t[:, hp * 256:(hp + 1) * 256], ALU.mult, eng="pool")
    S.tt(hm[:, hp * 256:(hp + 1) * 256], o32[:], gate_all[:, n, hp * 256:(hp + 1) * 256], ALU.mult, eng="pool")


def out_transpose(P, hm, hmTs, dstT, tg, ptb):
    S = P.S
    for c in range(4):
        S.tr(ptb[:, c * 128:(c + 1) * 128], hm[:, c * 128:(c + 1) * 128], P.identb[:])
    S.copy(hmTs[:], ptb[:, 0:512].rearrange("p (c t) -> p c t", c=4), eng="act")
    S.dma(dstT.rearrange("(c p) t -> p c t", p=128)[:, :, tg:tg + 128], hmTs[:])


def stage_B(P, l):
    S, G = P.S, P.G
    NCH = SEQ // 128
    with _scope(S):
        qkraw = S.sb("qkraw", [128, 4, SEQ + 3], F32)
        cacc = [S.sb("cacc%d" % i, [128, SEQ], F32) for i in range(2)]
        qkb = S.sb("qkb", [128, 4, SEQ], BF16)
        qA = S.sb("qA", [128, 2, SEQ], BF16)
        qB = S.sb("qB", [128, 2, SEQ], BF16)
        mconv = S.sb("mconv", [128, 4, 4], F32)
        mbias = S.sb("mbias", [128, 128], F32)
        mng = S.sb("mng", [128, 512], F32)
        gi = S.sb("gi", [128, NCH, 8], F32)
        ax = S.sb("ax", [128, NCH, 4], F32)
        lf = S.sb("lf", [128, NCH, 4], F32)
        lia = S.sb("lia", [128, NCH, 4], F32)
        e_all = S.sb("e_all", [128, NCH, 4], F32)
        rowsc = S.sb("rowsc", [128, NCH, 4], F32)
        g_all = S.sb("g_all", [128, NCH, 4], F32)
        v_all = S.sb("v_all", [128, NCH, 512], BF16)
        mo_all = S.sb("mo_all", [128, NCH, 512], BF16)
        vt = S.sb("vt", [128, NCH, 4, 130], BF16)
        ktok = [S.sb("ktok%d" % i, [128, 256], BF16) for i in range(2)]
        scTm = [S.sb("scTm%d" % i, [128, 256], BF16) for i in range(2)]
        P32 = S.sb("P32", [128, 2, 129], F32)
        Cb = S.sb("Cb", [128, 2, 130], BF16)
        hm = [S.sb("hm%d" % i, [128, 512], BF16) for i in range(2)]
        hmTs = [S.sb("hmTs%d" % i, [128, 4, 128], BF16) for i in range(2)]
        o32 = [S.sb("o32%d" % i, [128, 256], F32) for i in range(4)]
        d1_r = [S.sb("d1_%d" % i, [128, 2], F32) for i in range(4)]
        s2_r = [S.sb("s2_%d" % i, [128, 2], F32) for i in range(4)]
        tmp_r = [(S.sb("st6_%d" % i, [128, 2, 6], F32), S.sb("mv_%d" % i, [128, 2, 2], F32),
                  S.sb("t2_%d" % i, [128, 2], F32), S.sb("re_%d" % i, [128, 2], F32)) for i in range(4)]
        S.dma(mconv[:], G["mconv_%d" % l])
        S.dma(mbias[:], G["mbias_%d" % l])
        S.dma(mng[:], G["mng_%d" % l])
        S.memset(qkraw[:, :, 0:3], 0.0)
        S.memset(qA[64:128, :, :], 0.0)
        S.memset(qB[0:64, :, :], 0.0)
        S.memset(vt[:], 0.0)
        bank = 0
        for s in range(NSEQ):
            tb = s * SEQ
            for c in range(4):
                S.dma(qkraw[:, c, 3:SEQ + 3], G["qkT"][c * 128:(c + 1) * 128, tb:tb + SEQ])
            for c in range(4):
                acc = cacc[c % 2]
                S.ts(acc[:], qkraw[:, c, 0:SEQ], mconv[:, c, 0:1], None, ALU.mult)
                for j in range(1, 4):
                    S.stt(acc[:], qkraw[:, c, j:j + SEQ], mconv[:, c, j:j + 1], acc[:], ALU.mult, ALU.add)
                if c < 2:
                    S.act(qA[0:64, c, :], acc[0:64, :], AF.Silu)
                    S.act(qB[64:128, c, :], acc[64:128, :], AF.Silu)
                else:
                    S.act(qkb[:, c, :], acc[:], AF.Silu)
            dma_chunks(S, gi, G["zs"][tb:tb + SEQ, ZS_MI:ZS_MI + 8].rearrange("(n p) c -> p n c", p=128), NCH, 4)
            gif = gi[:].rearrange("p n c -> p (n c)")
            S.tt(gif, gif, mbias[:], ALU.add)
            S.act(ax[:], gi[:, :, 4:8], AF.Abs)
            S.act(ax[:], ax[:], AF.Exp, scale=-1.0)
            S.act(ax[:], ax[:], AF.Ln, bias=1.0)
            S.ts(lf[:], gi[:, :, 4:8], 0.0, None, ALU.min)
            S.tt(lf[:], lf[:], ax[:], ALU.subtract)
            pg = P.pbF[bank % 6]
            bank += 1
            lff = lf[:].rearrange("p n c -> p (n c)")
            S.mm(pg[:, 0:64], P.tri32[:], lff)
            S.mm(pg[:, 64:128], P.ones32[:], lff)
            S.tt(lia[:], gi[:, :, 0:4], pg[:, 0:64].rearrange("p (n c) -> p n c", c=4), ALU.subtract)
            S.act(e_all[:], lia[:], AF.Exp)
            S.act(rowsc[:].rearrange("p n c -> p (n c)"), pg[:, 0:64], AF.Exp, bias=math.log(0.125))
            S.act(g_all[:].rearrange("p n c -> p (n c)"), pg[:, 64:128], AF.Exp)
            dma_chunks(S, v_all, G["zv"][tb:tb + SEQ, :].rearrange("(n p) c -> p n c", p=128), NCH, 4)
            dma_chunks(S, mo_all, G["zo"][tb:tb + SEQ, :].rearrange("(n p) c -> p n c", p=128), NCH, 4)
            for n in range(NCH):
                for h in range(4):
                    S.ts(vt[:, n, h, 0:128], v_all[:, n, h * 128:(h + 1) * 128], e_all[:, n, h:h + 1], None,
                         ALU.mult)
            S.copy(vt[:, :, :, 128], e_all[:])
            pend = None
            for n in range(NCH):
                cs = slice(n * 128, (n + 1) * 128)
                kt = ktok[n % 2]
                ptb = P.pbB[n % 2]
                for hc in range(2):
                    S.tr(ptb[:, hc * 128:(hc + 1) * 128], qkb[:, 2 + hc, cs], P.identb[:])
                S.copy(kt[:], ptb[:, 0:256], eng="act")
                hmc = hm[n % 2]
                for hp in range(2):
                    psc = P.pbF[bank % 6]
                    bank += 1
                    for hh in range(2):
                        qX = qA if hh == 0 else qB
                        S.mm(psc[:, hh * 128:(hh + 1) * 128], qkb[:, 2 + hp, cs], qX[:, hp, cs])
                    sm = scTm[hp]
                    S.tt(sm[:], psc[:, 0:256], P.trib2[:], ALU.mult)
                    pNb = P.pbF[bank % 6]
                    bank += 1
                    pN = pNb[:, 0:258].rearrange("p (a b) -> p a b", a=2)
                    for hh in range(2):
                        h = 2 * hp + hh
                        pr = slice(64 * hh, 64 * hh + 64)
                        S.mm(pN[:, hh, :], sm[:, hh * 128:(hh + 1) * 128], vt[:, n, h, 0:129], start=True, stop=(n == 0))
                        if n > 0:
                            qX = qA if hh == 0 else qB
                            S.mm(pN[:, hh, :], qX[:, hp, cs], Cb[:, hp, 0:129], start=False, stop=True)
                    if n < NCH - 1:
                        pUb = P.pbF[bank % 6]
                        bank += 1
                        pU = pUb[:, 0:260].rearrange("p (a b) -> p a b", a=2)
                        S.mm(pUb[:, 0:260], kt[:, hp * 128:(hp + 1) * 128],
                             vt[:, n, 2 * hp:2 * hp + 2, :].rearrange("p a b -> p (a b)"))
                        for hh in range(2):
                            h = 2 * hp + hh
                            pr = slice(64 * hh, 64 * hh + 64)
                            if n == 0:
                                S.copy(P32[pr, hp, :], pU[pr, hh, 0:129])
                            else:
                                S.stt(P32[pr, hp, :], P32[pr, hp, :], g_all[pr, n - 1, h:h + 1], pU[pr, hh, 0:129],
                                      ALU.mult, ALU.add)
                            S.ts(Cb[pr, hp, 0:129], P32[pr, hp, :], g_all[pr, n, h:h + 1], None, ALU.mult)
                    rsl = rowsc[:, n, 2 * hp:2 * hp + 2]
                    d1, s2, tmp = d1_r[(2 * n + hp) % 4], s2_r[(2 * n + hp) % 4], tmp_r[(2 * n + hp) % 4]
                    S.tt(d1[:], pN[:, :, 128], rsl, ALU.mult)
                    S.act(d1[:], d1[:], AF.Abs)
                    S.ts(d1[:], d1[:], 1.0, None, ALU.max)
                    S.recip(d1[:], d1[:])
                    S.tt(s2[:], d1[:], rsl, ALU.mult)
                    ln_gate_out(P, pN, s2[:], hp, n, o32[(2 * n + hp) % 4], mng, mo_all, hmc, tmp)
                if pend is not None:
                    out_transpose(P, pend[0], pend[1], G["hmT"], pend[2], P.pbB[(n + 1) % 2])
                pend = (hmc, hmTs[n % 2], tb + n * 128)
            out_transpose(P, pend[0], pend[1], G["hmT"], pend[2], P.pbB[0])
            pend = None


def stage_D(P, l):
    S, G = P.S, P.G
    NCH = SEQ // 128
    gam = P.ret_g
    with _scope(S):
        rq = S.sb("rq", [128, 4, NTOK], BF16)
        rk = S.sb("rk", [128, 4, NTOK], BF16)
        rng = S.sb("rng", [128, 512], F32)
        rete = S.sb("rete", [128, 4], F32)
        retrow = S.sb("retrow", [128, 4], F32)
        v_all = S.sb("rv_all", [128, NCH, 512], BF16)
        rg_all = [S.sb("rg_all%d" % s, [128, NCH, 512], BF16) for s in range(NSEQ)]
        vt = [S.sb("rvt%d" % s, [128, NCH, 512], BF16) for s in range(NSEQ)]
        ktok = [[S.sb("rktok%d_%d" % (s, i), [128, 512], BF16) for i in range(2)] for s in range(NSEQ)]
        scTm = [[S.sb("rscTm%d_%d" % (s, i), [128, 256], BF16) for i in range(2)] for s in range(NSEQ)]
        P32 = [S.sb("rP32_%d" % s, [128, 4, 128], F32) for s in range(NSEQ)]
        Sb = [S.sb("rSb_%d" % s, [128, 4, 128], BF16) for s in range(NSEQ)]
        hm = [[S.sb("rhm%d_%d" % (s, i), [128, 512], BF16) for i in range(2)] for s in range(NSEQ)]
        hmTs = [[S.sb("rhmTs%d_%d" % (s, i), [128, 4, 128], BF16) for i in range(2)] for s in range(NSEQ)]
        o32 = [S.sb("ro32%d" % i, [128, 256], F32) for i in range(4)]
        tmp_r = [(S.sb("rst6_%d" % i, [128, 2, 6], F32), S.sb("rmv_%d" % i, [128, 2, 2], F32),
                  S.sb("rt2_%d" % i, [128, 2], F32), S.sb("rre_%d" % i, [128, 2], F32)) for i in range(4)]
        S.dma(rng[:], G["rng_%d" % l])
        S.dma(rete[:], G["c_rete"])
        S.dma(retrow[:], G["c_retrow"])
        bank = 0
        tcnt = 0
        ocnt = 0
        pend = None
        for s in range(NSEQ):
            tb = s * SEQ
            S.dma(rq[:, :, tb:tb + SEQ], G["rqT"].rearrange("(c p) t -> p c t", p=128)[:, :, tb:tb + SEQ])
            S.dma(rk[:, :, tb:tb + SEQ], G["rkT"].rearrange("(c p) t -> p c t", p=128)[:, :, tb:tb + SEQ])
            dma_chunks(S, v_all, G["rv"][tb:tb + SEQ, :].rearrange("(n p) c -> p n c", p=128), NCH, 4)
            dma_chunks(S, rg_all[s], G["rg"][tb:tb + SEQ, :].rearrange("(n p) c -> p n c", p=128), NCH, 4)
            for h in range(4):
                S.ts(vt[s][:, :, h * 128:(h + 1) * 128], v_all[:, :, h * 128:(h + 1) * 128], rete[:, h:h + 1], None,
                     ALU.mult)
        for n in range(NCH):
            for s in range(NSEQ):
                tb = s * SEQ
                cs = slice(tb + n * 128, tb + (n + 1) * 128)
                kt = ktok[s][n % 2]
                ptb = P.pbB[tcnt % 2]
                tcnt += 1
                for h in range(4):
                    S.tr(ptb[:, h * 128:(h + 1) * 128], rk[:, h, cs], P.identb[:])
                S.copy(kt[:], ptb[:, 0:512], eng="act")
                hmc = hm[s][n % 2]
                for hp in range(2):
                    psc = P.pbF[bank % 6]
                    bank += 1
                    for hh in range(2):
                        h = 2 * hp + hh
                        S.mm(psc[:, hh * 128:(hh + 1) * 128], rk[:, h, cs], rq[:, h, cs])
                    sm = scTm[s][hp]
                    S.tt(sm[:], psc[:, 0:256], P.trib2[:], ALU.mult)
                    pNb = P.pbF[bank % 6]
                    bank += 1
                    pN = pNb[:, 0:256].rearrange("p (a b) -> p a b", a=2)
                    for hh in range(2):
                        h = 2 * hp + hh
                        S.mm(pN[:, hh, :], sm[:, hh * 128:(hh + 1) * 128], vt[s][:, n, h * 128:(h + 1) * 128],
                             start=True, stop=(n == 0))
                        if n > 0:
                            S.mm(pN[:, hh, :], rq[:, h, cs], Sb[s][:, h, :], start=False, stop=True)
                    if n < NCH - 1:
                        pUb = P.pbF[bank % 6]
                        bank += 1
                        for hh in range(2):
                            h = 2 * hp + hh
                            S.mm(pUb[:, hh * 128:(hh + 1) * 128], kt[:, h * 128:(h + 1) * 128],
                                 vt[s][:, n, h * 128:(h + 1) * 128])
                        for hh in range(2):
                            h = 2 * hp + hh
                            pu = pUb[:, hh * 128:(hh + 1) * 128]
                            if n == 0:
                                S.copy(P32[s][:, h, :], pu)
                            else:
                                S.stt(P32[s][:, h, :], P32[s][:, h, :], gam[h], pu, ALU.mult, ALU.add)
                            S.ts(Sb[s][:, h, :], P32[s][:, h, :], gam[h], None, ALU.mult)
                    ri = ocnt % 4
                    ocnt += 1
                    ln_gate_out(P, pN, retrow[:, 2 * hp:2 * hp + 2], hp, n, o32[ri], rng, rg_all[s], hmc, tmp_r[ri])
                if pend is not None:
                    out_transpose(P, pend[0], pend[1], G["hrT"], pend[2], P.pbB[tcnt % 2])
                    tcnt += 1
                pend = (hmc, hmTs[s][n % 2], tb + n * 128)
        out_transpose(P, pend[0], pend[1], G["hrT"], pend[2], P.pbB[tcnt % 2])


def stage_C(P, l):
    S, G = P.S, P.G
    NT = SEQ // 128
    WSC = (8 ** -0.5) * (64 ** -0.5)
    with _scope(S):
        stg = [S.sb("stgC%d" % i, [128, 2048], F32) for i in range(2)]
        aqg = S.sb("aqg", [128, 2], F32)
        akg = S.sb("akg", [128, 1], F32)
        akvg = S.sb("akvg", [128, 128], F32)
        wuq = S.sb("wuq", [128, 2, 512], BF16)
        wuqi = S.sb("wuqi", [128, 2, 512], BF16)
        wuk = S.sb("wuk", [128, 1, 512], BF16)
        wuvz = S.sb("wuvz", [128, 8, 128], BF16)
        zt = [S.sb("ztC%d" % i, [128, ZS_N], F32) for i in range(4)]
        NR = 4
        junk_r = [S.sb("junkC%d" % i, [128, 256], BF16) for i in range(NR)]
        junk2_r = [S.sb("junk2C%d" % i, [128, 128], BF16) for i in range(NR)]
        ss_r = [S.sb("ssC%d" % i, [128, 2], F32) for i in range(NR)]
        rs_r = [S.sb("rsC%d" % i, [128, 2], F32) for i in range(NR)]
        st6_r = [S.sb("st6C%d" % i, [128, 6], F32) for i in range(NR)]
        mv_r = [S.sb("mvC%d" % i, [128, 2], F32) for i in range(NR)]
        cqn_r = [S.sb("cqn%d" % i, [128, 256], BF16) for i in range(NR)]
        kin_r = [S.sb("kin%d" % i, [128, 128], BF16) for i in range(NR)]
        ckn_r = [S.sb("ckn%d" % i, [128, 128], F32) for i in range(NR)]
        cqT = S.sb("cqT", [128, 2, SEQ], BF16)
        kidxT = S.sb("kidxT", [128, SEQ], BF16)
        ckv1 = S.sb("ckv1", [128, NT, 130], BF16)
        ckvT = S.sb("ckvT", [128, SEQ], BF16)
        qTa = S.sb("qTCa", [128, 4, 512], BF16)
        qTb = S.sb("qTCb", [128, 4, 512], BF16)
        qidxA = S.sb("qidxA", [128, 4, SEQ], BF16)
        qidxB = S.sb("qidxB", [128, 4, SEQ], BF16)
        qlatT = S.sb("qlatT", [128, 8, SEQ], BF16)
        wabs = S.sb("wabs", [128, NT, 8], F32)
        wsgn = S.sb("wsgn", [128, NT, 8], F32)
        acc2 = [S.sb("accC%d" % i, [128, SEQ], F32) for i in range(2)]
        NBIS = 20
        junkb = [S.sb("junkbC%d" % i, [128, SEQ], BF16) for i in range(2)]
        bis = [S.sb("bisC%d" % i, [128, 8], F32) for i in range(2)]
        dk = [S.sb("dkC%d" % i, [128, NBIS], F32) for i in range(2)]
        pw2 = S.sb("pw2C", [128, NBIS], F32)
        for k in range(NBIS):
            S.memset(pw2[:, k:k + 1], 2.0 ** -k, eng="pool")
        rbuf = [S.sb("rbufC%d" % i, [128, 512], F32) for i in range(3)]
        m8 = [S.sb("m8C%d" % i, [128, 8], F32) for i in range(2)]
        madd4 = [S.sb("maddC%d" % i, [128, SEQ], BF16) for i in range(4)]
        Eb = [S.sb("EbC%d" % i, [128, 512], BF16) for i in range(3)]
        otok = S.sb("otok", [128, 8, 128], BF16)
        olT = S.sb("olT", [128, 8, 128], BF16)
        haTs = [S.sb("haTs%d" % i, [128, 4, 128], BF16) for i in range(2)]
        rec = S.sb("recC", [128, 8], F32)
        S.dma(aqg[:], G["aqg_%d" % l])
        S.dma(akg[:], G["akg_%d" % l])
        S.dma(akvg[:], G["akvg_%d" % l])
        load_cast(P, wuq, G["wuq_%d" % l], 256, 512, gain=aqg, stg=stg)
        load_cast(P, wuqi, G["wuqi_%d" % l], 256, 512, gain=aqg, stg=stg)
        load_cast(P, wuk, G["wuk_%d" % l].rearrange("p a b -> p (a b)"), 128, 512, stg=stg)
        S.memset(wuvz[:], 0.0)
        sv = stg[P.stg_i % 2]
        P.stg_i += 1
        S.dma(sv[:, 0:512], G["wuv_%d" % l].rearrange("p a b -> p (a b)"))
        for h in range(8):
            S.copy(wuvz[:, h, (h % 2) * 64:(h % 2) * 64 + 64], sv[:, h * 64:(h + 1) * 64], eng="pool")
        S.memset(ckv1[:, :, 128:130], 1.0)
        S.memset(qTa[64:128, :, :], 0.0)
        S.memset(qTb[0:64, :, :], 0.0)
        S.memset(qidxA[64:128, :, :], 0.0)
        S.memset(qidxB[0:64, :, :], 0.0)
        bank = 0
        for s in range(NSEQ):
            tb = s * SEQ
            for i in range(NT):
                z = zt[i % 4]
                ri = i % NR
                junk, junk2, ss, rs, st6, mv = junk_r[ri], junk2_r[ri], ss_r[ri], rs_r[ri], st6_r[ri], mv_r[ri]
                cqn, kin, ckn = cqn_r[ri], kin_r[ri], ckn_r[ri]
                S.dma(z[:], G["zs"][tb + i * 128:tb + (i + 1) * 128, :])
                S.act(junk[:, 0:256], z[:, ZS_CQ:ZS_CQ + 256], AF.Square, accum_out=ss[:, 0:1])
                S.act(junk2[:, 0:128], z[:, ZS_CKV:ZS_CKV + 128], AF.Square, accum_out=ss[:, 1:2])
                S.ts(rs[:, 0:1], ss[:, 0:1], 1.0 / 256, EPS, ALU.mult, ALU.add)
                S.ts(rs[:, 1:2], ss[:, 1:2], 1.0 / 128, EPS, ALU.mult, ALU.add)
                S.act(rs[:], rs[:], AF.Sqrt)
                S.recip(rs[:], rs[:])
                S.ts(cqn[:], z[:, ZS_CQ:ZS_CQ + 256], rs[:, 0:1], None, ALU.mult)
                S.stt(ckv1[:, i, 0:128], z[:, ZS_CKV:ZS_CKV + 128], rs[:, 1:2], akvg[:], ALU.mult, ALU.mult)
                S.bnstats(st6[:], z[:, ZS_KIDX:ZS_KIDX + 64])
                S.bnaggr(mv[:], st6[:])
                S.ts(mv[:, 1:2], mv[:, 1:2], EPS, None, ALU.add)
                S.act(mv[:, 1:2], mv[:, 1:2], AF.Sqrt)
                S.recip(mv[:, 1:2], mv[:, 1:2])
                S.ts(kin[:, 0:64], z[:, ZS_KIDX:ZS_KIDX + 64], mv[:, 0:1], mv[:, 1:2], ALU.subtract, ALU.mult)
                S.ts(kin[:, 64:128], z[:, ZS_KIDX:ZS_KIDX + 64], mv[:, 0:1], mv[:, 1:2], ALU.subtract, ALU.mult)
                S.act(wabs[:, i, :], z[:, ZS_WIDX:ZS_WIDX + 8], AF.Abs, scale=WSC)
                S.ts(wsgn[:, i, :], z[:, ZS_WIDX:ZS_WIDX + 8], 0.0, None, ALU.is_gt)
                S.ts(wsgn[:, i, :], wsgn[:, i, :], 2.0, -1.0, ALU.mult, ALU.add)
                ptb = P.pbB[i % 2]
                S.tr(ptb[:, 0:128], cqn[:, 0:128], P.identb[:])
                S.tr(ptb[:, 128:256], cqn[:, 128:256], P.identb[:])
                S.tr(ptb[:, 256:384], kin[:], P.identb[:])
                S.tr(ptb[:, 384:512], ckv1[:, i, 0:128], P.identb[:])
                cs = slice(i * 128, (i + 1) * 128)
                S.copy(cqT[:, :, cs], ptb[:, 0:256].rearrange("p (c t) -> p c t", c=2), eng="act")
                S.copy(kidxT[:, cs], ptb[:, 256:384], eng="act")
                S.ts(kidxT[:, cs], kidxT[:, cs], akg[:, 0:1], None, ALU.mult)
                S.copy(ckvT[:, cs], ptb[:, 384:512], eng="act")
            for b4 in range(SEQ // 512):
                ts_ = slice(b4 * 512, (b4 + 1) * 512)
                for c in range(4):
                    ps = P.pbF[bank % 6]
                    bank += 1
                    for k in range(2):
                        S.mm(ps[:], wuq[:, k, c * 128:(c + 1) * 128], cqT[:, k, ts_], start=(k == 0), stop=(k == 1))
                    S.copy(qTa[0:64, c, :], ps[0:64, :], eng="act")
                    S.copy(qTb[64:128, c, :], ps[64:128, :], eng="act")
                for c in range(4):
                    ps = P.pbF[bank % 6]
                    bank += 1
                    for k in range(2):
                        S.mm(ps[:], wuqi[:, k, c * 128:(c + 1) * 128], cqT[:, k, ts_], start=(k == 0), stop=(k == 1))
                    S.copy(qidxA[0:64, c, ts_], ps[0:64, :], eng="dve")
                    S.copy(qidxB[64:128, c, ts_], ps[64:128, :], eng="dve")
                for h in range(8):
                    qTx = qTa if h % 2 == 0 else qTb
                    ps = P.pbF[bank % 6]
                    bank += 1
                    S.mm(ps[:], wuk[:, 0, (h // 2) * 128:(h // 2 + 1) * 128], qTx[:, h // 2, :])
                    S.act(qlatT[:, h, ts_], ps[:], AF.Copy, scale=0.125)
            def idx_topk_pair(ia):
                nonlocal bank
                tiles = (ia, ia + 1)
                for i in tiles:
                    W = (i + 1) * 128
                    cs = slice(i * 128, (i + 1) * 128)
                    nb = (W + 511) // 512
                    acc = acc2[i % 2]
                    for b in range(nb):
                        w = min(512, W - b * 512)
                        ks = slice(b * 512, b * 512 + w)
                        for g in range(8):
                            qix = qidxA if g % 2 == 0 else qidxB
                            ps = P.pbF[bank % 4]
                            bank += 1
                            S.mm(ps[:, 0:w], qix[:, g // 2, cs], kidxT[:, ks])
                            r = rbuf[g % 3]
                            S.act(r[:, 0:w], ps[:, 0:w], AF.Relu, scale=wabs[:, i, g:g + 1])
                            if g == 0:
                                S.ts(acc[:, ks], r[:, 0:w], wsgn[:, i, 0:1], None, ALU.mult)
                            else:
                                S.stt(acc[:, ks], r[:, 0:w], wsgn[:, i, g:g + 1], acc[:, ks], ALU.mult, ALU.add)
                    S.tt(acc[:, cs], acc[:, cs], P.negd[:], ALU.add)
                if ia >= 2:
                    for i in tiles:
                        W = (i + 1) * 128
                        acc, bs = acc2[i % 2], bis[i % 2]
                        S.max8(m8[i % 2][:], acc[:, 0:W])
                        S.treduce(bs[:, 0:1], acc[:, 0:i * 128], ALU.min)
                        S.ts(bs[:, 1:2], m8[i % 2][:, 0:1], bs[:, 0:1], 0.5, ALU.subtract, ALU.mult)
                        S.ts(bs[:, 2:3], m8[i % 2][:, 0:1], bs[:, 0:1], 0.5, ALU.add, ALU.mult)
                        S.ts(dk[i % 2][:], pw2[:], bs[:, 1:2], None, ALU.mult)
                    for k in range(NBIS):
                        for i in tiles:
                            W = (i + 1) * 128
                            S.ts_acc(junkb[i % 2][:, 0:W], acc2[i % 2][:, 0:W], bis[i % 2][:, 2:3], 0.0,
                                     ALU.is_ge, ALU.add, bis[i % 2][:, 3:4])
                        for i in tiles:
                            bs = bis[i % 2]
                            S.ts(bs[:, 4:5], bs[:, 3:4], 255.5, -0.5, ALU.is_ge, ALU.add)
                        for i in tiles:
                            bs = bis[i % 2]
                            S.stt(bs[:, 2:3], bs[:, 4:5], dk[i % 2][:, k:k + 1], bs[:, 2:3], ALU.mult, ALU.add)
                    for i in tiles:
                        W = (i + 1) * 128
                        bs = bis[i % 2]
                        S.tt(bs[:, 5:6], bs[:, 2:3], dk[i % 2][:, NBIS - 1:NBIS], ALU.subtract)
                        S.ts(madd4[i % 4][:, 0:W], acc2[i % 2][:, 0:W], bs[:, 5:6], 1.0, ALU.is_ge, ALU.subtract)
                else:
                    for i in tiles:
                        W = (i + 1) * 128
                        S.ts(madd4[i % 4][:, 0:W], acc2[i % 2][:, 0:W], -1.0e29, 1.0, ALU.is_ge, ALU.subtract)

            def attn(i):
                nonlocal bank
                cs = slice(i * 128, (i + 1) * 128)
                madd = madd4[i % 4]
                for h in range(8):
                    hh = h % 2
                    pvbank = P.pbF[4 + (h // 2) % 2]
                    pv = pvbank[:, hh * 129:(hh + 1) * 129]
                    for bg in range((i + 4) // 4):
                        j0 = bg * 4
                        nj = min(4, i + 1 - j0)
                        pl = P.pbF[bank % 4]
                        bank += 1
                        for jj in range(nj):
                            j = j0 + jj
                            near = j >= i - 1
                            blk = pl[:, jj * 128:(jj + 1) * 128]
                            S.mm(blk, ckvT[:, j * 128:(j + 1) * 128], qlatT[:, h, cs], start=True, stop=False)
                            S.mm(blk, madd[:, j * 128:(j + 1) * 128], P.i30k[:], start=False, stop=(not near))
                            if near:
                                S.mm(blk, P.biasb[:, h, i - j, :], P.identb[:], start=False, stop=True)
                        E = Eb[(h * 4 + bg) % 3]
                        S.act(E[:, 0:nj * 128], pl[:, 0:nj * 128], AF.Exp)
                        for jj in range(nj):
                            j = j0 + jj
                            S.mm(pv, E[:, jj * 128:(jj + 1) * 128], ckv1[:, j, 0:129], start=(j == 0), stop=(j == i))
                    S.act(rec[:, h:h + 1], pv[:, 128:129], AF.Ln)
                    S.act(rec[:, h:h + 1], rec[:, h:h + 1], AF.Exp, scale=-1.0)
                    S.act(otok[:, h, :], pv[:, 0:128], AF.Copy, scale=rec[:, h:h + 1])
                ptb = P.pbB[i % 2]
                for h in range(8):
                    S.tr(ptb[:, h * 128:(h + 1) * 128], otok[:, h, :], P.identb[:])
                S.copy(olT[:], ptb[:, :].rearrange("p (h t) -> p h t", h=8), eng="act")
                ph = P.pbF[bank % 4]
                bank += 1
                for hc in range(4):
                    S.mm(ph[:, hc * 128:(hc + 1) * 128], wuvz[:, 2 * hc, :], olT[:, 2 * hc, :], start=True, stop=False)
                    S.mm(ph[:, hc * 128:(hc + 1) * 128], wuvz[:, 2 * hc + 1, :], olT[:, 2 * hc + 1, :], start=False, stop=True)
                hs = haTs[i % 2]
                S.copy(hs[:], ph[:].rearrange("p (c t) -> p c t", c=4), eng="act")
                S.dma(G["haT"].rearrange("(c p) t -> p c t", p=128)[:, :, tb + i * 128:tb + (i + 1) * 128], hs[:])

            idx_topk_pair(0)
            for pp in range(NT // 2):
                if pp + 1 < NT // 2:
                    idx_topk_pair(2 * pp + 2)
                attn(2 * pp)
                attn(2 * pp + 1)


def stage_E(P, l, x_in):
    S, G = P.S, P.G
    with _scope(S):
        stg = [S.sb("stgE%d" % i, [128, 1024], F32) for i in range(3)]
        pw = [S.sb("pwE%d" % i, [128, 4, D], BF16) for i in range(3)]
        wo = S.sb("woE", [128, 8, D], BF16)
        hin = [[S.sb("hinE%d_%d" % (b, i), [128, 4, 512], BF16) for i in range(2)] for b in range(3)]
        gin = [S.sb("ginE%d" % i, [128, 24, 512], BF16) for i in range(2)]
        y32 = S.sb("y32E", [128, 512], F32)
        t32 = [S.sb("t32E%d" % i, [128, 512], F32) for i in range(2)]
        yT = S.sb("yTE", [128, 8, 512], BF16)
        xb = [S.sb("xbE%d" % i, [128, D], F32) for i in range(2)]
        for b, nm in enumerate(("p_m", "p_a", "p_r")):
            load_cast(P, pw[b], G["%s_%d" % (nm, l)], 512, D, stg=stg)
        load_cast(P, wo, G["w_out_%d" % l], D, D, stg=stg)
        srcs = [G["hmT"], G["haT"], G["hrT"]]
        bank = 0
        for st in range(NTOK // 512):
            t0 = st * 512
            for b in range(3):
                S.dma(hin[b][st % 2][:], srcs[b].rearrange("(c p) t -> p c t", p=128)[:, :, t0:t0 + 512])
            gt = gin[st % 2]
            dma_chunks(S, gt, G["gT"].rearrange("(c p) t -> p c t", p=128)[:, :, t0:t0 + 512], 24, 4)
            for c in range(8):
                pss = []
                for b in range(3):
                    ps = P.pbF[bank % 6]
                    bank += 1
                    for k in range(4):
                        S.mm(ps[:], pw[b][:, k, c * 128:(c + 1) * 128], hin[b][st % 2][:, k, :],
                             start=(k == 0), stop=(k == 3))
                    pss.append(ps)
                S.tt(y32[:], pss[0][:], gt[:, c, :], ALU.mult)
                S.tt(t32[0][:], pss[1][:], gt[:, 8 + c, :], ALU.mult)
                S.tt(t32[1][:], pss[2][:], gt[:, 16 + c, :], ALU.mult)
                S.tt(y32[:], y32[:], t32[0][:], ALU.add, eng="pool")
                S.tt(yT[:, c, :], y32[:], t32[1][:], ALU.add, eng="pool")
            for ti in range(4):
                r0 = t0 + ti * 128
                xt = xb[ti % 2]
                S.dma(xt[:], x_in[r0:r0 + 128, :])
                for hf in range(2):
                    ps = P.pbF[bank % 6]
                    bank += 1
                    for k in range(8):
                        S.mm(ps[:], yT[:, k, ti * 128:(ti + 1) * 128], wo[:, k, hf * 512:(hf + 1) * 512],
                             start=(k == 0), stop=(k == 7))
                    S.tt(xt[:, hf * 512:(hf + 1) * 512], xt[:, hf * 512:(hf + 1) * 512], ps[:], ALU.add)
                S.dma(G["x1"][r0:r0 + 128, :], xt[:])


def stage_F(P, l, x_out, final):
    S, G = P.S, P.G
    ST = 256
    NF = DFF // 128
    with _scope(S):
        stg = [S.sb("stgF%d" % i, [128, 1024], F32) for i in range(3)]
        n2g = S.sb("n2g", [128, 8], F32)
        wg = S.sb("wgF", [128, 8, DFF], BF16)
        wu = S.sb("wuF", [128, 8, DFF], BF16)
        wd = S.sb("wdF", [128, NF, D], BF16)
        xb = [S.sb("xbF%d" % i, [128, D], F32) for i in range(2)]
        hb = S.sb("hbF", [128, D], BF16)
        junk = S.sb("junkF", [128, D], BF16)
        ss = S.sb("ssF", [128, 1], F32)
        rs = S.sb("rsF", [128, 1], F32)
        hT = S.sb("hTF", [128, 8, ST], BF16)
        sg = [S.sb("sgF%d" % i, [128, ST], F32) for i in range(2)]
        aT = S.sb("aTF", [128, NF, ST], BF16)
        fng = None
        if final:
            fng = S.sb("fngF", [128, D], F32)
            S.dma(fng[:], G["fng"])
        S.dma(n2g[:], G["n2g_%d" % l])
        def prep(st):
            t0 = st * ST
            for ti in range(ST // 128):
                xt = xb[ti % 2]
                S.dma(xt[:], G["x1"][t0 + ti * 128:t0 + (ti + 1) * 128, :])
                norm_transpose(P, xt, hb, junk, ss, rs, hT, ti * 128, P.pbB[ti % 2])

        prep(0)
        for n0 in range(0, DFF, 1024):
            n1 = min(DFF, n0 + 1024)
            load_cast(P, wg, G["w_gate_%d" % l], D, DFF, gain=n2g, stg=stg, colkey=1024, cols=(n0, n1))
            load_cast(P, wu, G["w_up_%d" % l], D, DFF, gain=n2g, stg=stg, colkey=1024, cols=(n0, n1))
        load_cast(P, wd, G["w_down_%d" % l], DFF, D, stg=stg)
        bank = 0
        for st in range(NTOK // ST):
            t0 = st * ST
            if st > 0:
                prep(st)
            for f in range(NF):
                pg = P.pbF[bank % 6]
                pu = P.pbF[(bank + 1) % 6]
                bank += 2
                for k in range(8):
                    S.mm(pg[:, 0:ST], (wg[:, k, f * 128:(f + 1) * 128], "cb%d" % (f * 128 // 1024)), hT[:, k, :],
                         start=(k == 0), stop=(k == 7))
                for k in range(8):
                    S.mm(pu[:, 0:ST], (wu[:, k, f * 128:(f + 1) * 128], "cb%d" % (f * 128 // 1024)), hT[:, k, :],
                         start=(k == 0), stop=(k == 7))
                sgt = sg[f % 2]
                S.act(sgt[:], pg[:, 0:ST], AF.Silu)
                S.tt(aT[:, f, :], sgt[:], pu[:, 0:ST], ALU.mult)
            for ti in range(ST // 128):
                r0 = t0 + ti * 128
                xt = xb[ti % 2]
                S.dma(xt[:], G["x1"][r0:r0 + 128, :])
                for hf in range(2):
                    ps = P.pbF[bank % 6]
                    bank += 1
                    for f in range(NF):
                        S.mm(ps[:], aT[:, f, ti * 128:(ti + 1) * 128], wd[:, f, hf * 512:(hf + 1) * 512],
                             start=(f == 0), stop=(f == NF - 1))
                    S.tt(xt[:, hf * 512:(hf + 1) * 512], xt[:, hf * 512:(hf + 1) * 512], ps[:], ALU.add)
                if final:
                    S.act(junk[:], xt[:], AF.Square, accum_out=ss[:])
                    rms_rstd(P, rs[:], ss[:], D)
                    S.stt(xt[:], xt[:], rs[:, 0:1], fng[:], ALU.mult, ALU.mult)
                S.dma(x_out[r0:r0 + 128, :], xt[:])


SCRATCH = {
    "hT_d": ([D, NTOK], BF16), "qkT": ([512, NTOK], F32), "rqT": ([512, NTOK], BF16), "rkT": ([512, NTOK], BF16),
    "gT": ([3072, NTOK], BF16), "zv": ([NTOK, 512], BF16), "zo": ([NTOK, 512], BF16), "rv": ([NTOK, 512], BF16),
    "rg": ([NTOK, 512], BF16), "zs": ([NTOK, ZS_N], F32), "hmT": ([512, NTOK], BF16), "haT": ([512, NTOK], BF16),
    "hrT": ([512, NTOK], BF16), "x1": ([NTOK, D], F32), "xl": ([NTOK, D], F32),
}


def build_program(n_layers=2, dbg=(), stages="A1,A2,B,C,D,E,F"):
    stages = stages.split(",")
    nc = bass.Bass("TRN2", target_bir_lowering=False)
    P = Prog()
    G = {}
    P.G = G
    P.stg_i = 0
    G["x"] = nc.dram_tensor("x", [NTOK, D], F32, kind="ExternalInput").ap()
    for k, shp in CONST_SHAPES.items():
        G[k] = nc.dram_tensor(k, shp, F32, kind="ExternalInput").ap()
    for l in range(n_layers):
        for k, shp in LAYER_SHAPES.items():
            nm = "%s_%d" % (k, l)
            G[nm] = nc.dram_tensor(nm, shp, F32, kind="ExternalInput").ap()
    G["out"] = nc.dram_tensor("out", [NTOK, D], F32, kind="ExternalOutput").ap()
    for k, (shp, dt) in SCRATCH.items():
        G[k] = nc.dram_tensor(k, shp, dt, kind=("ExternalOutput" if k in dbg else "Internal")).ap()
    P.ret_g = host_constants()["ret_g"]
    with ExitStack() as es:
        S = Sched(nc, es)
        P.S = S
        P.pbF = [S.ps("pbF%d" % i, [128, 512], F32) for i in range(6)]
        P.pbB = [S.ps("pbB%d" % i, [128, 1024], BF16) for i in range(2)]
        P.identb = S.sb("identb", [128, 128], BF16)
        P.i30k = S.sb("i30k", [128, 128], BF16)
        P.trib2 = S.sb("trib2", [128, 256], BF16)
        P.tri32 = S.sb("tri32", [128, 128], F32)
        P.ones32 = S.sb("ones32", [128, 128], F32)
        P.negd = S.sb("negd", [128, 128], F32)
        P.biasb = S.sb("biasb", [128, 8, 2, 128], BF16)
        with _scope(S):
            c32 = S.sb("c32", [128, 128], F32)
            b32 = S.sb("b32", [128, 8, 2, 128], F32)
            rb31 = S.sb("rb31", [128, 8], F32)
            S.dma(c32[:], G["c_ident"])
            S.copy(P.identb[:], c32[:])
            S.ts(P.i30k[:], c32[:], 30000.0, None, ALU.mult)
            S.dma(P.tri32[:], G["c_tri"])
            S.copy(P.trib2[:, 0:128], P.tri32[:])
            S.copy(P.trib2[:, 128:256], P.tri32[:])
            S.dma(P.ones32[:], G["c_ones"])
            S.dma(P.negd[:], G["c_negd"])
            S.dma(b32[:], G["biasn"])
            S.dma(rb31[:], G["rb31"])
            for h in range(8):
                S.ts(P.biasb[:, h, :, :], b32[:, h, :, :], rb31[:, h:h + 1], None, ALU.subtract)
        x_in = G["x"]
        for l in range(n_layers):
            last = (l == n_layers - 1)
            x_out = G["out"] if last else G["xl"]
            if "A1" in stages:
                stage_A1(P, l, x_in)
            if "A2" in stages:
                stage_A2(P, l)
            if "B" in stages:
                stage_B(P, l)
            if "D" in stages:
                stage_D(P, l)
            if "C" in stages:
                stage_C(P, l)
            if "E" in stages:
                stage_E(P, l, x_in)
            if "F" in stages:
                stage_F(P, l, x_out, last)
            x_in = x_out
        S.barrier()
        P.n_inst = S.n_inst
    return nc, P


_CACHE = {}


def make_in_maps(inputs, n_layers=2, cores=range(NCORES)):
    hc = host_constants()
    shared = {k: np.ascontiguousarray(v, dtype=np.float32) for k, v in hc.items() if k.startswith("c_")}
    rel_bias = np.asarray(inputs["rel_bias"], np.float32)
    bi = bias_index()
    biasn = rel_bias[bi]
    shared["biasn"] = np.ascontiguousarray(biasn.transpose(0, 3, 1, 2))
    shared["rb31"] = np.ascontiguousarray(np.broadcast_to(rel_bias[31][None, :], (128, 8)))
    shared["fng"] = np.ascontiguousarray(np.broadcast_to(np.asarray(inputs["final_norm_g"], np.float32)[None, :], (128, D)))
    npin = {k: np.asarray(v) for k, v in inputs.items()}
    for l in range(n_layers):
        shared.update(prep_layer_weights(npin, l))
    x = npin["x"].astype(np.float32, copy=False)
    maps = []
    for c in cores:
        m = dict(shared)
        m["x"] = np.ascontiguousarray(x[NSEQ * c:NSEQ * (c + 1)].reshape(NTOK, D))
        maps.append(m)
    return maps


def kernel(**inputs):
    if "nc" not in _CACHE:
        _CACHE["nc"] = build_program()[0]
    nc = _CACHE["nc"]
    maps = make_in_maps(inputs)
    res = run_bass_kernel_spmd(nc, maps, core_ids=list(range(NCORES)))
    outs = [np.asarray(r["out"], dtype=np.float32).reshape(NSEQ, SEQ, D) for r in res.results]
    return np.concatenate(outs, axis=0)
```
